# Optimizing a Trainium2 kernel written in Bass

```python
import jax, jax.numpy as jnp
from jax import lax
import numpy as np

D_MODEL = 1024
BATCH = 4
SEQ = 8192
DEPTH = 4

HEAD_DIM = 64
N_HEADS_A = 8
N_HEADS_B = 8
N_HEADS = N_HEADS_A + N_HEADS_B
ROPE_THETA = 10000.0
DILATED_PATTERNS = ((128, 1), (512, 4), (2048, 16))
QBLK = 64
GRID_W = 64
NA_ROWS = 8
NA_COLS = 16
POOL_WINDOWS = (2, 4, 8, 16)
N_POOL = len(POOL_WINDOWS)
POOL_CH = D_MODEL // N_POOL
N_EXPERTS = 16
N_EXPERT_GROUPS = 4
EXPERTS_PER_GROUP = N_EXPERTS // N_EXPERT_GROUPS
TOP_K = 2
D_EXPERT = D_MODEL
EBLK = 256
N_EVEN = (DEPTH + 1) // 2
N_ODD = DEPTH // 2
DEEPNORM_ALPHA = (2.0 * DEPTH) ** 0.25
DEEPNORM_BETA = (8.0 * DEPTH) ** -0.25
LN_EPS = 1e-5
NEG_INF = -1e30

kernel_name = 'hybrid_dilated_neighbourhood_pool_moe_encoder'


def _layer_norm(x, g, b):
    xf = x.astype(jnp.float32)
    mu = jnp.mean(xf, axis=-1, keepdims=True)
    var = jnp.mean(jnp.square(xf - mu), axis=-1, keepdims=True)
    y = (xf - mu) * lax.rsqrt(var + LN_EPS) * g.astype(jnp.float32) + b.astype(jnp.float32)
    return y.astype(x.dtype)


def _rope_tables(seq):
    pos = jnp.arange(seq, dtype=jnp.float32)
    inv_freq = ROPE_THETA ** (-jnp.arange(0, HEAD_DIM, 2, dtype=jnp.float32) / HEAD_DIM)
    ang = pos[:, None] * inv_freq[None, :]
    return jnp.cos(ang), jnp.sin(ang)


def _rope(t, cos, sin):
    half = HEAD_DIM // 2
    c = cos[None, :, None, :].astype(t.dtype)
    s = sin[None, :, None, :].astype(t.dtype)
    t1, t2 = t[..., :half], t[..., half:]
    return jnp.concatenate([t1 * c - t2 * s, t2 * c + t1 * s], axis=-1)


def _band_attention(q, k, v, radius):
    n, length, h, hd = q.shape
    nb = -(-length // QBLK)
    lp = nb * QBLK
    kw = QBLK + 2 * radius
    qp = jnp.pad(q, ((0, 0), (0, lp - length), (0, 0), (0, 0)))
    kv_pad = ((0, 0), (radius, radius + lp - length), (0, 0), (0, 0))
    kp = jnp.pad(k, kv_pad)
    vp = jnp.pad(v, kv_pad)
    kidx = np.arange(nb)[:, None] * QBLK + np.arange(kw)[None, :]
    kpos = kidx - radius
    qpos = np.arange(lp).reshape(nb, QBLK)
    mask = ((np.abs(kpos[:, None, :] - qpos[:, :, None]) <= radius)
            & (kpos[:, None, :] >= 0) & (kpos[:, None, :] < length))
    kb = kp[:, kidx]
    vb = vp[:, kidx]
    s = jnp.einsum('nbqhd,nbkhd->nhbqk', qp.reshape(n, nb, QBLK, h, hd), kb).astype(jnp.float32)
    s = jnp.where(mask, s, NEG_INF)
    m = jnp.max(s, axis=-1, keepdims=True)
    p = jnp.exp(s - m)
    den = jnp.sum(p, axis=-1)
    o = jnp.einsum('nhbqk,nbkhd->nbqhd', p.astype(v.dtype), vb).astype(jnp.float32)
    o = o / jnp.moveaxis(den, 1, 3)[..., None]
    lse = jnp.moveaxis(m[..., 0] + jnp.log(den), 1, 3)
    o = o.reshape(n, lp, h, hd)[:, :length]
    lse = lse.reshape(n, lp, h)[:, :length]
    return o, lse


def _dilated_attention(q, k, v, window, dilation):
    b, s, h, hd = q.shape
    sub = s // dilation
    radius = window // (2 * dilation)

    def split(t):
        return t.reshape(b, sub, dilation, h, hd).transpose(0, 2, 1, 3, 4).reshape(b * dilation, sub, h, hd)

    o, lse = _band_attention(split(q), split(k), split(v), radius)
    o = o.reshape(b, dilation, sub, h, hd).transpose(0, 2, 1, 3, 4).reshape(b, s, h, hd)
    lse = lse.reshape(b, dilation, sub, h).transpose(0, 2, 1, 3).reshape(b, s, h)
    return o, lse


def _neighbourhood_attention(q, k, v, rpb):
    b, s, h, hd = q.shape
    rows = s // GRID_W
    wr = min(NA_ROWS, rows)
    r = np.arange(rows)
    ridx = np.clip(r - wr // 2, 0, rows - wr)[:, None] + np.arange(wr)[None, :]
    c = np.arange(GRID_W)
    cstart = np.clip(c - NA_COLS // 2, 0, GRID_W - NA_COLS)
    colmask = (c[None, :] >= cstart[:, None]) & (c[None, :] < cstart[:, None] + NA_COLS)
    mask = np.broadcast_to(colmask[:, None, :], (GRID_W, wr, GRID_W)).reshape(GRID_W, wr * GRID_W)
    roff = ridx - r[:, None] + (NA_ROWS - 1)
    coff = np.clip(c[None, :] - c[:, None], 1 - NA_COLS, NA_COLS - 1) + (NA_COLS - 1)
    bias = rpb[:, roff[:, None, :, None], coff[None, :, None, :]]
    bias = bias.reshape(h, rows, GRID_W, wr * GRID_W).astype(jnp.float32)
    qg = q.reshape(b, rows, GRID_W, h, hd)
    kg = k.reshape(b, rows, GRID_W, h, hd)[:, ridx].reshape(b, rows, wr * GRID_W, h, hd)
    vg = v.reshape(b, rows, GRID_W, h, hd)[:, ridx].reshape(b, rows, wr * GRID_W, h, hd)
    sc = jnp.einsum('brqhd,brkhd->bhrqk', qg, kg).astype(jnp.float32) + bias[None]
    sc = jnp.where(mask, sc, NEG_INF)
    p = jax.nn.softmax(sc, axis=-1)
    o = jnp.einsum('bhrqk,brkhd->brqhd', p.astype(v.dtype), vg)
    return o.reshape(b, s, h, hd)


def _attention_mixer(x, w_in, w_out, rpb, cos, sin):
    b, s, d = x.shape
    qkv = (x @ w_in).reshape(b, s, 3, N_HEADS, HEAD_DIM)
    q = qkv[:, :, 0] * (HEAD_DIM ** -0.5)
    k = qkv[:, :, 1]
    v = qkv[:, :, 2]
    qa = _rope(q[:, :, :N_HEADS_A], cos, sin)
    ka = _rope(k[:, :, :N_HEADS_A], cos, sin)
    va = v[:, :, :N_HEADS_A]
    outs, lses = [], []
    for window, dilation in DILATED_PATTERNS:
        o_i, l_i = _dilated_attention(qa, ka, va, window, dilation)
        outs.append(o_i)
        lses.append(l_i)
    wts = jax.nn.softmax(jnp.stack(lses), axis=0)
    oa = jnp.sum(wts[..., None] * jnp.stack(outs), axis=0).astype(x.dtype)
    ob = _neighbourhood_attention(q[:, :, N_HEADS_A:], k[:, :, N_HEADS_A:], v[:, :, N_HEADS_A:], rpb)
    o = jnp.concatenate([oa, ob], axis=2).reshape(b, s, d)
    return o @ w_out


def _pool_mixer(x, w, scale):
    b, s, d = x.shape
    xg = x.reshape(b, s, N_POOL, POOL_CH).astype(jnp.float32)
    cs = jnp.pad(jnp.cumsum(xg, axis=1), ((0, 0), (1, 0), (0, 0), (0, 0)))
    t = np.arange(s)[:, None]
    half = np.array(POOL_WINDOWS)[None, :] // 2
    lo = np.clip(t - half, 0, s)
    hi = np.clip(t + half, 0, s)
    gi = np.arange(N_POOL)[None, :]
    cnt = (hi - lo).astype(np.float32)[None, :, :, None]
    mean = (cs[:, hi, gi] - cs[:, lo, gi]) / cnt
    u = (mean - xg).astype(x.dtype)
    y = jnp.einsum('bsgc,gce->bsge', u, w).reshape(b, s, d)
    return y * scale


def _moe(xt, router_w, router_bias, w1, w3, w2):
    t_count, d = xt.shape
    aff = jax.nn.sigmoid(xt.astype(jnp.float32) @ router_w.astype(jnp.float32))
    sel = (aff + router_bias.astype(jnp.float32)).reshape(t_count, N_EXPERT_GROUPS, EXPERTS_PER_GROUP)
    gscore = jnp.sum(lax.top_k(sel, 2)[0], axis=-1)
    grp = jnp.argmax(gscore, axis=-1)
    sel_g = sel[jnp.arange(t_count), grp]
    _, local = lax.top_k(sel_g, TOP_K)
    eidx = grp[:, None] * EXPERTS_PER_GROUP + local
    gate = jnp.take_along_axis(aff, eidx, axis=1)
    gate = gate / jnp.sum(gate, axis=-1, keepdims=True)
    m = t_count * TOP_K
    flat_e = eidx.reshape(m)
    flat_t = jnp.arange(m, dtype=jnp.int32) // TOP_K
    order = jnp.argsort(flat_e)
    se = flat_e[order]
    st = flat_t[order]
    sg = gate.reshape(m)[order]
    counts = jnp.zeros((N_EXPERTS,), jnp.int32).at[flat_e].add(1)
    start = jnp.cumsum(counts) - counts
    padded = (counts + EBLK - 1) // EBLK * EBLK
    pend = jnp.cumsum(padded)
    pstart = pend - padded
    dest = pstart[se] + jnp.arange(m, dtype=jnp.int32) - start[se]
    p_rows = m + N_EXPERTS * EBLK
    nblk = p_rows // EBLK
    buf = jnp.zeros((p_rows, d), xt.dtype).at[dest].set(xt[st])
    blk_e = jnp.minimum(jnp.searchsorted(pend, jnp.arange(nblk, dtype=jnp.int32) * EBLK, side='right'), N_EXPERTS - 1)

    def expert_block(args):
        xb, e = args
        hb = jax.nn.silu(xb @ w1[e]) * (xb @ w3[e])
        return hb @ w2[e]

    yb = lax.map(expert_block, (buf.reshape(nblk, EBLK, d), blk_e)).reshape(p_rows, d)
    y = jnp.zeros((t_count, d), jnp.float32).at[st].add(yb[dest].astype(jnp.float32) * sg[:, None])
    return y.astype(xt.dtype)


def setup_inputs(seed: int = 0) -> dict:
    key = jax.random.key(seed)
    ks = jax.random.split(key, 13)
    d = D_MODEL
    x = jax.random.normal(ks[0], (BATCH, SEQ, d), jnp.float32)
    w_in = jax.random.normal(ks[1], (N_EVEN, d, 3 * d), jnp.float32) * d ** -0.5
    w_out = jax.random.normal(ks[2], (N_EVEN, d, d), jnp.float32) * (d ** -0.5 * DEEPNORM_BETA)
    rpb = jax.random.normal(ks[3], (N_EVEN, N_HEADS_B, 2 * NA_ROWS - 1, 2 * NA_COLS - 1), jnp.float32) * 0.1
    pool_w = jax.random.normal(ks[4], (N_ODD, N_POOL, POOL_CH, POOL_CH), jnp.float32) * (POOL_CH ** -0.5 * DEEPNORM_BETA)
    pool_scale = 1.0 + 0.1 * jax.random.normal(ks[5], (N_ODD, d), jnp.float32)
    router_w = jax.random.normal(ks[6], (d, N_EXPERTS), jnp.float32) * d ** -0.5
    router_bias = 0.01 * jax.random.normal(ks[7], (N_EXPERTS,), jnp.float32)
    moe_w1 = jax.random.normal(ks[8], (DEPTH, N_EXPERTS, d, D_EXPERT), jnp.float32) * d ** -0.5
    moe_w3 = jax.random.normal(ks[9], (DEPTH, N_EXPERTS, d, D_EXPERT), jnp.float32) * d ** -0.5
    moe_w2 = jax.random.normal(ks[10], (DEPTH, N_EXPERTS, D_EXPERT, d), jnp.float32) * (D_EXPERT ** -0.5 * DEEPNORM_BETA)
    ln_g = 1.0 + 0.05 * jax.random.normal(ks[11], (DEPTH, 2, d), jnp.float32)
    ln_b = 0.02 * jax.random.normal(ks[12], (DEPTH, 2, d), jnp.float32)
    return {'x': x, 'w_in': w_in, 'w_out': w_out, 'rpb': rpb, 'pool_w': pool_w,
            'pool_scale': pool_scale, 'router_w': router_w, 'router_bias': router_bias,
            'moe_w1': moe_w1, 'moe_w3': moe_w3, 'moe_w2': moe_w2, 'ln_g': ln_g, 'ln_b': ln_b}


def reference(x, w_in, w_out, rpb, pool_w, pool_scale, router_w, router_bias, moe_w1, moe_w3, moe_w2, ln_g, ln_b):
    b, s, d = x.shape
    cos, sin = _rope_tables(s)
    for layer in range(DEPTH):
        i = layer // 2
        if layer % 2 == 0:
            h = _attention_mixer(x, w_in[i], w_out[i], rpb[i], cos, sin)
        else:
            h = _pool_mixer(x, pool_w[i], pool_scale[i])
        x = _layer_norm(DEEPNORM_ALPHA * x + h, ln_g[layer, 0], ln_b[layer, 0])
        f = _moe(x.reshape(b * s, d), router_w, router_bias, moe_w1[layer], moe_w3[layer], moe_w2[layer]).reshape(b, s, d)
        x = _layer_norm(DEEPNORM_ALPHA * x + f, ln_g[layer, 1], ln_b[layer, 1])
    return x
```

```python
import numpy as np
import concourse.bass as bass
import concourse.mybir as mybir
from concourse.bass_utils import run_bass_kernel_spmd
from contextlib import ExitStack

F32 = mybir.dt.float32
BF16 = mybir.dt.bfloat16
U32 = mybir.dt.uint32
I32 = mybir.dt.int32
AF = mybir.ActivationFunctionType
ALU = mybir.AluOpType
AX = mybir.AxisListType

ENGS = ['pe', 'act', 'dve', 'pool', 'sp']
ENG_ATTR = {'pe': 'tensor', 'act': 'scalar', 'dve': 'vector', 'pool': 'gpsimd', 'sp': 'sync'}


class Tok:
    __slots__ = ('sem', 'val')

    def __init__(self, sem, val):
        self.sem = sem
        self.val = val


class Buf:
    def __init__(self, kb, t, name):
        self.kb = kb
        self.t = t
        self.name = name
        self.w = []
        self.r = {}
        self._slot = None

    def __getitem__(self, k):
        return self.t[k]

    def slot(self):
        if self._slot is None:
            self._slot = self.kb.new_sem()
            self._slotval = 0
        return self._slot


class KB:
    def __init__(self, nc, es, same_eng_sync=True):
        self.nc = nc
        self.es = es
        self.ops = {e: [] for e in ENGS}
        self.nsem = 0
        self.sems = {e: self.new_sem() for e in ENGS}
        self.cnt = {e: 0 for e in ENGS}
        self.waited = {e: {} for e in ENGS}
        self.same_eng_sync = same_eng_sync
        self.nbuf = 0

    def new_sem(self):
        self.nsem += 1
        return self.es.enter_context(self.nc.semaphore(f"sem{self.nsem}"))

    def sb(self, shape, dtype, name=None):
        self.nbuf += 1
        name = name or f"sb{self.nbuf}"
        return Buf(self, self.es.enter_context(self.nc.sbuf_tensor(name, list(shape), dtype)), name)

    def ps(self, shape, dtype=F32, name=None):
        self.nbuf += 1
        name = name or f"ps{self.nbuf}"
        return Buf(self, self.es.enter_context(self.nc.psum_tensor(name, list(shape), dtype)), name)

    def view(self, name="v"):
        return Buf(self, None, name)

    def _waits(self, eng, deps):
        waits = []
        for d in deps:
            if d is None:
                continue
            if isinstance(d, (list, tuple)):
                waits += self._waits(eng, d)
                continue
            if (not self.same_eng_sync) and d.sem is self.sems[eng]:
                continue
            key = id(d.sem)
            if self.waited[eng].get(key, 0) >= d.val:
                continue
            self.waited[eng][key] = d.val
            waits.append((d.sem, d.val))
        return waits

    def _deps(self, reads, writes, deps):
        ds = list(deps)
        for b in reads:
            ds += b.w
        for b in writes:
            ds += b.w
            ds += list(b.r.values())
        return ds

    def _commit(self, tok, reads, writes):
        for b in writes:
            b.w = [tok]
            b.r = {}
        for b in reads:
            if b not in writes:
                o = b.r.get(id(tok.sem))
                if o is None or o.val < tok.val:
                    b.r[id(tok.sem)] = tok

    def op(self, eng, fn, reads=(), writes=(), deps=()):
        waits = self._waits(eng, self._deps(reads, writes, deps))
        self.cnt[eng] += 1
        self.ops[eng].append((waits, fn, (self.sems[eng], 1)))
        tok = Tok(self.sems[eng], self.cnt[eng])
        self._commit(tok, reads, writes)
        return tok

    def group(self, eng, fns, reads=(), writes=(), deps=()):
        waits = self._waits(eng, self._deps(reads, writes, deps))
        for i, fn in enumerate(fns):
            self.cnt[eng] += 1
            self.ops[eng].append((waits if i == 0 else [], fn, (self.sems[eng], 1)))
        tok = Tok(self.sems[eng], self.cnt[eng])
        self._commit(tok, reads, writes)
        return tok

    def dma(self, eng, out, in_, slotbuf, reads=(), writes=(), deps=(), **kw):
        sem = slotbuf.slot()
        ds = list(deps)
        for b in reads:
            ds += b.w
        for b in writes:
            ds += [t for t in b.w if t.sem is not sem]
            ds += list(b.r.values())
        waits = self._waits(eng, ds)
        slotbuf._slotval += 16
        self.ops[eng].append((waits, lambda e: e.dma_start(out=out, in_=in_, **kw), (sem, 16)))
        tok = Tok(sem, slotbuf._slotval)
        self._commit(tok, reads, writes)
        return tok

    def wait_only(self, eng, deps):
        waits = self._waits(eng, deps)
        if waits:
            self.ops[eng].append((waits, None, None))

    def emit(self):
        with self.nc.Block() as block:
            for e in ENGS:
                dec = getattr(block, ENG_ATTR[e])
                ops = self.ops[e]

                def body(eng, ops=ops):
                    for waits, fn, inc in ops:
                        for s, v in waits:
                            eng.wait_ge(s, v)
                        if fn is None:
                            continue
                        inst = fn(eng)
                        if inc is not None:
                            inst.then_inc(inc[0], inc[1])
                dec(body)


D = 1024
ALPHA = 8.0 ** 0.25
LN_EPS = 1e-5


def make_ident(kb, ident):
    kb.op('pool', lambda e: e.memset(ident[:], 0.0), writes=[ident])
    kb.op('pool', lambda e: e.affine_select(out=ident[:], in_=ident[:], compare_op=ALU.not_equal, fill=1.0,
                                            base=0, pattern=[[-1, 128]], channel_multiplier=1), writes=[ident])


class LNBufs:
    def __init__(self, kb, nbuf=2):
        self.kb = kb
        self.n = nbuf
        self.i = 0
        self.z = [kb.sb([128, D], F32) for _ in range(nbuf)]
        self.o = [kb.sb([128, D], F32) for _ in range(nbuf)]
        self.st = [kb.sb([128, 12], F32) for _ in range(nbuf)]
        self.mv = [kb.sb([128, 2], F32) for _ in range(nbuf)]
        self.rs = [kb.sb([128, 1], F32) for _ in range(nbuf)]
        self.nb = [kb.sb([128, 1], F32) for _ in range(nbuf)]
        self.eps = kb.sb([128, 1], F32)
        kb.op('pool', lambda e: e.memset(self.eps[:], LN_EPS), writes=[self.eps])


def emit_ln(kb, L, xbuf, x_ap, h_aps, h_bufs, g, b, out_dram_ap, dma_eng='sp'):
    i = L.i
    L.i = (L.i + 1) % L.n
    z, o, st, mv, rs, nb = L.z[i], L.o[i], L.st[i], L.mv[i], L.rs[i], L.nb[i]
    for hf in range(2):
        sl = slice(hf * 512, (hf + 1) * 512)
        kb.op('dve', lambda e, sl=sl, hf=hf: e.scalar_tensor_tensor(out=z[:, sl], in0=x_ap[:, sl], scalar=ALPHA,
                                                                    in1=h_aps[hf], op0=ALU.mult, op1=ALU.add),
              reads=[xbuf, h_bufs[hf]], writes=[z] if hf == 0 else [], deps=z.w if hf == 1 else ())
    z.w = [Tok(kb.sems['dve'], kb.cnt['dve'])]
    for hf in range(2):
        sl = slice(hf * 512, (hf + 1) * 512)
        kb.op('dve', lambda e, sl=sl, hf=hf: e.bn_stats(out=st[:, hf * 6:(hf + 1) * 6], in_=z[:, sl]),
              reads=[z], writes=[st] if hf == 0 else [], deps=st.w if hf == 1 else ())
    st.w = [Tok(kb.sems['dve'], kb.cnt['dve'])]
    kb.op('dve', lambda e: e.bn_aggr(out=mv[:], in_=st[:]), reads=[st], writes=[mv])
    kb.op('act', lambda e: e.activation(out=rs[:], in_=mv[:, 1:2], func=AF.Sqrt, bias=L.eps[:], scale=1.0),
          reads=[mv, L.eps], writes=[rs])
    kb.op('dve', lambda e: e.reciprocal(out=rs[:], in_=rs[:]), reads=[rs], writes=[rs])
    kb.op('dve', lambda e: e.scalar_tensor_tensor(out=nb[:], in0=mv[:, 0:1], scalar=-1.0, in1=rs[:],
                                                  op0=ALU.mult, op1=ALU.mult), reads=[mv, rs], writes=[nb])
    kb.op('act', lambda e: e.activation(out=o[:], in_=z[:], func=AF.Identity, bias=nb[:], scale=rs[:]),
          reads=[z, rs, nb], writes=[o])
    kb.op('pool', lambda e: e.tensor_tensor(out=o[:], in0=o[:], in1=g[:], op=ALU.mult), reads=[o, g], writes=[o])
    kb.op('pool', lambda e: e.tensor_tensor(out=o[:], in0=o[:], in1=b[:], op=ALU.add), reads=[o, b], writes=[o])
    return kb.dma(dma_eng, out_dram_ap, o[:], o, reads=[o])


def build_moe(NT=32, GT=8, NE=16, debug=False):
    nc = bass.Bass("TRN2", target_bir_lowering=False)
    T = NT * 128
    x = nc.dram_tensor("x", [T, D], F32, kind="ExternalInput").ap()
    rw = nc.dram_tensor("rw", [D, 16], F32, kind="ExternalInput").ap()
    rbias = nc.dram_tensor("rbias", [128, GT * 16], F32, kind="ExternalInput").ap()
    w1 = nc.dram_tensor("w1", [16, D, D], F32, kind="ExternalInput").ap()
    w3 = nc.dram_tensor("w3", [16, D, D], F32, kind="ExternalInput").ap()
    w2 = nc.dram_tensor("w2", [16, D, D], F32, kind="ExternalInput").ap()
    lng = nc.dram_tensor("lng", [128, D], F32, kind="ExternalInput").ap()
    lnb = nc.dram_tensor("lnb", [128, D], F32, kind="ExternalInput").ap()
    y = nc.dram_tensor("y", [T, D], F32, kind="ExternalOutput").ap()
    if debug:
        gdbg = nc.dram_tensor("gdbg", [NT // GT, 128, GT * 16], F32, kind="ExternalOutput").ap()
    with ExitStack() as es:
        kb = KB(nc, es)
        ident = kb.sb([128, 128], F32)
        make_ident(kb, ident)
        rws = kb.sb([128, 8, 16], F32)
        kb.dma('sp', rws[:], rw.rearrange("(c p) e -> p c e", p=128), rws, writes=[rws])
        rb = kb.sb([128, GT * 16], F32)
        kb.dma('sp', rb[:], rbias[:, :], rb, writes=[rb])
        g = kb.sb([128, D], F32)
        b = kb.sb([128, D], F32)
        kb.dma('sp', g[:], lng[:, :], g, writes=[g])
        kb.dma('sp', b[:], lnb[:, :], b, writes=[b])
        L = LNBufs(kb, nbuf=1)

        xin = [kb.sb([128, D], F32) for _ in range(2)]
        xT32 = [kb.sb([128, 8, 128], F32) for _ in range(2)]
        xT = kb.sb([128, 8, GT * 128], BF16)
        xTv = [kb.view(f"xTv{t}") for t in range(GT)]
        acc = kb.sb([128, GT, D], F32)
        accv = [[kb.view() for _ in range(2)] for t in range(GT)]
        wb = [[kb.sb([128, 8, D], BF16) for _ in range(3)] for _ in range(2)]
        hT = [kb.sb([128, 8, 512], BF16) for _ in range(1)]
        hTv = [[kb.view() for _ in range(8)] for _ in range(1)]
        sil = [kb.sb([128, 512], BF16) for _ in range(2)]
        banks = [kb.ps([128, 512], F32) for _ in range(8)]
        NG = GT * 16
        aff = kb.sb([128, NG], F32)
        affv = [kb.view() for _ in range(GT)]
        S8 = kb.sb([128, GT * 4, 8], F32)
        Pt = kb.sb([128, GT * 4, 6], F32)
        gs = kb.sb([128, GT * 4], F32)
        gmax = kb.sb([128, GT], F32)
        ghot = kb.sb([128, GT * 4], F32)
        c1 = kb.sb([128, GT * 4, 4], F32)
        c2 = kb.sb([128, GT * 4, 4], F32)
        GA = kb.sb([128, GT, 16], F32)
        den = kb.sb([128, GT], F32)
        G = kb.sb([128, GT, 16], F32)

        wsrc = [w1, w3, w2]
        nsteps = (NT // GT) * NE
        step_list = [(gi, e) for gi in range(NT // GT) for e in range(NE)]

        def load_weights(si):
            gi, e = step_list[si]
            slot = si % 2
            for j in range(3):
                kb.dma('pool', wb[slot][j][:], wsrc[j][e].rearrange("(c p) f -> p c f", p=128), wb[slot][j],
                       writes=[wb[slot][j]])

        if nsteps > 0:
            load_weights(0)
        ybank = 0
        out_toks = []
        for gi in range(NT // GT):
            for t in range(GT):
                tile = gi * GT + t
                xi = xin[t % 2]
                x32 = xT32[t % 2]
                kb.dma('sp', xi[:], x[tile * 128:(tile + 1) * 128, :], xi, writes=[xi])
                for hf in range(2):
                    bk = banks[4 + hf]
                    kb.group('pe', [lambda e, c=c, bk=bk, xi=xi: e.transpose(bk[:, (c % 4) * 128:(c % 4 + 1) * 128],
                                                                               xi[:, c * 128:(c + 1) * 128], ident[:])
                                    for c in range(hf * 4, hf * 4 + 4)], reads=[xi, ident], writes=[bk])
                    if True:
                        kb.op('act', lambda e, bk=bk, hf=hf, x32=x32: e.copy(
                            out=x32[:, hf * 4:(hf + 1) * 4, :], in_=bk[:].rearrange("p (c t) -> p c t", c=4)),
                            reads=[bk], writes=[x32] if hf == 0 else [], deps=x32.w if hf == 1 else ())
                    kb.op('dve', lambda e, hf=hf, t=t, x32=x32: e.tensor_copy(
                        out=xT[:, hf * 4:(hf + 1) * 4, t * 128:(t + 1) * 128], in_=x32[:, hf * 4:(hf + 1) * 4, :]),
                        reads=[], writes=[xTv[t]] if hf == 0 else [], deps=list(xTv[t].w if hf == 1 else ()) + [Tok(kb.sems['act'], kb.cnt['act'])])
                x32.w = [Tok(kb.sems['act'], kb.cnt['act'])]
                xTv[t].w = [Tok(kb.sems['dve'], kb.cnt['dve'])]
                x32.r[id(kb.sems['dve'])] = Tok(kb.sems['dve'], kb.cnt['dve'])
                rbk = banks[6]
                kb.group('pe', [lambda e, c=c, x32=x32, rbk=rbk: e.matmul(rbk[:, 0:16], x32[:, c, :], rws[:, c, :],
                                                                         start=(c == 0), stop=(c == 7))
                                for c in range(8)], reads=[x32, rws], writes=[rbk])
                kb.op('act', lambda e, t=t, rbk=rbk: e.activation(out=aff[:, t * 16:(t + 1) * 16], in_=rbk[:, 0:16],
                                                                   func=AF.Sigmoid), reads=[rbk], writes=[affv[t]])
            S4 = S8[:, :, 0:4]
            aff4 = aff[:].rearrange("p (g j) -> p g j", j=4)
            kb.op('dve', lambda e: e.tensor_tensor(out=S8[:, :, 0:4], in0=aff4, in1=rb[:].rearrange("p (g j) -> p g j", j=4),
                                                   op=ALU.add), reads=affv + [rb], writes=[S8])
            kb.op('dve', lambda e: e.tensor_copy(out=S8[:, :, 4:8], in_=S8[:, :, 0:4]), reads=[S8], writes=[S8])
            pairs = [(0, 1), (0, 2), (0, 3), (1, 2), (1, 3), (2, 3)]
            for i, (a, bb) in enumerate(pairs):
                kb.op('dve', lambda e, i=i, a=a, bb=bb: e.tensor_tensor(out=Pt[:, :, i], in0=S8[:, :, a], in1=S8[:, :, bb],
                                                                         op=ALU.add), reads=[S8], writes=[Pt])
            kb.op('dve', lambda e: e.tensor_reduce(out=gs[:], in_=Pt[:], axis=AX.X, op=ALU.max), reads=[Pt], writes=[gs])
            kb.op('dve', lambda e: e.tensor_reduce(out=gmax[:], in_=gs[:].rearrange("p (t g) -> p t g", g=4), axis=AX.X,
                                                   op=ALU.max), reads=[gs], writes=[gmax])
            kb.op('dve', lambda e: e.tensor_tensor(out=ghot[:].rearrange("p (t g) -> p t g", g=4),
                                                   in0=gs[:].rearrange("p (t g) -> p t g", g=4),
                                                   in1=gmax[:].unsqueeze(2).broadcast_to([128, GT, 4]), op=ALU.is_ge),
                  reads=[gs, gmax], writes=[ghot])
            kb.op('dve', lambda e: e.tensor_tensor(out=c1[:], in0=S8[:, :, 0:4], in1=S8[:, :, 1:5], op=ALU.is_gt),
                  reads=[S8], writes=[c1])
            kb.op('dve', lambda e: e.tensor_tensor(out=c2[:], in0=S8[:, :, 0:4], in1=S8[:, :, 2:6], op=ALU.is_gt),
                  reads=[S8], writes=[c2])
            kb.op('dve', lambda e: e.tensor_tensor(out=c1[:], in0=c1[:], in1=c2[:], op=ALU.add), reads=[c1, c2], writes=[c1])
            kb.op('dve', lambda e: e.tensor_tensor(out=c2[:], in0=S8[:, :, 0:4], in1=S8[:, :, 3:7], op=ALU.is_gt),
                  reads=[S8], writes=[c2])
            kb.op('dve', lambda e: e.tensor_tensor(out=c1[:], in0=c1[:], in1=c2[:], op=ALU.add), reads=[c1, c2], writes=[c1])
            kb.op('dve', lambda e: e.scalar_tensor_tensor(out=c1[:], in0=c1[:], scalar=2.0,
                                                          in1=ghot[:].unsqueeze(2).broadcast_to([128, GT * 4, 4]),
                                                          op0=ALU.is_ge, op1=ALU.mult), reads=[c1, ghot], writes=[c1])
            kb.op('dve', lambda e: e.tensor_tensor(out=GA[:].rearrange("p t (g j) -> p (t g) j", j=4), in0=c1[:], in1=aff4,
                                                   op=ALU.mult), reads=[c1] + affv, writes=[GA])
            kb.op('dve', lambda e: e.tensor_reduce(out=den[:], in_=GA[:], axis=AX.X, op=ALU.add), reads=[GA], writes=[den])
            kb.op('dve', lambda e: e.reciprocal(out=den[:], in_=den[:]), reads=[den], writes=[den])
            kb.op('dve', lambda e: e.tensor_tensor(out=G[:], in0=GA[:], in1=den[:].unsqueeze(2).broadcast_to([128, GT, 16]),
                                                   op=ALU.mult), reads=[GA, den], writes=[G])
            if debug:
                out_toks.append(kb.dma('sp', gdbg[gi], G[:].rearrange("p t e -> p (t e)"), G, reads=[G]))
            for e_i in range(NE):
                si = gi * NE + e_i
                slot = si % 2
                if si + 1 < nsteps:
                    load_weights(si + 1)
                w1b, w3b, w2b = wb[slot]
                for tt in range(GT // 4):
                    hb = hT[0]
                    hv = hTv[0]
                    tsl = slice(tt * 512, (tt + 1) * 512)
                    for fc in range(8):
                        b1 = banks[fc % 2]
                        b3 = banks[2 + fc % 2]
                        sl_ = sil[fc % 2]
                        fsl = slice(fc * 128, (fc + 1) * 128)
                        kb.group('pe', [lambda e, c=c, b1=b1, fsl=fsl, w1b=w1b, tsl=tsl: e.matmul(b1[:], w1b[:, c, fsl], xT[:, c, tsl],
                                                                                 start=(c == 0), stop=(c == 7))
                                        for c in range(8)], reads=[w1b] + xTv[tt * 4:(tt + 1) * 4], writes=[b1])
                        kb.group('pe', [lambda e, c=c, b3=b3, fsl=fsl, w3b=w3b, tsl=tsl: e.matmul(b3[:], w3b[:, c, fsl], xT[:, c, tsl],
                                                                                 start=(c == 0), stop=(c == 7))
                                        for c in range(8)], reads=[w3b] + xTv[tt * 4:(tt + 1) * 4], writes=[b3])
                        kb.op('act', lambda e, b1=b1, sl_=sl_: e.activation(out=sl_[:], in_=b1[:], func=AF.Silu),
                              reads=[b1], writes=[sl_])
                        kb.op('dve', lambda e, b3=b3, sl_=sl_, fc=fc, hb=hb: e.tensor_tensor(out=hb[:, fc, :], in0=b3[:],
                                                                                          in1=sl_[:], op=ALU.mult),
                              reads=[b3, sl_], writes=[hv[fc]])
                    for t4 in range(4):
                        t = tt * 4 + t4
                        for oh in range(2):
                            yb = banks[4 + ybank % 4]
                            ybank += 1
                            osl = slice(oh * 512, (oh + 1) * 512)
                            kb.group('pe', [lambda e, fc=fc, yb=yb, t4=t4, osl=osl, hb=hb, w2b=w2b: e.matmul(
                                yb[:], hb[:, fc, t4 * 128:(t4 + 1) * 128], w2b[:, fc, osl], start=(fc == 0), stop=(fc == 7))
                                for fc in range(8)], reads=[w2b] + hv, writes=[yb])
                            av = accv[t][oh]
                            if e_i == 0:
                                kb.op('dve', lambda e, yb=yb, t=t, osl=osl, e_i=e_i: e.tensor_scalar(
                                    out=acc[:, t, osl], in0=yb[:], scalar1=G[:, t, e_i:e_i + 1], scalar2=None, op0=ALU.mult),
                                    reads=[yb, G], writes=[av])
                            else:
                                kb.op('dve', lambda e, yb=yb, t=t, osl=osl, e_i=e_i: e.scalar_tensor_tensor(
                                    out=acc[:, t, osl], in0=yb[:], scalar=G[:, t, e_i:e_i + 1], in1=acc[:, t, osl],
                                    op0=ALU.mult, op1=ALU.add), reads=[yb, G], writes=[av])
            for t in range(GT):
                tile = gi * GT + t
                xi = xin[t % 2]
                kb.dma('sp', xi[:], x[tile * 128:(tile + 1) * 128, :], xi, writes=[xi])
                out_toks.append(emit_ln(kb, L, xi, xi, [acc[:, t, 0:512], acc[:, t, 512:1024]], accv[t], g, b,
                                        y[tile * 128:(tile + 1) * 128, :]))
        kb.wait_only('sp', out_toks)
        kb.emit()
    return nc


def pool_tables(first_is_start, last_is_end):
    Bm = np.zeros((3, 4, 128, 128), np.float32)
    Bh = np.zeros((3, 4, 16, 128), np.float32)
    for var in range(3):
        start_edge = (var == 0 and first_is_start)
        end_edge = (var == 2 and last_is_end)
        for g, w in enumerate((2, 4, 8, 16)):
            half = w // 2
            for t in range(128):
                lo, hi = t - half, t + half - 1
                if start_edge:
                    lo = max(lo, 0)
                if end_edge:
                    hi = min(hi, 127)
                cnt = hi - lo + 1
                for s in range(lo, hi + 1):
                    if 0 <= s < 128:
                        Bm[var, g, s, t] += 1.0 / cnt
                    elif s < 0:
                        Bh[var, g, 8 + s, t] += 1.0 / cnt
                    else:
                        Bh[var, g, 8 + (s - 128), t] += 1.0 / cnt
                Bm[var, g, t, t] -= 1.0
    return Bm, Bh


def build_pool(NT=32):
    nc = bass.Bass("TRN2", target_bir_lowering=False)
    T = NT * 128
    xp = nc.dram_tensor("xp", [T + 16, D], F32, kind="ExternalInput").ap()
    bm = nc.dram_tensor("bm", [128, 12 * 128], F32, kind="ExternalInput").ap()
    bh = nc.dram_tensor("bh", [16, 12 * 128], F32, kind="ExternalInput").ap()
    pw = nc.dram_tensor("pw", [4, 256, 256], F32, kind="ExternalInput").ap()
    psc = nc.dram_tensor("psc", [128, D], F32, kind="ExternalInput").ap()
    lng = nc.dram_tensor("lng", [128, D], F32, kind="ExternalInput").ap()
    lnb = nc.dram_tensor("lnb", [128, D], F32, kind="ExternalInput").ap()
    y = nc.dram_tensor("y", [T, D], F32, kind="ExternalOutput").ap()
    with ExitStack() as es:
        kb = KB(nc, es)
        g = kb.sb([128, D], F32)
        b = kb.sb([128, D], F32)
        kb.dma('sp', g[:], lng[:, :], g, writes=[g])
        kb.dma('sp', b[:], lnb[:, :], b, writes=[b])
        L = LNBufs(kb, nbuf=2)
        Bm = kb.sb([128, 12 * 128], BF16)
        Bh = kb.sb([16, 12 * 128], BF16)
        kb.dma('pool', Bm[:], bm[:, :], Bm, writes=[Bm])
        kb.dma('pool', Bh[:], bh[:, :], Bh, writes=[Bh])
        W32 = kb.sb([128, 4, 2, 256], F32)
        sc = kb.sb([128, D], F32)
        Wb = kb.sb([128, 4, 2, 256], BF16)
        kb.dma('sp', W32[:], pw.rearrange("g (hh p) e -> p g hh e", p=128), W32, writes=[W32])
        kb.dma('sp', sc[:], psc[:, :], sc, writes=[sc])
        kb.op('dve', lambda e: e.tensor_tensor(out=Wb[:], in0=W32[:],
                                               in1=sc[:].rearrange("p (g e) -> p g e", g=4).unsqueeze(2).broadcast_to([128, 4, 2, 256]),
                                               op=ALU.mult), reads=[W32, sc], writes=[Wb])
        xin = [kb.sb([128, D], F32) for _ in range(3)]
        xh = [kb.sb([16, D], F32) for _ in range(2)]
        xb = [kb.sb([128, D], BF16) for _ in range(2)]
        xhb = [kb.sb([16, D], BF16) for _ in range(2)]
        uT = [kb.sb([128, 8, 128], BF16) for _ in range(2)]
        uTa = [kb.view() for _ in range(2)]
        uTb = [kb.view() for _ in range(2)]
        pu = [[kb.ps([128, 512], F32) for _ in range(2)] for _ in range(2)]
        py = [[kb.ps([128, 512], F32) for _ in range(2)] for _ in range(2)]
        out_toks = []
        for i in range(NT):
            var = 0 if i == 0 else (2 if i == NT - 1 else 1)
            xi = xin[i % 3]
            xhi = xh[i % 2]
            xbi = xb[i % 2]
            xhbi = xhb[i % 2]
            u = uT[i % 2]
            ua, ub = uTa[i % 2], uTb[i % 2]
            pui = pu[i % 2]
            pyi = py[i % 2]
            kb.dma('sp', xi[:], xp[8 + i * 128: 8 + (i + 1) * 128, :], xi, writes=[xi])
            kb.dma('sp', xhi[0:8, :], xp[i * 128: i * 128 + 8, :], xhi, writes=[xhi])
            kb.dma('sp', xhi[8:16, :], xp[8 + (i + 1) * 128: 16 + (i + 1) * 128, :], xhi, writes=[xhi])
            kb.op('act', lambda e, xi=xi, xbi=xbi: e.copy(out=xbi[:], in_=xi[:]), reads=[xi], writes=[xbi])
            kb.op('dve', lambda e, xhi=xhi, xhbi=xhbi: e.tensor_copy(out=xhbi[:], in_=xhi[:]), reads=[xhi], writes=[xhbi])
            for hf in range(2):
                fns = []
                for fc in range(hf * 4, hf * 4 + 4):
                    col = (var * 4 + fc // 2) * 128
                    fns.append(lambda e, fc=fc, col=col, xbi=xbi, bk=pui[hf]: e.matmul(
                        bk[:, (fc % 4) * 128:(fc % 4 + 1) * 128], xbi[:, fc * 128:(fc + 1) * 128], Bm[:, col:col + 128],
                        start=True, stop=False))
                    fns.append(lambda e, fc=fc, col=col, xhbi=xhbi, bk=pui[hf]: e.matmul(
                        bk[:, (fc % 4) * 128:(fc % 4 + 1) * 128], xhbi[:, fc * 128:(fc + 1) * 128], Bh[:, col:col + 128],
                        start=False, stop=True))
                kb.group('pe', fns, reads=[xbi, xhbi, Bm, Bh], writes=[pui[hf]])
            kb.op('act', lambda e, u=u, bk=pui[0]: e.copy(out=u[:, 0:4, :], in_=bk[:].rearrange("p (c t) -> p c t", c=4)),
                  reads=[pui[0]], writes=[ua])
            kb.op('dve', lambda e, u=u, bk=pui[1]: e.tensor_copy(out=u[:, 4:8, :], in_=bk[:].rearrange("p (c t) -> p c t", c=4)),
                  reads=[pui[1]], writes=[ub])
            for bkidx in range(2):
                fns = []
                for gg in range(bkidx * 2, bkidx * 2 + 2):
                    for hh in range(2):
                        fns.append(lambda e, gg=gg, hh=hh, u=u, bk=pyi[bkidx]: e.matmul(
                            bk[:, (gg % 2) * 256:(gg % 2 + 1) * 256], u[:, gg * 2 + hh, :], Wb[:, gg, hh, :],
                            start=(hh == 0), stop=(hh == 1)))
                kb.group('pe', fns, reads=[ua if bkidx == 0 else ub, Wb], writes=[pyi[bkidx]])
            out_toks.append(emit_ln(kb, L, xi, xi, [pyi[0][:], pyi[1][:]], pyi, g, b, y[i * 128:(i + 1) * 128, :]))
        kb.wait_only('sp', out_toks)
        kb.emit()
    return nc


def build_qkv(NT=32):
    nc = bass.Bass("TRN2", target_bir_lowering=False)
    T = NT * 128
    x = nc.dram_tensor("x", [T, D], F32, kind="ExternalInput").ap()
    win = nc.dram_tensor("win", [D, 3 * D], F32, kind="ExternalInput").ap()
    cs = nc.dram_tensor("cs", [T, 64], F32, kind="ExternalInput").ap()
    qkv = nc.dram_tensor("qkv", [T, 3 * D], BF16, kind="ExternalOutput").ap()
    with ExitStack() as es:
        kb = KB(nc, es)
        ident = kb.sb([128, 128], F32)
        make_ident(kb, ident)
        wb = kb.sb([128, 8, 3 * D], BF16)
        wv = [kb.view() for _ in range(8)]
        for c in range(8):
            kb.dma('pool', wb[:, c, :], win[c * 128:(c + 1) * 128, :], wv[c], writes=[wv[c]])
        kb.op('pool', lambda e: e.tensor_scalar(out=wb[:, :, 0:D], in0=wb[:, :, 0:D], scalar1=0.125, scalar2=None, op0=ALU.mult),
              reads=[], writes=wv)
        xin = [kb.sb([128, D], F32) for _ in range(2)]
        cst = [kb.sb([128, 64], F32) for _ in range(2)]
        xT = [kb.sb([128, 8, 128], BF16) for _ in range(2)]
        kro = [kb.sb([128, 512], F32) for _ in range(2)]
        tm = [kb.sb([128, 8, 32], F32) for _ in range(4)]
        tp = [kb.sb([128, 8, 32], F32) for _ in range(4)]
        ost = [kb.sb([128, 3 * D], BF16) for _ in range(2)]
        ostv = [[kb.view() for _ in range(6)] for _ in range(2)]
        banks = [kb.ps([128, 512], F32) for _ in range(8)]
        nb = 0
        out_toks = []
        for i in range(NT):
            xi = xin[i % 2]
            ci = cst[i % 2]
            xt = xT[i % 2]
            oi = ost[i % 2]
            ov = ostv[i % 2]
            kr = kro[i % 2]
            kb.dma('sp', xi[:], x[i * 128:(i + 1) * 128, :], xi, writes=[xi])
            kb.dma('sp', ci[:], cs[i * 128:(i + 1) * 128, :], ci, writes=[ci])
            for hf in range(2):
                bk = banks[nb % 8]; nb += 1
                kb.group('pe', [lambda e, c=c, bk=bk, xi=xi: e.transpose(bk[:, (c % 4) * 128:(c % 4 + 1) * 128],
                                                                           xi[:, c * 128:(c + 1) * 128], ident[:])
                                for c in range(hf * 4, hf * 4 + 4)], reads=[xi, ident], writes=[bk])
                eng = 'act' if hf == 0 else 'dve'
                fn = (lambda e, bk=bk, hf=hf, xt=xt: e.copy(out=xt[:, hf * 4:(hf + 1) * 4, :], in_=bk[:].rearrange("p (c t) -> p c t", c=4))) \
                    if hf == 0 else (lambda e, bk=bk, hf=hf, xt=xt: e.tensor_copy(out=xt[:, hf * 4:(hf + 1) * 4, :], in_=bk[:].rearrange("p (c t) -> p c t", c=4)))
                if hf == 0:
                    t_a = kb.op(eng, fn, reads=[bk], writes=[xt])
                else:
                    t_b = kb.op(eng, fn, reads=[bk], deps=[t_a])
                    xt.w = [t_a, t_b]
            cosb = ci[:, 0:32].unsqueeze(1).broadcast_to([128, 8, 32])
            sinb = ci[:, 32:64].unsqueeze(1).broadcast_to([128, 8, 32])
            for blk in range(6):
                bk = banks[nb % 8]; nb += 1
                kb.group('pe', [lambda e, c=c, bk=bk, blk=blk, xt=xt: e.matmul(bk[:], xt[:, c, :], wb[:, c, blk * 512:(blk + 1) * 512],
                                                                               start=(c == 0), stop=(c == 7)) for c in range(8)],
                         reads=[xt] + wv, writes=[bk])
                osl = slice(blk * 512, (blk + 1) * 512)
                if blk in (0, 2):
                    if blk == 0:
                        eng, tt, src, sbuf = 'dve', tm, bk, bk
                    else:
                        kb.op('act', lambda e, bk=bk, kr=kr: e.copy(out=kr[:], in_=bk[:]), reads=[bk], writes=[kr])
                        eng, tt, src, sbuf = 'pool', tp, kr, kr
                    s4 = src[:].rearrange("p (h two j) -> p h two j", two=2, j=32)
                    o4 = oi[:, osl].rearrange("p (h two j) -> p h two j", two=2, j=32)
                    t1, t2 = s4[:, :, 0, :], s4[:, :, 1, :]
                    kb.op(eng, lambda e, t1=t1, tt=tt, cosb=cosb: e.tensor_tensor(out=tt[0][:], in0=t1, in1=cosb, op=ALU.mult), reads=[sbuf, ci], writes=[tt[0]])
                    kb.op(eng, lambda e, t2=t2, tt=tt, sinb=sinb: e.tensor_tensor(out=tt[1][:], in0=t2, in1=sinb, op=ALU.mult), reads=[sbuf, ci], writes=[tt[1]])
                    kb.op(eng, lambda e, t2=t2, tt=tt, cosb=cosb: e.tensor_tensor(out=tt[2][:], in0=t2, in1=cosb, op=ALU.mult), reads=[sbuf, ci], writes=[tt[2]])
                    kb.op(eng, lambda e, t1=t1, tt=tt, sinb=sinb: e.tensor_tensor(out=tt[3][:], in0=t1, in1=sinb, op=ALU.mult), reads=[sbuf, ci], writes=[tt[3]])
                    kb.op(eng, lambda e, tt=tt, o4=o4: e.tensor_tensor(out=o4[:, :, 0, :], in0=tt[0][:], in1=tt[1][:], op=ALU.subtract), reads=[tt[0], tt[1]], writes=[ov[blk]])
                    tk = kb.op(eng, lambda e, tt=tt, o4=o4: e.tensor_tensor(out=o4[:, :, 1, :], in0=tt[2][:], in1=tt[3][:], op=ALU.add), reads=[tt[2], tt[3]], deps=ov[blk].w)
                    ov[blk].w = ov[blk].w + [tk]
                else:
                    kb.op('act', lambda e, bk=bk, oi=oi, osl=osl: e.copy(out=oi[:, osl], in_=bk[:]), reads=[bk], writes=[ov[blk]])
            out_toks.append(kb.dma('sp', qkv[i * 128:(i + 1) * 128, :], oi[:], oi, reads=ov))
        kb.wait_only('sp', out_toks)
        kb.emit()
    return nc


def build_attn(NB=4):
    nc = bass.Bass("TRN2", target_bir_lowering=False)
    S = S_LEN
    qA, kA, vA, oA = [], [], [], []
    for di, d in enumerate(DILS):
        Ls = S // d
        nt = Ls // 128
        qA.append(nc.dram_tensor(f"qA{di}", [NB, 64, S], BF16, kind="ExternalInput").ap())
        kA.append(nc.dram_tensor(f"kA{di}", [NB, 64, d * (Ls + 128)], BF16, kind="ExternalInput").ap())
        vA.append(nc.dram_tensor(f"vA{di}", [NB, 128, d * (nt + 1) * 65], BF16, kind="ExternalInput").ap())
        oA.append(nc.dram_tensor(f"oA{di}", [NB, 128, 64 * 65], F32, kind="ExternalOutput").ap())
    qB = nc.dram_tensor("qB", [NB, 64, S], BF16, kind="ExternalInput").ap()
    kB = nc.dram_tensor("kB", [NB, 64, S], BF16, kind="ExternalInput").ap()
    vB = nc.dram_tensor("vB", [NB, 128, 64 * 65], BF16, kind="ExternalInput").ap()
    oB = nc.dram_tensor("oB", [NB, 128, 64 * 64], F32, kind="ExternalOutput").ap()
    ebias = nc.dram_tensor("ebias", [128, 25 * 128], F32, kind="ExternalInput").ap()
    maskd = nc.dram_tensor("maskd", [128, 256], F32, kind="ExternalInput").ap()
    with ExitStack() as es:
        kb = KB(nc, es)
        mask = kb.sb([128, 256], BF16)
        kb.dma('pool', mask[:], maskd[:, :], mask, writes=[mask])
        eb32 = kb.sb([128, 25 * 128], F32)
        E = kb.sb([128, 25 * 128], BF16)
        kb.dma('sp', eb32[:], ebias[:, :], eb32, writes=[eb32])
        kb.op('act', lambda e: e.activation(out=E[:], in_=eb32[:], func=AF.Exp), reads=[eb32], writes=[E])
        KMAX = 16 * (512 + 128)
        qT = [kb.sb([64, S], BF16) for _ in range(2)]
        kT = [kb.sb([64, KMAX], BF16) for _ in range(2)]
        V = [kb.sb([128, 80 * 65], BF16) for _ in range(2)]
        osb = [kb.sb([128, 64 * 65], F32) for _ in range(2)]
        pT = [kb.sb([128, 256], BF16) for _ in range(4)]
        rden = [kb.sb([128, 1], F32) for _ in range(2)]
        sbk = [kb.ps([128, 512], F32) for _ in range(3)]
        obk = [kb.ps([128, 512], F32) for _ in range(4)]
        stages = []
        for b in range(NB):
            for di in range(3):
                stages.append(('A', b, di))
            stages.append(('B', b, None))

        def load(si):
            kind, b, di = stages[si]
            sl = si % 2
            if kind == 'A':
                d = DILS[di]
                Ls = S // d
                nt = Ls // 128
                kb.dma('sp', qT[sl][:], qA[di][b], qT[sl], writes=[qT[sl]])
                kb.dma('sp', kT[sl][:, 0:d * (Ls + 128)], kA[di][b], kT[sl], writes=[kT[sl]])
                kb.dma('sp', V[sl][:, 0:d * (nt + 1) * 65], vA[di][b], V[sl], writes=[V[sl]])
            else:
                kb.dma('sp', qT[sl][:], qB[b], qT[sl], writes=[qT[sl]])
                kb.dma('sp', kT[sl][:, 0:S], kB[b], kT[sl], writes=[kT[sl]])
                kb.dma('sp', V[sl][:, 0:64 * 65], vB[b], V[sl], writes=[V[sl]])

        load(0)
        step = 0
        out_toks = []
        for si, (kind, b, di) in enumerate(stages):
            if si + 1 < len(stages):
                load(si + 1)
            sl = si % 2
            q, k, v, ob = qT[sl], kT[sl], V[sl], osb[sl]
            if kind == 'A':
                d = DILS[di]
                Ls = S // d
                nt = Ls // 128
                for r in range(d):
                    for j in range(nt + 1):
                        q0 = 128 * (j - 1) if j >= 1 else 0
                        q1 = 128 * (j + 1) if j <= nt - 1 else 128 * nt
                        w = q1 - q0
                        moff = 128 if j == 0 else 0
                        sb_ = sbk[step % 3]
                        p = pT[step % 4]
                        koff = r * (Ls + 128) + 128 * j
                        vt = r * (nt + 1) + j
                        kb.op('pe', lambda e, sb_=sb_, k=k, q=q, koff=koff, qa=r * Ls + q0, w=w: e.matmul(
                            sb_[:, 0:w], k[:, koff:koff + 128], q[:, qa:qa + w], start=True, stop=True),
                            reads=[k, q], writes=[sb_])
                        kb.op('act', lambda e, sb_=sb_, p=p, w=w: e.activation(out=p[:, 0:w], in_=sb_[:, 0:w], func=AF.Exp),
                              reads=[sb_], writes=[p])
                        meng = 'dve' if step % 2 == 0 else 'pool'
                        kb.op(meng, lambda e, p=p, w=w, moff=moff: e.tensor_tensor(out=p[:, 0:w], in0=p[:, 0:w],
                                                                                 in1=mask[:, moff:moff + w], op=ALU.mult),
                              reads=[p, mask], writes=[p])
                        if j >= 1:
                            o_ = obk[(j - 1) % 4]
                            kb.op('pe', lambda e, o_=o_, p=p, v=v, vt=vt: e.matmul(o_[:, 0:65], p[:, 0:128], v[:, vt * 65:(vt + 1) * 65],
                                                                                   start=False, stop=True),
                                  reads=[p, v], writes=[], deps=o_.w + list(o_.r.values()))
                            o_.w = [Tok(kb.sems['pe'], kb.cnt['pe'])]
                            blk = r * nt + (j - 1)
                            kb.op('dve', lambda e, o_=o_, ob=ob, blk=blk: e.tensor_copy(out=ob[:, blk * 65:(blk + 1) * 65], in_=o_[:, 0:65]),
                                  reads=[o_], writes=[], deps=list(ob.r.values()))
                            ob.w = [Tok(kb.sems['dve'], kb.cnt['dve'])]
                        if j <= nt - 1:
                            o_ = obk[j % 4]
                            pc = 128 if j >= 1 else 0
                            kb.op('pe', lambda e, o_=o_, p=p, v=v, vt=vt, pc=pc: e.matmul(o_[:, 0:65], p[:, pc:pc + 128],
                                                                                         v[:, vt * 65:(vt + 1) * 65], start=True, stop=False),
                                  reads=[p, v], writes=[o_])
                        step += 1
                out_toks.append(kb.dma('sp', oA[di][b], ob[:], ob, reads=[ob]))
            else:
                for m in range(64):
                    cls = {0: 0, 1: 1, 62: 3, 63: 4}.get(m, 2)
                    bt = min(max(m - 2, 0), 59)
                    o_ = obk[m % 4]
                    for j in range(5):
                        tile = bt + j
                        sb_ = sbk[step % 3]
                        p = pT[step % 4]
                        kb.op('pe', lambda e, sb_=sb_, k=k, q=q, tile=tile, m=m: e.matmul(
                            sb_[:, 0:128], k[:, tile * 128:(tile + 1) * 128], q[:, m * 128:(m + 1) * 128], start=True, stop=True),
                            reads=[k, q], writes=[sb_])
                        kb.op('act', lambda e, sb_=sb_, p=p: e.activation(out=p[:, 0:128], in_=sb_[:, 0:128], func=AF.Exp),
                              reads=[sb_], writes=[p])
                        meng = 'dve' if step % 2 == 0 else 'pool'
                        ecol = (cls * 5 + j) * 128
                        kb.op(meng, lambda e, p=p, ecol=ecol: e.tensor_tensor(out=p[:, 0:128], in0=p[:, 0:128],
                                                                             in1=E[:, ecol:ecol + 128], op=ALU.mult),
                              reads=[p, E], writes=[p])
                        if j == 0:
                            kb.op('pe', lambda e, o_=o_, p=p, v=v, tile=tile: e.matmul(o_[:, 0:65], p[:, 0:128], v[:, tile * 65:(tile + 1) * 65],
                                                                                     start=True, stop=False),
                                  reads=[p, v], writes=[o_])
                        else:
                            kb.op('pe', lambda e, o_=o_, p=p, v=v, tile=tile, j=j: e.matmul(o_[:, 0:65], p[:, 0:128], v[:, tile * 65:(tile + 1) * 65],
                                                                                          start=False, stop=(j == 4)),
                                  reads=[p, v], writes=[], deps=o_.w)
                            o_.w = [Tok(kb.sems['pe'], kb.cnt['pe'])]
                        step += 1
                    rd = rden[m % 2]
                    kb.op('dve', lambda e, o_=o_, rd=rd: e.reciprocal(out=rd[:], in_=o_[:, 64:65]), reads=[o_], writes=[rd])
                    kb.op('dve', lambda e, o_=o_, rd=rd, ob=ob, m=m: e.tensor_scalar(out=ob[:, m * 64:(m + 1) * 64], in0=o_[:, 0:64],
                                                                                   scalar1=rd[:, 0:1], scalar2=None, op0=ALU.mult),
                          reads=[o_, rd], writes=[], deps=list(ob.r.values()))
                    ob.w = [Tok(kb.sems['dve'], kb.cnt['dve'])]
                out_toks.append(kb.dma('sp', oB[b], ob[:, 0:64 * 64], ob, reads=[ob]))
        kb.wait_only('sp', out_toks)
        kb.emit()
    return nc


def build_oproj(NT=32):
    nc = bass.Bass("TRN2", target_bir_lowering=False)
    T = NT * 128
    x = nc.dram_tensor("x", [T, D], F32, kind="ExternalInput").ap()
    oa = nc.dram_tensor("oa", [3, T, 8 * 65], F32, kind="ExternalInput").ap()
    obd = nc.dram_tensor("ob", [T, 512], F32, kind="ExternalInput").ap()
    wout = nc.dram_tensor("wout", [D, D], F32, kind="ExternalInput").ap()
    lng = nc.dram_tensor("lng", [128, D], F32, kind="ExternalInput").ap()
    lnb = nc.dram_tensor("lnb", [128, D], F32, kind="ExternalInput").ap()
    y = nc.dram_tensor("y", [T, D], F32, kind="ExternalOutput").ap()
    with ExitStack() as es:
        kb = KB(nc, es)
        ident = kb.sb([128, 128], F32)
        make_ident(kb, ident)
        g = kb.sb([128, D], F32)
        b = kb.sb([128, D], F32)
        kb.dma('sp', g[:], lng[:, :], g, writes=[g])
        kb.dma('sp', b[:], lnb[:, :], b, writes=[b])
        L = LNBufs(kb, nbuf=2)
        wb = kb.sb([128, 8, D], BF16)
        kb.dma('pool', wb[:], wout.rearrange("(c p) f -> p c f", p=128), wb, writes=[wb])
        xin = [kb.sb([128, D], F32) for _ in range(3)]
        oat = [[kb.sb([128, 8, 65], F32) for _ in range(3)] for _ in range(2)]
        rd = [kb.sb([128, 8], F32) for _ in range(2)]
        ot = [kb.sb([128, D], F32) for _ in range(2)]
        ota = [kb.view() for _ in range(2)]
        otb = [kb.view() for _ in range(2)]
        oT = [kb.sb([128, 8, 128], BF16) for _ in range(2)]
        oTa = [kb.view() for _ in range(2)]
        oTb = [kb.view() for _ in range(2)]
        pt = [[kb.ps([128, 512], F32) for _ in range(2)] for _ in range(2)]
        py = [[kb.ps([128, 512], F32) for _ in range(2)] for _ in range(2)]
        out_toks = []
        for i in range(NT):
            xi = xin[i % 3]
            a3 = oat[i % 2]
            o = ot[i % 2]
            r_ = rd[i % 2]
            oTi = oT[i % 2]
            rows = slice(i * 128, (i + 1) * 128)
            kb.dma('sp', xi[:], x[rows, :], xi, writes=[xi])
            for br in range(3):
                kb.dma('sp', a3[br][:], oa[br, rows, :].rearrange("p (h c) -> p h c", c=65), a3[br], writes=[a3[br]])
            kb.dma('sp', o[:, 512:1024], obd[rows, :], otb[i % 2], writes=[otb[i % 2]])
            kb.op('dve', lambda e, a3=a3: e.tensor_tensor(out=a3[0][:], in0=a3[0][:], in1=a3[1][:], op=ALU.add), reads=[a3[1]], writes=[a3[0]])
            kb.op('dve', lambda e, a3=a3: e.tensor_tensor(out=a3[0][:], in0=a3[0][:], in1=a3[2][:], op=ALU.add), reads=[a3[2]], writes=[a3[0]])
            kb.op('dve', lambda e, a3=a3, r_=r_: e.reciprocal(out=r_[:], in_=a3[0][:, :, 64]), reads=[a3[0]], writes=[r_])
            kb.op('dve', lambda e, a3=a3, r_=r_, o=o: e.tensor_tensor(out=o[:, 0:512].rearrange("p (h c) -> p h c", c=64), in0=a3[0][:, :, 0:64],
                                                                   in1=r_[:].unsqueeze(2).broadcast_to([128, 8, 64]), op=ALU.mult),
                  reads=[a3[0], r_], writes=[ota[i % 2]])
            for hf in range(2):
                bk = pt[i % 2][hf]
                kb.group('pe', [lambda e, c=c, bk=bk, o=o: e.transpose(bk[:, (c % 4) * 128:(c % 4 + 1) * 128],
                                                                        o[:, c * 128:(c + 1) * 128], ident[:])
                                for c in range(hf * 4, hf * 4 + 4)], reads=[ota[i % 2] if hf == 0 else otb[i % 2], ident], writes=[bk])
                if hf == 0:
                    kb.op('act', lambda e, bk=bk, oTi=oTi: e.copy(out=oTi[:, 0:4, :], in_=bk[:].rearrange("p (c t) -> p c t", c=4)),
                          reads=[bk], writes=[oTa[i % 2]])
                else:
                    kb.op('dve', lambda e, bk=bk, oTi=oTi: e.tensor_copy(out=oTi[:, 4:8, :], in_=bk[:].rearrange("p (c t) -> p c t", c=4)),
                          reads=[bk], writes=[oTb[i % 2]])
            for oh in range(2):
                bk = py[i % 2][oh]
                kb.group('pe', [lambda e, c=c, bk=bk, oTi=oTi, oh=oh: e.matmul(bk[:], oTi[:, c, :], wb[:, c, oh * 512:(oh + 1) * 512],
                                                                               start=(c == 0), stop=(c == 7)) for c in range(8)],
                         reads=[oTa[i % 2], oTb[i % 2], wb], writes=[bk])
            out_toks.append(emit_ln(kb, L, xi, xi, [py[i % 2][0][:], py[i % 2][1][:]], py[i % 2], g, b, y[rows, :]))
        kb.wait_only('sp', out_toks)
        kb.emit()
    return nc

import ml_dtypes
BF = ml_dtypes.bfloat16
S_LEN = 8192
DILS = (1, 4, 16)


def rope_cs(pos):
    pos = pos.astype(np.float32)
    inv = (np.float32(10000.0) ** (-np.arange(0, 64, 2, dtype=np.float32) / np.float32(64))).astype(np.float32)
    ang = (pos[:, None] * inv[None, :]).astype(np.float32)
    return np.concatenate([np.cos(ang), np.sin(ang)], 1).astype(np.float32)


def attn_mask():
    p = np.arange(128)[:, None]
    a = np.arange(128)[None, :]
    return np.concatenate([(p <= a), (p >= a)], 1).astype(np.float32)


def na_bias_table(rpb_h):
    out = np.full((128, 25, 128), -30000.0, np.float32)
    kk = np.arange(128)
    qq = np.arange(128)
    for cls, m in enumerate((0, 1, 2, 62, 63)):
        base = min(max(2 * m - 4, 0), 118)
        qrow = 2 * m + qq // 64
        qcol = qq % 64
        rstart = np.clip(qrow - 4, 0, 120)
        cstart = np.clip(qcol - 8, 0, 48)
        for j in range(5):
            krow = base + 2 * j + kk // 64
            kcol = kk % 64
            ok = ((krow[:, None] >= rstart[None, :]) & (krow[:, None] < rstart[None, :] + 8)
                  & (kcol[:, None] >= cstart[None, :]) & (kcol[:, None] < cstart[None, :] + 16))
            roff = np.clip(krow[:, None] - qrow[None, :] + 7, 0, 14)
            coff = np.clip(kcol[:, None] - qcol[None, :], -15, 15) + 15
            vals = rpb_h[roff, coff]
            out[:, cls * 5 + j, :] = np.where(ok, vals, np.float32(-30000.0))
    return out.reshape(128, 25 * 128)


def attn_core_inputs(qkv, c, rpb_i):
    B, S = qkv.shape[0], qkv.shape[1]
    ha, hb = c, 8 + c
    im = {}
    q_h, k_h, v_h = qkv[:, :, 0, ha], qkv[:, :, 1, ha], qkv[:, :, 2, ha]
    one = np.ones((), BF)
    for di, d in enumerate(DILS):
        Ls = S // d
        nt = Ls // 128
        im[f"qA{di}"] = np.ascontiguousarray(q_h.reshape(B, Ls, d, 64).transpose(0, 3, 2, 1)).reshape(B, 64, d * Ls)
        kk = np.zeros((B, 64, d, Ls + 128), BF)
        kk[:, :, :, 64:64 + Ls] = k_h.reshape(B, Ls, d, 64).transpose(0, 3, 2, 1)
        im[f"kA{di}"] = kk.reshape(B, 64, d * (Ls + 128))
        vv = np.zeros((B, d, Ls + 128, 65), BF)
        vv[:, :, 64:64 + Ls, :64] = v_h.reshape(B, Ls, d, 64).transpose(0, 2, 1, 3)
        vv[:, :, 64:64 + Ls, 64] = one
        im[f"vA{di}"] = np.ascontiguousarray(vv.reshape(B, d, nt + 1, 128, 65).transpose(0, 3, 1, 2, 4)).reshape(B, 128, d * (nt + 1) * 65)
    im["qB"] = np.ascontiguousarray(qkv[:, :, 0, hb].transpose(0, 2, 1))
    im["kB"] = np.ascontiguousarray(qkv[:, :, 1, hb].transpose(0, 2, 1))
    vb = np.zeros((B, S, 65), BF)
    vb[:, :, :64] = qkv[:, :, 2, hb]
    vb[:, :, 64] = one
    im["vB"] = np.ascontiguousarray(vb.reshape(B, 64, 128, 65).transpose(0, 2, 1, 3)).reshape(B, 128, 64 * 65)
    im["ebias"] = na_bias_table(rpb_i[c])
    im["maskd"] = attn_mask()
    return im


def attn_core_outputs(results, B, S):
    oa = np.zeros((3, B, S, 8, 65), np.float32)
    ob = np.zeros((B, S, 8, 64), np.float32)
    for c, r in enumerate(results):
        for di, d in enumerate(DILS):
            Ls = S // d
            nt = Ls // 128
            a = r[f"oA{di}"].reshape(B, 128, d, nt, 65).transpose(0, 3, 1, 2, 4).reshape(B, S, 65)
            oa[di, :, :, c, :] = a
        ob[:, :, c, :] = r["oB"].reshape(B, 128, 64, 64).transpose(0, 2, 1, 3).reshape(B, S, 64)
    return oa.reshape(3, B, S, 8 * 65), ob.reshape(B, S, 512)


N_CORES = 8
_PROGS = {}


def _prog(name, fn):
    if name not in _PROGS:
        _PROGS[name] = fn()
    return _PROGS[name]


def _run(nc, in_maps):
    return run_bass_kernel_spmd(nc, in_maps, core_ids=list(range(N_CORES))).results


def _rep(v):
    return np.ascontiguousarray(np.broadcast_to(np.asarray(v, np.float32)[None], (128, 1024)))


def _attn_layer(xf, w_in, w_out, rpb_i, g, b, B, S):
    T = xf.shape[0] // N_CORES
    NT = T // 128
    halves = S // T
    nc1 = _prog('qkv', lambda: build_qkv(NT=NT))
    ims = []
    for c in range(N_CORES):
        p0 = (c % halves) * T
        ims.append({"x": xf[c * T:(c + 1) * T], "win": w_in, "cs": rope_cs(np.arange(p0, p0 + T))})
    r1 = _run(nc1, ims)
    qkv = np.concatenate([r["qkv"] for r in r1], 0).reshape(B, S, 3, 16, 64)
    nc2 = _prog('attn', lambda: build_attn(NB=B))
    r2 = _run(nc2, [attn_core_inputs(qkv, c, rpb_i) for c in range(N_CORES)])
    oa, ob = attn_core_outputs(r2, B, S)
    oa = oa.reshape(3, B * S, 8 * 65)
    ob = ob.reshape(B * S, 512)
    nc3 = _prog('oproj', lambda: build_oproj(NT=NT))
    ims = [{"x": xf[c * T:(c + 1) * T], "oa": np.ascontiguousarray(oa[:, c * T:(c + 1) * T]),
            "ob": np.ascontiguousarray(ob[c * T:(c + 1) * T]), "wout": w_out, "lng": _rep(g), "lnb": _rep(b)}
           for c in range(N_CORES)]
    r3 = _run(nc3, ims)
    return np.concatenate([r["y"] for r in r3], 0)


def _pool_layer(xf, pw, psc, g, b, B, S):
    T = xf.shape[0] // N_CORES
    NT = T // 128
    halves = S // T
    ncp = _prog('pool', lambda: build_pool(NT=NT))
    ims = []
    for c in range(N_CORES):
        h = c % halves
        xp = np.zeros((T + 16, 1024), np.float32)
        lo = c * T - (8 if h > 0 else 0)
        hi = (c + 1) * T + (8 if h < halves - 1 else 0)
        xp[8 - (c * T - lo): 8 + T + (hi - (c + 1) * T)] = xf[lo:hi]
        Bm, Bh = pool_tables(h == 0, h == halves - 1)
        ims.append({"xp": xp, "bm": np.ascontiguousarray(Bm.transpose(2, 0, 1, 3).reshape(128, 12 * 128)),
                    "bh": np.ascontiguousarray(Bh.transpose(2, 0, 1, 3).reshape(16, 12 * 128)),
                    "pw": pw, "psc": _rep(psc), "lng": _rep(g), "lnb": _rep(b)})
    r = _run(ncp, ims)
    return np.concatenate([q["y"] for q in r], 0)


def _moe_layer(xf, rw, rbias, w1, w3, w2, g, b):
    T = xf.shape[0] // N_CORES
    NT = T // 128
    GT = 8
    ncm = _prog('moe', lambda: build_moe(NT=NT, GT=GT, NE=16))
    rb = np.ascontiguousarray(np.broadcast_to(np.tile(np.asarray(rbias, np.float32), GT)[None], (128, GT * 16)))
    ims = [{"x": xf[c * T:(c + 1) * T], "rw": rw, "rbias": rb, "w1": w1, "w3": w3, "w2": w2, "lng": _rep(g), "lnb": _rep(b)}
           for c in range(N_CORES)]
    r = _run(ncm, ims)
    return np.concatenate([q["y"] for q in r], 0)


def kernel(x, w_in, w_out, rpb, pool_w, pool_scale, router_w, router_bias, moe_w1, moe_w3, moe_w2, ln_g, ln_b):
    f = lambda a: np.ascontiguousarray(np.asarray(a, np.float32))
    x = f(x)
    B, S, Dm = x.shape
    xf = x.reshape(B * S, Dm)
    depth = moe_w1.shape[0]
    for layer in range(depth):
        i = layer // 2
        if layer % 2 == 0:
            xf = _attn_layer(xf, f(w_in[i]), f(w_out[i]), f(rpb[i]), ln_g[layer, 0], ln_b[layer, 0], B, S)
        else:
            xf = _pool_layer(xf, f(pool_w[i]), pool_scale[i], ln_g[layer, 0], ln_b[layer, 0], B, S)
        xf = _moe_layer(xf, f(router_w), router_bias, f(moe_w1[layer]), f(moe_w3[layer]), f(moe_w2[layer]),
                        ln_g[layer, 1], ln_b[layer, 1])
    return xf.reshape(B, S, Dm).astype(np.float32)
```

```python
import numpy as np
import concourse.bass as bass
import concourse.mybir as mybir
from concourse.bass_utils import run_bass_kernel_spmd
from contextlib import ExitStack

F32 = mybir.dt.float32
BF16 = mybir.dt.bfloat16
U32 = mybir.dt.uint32
I32 = mybir.dt.int32
AF = mybir.ActivationFunctionType
ALU = mybir.AluOpType
AX = mybir.AxisListType

ENGS = ['pe', 'act', 'dve', 'pool', 'sp']
ENG_ATTR = {'pe': 'tensor', 'act': 'scalar', 'dve': 'vector', 'pool': 'gpsimd', 'sp': 'sync'}


class Tok:
    __slots__ = ('sem', 'val')

    def __init__(self, sem, val):
        self.sem = sem
        self.val = val


class Buf:
    def __init__(self, kb, t, name):
        self.kb = kb
        self.t = t
        self.name = name
        self.w = []
        self.r = {}
        self._slot = None

    def __getitem__(self, k):
        return self.t[k]

    def slot(self):
        if self._slot is None:
            self._slot = self.kb.new_sem()
            self._slotval = 0
        return self._slot


class KB:
    def __init__(self, nc, es, same_eng_sync=True):
        self.nc = nc
        self.es = es
        self.ops = {e: [] for e in ENGS}
        self.nsem = 0
        self.sems = {e: self.new_sem() for e in ENGS}
        self.cnt = {e: 0 for e in ENGS}
        self.waited = {e: {} for e in ENGS}
        self.same_eng_sync = same_eng_sync
        self.nbuf = 0

    def new_sem(self):
        self.nsem += 1
        return self.es.enter_context(self.nc.semaphore(f"sem{self.nsem}"))

    def sb(self, shape, dtype, name=None):
        self.nbuf += 1
        name = name or f"sb{self.nbuf}"
        return Buf(self, self.es.enter_context(self.nc.sbuf_tensor(name, list(shape), dtype)), name)

    def ps(self, shape, dtype=F32, name=None):
        self.nbuf += 1
        name = name or f"ps{self.nbuf}"
        return Buf(self, self.es.enter_context(self.nc.psum_tensor(name, list(shape), dtype)), name)

    def view(self, name="v"):
        return Buf(self, None, name)

    def _waits(self, eng, deps):
        waits = []
        for d in deps:
            if d is None:
                continue
            if isinstance(d, (list, tuple)):
                waits += self._waits(eng, d)
                continue
            if (not self.same_eng_sync) and d.sem is self.sems[eng]:
                continue
            key = id(d.sem)
            if self.waited[eng].get(key, 0) >= d.val:
                continue
            self.waited[eng][key] = d.val
            waits.append((d.sem, d.val))
        return waits

    def _deps(self, reads, writes, deps):
        ds = list(deps)
        for b in reads:
            ds += b.w
        for b in writes:
            ds += b.w
            ds += list(b.r.values())
        return ds

    def _commit(self, tok, reads, writes):
        for b in writes:
            b.w = [tok]
            b.r = {}
        for b in reads:
            if b not in writes:
                o = b.r.get(id(tok.sem))
                if o is None or o.val < tok.val:
                    b.r[id(tok.sem)] = tok

    def op(self, eng, fn, reads=(), writes=(), deps=()):
        waits = self._waits(eng, self._deps(reads, writes, deps))
        self.cnt[eng] += 1
        self.ops[eng].append((waits, fn, (self.sems[eng], 1)))
        tok = Tok(self.sems[eng], self.cnt[eng])
        self._commit(tok, reads, writes)
        return tok

    def group(self, eng, fns, reads=(), writes=(), deps=()):
        waits = self._waits(eng, self._deps(reads, writes, deps))
        for i, fn in enumerate(fns):
            self.cnt[eng] += 1
            self.ops[eng].append((waits if i == 0 else [], fn, (self.sems[eng], 1)))
        tok = Tok(self.sems[eng], self.cnt[eng])
        self._commit(tok, reads, writes)
        return tok

    def dma(self, eng, out, in_, slotbuf, reads=(), writes=(), deps=(), **kw):
        sem = slotbuf.slot()
        ds = list(deps)
        for b in reads:
            ds += b.w
        for b in writes:
            ds += [t for t in b.w if t.sem is not sem]
            ds += list(b.r.values())
        waits = self._waits(eng, ds)
        slotbuf._slotval += 16
        self.ops[eng].append((waits, lambda e: e.dma_start(out=out, in_=in_, **kw), (sem, 16)))
        tok = Tok(sem, slotbuf._slotval)
        self._commit(tok, reads, writes)
        return tok

    def wait_only(self, eng, deps):
        waits = self._waits(eng, deps)
        if waits:
            self.ops[eng].append((waits, None, None))

    def emit(self):
        with self.nc.Block() as block:
            for e in ENGS:
                dec = getattr(block, ENG_ATTR[e])
                ops = self.ops[e]

                def body(eng, ops=ops):
                    for waits, fn, inc in ops:
                        for s, v in waits:
                            eng.wait_ge(s, v)
                        if fn is None:
                            continue
                        inst = fn(eng)
                        if inc is not None:
                            inst.then_inc(inc[0], inc[1])
                dec(body)


D = 1024
ALPHA = 8.0 ** 0.25
LN_EPS = 1e-5


def make_ident(kb, ident):
    kb.op('pool', lambda e: e.memset(ident[:], 0.0), writes=[ident])
    kb.op('pool', lambda e: e.affine_select(out=ident[:], in_=ident[:], compare_op=ALU.not_equal, fill=1.0,
                                            base=0, pattern=[[-1, 128]], channel_multiplier=1), writes=[ident])


class LNBufs:
    def __init__(self, kb, nbuf=2):
        self.kb = kb
        self.n = nbuf
        self.i = 0
        self.z = [kb.sb([128, D], F32) for _ in range(nbuf)]
        self.o = [kb.sb([128, D], F32) for _ in range(nbuf)]
        self.st = [kb.sb([128, 12], F32) for _ in range(nbuf)]
        self.mv = [kb.sb([128, 2], F32) for _ in range(nbuf)]
        self.rs = [kb.sb([128, 1], F32) for _ in range(nbuf)]
        self.nb = [kb.sb([128, 1], F32) for _ in range(nbuf)]
        self.eps = kb.sb([128, 1], F32)
        kb.op('pool', lambda e: e.memset(self.eps[:], LN_EPS), writes=[self.eps])


def emit_ln(kb, L, xbuf, x_ap, h_aps, h_bufs, g, b, out_dram_ap, dma_eng='sp'):
    i = L.i
    L.i = (L.i + 1) % L.n
    z, o, st, mv, rs, nb = L.z[i], L.o[i], L.st[i], L.mv[i], L.rs[i], L.nb[i]
    for hf in range(2):
        sl = slice(hf * 512, (hf + 1) * 512)
        kb.op('dve', lambda e, sl=sl, hf=hf: e.scalar_tensor_tensor(out=z[:, sl], in0=x_ap[:, sl], scalar=ALPHA,
                                                                    in1=h_aps[hf], op0=ALU.mult, op1=ALU.add),
              reads=[xbuf, h_bufs[hf]], writes=[z] if hf == 0 else [], deps=z.w if hf == 1 else ())
    z.w = [Tok(kb.sems['dve'], kb.cnt['dve'])]
    for hf in range(2):
        sl = slice(hf * 512, (hf + 1) * 512)
        kb.op('dve', lambda e, sl=sl, hf=hf: e.bn_stats(out=st[:, hf * 6:(hf + 1) * 6], in_=z[:, sl]),
              reads=[z], writes=[st] if hf == 0 else [], deps=st.w if hf == 1 else ())
    st.w = [Tok(kb.sems['dve'], kb.cnt['dve'])]
    kb.op('dve', lambda e: e.bn_aggr(out=mv[:], in_=st[:]), reads=[st], writes=[mv])
    kb.op('act', lambda e: e.activation(out=rs[:], in_=mv[:, 1:2], func=AF.Sqrt, bias=L.eps[:], scale=1.0),
          reads=[mv, L.eps], writes=[rs])
    kb.op('dve', lambda e: e.reciprocal(out=rs[:], in_=rs[:]), reads=[rs], writes=[rs])
    kb.op('dve', lambda e: e.scalar_tensor_tensor(out=nb[:], in0=mv[:, 0:1], scalar=-1.0, in1=rs[:],
                                                  op0=ALU.mult, op1=ALU.mult), reads=[mv, rs], writes=[nb])
    kb.op('act', lambda e: e.activation(out=o[:], in_=z[:], func=AF.Identity, bias=nb[:], scale=rs[:]),
          reads=[z, rs, nb], writes=[o])
    kb.op('pool', lambda e: e.tensor_tensor(out=o[:], in0=o[:], in1=g[:], op=ALU.mult), reads=[o, g], writes=[o])
    kb.op('pool', lambda e: e.tensor_tensor(out=o[:], in0=o[:], in1=b[:], op=ALU.add), reads=[o, b], writes=[o])
    return kb.dma(dma_eng, out_dram_ap, o[:], o, reads=[o])


def build_moe(NT=32, GT=8, NE=16, debug=False):
    nc = bass.Bass("TRN2", target_bir_lowering=False)
    T = NT * 128
    x = nc.dram_tensor("x", [T, D], F32, kind="ExternalInput").ap()
    rw = nc.dram_tensor("rw", [D, 16], F32, kind="ExternalInput").ap()
    rbias = nc.dram_tensor("rbias", [128, GT * 16], F32, kind="ExternalInput").ap()
    w1 = nc.dram_tensor("w1", [16, D, D], F32, kind="ExternalInput").ap()
    w3 = nc.dram_tensor("w3", [16, D, D], F32, kind="ExternalInput").ap()
    w2 = nc.dram_tensor("w2", [16, D, D], F32, kind="ExternalInput").ap()
    lng = nc.dram_tensor("lng", [128, D], F32, kind="ExternalInput").ap()
    lnb = nc.dram_tensor("lnb", [128, D], F32, kind="ExternalInput").ap()
    y = nc.dram_tensor("y", [T, D], F32, kind="ExternalOutput").ap()
    if debug:
        gdbg = nc.dram_tensor("gdbg", [NT // GT, 128, GT * 16], F32, kind="ExternalOutput").ap()
    with ExitStack() as es:
        kb = KB(nc, es)
        ident = kb.sb([128, 128], F32)
        make_ident(kb, ident)
        rws = kb.sb([128, 8, 16], F32)
        kb.dma('sp', rws[:], rw.rearrange("(c p) e -> p c e", p=128), rws, writes=[rws])
        rb = kb.sb([128, GT * 16], F32)
        kb.dma('sp', rb[:], rbias[:, :], rb, writes=[rb])
        g = kb.sb([128, D], F32)
        b = kb.sb([128, D], F32)
        kb.dma('sp', g[:], lng[:, :], g, writes=[g])
        kb.dma('sp', b[:], lnb[:, :], b, writes=[b])
        L = LNBufs(kb, nbuf=1)

        xin = [kb.sb([128, D], F32) for _ in range(2)]
        xT32 = [kb.sb([128, 8, 128], F32) for _ in range(2)]
        xT = kb.sb([128, 8, GT * 128], BF16)
        xTv = [kb.view(f"xTv{t}") for t in range(GT)]
        acc = kb.sb([128, GT, D], F32)
        accv = [[kb.view() for _ in range(2)] for t in range(GT)]
        wb = [[kb.sb([128, 8, D], BF16) for _ in range(3)] for _ in range(2)]
        hT = [kb.sb([128, 8, 512], BF16) for _ in range(1)]
        hTv = [[kb.view() for _ in range(8)] for _ in range(1)]
        sil = [kb.sb([128, 512], BF16) for _ in range(2)]
        banks = [kb.ps([128, 512], F32) for _ in range(8)]
        NG = GT * 16
        aff = kb.sb([128, NG], F32)
        affv = [kb.view() for _ in range(GT)]
        S8 = kb.sb([128, GT * 4, 8], F32)
        Pt = kb.sb([128, GT * 4, 6], F32)
        gs = kb.sb([128, GT * 4], F32)
        gmax = kb.sb([128, GT], F32)
        ghot = kb.sb([128, GT * 4], F32)
        c1 = kb.sb([128, GT * 4, 4], F32)
        c2 = kb.sb([128, GT * 4, 4], F32)
        GA = kb.sb([128, GT, 16], F32)
        den = kb.sb([128, GT], F32)
        G = kb.sb([128, GT, 16], F32)

        wsrc = [w1, w3, w2]
        nsteps = (NT // GT) * NE
        step_list = [(gi, e) for gi in range(NT // GT) for e in range(NE)]

        def load_weights(si):
            gi, e = step_list[si]
            slot = si % 2
            for j in range(3):
                kb.dma('pool', wb[slot][j][:], wsrc[j][e].rearrange("(c p) f -> p c f", p=128), wb[slot][j],
                       writes=[wb[slot][j]])

        if nsteps > 0:
            load_weights(0)
        ybank = 0
        out_toks = []
        for gi in range(NT // GT):
            for t in range(GT):
                tile = gi * GT + t
                xi = xin[t % 2]
                x32 = xT32[t % 2]
                kb.dma('sp', xi[:], x[tile * 128:(tile + 1) * 128, :], xi, writes=[xi])
                for hf in range(2):
                    bk = banks[4 + hf]
                    kb.group('pe', [lambda e, c=c, bk=bk, xi=xi: e.transpose(bk[:, (c % 4) * 128:(c % 4 + 1) * 128],
                                                                               xi[:, c * 128:(c + 1) * 128], ident[:])
                                    for c in range(hf * 4, hf * 4 + 4)], reads=[xi, ident], writes=[bk])
                    if True:
                        kb.op('act', lambda e, bk=bk, hf=hf, x32=x32: e.copy(
                            out=x32[:, hf * 4:(hf + 1) * 4, :], in_=bk[:].rearrange("p (c t) -> p c t", c=4)),
                            reads=[bk], writes=[x32] if hf == 0 else [], deps=x32.w if hf == 1 else ())
                    kb.op('dve', lambda e, hf=hf, t=t, x32=x32: e.tensor_copy(
                        out=xT[:, hf * 4:(hf + 1) * 4, t * 128:(t + 1) * 128], in_=x32[:, hf * 4:(hf + 1) * 4, :]),
                        reads=[], writes=[xTv[t]] if hf == 0 else [], deps=list(xTv[t].w if hf == 1 else ()) + [Tok(kb.sems['act'], kb.cnt['act'])])
                x32.w = [Tok(kb.sems['act'], kb.cnt['act'])]
                xTv[t].w = [Tok(kb.sems['dve'], kb.cnt['dve'])]
                x32.r[id(kb.sems['dve'])] = Tok(kb.sems['dve'], kb.cnt['dve'])
                rbk = banks[6]
                kb.group('pe', [lambda e, c=c, x32=x32, rbk=rbk: e.matmul(rbk[:, 0:16], x32[:, c, :], rws[:, c, :],
                                                                         start=(c == 0), stop=(c == 7))
                                for c in range(8)], reads=[x32, rws], writes=[rbk])
                kb.op('act', lambda e, t=t, rbk=rbk: e.activation(out=aff[:, t * 16:(t + 1) * 16], in_=rbk[:, 0:16],
                                                                   func=AF.Sigmoid), reads=[rbk], writes=[affv[t]])
            S4 = S8[:, :, 0:4]
            aff4 = aff[:].rearrange("p (g j) -> p g j", j=4)
            kb.op('dve', lambda e: e.tensor_tensor(out=S8[:, :, 0:4], in0=aff4, in1=rb[:].rearrange("p (g j) -> p g j", j=4),
                                                   op=ALU.add), reads=affv + [rb], writes=[S8])
            kb.op('dve', lambda e: e.tensor_copy(out=S8[:, :, 4:8], in_=S8[:, :, 0:4]), reads=[S8], writes=[S8])
            pairs = [(0, 1), (0, 2), (0, 3), (1, 2), (1, 3), (2, 3)]
            for i, (a, bb) in enumerate(pairs):
                kb.op('dve', lambda e, i=i, a=a, bb=bb: e.tensor_tensor(out=Pt[:, :, i], in0=S8[:, :, a], in1=S8[:, :, bb],
                                                                         op=ALU.add), reads=[S8], writes=[Pt])
            kb.op('dve', lambda e: e.tensor_reduce(out=gs[:], in_=Pt[:], axis=AX.X, op=ALU.max), reads=[Pt], writes=[gs])
            kb.op('dve', lambda e: e.tensor_reduce(out=gmax[:], in_=gs[:].rearrange("p (t g) -> p t g", g=4), axis=AX.X,
                                                   op=ALU.max), reads=[gs], writes=[gmax])
            kb.op('dve', lambda e: e.tensor_tensor(out=ghot[:].rearrange("p (t g) -> p t g", g=4),
                                                   in0=gs[:].rearrange("p (t g) -> p t g", g=4),
                                                   in1=gmax[:].unsqueeze(2).broadcast_to([128, GT, 4]), op=ALU.is_ge),
                  reads=[gs, gmax], writes=[ghot])
            kb.op('dve', lambda e: e.tensor_tensor(out=c1[:], in0=S8[:, :, 0:4], in1=S8[:, :, 1:5], op=ALU.is_gt),
                  reads=[S8], writes=[c1])
            kb.op('dve', lambda e: e.tensor_tensor(out=c2[:], in0=S8[:, :, 0:4], in1=S8[:, :, 2:6], op=ALU.is_gt),
                  reads=[S8], writes=[c2])
            kb.op('dve', lambda e: e.tensor_tensor(out=c1[:], in0=c1[:], in1=c2[:], op=ALU.add), reads=[c1, c2], writes=[c1])
            kb.op('dve', lambda e: e.tensor_tensor(out=c2[:], in0=S8[:, :, 0:4], in1=S8[:, :, 3:7], op=ALU.is_gt),
                  reads=[S8], writes=[c2])
            kb.op('dve', lambda e: e.tensor_tensor(out=c1[:], in0=c1[:], in1=c2[:], op=ALU.add), reads=[c1, c2], writes=[c1])
            kb.op('dve', lambda e: e.scalar_tensor_tensor(out=c1[:], in0=c1[:], scalar=2.0,
                                                          in1=ghot[:].unsqueeze(2).broadcast_to([128, GT * 4, 4]),
                                                          op0=ALU.is_ge, op1=ALU.mult), reads=[c1, ghot], writes=[c1])
            kb.op('dve', lambda e: e.tensor_tensor(out=GA[:].rearrange("p t (g j) -> p (t g) j", j=4), in0=c1[:], in1=aff4,
                                                   op=ALU.mult), reads=[c1] + affv, writes=[GA])
            kb.op('dve', lambda e: e.tensor_reduce(out=den[:], in_=GA[:], axis=AX.X, op=ALU.add), reads=[GA], writes=[den])
            kb.op('dve', lambda e: e.reciprocal(out=den[:], in_=den[:]), reads=[den], writes=[den])
            kb.op('dve', lambda e: e.tensor_tensor(out=G[:], in0=GA[:], in1=den[:].unsqueeze(2).broadcast_to([128, GT, 16]),
                                                   op=ALU.mult), reads=[GA, den], writes=[G])
            if debug:
                out_toks.append(kb.dma('sp', gdbg[gi], G[:].rearrange("p t e -> p (t e)"), G, reads=[G]))
            for e_i in range(NE):
                si = gi * NE + e_i
                slot = si % 2
                if si + 1 < nsteps:
                    load_weights(si + 1)
                w1b, w3b, w2b = wb[slot]
                for tt in range(GT // 4):
                    hb = hT[0]
                    hv = hTv[0]
                    tsl = slice(tt * 512, (tt + 1) * 512)
                    for fc in range(8):
                        b1 = banks[fc % 2]
                        b3 = banks[2 + fc % 2]
                        sl_ = sil[fc % 2]
                        fsl = slice(fc * 128, (fc + 1) * 128)
                        kb.group('pe', [lambda e, c=c, b1=b1, fsl=fsl, w1b=w1b, tsl=tsl: e.matmul(b1[:], w1b[:, c, fsl], xT[:, c, tsl],
                                                                                 start=(c == 0), stop=(c == 7))
                                        for c in range(8)], reads=[w1b] + xTv[tt * 4:(tt + 1) * 4], writes=[b1])
                        kb.group('pe', [lambda e, c=c, b3=b3, fsl=fsl, w3b=w3b, tsl=tsl: e.matmul(b3[:], w3b[:, c, fsl], xT[:, c, tsl],
                                                                                 start=(c == 0), stop=(c == 7))
                                        for c in range(8)], reads=[w3b] + xTv[tt * 4:(tt + 1) * 4], writes=[b3])
                        kb.op('act', lambda e, b1=b1, sl_=sl_: e.activation(out=sl_[:], in_=b1[:], func=AF.Silu),
                              reads=[b1], writes=[sl_])
                        kb.op('dve', lambda e, b3=b3, sl_=sl_, fc=fc, hb=hb: e.tensor_tensor(out=hb[:, fc, :], in0=b3[:],
                                                                                          in1=sl_[:], op=ALU.mult),
                              reads=[b3, sl_], writes=[hv[fc]])
                    for t4 in range(4):
                        t = tt * 4 + t4
                        for oh in range(2):
                            yb = banks[4 + ybank % 4]
                            ybank += 1
                            osl = slice(oh * 512, (oh + 1) * 512)
                            kb.group('pe', [lambda e, fc=fc, yb=yb, t4=t4, osl=osl, hb=hb, w2b=w2b: e.matmul(
                                yb[:], hb[:, fc, t4 * 128:(t4 + 1) * 128], w2b[:, fc, osl], start=(fc == 0), stop=(fc == 7))
                                for fc in range(8)], reads=[w2b] + hv, writes=[yb])
                            av = accv[t][oh]
                            if e_i == 0:
                                kb.op('dve', lambda e, yb=yb, t=t, osl=osl, e_i=e_i: e.tensor_scalar(
                                    out=acc[:, t, osl], in0=yb[:], scalar1=G[:, t, e_i:e_i + 1], scalar2=None, op0=ALU.mult),
                                    reads=[yb, G], writes=[av])
                            else:
                                kb.op('dve', lambda e, yb=yb, t=t, osl=osl, e_i=e_i: e.scalar_tensor_tensor(
                                    out=acc[:, t, osl], in0=yb[:], scalar=G[:, t, e_i:e_i + 1], in1=acc[:, t, osl],
                                    op0=ALU.mult, op1=ALU.add), reads=[yb, G], writes=[av])
            for t in range(GT):
                tile = gi * GT + t
                xi = xin[t % 2]
                kb.dma('sp', xi[:], x[tile * 128:(tile + 1) * 128, :], xi, writes=[xi])
                out_toks.append(emit_ln(kb, L, xi, xi, [acc[:, t, 0:512], acc[:, t, 512:1024]], accv[t], g, b,
                                        y[tile * 128:(tile + 1) * 128, :]))
        kb.wait_only('sp', out_toks)
        kb.emit()
    return nc


def pool_tables(first_is_start, last_is_end):
    Bm = np.zeros((3, 4, 128, 128), np.float32)
    Bh = np.zeros((3, 4, 16, 128), np.float32)
    for var in range(3):
        start_edge = (var == 0 and first_is_start)
        end_edge = (var == 2 and last_is_end)
        for g, w in enumerate((2, 4, 8, 16)):
            half = w // 2
            for t in range(128):
                lo, hi = t - half, t + half - 1
                if start_edge:
                    lo = max(lo, 0)
                if end_edge:
                    hi = min(hi, 127)
                cnt = hi - lo + 1
                for s in range(lo, hi + 1):
                    if 0 <= s < 128:
                        Bm[var, g, s, t] += 1.0 / cnt
                    elif s < 0:
                        Bh[var, g, 8 + s, t] += 1.0 / cnt
                    else:
                        Bh[var, g, 8 + (s - 128), t] += 1.0 / cnt
                Bm[var, g, t, t] -= 1.0
    return Bm, Bh


def build_pool(NT=32):
    nc = bass.Bass("TRN2", target_bir_lowering=False)
    T = NT * 128
    xp = nc.dram_tensor("xp", [T + 16, D], F32, kind="ExternalInput").ap()
    bm = nc.dram_tensor("bm", [128, 12 * 128], F32, kind="ExternalInput").ap()
    bh = nc.dram_tensor("bh", [16, 12 * 128], F32, kind="ExternalInput").ap()
    pw = nc.dram_tensor("pw", [4, 256, 256], F32, kind="ExternalInput").ap()
    psc = nc.dram_tensor("psc", [128, D], F32, kind="ExternalInput").ap()
    lng = nc.dram_tensor("lng", [128, D], F32, kind="ExternalInput").ap()
    lnb = nc.dram_tensor("lnb", [128, D], F32, kind="ExternalInput").ap()
    y = nc.dram_tensor("y", [T, D], F32, kind="ExternalOutput").ap()
    with ExitStack() as es:
        kb = KB(nc, es)
        g = kb.sb([128, D], F32)
        b = kb.sb([128, D], F32)
        kb.dma('sp', g[:], lng[:, :], g, writes=[g])
        kb.dma('sp', b[:], lnb[:, :], b, writes=[b])
        L = LNBufs(kb, nbuf=2)
        Bm = kb.sb([128, 12 * 128], BF16)
        Bh = kb.sb([16, 12 * 128], BF16)
        kb.dma('pool', Bm[:], bm[:, :], Bm, writes=[Bm])
        kb.dma('pool', Bh[:], bh[:, :], Bh, writes=[Bh])
        W32 = kb.sb([128, 4, 2, 256], F32)
        sc = kb.sb([128, D], F32)
        Wb = kb.sb([128, 4, 2, 256], BF16)
        kb.dma('sp', W32[:], pw.rearrange("g (hh p) e -> p g hh e", p=128), W32, writes=[W32])
        kb.dma('sp', sc[:], psc[:, :], sc, writes=[sc])
        kb.op('dve', lambda e: e.tensor_tensor(out=Wb[:], in0=W32[:],
                                               in1=sc[:].rearrange("p (g e) -> p g e", g=4).unsqueeze(2).broadcast_to([128, 4, 2, 256]),
                                               op=ALU.mult), reads=[W32, sc], writes=[Wb])
        xin = [kb.sb([128, D], F32) for _ in range(3)]
        xh = [kb.sb([16, D], F32) for _ in range(2)]
        xb = [kb.sb([128, D], BF16) for _ in range(2)]
        xhb = [kb.sb([16, D], BF16) for _ in range(2)]
        uT = [kb.sb([128, 8, 128], BF16) for _ in range(2)]
        uTa = [kb.view() for _ in range(2)]
        uTb = [kb.view() for _ in range(2)]
        pu = [[kb.ps([128, 512], F32) for _ in range(2)] for _ in range(2)]
        py = [[kb.ps([128, 512], F32) for _ in range(2)] for _ in range(2)]
        out_toks = []
        for i in range(NT):
            var = 0 if i == 0 else (2 if i == NT - 1 else 1)
            xi = xin[i % 3]
            xhi = xh[i % 2]
            xbi = xb[i % 2]
            xhbi = xhb[i % 2]
            u = uT[i % 2]
            ua, ub = uTa[i % 2], uTb[i % 2]
            pui = pu[i % 2]
            pyi = py[i % 2]
            kb.dma('sp', xi[:], xp[8 + i * 128: 8 + (i + 1) * 128, :], xi, writes=[xi])
            kb.dma('sp', xhi[0:8, :], xp[i * 128: i * 128 + 8, :], xhi, writes=[xhi])
            kb.dma('sp', xhi[8:16, :], xp[8 + (i + 1) * 128: 16 + (i + 1) * 128, :], xhi, writes=[xhi])
            kb.op('act', lambda e, xi=xi, xbi=xbi: e.copy(out=xbi[:], in_=xi[:]), reads=[xi], writes=[xbi])
            kb.op('dve', lambda e, xhi=xhi, xhbi=xhbi: e.tensor_copy(out=xhbi[:], in_=xhi[:]), reads=[xhi], writes=[xhbi])
            for hf in range(2):
                fns = []
                for fc in range(hf * 4, hf * 4 + 4):
                    col = (var * 4 + fc // 2) * 128
                    fns.append(lambda e, fc=fc, col=col, xbi=xbi, bk=pui[hf]: e.matmul(
                        bk[:, (fc % 4) * 128:(fc % 4 + 1) * 128], xbi[:, fc * 128:(fc + 1) * 128], Bm[:, col:col + 128],
                        start=True, stop=False))
                    fns.append(lambda e, fc=fc, col=col, xhbi=xhbi, bk=pui[hf]: e.matmul(
                        bk[:, (fc % 4) * 128:(fc % 4 + 1) * 128], xhbi[:, fc * 128:(fc + 1) * 128], Bh[:, col:col + 128],
                        start=False, stop=True))
                kb.group('pe', fns, reads=[xbi, xhbi, Bm, Bh], writes=[pui[hf]])
            kb.op('act', lambda e, u=u, bk=pui[0]: e.copy(out=u[:, 0:4, :], in_=bk[:].rearrange("p (c t) -> p c t", c=4)),
                  reads=[pui[0]], writes=[ua])
            kb.op('dve', lambda e, u=u, bk=pui[1]: e.tensor_copy(out=u[:, 4:8, :], in_=bk[:].rearrange("p (c t) -> p c t", c=4)),
                  reads=[pui[1]], writes=[ub])
            for bkidx in range(2):
                fns = []
                for gg in range(bkidx * 2, bkidx * 2 + 2):
                    for hh in range(2):
                        fns.append(lambda e, gg=gg, hh=hh, u=u, bk=pyi[bkidx]: e.matmul(
                            bk[:, (gg % 2) * 256:(gg % 2 + 1) * 256], u[:, gg * 2 + hh, :], Wb[:, gg, hh, :],
                            start=(hh == 0), stop=(hh == 1)))
                kb.group('pe', fns, reads=[ua if bkidx == 0 else ub, Wb], writes=[pyi[bkidx]])
            out_toks.append(emit_ln(kb, L, xi, xi, [pyi[0][:], pyi[1][:]], pyi, g, b, y[i * 128:(i + 1) * 128, :]))
        kb.wait_only('sp', out_toks)
        kb.emit()
    return nc


def build_qkv(NT=32):
    nc = bass.Bass("TRN2", target_bir_lowering=False)
    T = NT * 128
    x = nc.dram_tensor("x", [T, D], F32, kind="ExternalInput").ap()
    win = nc.dram_tensor("win", [D, 3 * D], F32, kind="ExternalInput").ap()
    cs = nc.dram_tensor("cs", [T, 64], F32, kind="ExternalInput").ap()
    qkv = nc.dram_tensor("qkv", [T, 3 * D], BF16, kind="ExternalOutput").ap()
    with ExitStack() as es:
        kb = KB(nc, es)
        ident = kb.sb([128, 128], F32)
        make_ident(kb, ident)
        wb = kb.sb([128, 8, 3 * D], BF16)
        wv = [kb.view() for _ in range(8)]
        for c in range(8):
            kb.dma('pool', wb[:, c, :], win[c * 128:(c + 1) * 128, :], wv[c], writes=[wv[c]])
        kb.op('pool', lambda e: e.tensor_scalar(out=wb[:, :, 0:D], in0=wb[:, :, 0:D], scalar1=0.125, scalar2=None, op0=ALU.mult),
              reads=[], writes=wv)
        xin = [kb.sb([128, D], F32) for _ in range(2)]
        cst = [kb.sb([128, 64], F32) for _ in range(2)]
        xT = [kb.sb([128, 8, 128], BF16) for _ in range(2)]
        kro = [kb.sb([128, 512], F32) for _ in range(2)]
        tm = [kb.sb([128, 8, 32], F32) for _ in range(4)]
        tp = [kb.sb([128, 8, 32], F32) for _ in range(4)]
        ost = [kb.sb([128, 3 * D], BF16) for _ in range(2)]
        ostv = [[kb.view() for _ in range(6)] for _ in range(2)]
        banks = [kb.ps([128, 512], F32) for _ in range(8)]
        nb = 0
        out_toks = []
        for i in range(NT):
            xi = xin[i % 2]
            ci = cst[i % 2]
            xt = xT[i % 2]
            oi = ost[i % 2]
            ov = ostv[i % 2]
            kr = kro[i % 2]
            kb.dma('sp', xi[:], x[i * 128:(i + 1) * 128, :], xi, writes=[xi])
            kb.dma('sp', ci[:], cs[i * 128:(i + 1) * 128, :], ci, writes=[ci])
            for hf in range(2):
                bk = banks[nb % 8]; nb += 1
                kb.group('pe', [lambda e, c=c, bk=bk, xi=xi: e.transpose(bk[:, (c % 4) * 128:(c % 4 + 1) * 128],
                                                                           xi[:, c * 128:(c + 1) * 128], ident[:])
                                for c in range(hf * 4, hf * 4 + 4)], reads=[xi, ident], writes=[bk])
                eng = 'act' if hf == 0 else 'dve'
                fn = (lambda e, bk=bk, hf=hf, xt=xt: e.copy(out=xt[:, hf * 4:(hf + 1) * 4, :], in_=bk[:].rearrange("p (c t) -> p c t", c=4))) \
                    if hf == 0 else (lambda e, bk=bk, hf=hf, xt=xt: e.tensor_copy(out=xt[:, hf * 4:(hf + 1) * 4, :], in_=bk[:].rearrange("p (c t) -> p c t", c=4)))
                if hf == 0:
                    t_a = kb.op(eng, fn, reads=[bk], writes=[xt])
                else:
                    t_b = kb.op(eng, fn, reads=[bk], deps=[t_a])
                    xt.w = [t_a, t_b]
            cosb = ci[:, 0:32].unsqueeze(1).broadcast_to([128, 8, 32])
            sinb = ci[:, 32:64].unsqueeze(1).broadcast_to([128, 8, 32])
            for blk in range(6):
                bk = banks[nb % 8]; nb += 1
                kb.group('pe', [lambda e, c=c, bk=bk, blk=blk, xt=xt: e.matmul(bk[:], xt[:, c, :], wb[:, c, blk * 512:(blk + 1) * 512],
                                                                               start=(c == 0), stop=(c == 7)) for c in range(8)],
                         reads=[xt] + wv, writes=[bk])
                osl = slice(blk * 512, (blk + 1) * 512)
                if blk in (0, 2):
                    if blk == 0:
                        eng, tt, src, sbuf = 'dve', tm, bk, bk
                    else:
                        kb.op('act', lambda e, bk=bk, kr=kr: e.copy(out=kr[:], in_=bk[:]), reads=[bk], writes=[kr])
                        eng, tt, src, sbuf = 'pool', tp, kr, kr
                    s4 = src[:].rearrange("p (h two j) -> p h two j", two=2, j=32)
                    o4 = oi[:, osl].rearrange("p (h two j) -> p h two j", two=2, j=32)
                    t1, t2 = s4[:, :, 0, :], s4[:, :, 1, :]
                    kb.op(eng, lambda e, t1=t1, tt=tt, cosb=cosb: e.tensor_tensor(out=tt[0][:], in0=t1, in1=cosb, op=ALU.mult), reads=[sbuf, ci], writes=[tt[0]])
                    kb.op(eng, lambda e, t2=t2, tt=tt, sinb=sinb: e.tensor_tensor(out=tt[1][:], in0=t2, in1=sinb, op=ALU.mult), reads=[sbuf, ci], writes=[tt[1]])
                    kb.op(eng, lambda e, t2=t2, tt=tt, cosb=cosb: e.tensor_tensor(out=tt[2][:], in0=t2, in1=cosb, op=ALU.mult), reads=[sbuf, ci], writes=[tt[2]])
                    kb.op(eng, lambda e, t1=t1, tt=tt, sinb=sinb: e.tensor_tensor(out=tt[3][:], in0=t1, in1=sinb, op=ALU.mult), reads=[sbuf, ci], writes=[tt[3]])
                    kb.op(eng, lambda e, tt=tt, o4=o4: e.tensor_tensor(out=o4[:, :, 0, :], in0=tt[0][:], in1=tt[1][:], op=ALU.subtract), reads=[tt[0], tt[1]], writes=[ov[blk]])
                    tk = kb.op(eng, lambda e, tt=tt, o4=o4: e.tensor_tensor(out=o4[:, :, 1, :], in0=tt[2][:], in1=tt[3][:], op=ALU.add), reads=[tt[2], tt[3]], deps=ov[blk].w)
                    ov[blk].w = ov[blk].w + [tk]
                else:
                    kb.op('act', lambda e, bk=bk, oi=oi, osl=osl: e.copy(out=oi[:, osl], in_=bk[:]), reads=[bk], writes=[ov[blk]])
            out_toks.append(kb.dma('sp', qkv[i * 128:(i + 1) * 128, :], oi[:], oi, reads=ov))
        kb.wait_only('sp', out_toks)
        kb.emit()
    return nc


def build_attn(NB=4):
    nc = bass.Bass("TRN2", target_bir_lowering=False)
    S = S_LEN
    qA, kA, vA, oA = [], [], [], []
    for di, d in enumerate(DILS):
        Ls = S // d
        nt = Ls // 128
        qA.append(nc.dram_tensor(f"qA{di}", [NB, 64, S], BF16, kind="ExternalInput").ap())
        kA.append(nc.dram_tensor(f"kA{di}", [NB, 64, d * (Ls + 128)], BF16, kind="ExternalInput").ap())
        vA.append(nc.dram_tensor(f"vA{di}", [NB, 128, d * (nt + 1) * 65], BF16, kind="ExternalInput").ap())
        oA.append(nc.dram_tensor(f"oA{di}", [NB, 128, 64 * 65], F32, kind="ExternalOutput").ap())
    qB = nc.dram_tensor("qB", [NB, 64, S], BF16, kind="ExternalInput").ap()
    kB = nc.dram_tensor("kB", [NB, 64, S], BF16, kind="ExternalInput").ap()
    vB = nc.dram_tensor("vB", [NB, 128, 64 * 65], BF16, kind="ExternalInput").ap()
    oB = nc.dram_tensor("oB", [NB, 128, 64 * 64], F32, kind="ExternalOutput").ap()
    ebias = nc.dram_tensor("ebias", [128, 25 * 128], F32, kind="ExternalInput").ap()
    maskd = nc.dram_tensor("maskd", [128, 256], F32, kind="ExternalInput").ap()
    with ExitStack() as es:
        kb = KB(nc, es)
        mask = kb.sb([128, 256], BF16)
        kb.dma('pool', mask[:], maskd[:, :], mask, writes=[mask])
        eb32 = kb.sb([128, 25 * 128], F32)
        E = kb.sb([128, 25 * 128], BF16)
        kb.dma('sp', eb32[:], ebias[:, :], eb32, writes=[eb32])
        kb.op('act', lambda e: e.activation(out=E[:], in_=eb32[:], func=AF.Exp), reads=[eb32], writes=[E])
        KMAX = 16 * (512 + 128)
        qT = [kb.sb([64, S], BF16) for _ in range(2)]
        kT = [kb.sb([64, KMAX], BF16) for _ in range(2)]
        V = [kb.sb([128, 80 * 65], BF16) for _ in range(2)]
        osb = [kb.sb([128, 64 * 65], F32) for _ in range(2)]
        pT = [kb.sb([128, 256], BF16) for _ in range(4)]
        rden = [kb.sb([128, 1], F32) for _ in range(2)]
        sbk = [kb.ps([128, 512], F32) for _ in range(3)]
        obk = [kb.ps([128, 512], F32) for _ in range(4)]
        stages = []
        for b in range(NB):
            for di in range(3):
                stages.append(('A', b, di))
            stages.append(('B', b, None))

        def load(si):
            kind, b, di = stages[si]
            sl = si % 2
            if kind == 'A':
                d = DILS[di]
                Ls = S // d
                nt = Ls // 128
                kb.dma('sp', qT[sl][:], qA[di][b], qT[sl], writes=[qT[sl]])
                kb.dma('sp', kT[sl][:, 0:d * (Ls + 128)], kA[di][b], kT[sl], writes=[kT[sl]])
                kb.dma('sp', V[sl][:, 0:d * (nt + 1) * 65], vA[di][b], V[sl], writes=[V[sl]])
            else:
                kb.dma('sp', qT[sl][:], qB[b], qT[sl], writes=[qT[sl]])
                kb.dma('sp', kT[sl][:, 0:S], kB[b], kT[sl], writes=[kT[sl]])
                kb.dma('sp', V[sl][:, 0:64 * 65], vB[b], V[sl], writes=[V[sl]])

        load(0)
        out_toks = []
        fronts, backs = [], []
        LOOK = 2

        def add_step(si, first, last, front_fn, back_fn):
            n = len(fronts)

            def front(n=n):
                front_fn(n)

            def back(n=n):
                if first and si + 1 < len(stages):
                    load(si + 1)
                back_fn(n)
                if last:
                    kind, b, di = stages[si]
                    ob = osb[si % 2]
                    if kind == 'A':
                        out_toks.append(kb.dma('sp', oA[di][b], ob[:], ob, reads=[ob]))
                    else:
                        out_toks.append(kb.dma('sp', oB[b], ob[:, 0:64 * 64], ob, reads=[ob]))
            fronts.append(front)
            backs.append(back)

        for si, (kind, b, di) in enumerate(stages):
            sl = si % 2
            q, k, v, ob = qT[sl], kT[sl], V[sl], osb[sl]
            if kind == 'A':
                d = DILS[di]
                Ls = S // d
                nt = Ls // 128
                for r in range(d):
                    for j in range(nt + 1):
                        q0 = 128 * (j - 1) if j >= 1 else 0
                        q1 = 128 * (j + 1) if j <= nt - 1 else 128 * nt
                        w = q1 - q0
                        moff = 128 if j == 0 else 0
                        koff = r * (Ls + 128) + 128 * j
                        vt = r * (nt + 1) + j
                        qa = r * Ls + q0

                        def front_fn(step, k=k, q=q, koff=koff, qa=qa, w=w, moff=moff):
                            sb_ = sbk[step % 3]
                            p = pT[step % 4]
                            kb.op('pe', lambda e: e.matmul(sb_[:, 0:w], k[:, koff:koff + 128], q[:, qa:qa + w], start=True, stop=True),
                                  reads=[k, q], writes=[sb_])
                            kb.op('act', lambda e: e.activation(out=p[:, 0:w], in_=sb_[:, 0:w], func=AF.Exp), reads=[sb_], writes=[p])
                            meng = 'dve' if step % 2 == 0 else 'pool'
                            kb.op(meng, lambda e: e.tensor_tensor(out=p[:, 0:w], in0=p[:, 0:w], in1=mask[:, moff:moff + w], op=ALU.mult),
                                  reads=[p, mask], writes=[p])

                        def back_fn(step, j=j, nt=nt, r=r, v=v, vt=vt, ob=ob):
                            p = pT[step % 4]
                            if j >= 1:
                                o_ = obk[(j - 1) % 4]
                                kb.op('pe', lambda e: e.matmul(o_[:, 0:65], p[:, 0:128], v[:, vt * 65:(vt + 1) * 65], start=False, stop=True),
                                      reads=[p, v], writes=[], deps=o_.w + list(o_.r.values()))
                                o_.w = [Tok(kb.sems['pe'], kb.cnt['pe'])]
                                blk = r * nt + (j - 1)
                                kb.op('dve', lambda e: e.tensor_copy(out=ob[:, blk * 65:(blk + 1) * 65], in_=o_[:, 0:65]),
                                      reads=[o_], writes=[], deps=list(ob.r.values()))
                                ob.w = [Tok(kb.sems['dve'], kb.cnt['dve'])]
                            if j <= nt - 1:
                                o2 = obk[j % 4]
                                pc = 128 if j >= 1 else 0
                                kb.op('pe', lambda e: e.matmul(o2[:, 0:65], p[:, pc:pc + 128], v[:, vt * 65:(vt + 1) * 65], start=True, stop=False),
                                      reads=[p, v], writes=[o2])
                        add_step(si, r == 0 and j == 0, r == d - 1 and j == nt, front_fn, back_fn)
            else:
                for m in range(64):
                    cls = {0: 0, 1: 1, 62: 3, 63: 4}.get(m, 2)
                    bt = min(max(m - 2, 0), 59)
                    for j in range(5):
                        tile = bt + j
                        ecol = (cls * 5 + j) * 128

                        def front_fn(step, k=k, q=q, tile=tile, m=m, ecol=ecol):
                            sb_ = sbk[step % 3]
                            p = pT[step % 4]
                            kb.op('pe', lambda e: e.matmul(sb_[:, 0:128], k[:, tile * 128:(tile + 1) * 128], q[:, m * 128:(m + 1) * 128],
                                                           start=True, stop=True), reads=[k, q], writes=[sb_])
                            kb.op('act', lambda e: e.activation(out=p[:, 0:128], in_=sb_[:, 0:128], func=AF.Exp), reads=[sb_], writes=[p])
                            meng = 'dve' if step % 2 == 0 else 'pool'
                            kb.op(meng, lambda e: e.tensor_tensor(out=p[:, 0:128], in0=p[:, 0:128], in1=E[:, ecol:ecol + 128], op=ALU.mult),
                                  reads=[p, E], writes=[p])

                        def back_fn(step, j=j, m=m, v=v, tile=tile, ob=ob):
                            p = pT[step % 4]
                            o_ = obk[m % 4]
                            if j == 0:
                                kb.op('pe', lambda e: e.matmul(o_[:, 0:65], p[:, 0:128], v[:, tile * 65:(tile + 1) * 65], start=True, stop=False),
                                      reads=[p, v], writes=[o_])
                            else:
                                kb.op('pe', lambda e: e.matmul(o_[:, 0:65], p[:, 0:128], v[:, tile * 65:(tile + 1) * 65], start=False, stop=(j == 4)),
                                      reads=[p, v], writes=[], deps=o_.w)
                                o_.w = [Tok(kb.sems['pe'], kb.cnt['pe'])]
                            if j == 4:
                                rd = rden[m % 2]
                                kb.op('dve', lambda e: e.reciprocal(out=rd[:], in_=o_[:, 64:65]), reads=[o_], writes=[rd])
                                kb.op('dve', lambda e: e.tensor_scalar(out=ob[:, m * 64:(m + 1) * 64], in0=o_[:, 0:64], scalar1=rd[:, 0:1],
                                                                       scalar2=None, op0=ALU.mult),
                                      reads=[o_, rd], writes=[], deps=list(ob.r.values()))
                                ob.w = [Tok(kb.sems['dve'], kb.cnt['dve'])]
                        add_step(si, m == 0 and j == 0, m == 63 and j == 4, front_fn, back_fn)
        nsteps = len(fronts)
        for n in range(min(LOOK, nsteps)):
            fronts[n]()
        for n in range(nsteps):
            if n + LOOK < nsteps:
                fronts[n + LOOK]()
            backs[n]()
        kb.wait_only('sp', out_toks)
        kb.emit()
    return nc


def build_oproj(NT=32):
    nc = bass.Bass("TRN2", target_bir_lowering=False)
    T = NT * 128
    x = nc.dram_tensor("x", [T, D], F32, kind="ExternalInput").ap()
    oa = nc.dram_tensor("oa", [3, T, 8 * 65], F32, kind="ExternalInput").ap()
    obd = nc.dram_tensor("ob", [T, 512], F32, kind="ExternalInput").ap()
    wout = nc.dram_tensor("wout", [D, D], F32, kind="ExternalInput").ap()
    lng = nc.dram_tensor("lng", [128, D], F32, kind="ExternalInput").ap()
    lnb = nc.dram_tensor("lnb", [128, D], F32, kind="ExternalInput").ap()
    y = nc.dram_tensor("y", [T, D], F32, kind="ExternalOutput").ap()
    with ExitStack() as es:
        kb = KB(nc, es)
        ident = kb.sb([128, 128], F32)
        make_ident(kb, ident)
        g = kb.sb([128, D], F32)
        b = kb.sb([128, D], F32)
        kb.dma('sp', g[:], lng[:, :], g, writes=[g])
        kb.dma('sp', b[:], lnb[:, :], b, writes=[b])
        L = LNBufs(kb, nbuf=2)
        wb = kb.sb([128, 8, D], BF16)
        kb.dma('pool', wb[:], wout.rearrange("(c p) f -> p c f", p=128), wb, writes=[wb])
        xin = [kb.sb([128, D], F32) for _ in range(3)]
        oat = [[kb.sb([128, 8, 65], F32) for _ in range(3)] for _ in range(2)]
        rd = [kb.sb([128, 8], F32) for _ in range(2)]
        ot = [kb.sb([128, D], F32) for _ in range(2)]
        ota = [kb.view() for _ in range(2)]
        otb = [kb.view() for _ in range(2)]
        oT = [kb.sb([128, 8, 128], BF16) for _ in range(2)]
        oTa = [kb.view() for _ in range(2)]
        oTb = [kb.view() for _ in range(2)]
        pt = [[kb.ps([128, 512], F32) for _ in range(2)] for _ in range(2)]
        py = [[kb.ps([128, 512], F32) for _ in range(2)] for _ in range(2)]
        out_toks = []
        for i in range(NT):
            xi = xin[i % 3]
            a3 = oat[i % 2]
            o = ot[i % 2]
            r_ = rd[i % 2]
            oTi = oT[i % 2]
            rows = slice(i * 128, (i + 1) * 128)
            kb.dma('sp', xi[:], x[rows, :], xi, writes=[xi])
            for br in range(3):
                kb.dma('sp', a3[br][:], oa[br, rows, :].rearrange("p (h c) -> p h c", c=65), a3[br], writes=[a3[br]])
            kb.dma('sp', o[:, 512:1024], obd[rows, :], otb[i % 2], writes=[otb[i % 2]])
            kb.op('dve', lambda e, a3=a3: e.tensor_tensor(out=a3[0][:], in0=a3[0][:], in1=a3[1][:], op=ALU.add), reads=[a3[1]], writes=[a3[0]])
            kb.op('dve', lambda e, a3=a3: e.tensor_tensor(out=a3[0][:], in0=a3[0][:], in1=a3[2][:], op=ALU.add), reads=[a3[2]], writes=[a3[0]])
            kb.op('dve', lambda e, a3=a3, r_=r_: e.reciprocal(out=r_[:], in_=a3[0][:, :, 64]), reads=[a3[0]], writes=[r_])
            kb.op('dve', lambda e, a3=a3, r_=r_, o=o: e.tensor_tensor(out=o[:, 0:512].rearrange("p (h c) -> p h c", c=64), in0=a3[0][:, :, 0:64],
                                                                   in1=r_[:].unsqueeze(2).broadcast_to([128, 8, 64]), op=ALU.mult),
                  reads=[a3[0], r_], writes=[ota[i % 2]])
            for hf in range(2):
                bk = pt[i % 2][hf]
                kb.group('pe', [lambda e, c=c, bk=bk, o=o: e.transpose(bk[:, (c % 4) * 128:(c % 4 + 1) * 128],
                                                                        o[:, c * 128:(c + 1) * 128], ident[:])
                                for c in range(hf * 4, hf * 4 + 4)], reads=[ota[i % 2] if hf == 0 else otb[i % 2], ident], writes=[bk])
                if hf == 0:
                    kb.op('act', lambda e, bk=bk, oTi=oTi: e.copy(out=oTi[:, 0:4, :], in_=bk[:].rearrange("p (c t) -> p c t", c=4)),
                          reads=[bk], writes=[oTa[i % 2]])
                else:
                    kb.op('dve', lambda e, bk=bk, oTi=oTi: e.tensor_copy(out=oTi[:, 4:8, :], in_=bk[:].rearrange("p (c t) -> p c t", c=4)),
                          reads=[bk], writes=[oTb[i % 2]])
            for oh in range(2):
                bk = py[i % 2][oh]
                kb.group('pe', [lambda e, c=c, bk=bk, oTi=oTi, oh=oh: e.matmul(bk[:], oTi[:, c, :], wb[:, c, oh * 512:(oh + 1) * 512],
                                                                               start=(c == 0), stop=(c == 7)) for c in range(8)],
                         reads=[oTa[i % 2], oTb[i % 2], wb], writes=[bk])
            out_toks.append(emit_ln(kb, L, xi, xi, [py[i % 2][0][:], py[i % 2][1][:]], py[i % 2], g, b, y[rows, :]))
        kb.wait_only('sp', out_toks)
        kb.emit()
    return nc

import ml_dtypes
BF = ml_dtypes.bfloat16
S_LEN = 8192
DILS = (1, 4, 16)


def rope_cs(pos):
    pos = pos.astype(np.float32)
    inv = (np.float32(10000.0) ** (-np.arange(0, 64, 2, dtype=np.float32) / np.float32(64))).astype(np.float32)
    ang = (pos[:, None] * inv[None, :]).astype(np.float32)
    return np.concatenate([np.cos(ang), np.sin(ang)], 1).astype(np.float32)


def attn_mask():
    p = np.arange(128)[:, None]
    a = np.arange(128)[None, :]
    return np.concatenate([(p <= a), (p >= a)], 1).astype(np.float32)


def na_bias_table(rpb_h):
    out = np.full((128, 25, 128), -30000.0, np.float32)
    kk = np.arange(128)
    qq = np.arange(128)
    for cls, m in enumerate((0, 1, 2, 62, 63)):
        base = min(max(2 * m - 4, 0), 118)
        qrow = 2 * m + qq // 64
        qcol = qq % 64
        rstart = np.clip(qrow - 4, 0, 120)
        cstart = np.clip(qcol - 8, 0, 48)
        for j in range(5):
            krow = base + 2 * j + kk // 64
            kcol = kk % 64
            ok = ((krow[:, None] >= rstart[None, :]) & (krow[:, None] < rstart[None, :] + 8)
                  & (kcol[:, None] >= cstart[None, :]) & (kcol[:, None] < cstart[None, :] + 16))
            roff = np.clip(krow[:, None] - qrow[None, :] + 7, 0, 14)
            coff = np.clip(kcol[:, None] - qcol[None, :], -15, 15) + 15
            vals = rpb_h[roff, coff]
            out[:, cls * 5 + j, :] = np.where(ok, vals, np.float32(-30000.0))
    return out.reshape(128, 25 * 128)


def attn_core_inputs(qkv, c, rpb_i):
    B, S = qkv.shape[0], qkv.shape[1]
    ha, hb = c, 8 + c
    im = {}
    q_h, k_h, v_h = qkv[:, :, 0, ha], qkv[:, :, 1, ha], qkv[:, :, 2, ha]
    one = np.ones((), BF)
    for di, d in enumerate(DILS):
        Ls = S // d
        nt = Ls // 128
        im[f"qA{di}"] = np.ascontiguousarray(q_h.reshape(B, Ls, d, 64).transpose(0, 3, 2, 1)).reshape(B, 64, d * Ls)
        kk = np.zeros((B, 64, d, Ls + 128), BF)
        kk[:, :, :, 64:64 + Ls] = k_h.reshape(B, Ls, d, 64).transpose(0, 3, 2, 1)
        im[f"kA{di}"] = kk.reshape(B, 64, d * (Ls + 128))
        vv = np.zeros((B, d, Ls + 128, 65), BF)
        vv[:, :, 64:64 + Ls, :64] = v_h.reshape(B, Ls, d, 64).transpose(0, 2, 1, 3)
        vv[:, :, 64:64 + Ls, 64] = one
        im[f"vA{di}"] = np.ascontiguousarray(vv.reshape(B, d, nt + 1, 128, 65).transpose(0, 3, 1, 2, 4)).reshape(B, 128, d * (nt + 1) * 65)
    im["qB"] = np.ascontiguousarray(qkv[:, :, 0, hb].transpose(0, 2, 1))
    im["kB"] = np.ascontiguousarray(qkv[:, :, 1, hb].transpose(0, 2, 1))
    vb = np.zeros((B, S, 65), BF)
    vb[:, :, :64] = qkv[:, :, 2, hb]
    vb[:, :, 64] = one
    im["vB"] = np.ascontiguousarray(vb.reshape(B, 64, 128, 65).transpose(0, 2, 1, 3)).reshape(B, 128, 64 * 65)
    im["ebias"] = na_bias_table(rpb_i[c])
    im["maskd"] = attn_mask()
    return im


def attn_core_outputs(results, B, S):
    oa = np.zeros((3, B, S, 8, 65), np.float32)
    ob = np.zeros((B, S, 8, 64), np.float32)
    for c, r in enumerate(results):
        for di, d in enumerate(DILS):
            Ls = S // d
            nt = Ls // 128
            a = r[f"oA{di}"].reshape(B, 128, d, nt, 65).transpose(0, 3, 1, 2, 4).reshape(B, S, 65)
            oa[di, :, :, c, :] = a
        ob[:, :, c, :] = r["oB"].reshape(B, 128, 64, 64).transpose(0, 2, 1, 3).reshape(B, S, 64)
    return oa.reshape(3, B, S, 8 * 65), ob.reshape(B, S, 512)


N_CORES = 8
_PROGS = {}


def _prog(name, fn):
    if name not in _PROGS:
        _PROGS[name] = fn()
    return _PROGS[name]


def _run(nc, in_maps):
    return run_bass_kernel_spmd(nc, in_maps, core_ids=list(range(N_CORES))).results


def _rep(v):
    return np.ascontiguousarray(np.broadcast_to(np.asarray(v, np.float32)[None], (128, 1024)))


def _attn_layer(xf, w_in, w_out, rpb_i, g, b, B, S):
    T = xf.shape[0] // N_CORES
    NT = T // 128
    halves = S // T
    nc1 = _prog('qkv', lambda: build_qkv(NT=NT))
    ims = []
    for c in range(N_CORES):
        p0 = (c % halves) * T
        ims.append({"x": xf[c * T:(c + 1) * T], "win": w_in, "cs": rope_cs(np.arange(p0, p0 + T))})
    r1 = _run(nc1, ims)
    qkv = np.concatenate([r["qkv"] for r in r1], 0).reshape(B, S, 3, 16, 64)
    nc2 = _prog('attn', lambda: build_attn(NB=B))
    r2 = _run(nc2, [attn_core_inputs(qkv, c, rpb_i) for c in range(N_CORES)])
    oa, ob = attn_core_outputs(r2, B, S)
    oa = oa.reshape(3, B * S, 8 * 65)
    ob = ob.reshape(B * S, 512)
    nc3 = _prog('oproj', lambda: build_oproj(NT=NT))
    ims = [{"x": xf[c * T:(c + 1) * T], "oa": np.ascontiguousarray(oa[:, c * T:(c + 1) * T]),
            "ob": np.ascontiguousarray(ob[c * T:(c + 1) * T]), "wout": w_out, "lng": _rep(g), "lnb": _rep(b)}
           for c in range(N_CORES)]
    r3 = _run(nc3, ims)
    return np.concatenate([r["y"] for r in r3], 0)


def _pool_layer(xf, pw, psc, g, b, B, S):
    T = xf.shape[0] // N_CORES
    NT = T // 128
    halves = S // T
    ncp = _prog('pool', lambda: build_pool(NT=NT))
    ims = []
    for c in range(N_CORES):
        h = c % halves
        xp = np.zeros((T + 16, 1024), np.float32)
        lo = c * T - (8 if h > 0 else 0)
        hi = (c + 1) * T + (8 if h < halves - 1 else 0)
        xp[8 - (c * T - lo): 8 + T + (hi - (c + 1) * T)] = xf[lo:hi]
        Bm, Bh = pool_tables(h == 0, h == halves - 1)
        ims.append({"xp": xp, "bm": np.ascontiguousarray(Bm.transpose(2, 0, 1, 3).reshape(128, 12 * 128)),
                    "bh": np.ascontiguousarray(Bh.transpose(2, 0, 1, 3).reshape(16, 12 * 128)),
                    "pw": pw, "psc": _rep(psc), "lng": _rep(g), "lnb": _rep(b)})
    r = _run(ncp, ims)
    return np.concatenate([q["y"] for q in r], 0)


def _moe_layer(xf, rw, rbias, w1, w3, w2, g, b):
    T = xf.shape[0] // N_CORES
    NT = T // 128
    GT = 8
    ncm = _prog('moe', lambda: build_moe(NT=NT, GT=GT, NE=16))
    rb = np.ascontiguousarray(np.broadcast_to(np.tile(np.asarray(rbias, np.float32), GT)[None], (128, GT * 16)))
    ims = [{"x": xf[c * T:(c + 1) * T], "rw": rw, "rbias": rb, "w1": w1, "w3": w3, "w2": w2, "lng": _rep(g), "lnb": _rep(b)}
           for c in range(N_CORES)]
    r = _run(ncm, ims)
    return np.concatenate([q["y"] for q in r], 0)


def kernel(x, w_in, w_out, rpb, pool_w, pool_scale, router_w, router_bias, moe_w1, moe_w3, moe_w2, ln_g, ln_b):
    f = lambda a: np.ascontiguousarray(np.asarray(a, np.float32))
    x = f(x)
    B, S, Dm = x.shape
    xf = x.reshape(B * S, Dm)
    depth = moe_w1.shape[0]
    for layer in range(depth):
        i = layer // 2
        if layer % 2 == 0:
            xf = _attn_layer(xf, f(w_in[i]), f(w_out[i]), f(rpb[i]), ln_g[layer, 0], ln_b[layer, 0], B, S)
        else:
            xf = _pool_layer(xf, f(pool_w[i]), pool_scale[i], ln_g[layer, 0], ln_b[layer, 0], B, S)
        xf = _moe_layer(xf, f(router_w), router_bias, f(moe_w1[layer]), f(moe_w3[layer]), f(moe_w2[layer]),
                        ln_g[layer, 1], ln_b[layer, 1])
    return xf.reshape(B, S, Dm).astype(np.float32)
```

```python
import numpy as np
import concourse.bass as bass
import concourse.mybir as mybir
from concourse.bass_utils import run_bass_kernel_spmd
from contextlib import ExitStack

F32 = mybir.dt.float32
BF16 = mybir.dt.bfloat16
U32 = mybir.dt.uint32
I32 = mybir.dt.int32
AF = mybir.ActivationFunctionType
ALU = mybir.AluOpType
AX = mybir.AxisListType

ENGS = ['pe', 'act', 'dve', 'pool', 'sp']
ENG_ATTR = {'pe': 'tensor', 'act': 'scalar', 'dve': 'vector', 'pool': 'gpsimd', 'sp': 'sync'}


class Tok:
    __slots__ = ('sem', 'val')

    def __init__(self, sem, val):
        self.sem = sem
        self.val = val


class Buf:
    def __init__(self, kb, t, name):
        self.kb = kb
        self.t = t
        self.name = name
        self.w = []
        self.r = {}
        self._slot = None

    def __getitem__(self, k):
        return self.t[k]

    def slot(self):
        if self._slot is None:
            self._slot = self.kb.new_sem()
            self._slotval = 0
        return self._slot


class KB:
    def __init__(self, nc, es, same_eng_sync=True):
        self.nc = nc
        self.es = es
        self.ops = {e: [] for e in ENGS}
        self.nsem = 0
        self.sems = {e: self.new_sem() for e in ENGS}
        self.cnt = {e: 0 for e in ENGS}
        self.waited = {e: {} for e in ENGS}
        self.same_eng_sync = same_eng_sync
        self.nbuf = 0

    def new_sem(self):
        self.nsem += 1
        return self.es.enter_context(self.nc.semaphore(f"sem{self.nsem}"))

    def sb(self, shape, dtype, name=None):
        self.nbuf += 1
        name = name or f"sb{self.nbuf}"
        return Buf(self, self.es.enter_context(self.nc.sbuf_tensor(name, list(shape), dtype)), name)

    def ps(self, shape, dtype=F32, name=None):
        self.nbuf += 1
        name = name or f"ps{self.nbuf}"
        return Buf(self, self.es.enter_context(self.nc.psum_tensor(name, list(shape), dtype)), name)

    def view(self, name="v"):
        return Buf(self, None, name)

    def _waits(self, eng, deps):
        waits = []
        for d in deps:
            if d is None:
                continue
            if isinstance(d, (list, tuple)):
                waits += self._waits(eng, d)
                continue
            if (not self.same_eng_sync) and d.sem is self.sems[eng]:
                continue
            key = id(d.sem)
            if self.waited[eng].get(key, 0) >= d.val:
                continue
            self.waited[eng][key] = d.val
            waits.append((d.sem, d.val))
        return waits

    def _deps(self, reads, writes, deps):
        ds = list(deps)
        for b in reads:
            ds += b.w
        for b in writes:
            ds += b.w
            ds += list(b.r.values())
        return ds

    def _commit(self, tok, reads, writes):
        for b in writes:
            b.w = [tok]
            b.r = {}
        for b in reads:
            if b not in writes:
                o = b.r.get(id(tok.sem))
                if o is None or o.val < tok.val:
                    b.r[id(tok.sem)] = tok

    def op(self, eng, fn, reads=(), writes=(), deps=()):
        waits = self._waits(eng, self._deps(reads, writes, deps))
        self.cnt[eng] += 1
        self.ops[eng].append((waits, fn, (self.sems[eng], 1)))
        tok = Tok(self.sems[eng], self.cnt[eng])
        self._commit(tok, reads, writes)
        return tok

    def group(self, eng, fns, reads=(), writes=(), deps=()):
        waits = self._waits(eng, self._deps(reads, writes, deps))
        for i, fn in enumerate(fns):
            self.cnt[eng] += 1
            self.ops[eng].append((waits if i == 0 else [], fn, (self.sems[eng], 1)))
        tok = Tok(self.sems[eng], self.cnt[eng])
        self._commit(tok, reads, writes)
        return tok

    def dma(self, eng, out, in_, slotbuf, reads=(), writes=(), deps=(), **kw):
        sem = slotbuf.slot()
        ds = list(deps)
        for b in reads:
            ds += b.w
        for b in writes:
            ds += [t for t in b.w if t.sem is not sem]
            ds += list(b.r.values())
        waits = self._waits(eng, ds)
        slotbuf._slotval += 16
        self.ops[eng].append((waits, lambda e: e.dma_start(out=out, in_=in_, **kw), (sem, 16)))
        tok = Tok(sem, slotbuf._slotval)
        self._commit(tok, reads, writes)
        return tok

    def wait_only(self, eng, deps):
        waits = self._waits(eng, deps)
        if waits:
            self.ops[eng].append((waits, None, None))

    def emit(self):
        with self.nc.Block() as block:
            for e in ENGS:
                dec = getattr(block, ENG_ATTR[e])
                ops = self.ops[e]

                def body(eng, ops=ops):
                    for waits, fn, inc in ops:
                        for s, v in waits:
                            eng.wait_ge(s, v)
                        if fn is None:
                            continue
                        inst = fn(eng)
                        if inc is not None:
                            inst.then_inc(inc[0], inc[1])
                dec(body)


def _idma(self, out, in_, idx_ap, scatter, slotbuf, bound, reads=(), writes=(), deps=()):
    sem = slotbuf.slot()
    ds = list(deps)
    for b in reads:
        ds += b.w
    for b in writes:
        ds += [t for t in b.w if t.sem is not sem]
        ds += list(b.r.values())
    waits = self._waits('pool', ds)
    slotbuf._slotval += 16
    if not hasattr(self, '_bregs'):
        self._bregs = {}

    def breg(e):
        if bound not in self._bregs:
            self._bregs[bound] = e.to_reg(bound)
        return self._bregs[bound]
    if scatter:
        fn = lambda e: e.indirect_dma_start(out=out, out_offset=bass.IndirectOffsetOnAxis(ap=idx_ap, axis=0), in_=in_,
                                            in_offset=None, bounds_check=breg(e), oob_is_err=False)
    else:
        fn = lambda e: e.indirect_dma_start(out=out, out_offset=None, in_=in_,
                                            in_offset=bass.IndirectOffsetOnAxis(ap=idx_ap, axis=0), bounds_check=breg(e),
                                            oob_is_err=False)
    self.ops['pool'].append((waits, fn, (sem, 16)))
    tok = Tok(sem, slotbuf._slotval)
    self._commit(tok, reads, writes)
    return tok


KB.idma = _idma


D = 1024
ALPHA = 8.0 ** 0.25
LN_EPS = 1e-5


def make_ident(kb, ident):
    kb.op('pool', lambda e: e.memset(ident[:], 0.0), writes=[ident])
    kb.op('pool', lambda e: e.affine_select(out=ident[:], in_=ident[:], compare_op=ALU.not_equal, fill=1.0,
                                            base=0, pattern=[[-1, 128]], channel_multiplier=1), writes=[ident])


class LNBufs:
    def __init__(self, kb, nbuf=2):
        self.kb = kb
        self.n = nbuf
        self.i = 0
        self.z = [kb.sb([128, D], F32) for _ in range(nbuf)]
        self.o = [kb.sb([128, D], F32) for _ in range(nbuf)]
        self.st = [kb.sb([128, 12], F32) for _ in range(nbuf)]
        self.mv = [kb.sb([128, 2], F32) for _ in range(nbuf)]
        self.rs = [kb.sb([128, 1], F32) for _ in range(nbuf)]
        self.nb = [kb.sb([128, 1], F32) for _ in range(nbuf)]
        self.eps = kb.sb([128, 1], F32)
        kb.op('pool', lambda e: e.memset(self.eps[:], LN_EPS), writes=[self.eps])


def emit_ln(kb, L, xbuf, x_ap, h_aps, h_bufs, g, b, out_dram_ap, dma_eng='sp'):
    i = L.i
    L.i = (L.i + 1) % L.n
    z, o, st, mv, rs, nb = L.z[i], L.o[i], L.st[i], L.mv[i], L.rs[i], L.nb[i]
    for hf in range(2):
        sl = slice(hf * 512, (hf + 1) * 512)
        kb.op('dve', lambda e, sl=sl, hf=hf: e.scalar_tensor_tensor(out=z[:, sl], in0=x_ap[:, sl], scalar=ALPHA,
                                                                    in1=h_aps[hf], op0=ALU.mult, op1=ALU.add),
              reads=[xbuf, h_bufs[hf]], writes=[z] if hf == 0 else [], deps=z.w if hf == 1 else ())
    z.w = [Tok(kb.sems['dve'], kb.cnt['dve'])]
    for hf in range(2):
        sl = slice(hf * 512, (hf + 1) * 512)
        kb.op('dve', lambda e, sl=sl, hf=hf: e.bn_stats(out=st[:, hf * 6:(hf + 1) * 6], in_=z[:, sl]),
              reads=[z], writes=[st] if hf == 0 else [], deps=st.w if hf == 1 else ())
    st.w = [Tok(kb.sems['dve'], kb.cnt['dve'])]
    kb.op('dve', lambda e: e.bn_aggr(out=mv[:], in_=st[:]), reads=[st], writes=[mv])
    kb.op('act', lambda e: e.activation(out=rs[:], in_=mv[:, 1:2], func=AF.Sqrt, bias=L.eps[:], scale=1.0),
          reads=[mv, L.eps], writes=[rs])
    kb.op('dve', lambda e: e.reciprocal(out=rs[:], in_=rs[:]), reads=[rs], writes=[rs])
    kb.op('dve', lambda e: e.scalar_tensor_tensor(out=nb[:], in0=mv[:, 0:1], scalar=-1.0, in1=rs[:],
                                                  op0=ALU.mult, op1=ALU.mult), reads=[mv, rs], writes=[nb])
    kb.op('act', lambda e: e.activation(out=o[:], in_=z[:], func=AF.Identity, bias=nb[:], scale=rs[:]),
          reads=[z, rs, nb], writes=[o])
    kb.op('pool', lambda e: e.tensor_tensor(out=o[:], in0=o[:], in1=g[:], op=ALU.mult), reads=[o, g], writes=[o])
    kb.op('pool', lambda e: e.tensor_tensor(out=o[:], in0=o[:], in1=b[:], op=ALU.add), reads=[o, b], writes=[o])
    return kb.dma(dma_eng, out_dram_ap, o[:], o, reads=[o])


def build_moe(NT=32, GT=8, NE=16, debug=False):
    nc = bass.Bass("TRN2", target_bir_lowering=False)
    T = NT * 128
    x = nc.dram_tensor("x", [T, D], F32, kind="ExternalInput").ap()
    rw = nc.dram_tensor("rw", [D, 16], F32, kind="ExternalInput").ap()
    rbias = nc.dram_tensor("rbias", [128, GT * 16], F32, kind="ExternalInput").ap()
    w1 = nc.dram_tensor("w1", [16, D, D], F32, kind="ExternalInput").ap()
    w3 = nc.dram_tensor("w3", [16, D, D], F32, kind="ExternalInput").ap()
    w2 = nc.dram_tensor("w2", [16, D, D], F32, kind="ExternalInput").ap()
    lng = nc.dram_tensor("lng", [128, D], F32, kind="ExternalInput").ap()
    lnb = nc.dram_tensor("lnb", [128, D], F32, kind="ExternalInput").ap()
    y = nc.dram_tensor("y", [T, D], F32, kind="ExternalOutput").ap()
    if debug:
        gdbg = nc.dram_tensor("gdbg", [NT // GT, 128, GT * 16], F32, kind="ExternalOutput").ap()
    with ExitStack() as es:
        kb = KB(nc, es)
        ident = kb.sb([128, 128], F32)
        make_ident(kb, ident)
        rws = kb.sb([128, 8, 16], F32)
        kb.dma('sp', rws[:], rw.rearrange("(c p) e -> p c e", p=128), rws, writes=[rws])
        rb = kb.sb([128, GT * 16], F32)
        kb.dma('sp', rb[:], rbias[:, :], rb, writes=[rb])
        g = kb.sb([128, D], F32)
        b = kb.sb([128, D], F32)
        kb.dma('sp', g[:], lng[:, :], g, writes=[g])
        kb.dma('sp', b[:], lnb[:, :], b, writes=[b])
        L = LNBufs(kb, nbuf=1)

        xin = [kb.sb([128, D], F32) for _ in range(2)]
        xT32 = [kb.sb([128, 8, 128], F32) for _ in range(2)]
        xT = kb.sb([128, 8, GT * 128], BF16)
        xTv = [kb.view(f"xTv{t}") for t in range(GT)]
        acc = kb.sb([128, GT, D], F32)
        accv = [[kb.view() for _ in range(2)] for t in range(GT)]
        wb = [[kb.sb([128, 8, D], BF16) for _ in range(3)] for _ in range(2)]
        hT = [kb.sb([128, 8, 512], BF16) for _ in range(1)]
        hTv = [[kb.view() for _ in range(8)] for _ in range(1)]
        sil = [kb.sb([128, 512], BF16) for _ in range(2)]
        banks = [kb.ps([128, 512], F32) for _ in range(8)]
        NG = GT * 16
        aff = kb.sb([128, NG], F32)
        affv = [kb.view() for _ in range(GT)]
        S8 = kb.sb([128, GT * 4, 8], F32)
        Pt = kb.sb([128, GT * 4, 6], F32)
        gs = kb.sb([128, GT * 4], F32)
        gmax = kb.sb([128, GT], F32)
        ghot = kb.sb([128, GT * 4], F32)
        c1 = kb.sb([128, GT * 4, 4], F32)
        c2 = kb.sb([128, GT * 4, 4], F32)
        GA = kb.sb([128, GT, 16], F32)
        den = kb.sb([128, GT], F32)
        G = kb.sb([128, GT, 16], F32)

        wsrc = [w1, w3, w2]
        nsteps = (NT // GT) * NE
        step_list = [(gi, e) for gi in range(NT // GT) for e in range(NE)]

        def load_weights(si):
            gi, e = step_list[si]
            slot = si % 2
            for j in range(3):
                kb.dma('pool', wb[slot][j][:], wsrc[j][e].rearrange("(c p) f -> p c f", p=128), wb[slot][j],
                       writes=[wb[slot][j]])

        if nsteps > 0:
            load_weights(0)
        ybank = 0
        out_toks = []
        for gi in range(NT // GT):
            for t in range(GT):
                tile = gi * GT + t
                xi = xin[t % 2]
                x32 = xT32[t % 2]
                kb.dma('sp', xi[:], x[tile * 128:(tile + 1) * 128, :], xi, writes=[xi])
                for hf in range(2):
                    bk = banks[4 + hf]
                    kb.group('pe', [lambda e, c=c, bk=bk, xi=xi: e.transpose(bk[:, (c % 4) * 128:(c % 4 + 1) * 128],
                                                                               xi[:, c * 128:(c + 1) * 128], ident[:])
                                    for c in range(hf * 4, hf * 4 + 4)], reads=[xi, ident], writes=[bk])
                    if True:
                        kb.op('act', lambda e, bk=bk, hf=hf, x32=x32: e.copy(
                            out=x32[:, hf * 4:(hf + 1) * 4, :], in_=bk[:].rearrange("p (c t) -> p c t", c=4)),
                            reads=[bk], writes=[x32] if hf == 0 else [], deps=x32.w if hf == 1 else ())
                    kb.op('dve', lambda e, hf=hf, t=t, x32=x32: e.tensor_copy(
                        out=xT[:, hf * 4:(hf + 1) * 4, t * 128:(t + 1) * 128], in_=x32[:, hf * 4:(hf + 1) * 4, :]),
                        reads=[], writes=[xTv[t]] if hf == 0 else [], deps=list(xTv[t].w if hf == 1 else ()) + [Tok(kb.sems['act'], kb.cnt['act'])])
                x32.w = [Tok(kb.sems['act'], kb.cnt['act'])]
                xTv[t].w = [Tok(kb.sems['dve'], kb.cnt['dve'])]
                x32.r[id(kb.sems['dve'])] = Tok(kb.sems['dve'], kb.cnt['dve'])
                rbk = banks[6]
                kb.group('pe', [lambda e, c=c, x32=x32, rbk=rbk: e.matmul(rbk[:, 0:16], x32[:, c, :], rws[:, c, :],
                                                                         start=(c == 0), stop=(c == 7))
                                for c in range(8)], reads=[x32, rws], writes=[rbk])
                kb.op('act', lambda e, t=t, rbk=rbk: e.activation(out=aff[:, t * 16:(t + 1) * 16], in_=rbk[:, 0:16],
                                                                   func=AF.Sigmoid), reads=[rbk], writes=[affv[t]])
            S4 = S8[:, :, 0:4]
            aff4 = aff[:].rearrange("p (g j) -> p g j", j=4)
            kb.op('dve', lambda e: e.tensor_tensor(out=S8[:, :, 0:4], in0=aff4, in1=rb[:].rearrange("p (g j) -> p g j", j=4),
                                                   op=ALU.add), reads=affv + [rb], writes=[S8])
            kb.op('dve', lambda e: e.tensor_copy(out=S8[:, :, 4:8], in_=S8[:, :, 0:4]), reads=[S8], writes=[S8])
            pairs = [(0, 1), (0, 2), (0, 3), (1, 2), (1, 3), (2, 3)]
            for i, (a, bb) in enumerate(pairs):
                kb.op('dve', lambda e, i=i, a=a, bb=bb: e.tensor_tensor(out=Pt[:, :, i], in0=S8[:, :, a], in1=S8[:, :, bb],
                                                                         op=ALU.add), reads=[S8], writes=[Pt])
            kb.op('dve', lambda e: e.tensor_reduce(out=gs[:], in_=Pt[:], axis=AX.X, op=ALU.max), reads=[Pt], writes=[gs])
            kb.op('dve', lambda e: e.tensor_reduce(out=gmax[:], in_=gs[:].rearrange("p (t g) -> p t g", g=4), axis=AX.X,
                                                   op=ALU.max), reads=[gs], writes=[gmax])
            kb.op('dve', lambda e: e.tensor_tensor(out=ghot[:].rearrange("p (t g) -> p t g", g=4),
                                                   in0=gs[:].rearrange("p (t g) -> p t g", g=4),
                                                   in1=gmax[:].unsqueeze(2).broadcast_to([128, GT, 4]), op=ALU.is_ge),
                  reads=[gs, gmax], writes=[ghot])
            kb.op('dve', lambda e: e.tensor_tensor(out=c1[:], in0=S8[:, :, 0:4], in1=S8[:, :, 1:5], op=ALU.is_gt),
                  reads=[S8], writes=[c1])
            kb.op('dve', lambda e: e.tensor_tensor(out=c2[:], in0=S8[:, :, 0:4], in1=S8[:, :, 2:6], op=ALU.is_gt),
                  reads=[S8], writes=[c2])
            kb.op('dve', lambda e: e.tensor_tensor(out=c1[:], in0=c1[:], in1=c2[:], op=ALU.add), reads=[c1, c2], writes=[c1])
            kb.op('dve', lambda e: e.tensor_tensor(out=c2[:], in0=S8[:, :, 0:4], in1=S8[:, :, 3:7], op=ALU.is_gt),
                  reads=[S8], writes=[c2])
            kb.op('dve', lambda e: e.tensor_tensor(out=c1[:], in0=c1[:], in1=c2[:], op=ALU.add), reads=[c1, c2], writes=[c1])
            kb.op('dve', lambda e: e.scalar_tensor_tensor(out=c1[:], in0=c1[:], scalar=2.0,
                                                          in1=ghot[:].unsqueeze(2).broadcast_to([128, GT * 4, 4]),
                                                          op0=ALU.is_ge, op1=ALU.mult), reads=[c1, ghot], writes=[c1])
            kb.op('dve', lambda e: e.tensor_tensor(out=GA[:].rearrange("p t (g j) -> p (t g) j", j=4), in0=c1[:], in1=aff4,
                                                   op=ALU.mult), reads=[c1] + affv, writes=[GA])
            kb.op('dve', lambda e: e.tensor_reduce(out=den[:], in_=GA[:], axis=AX.X, op=ALU.add), reads=[GA], writes=[den])
            kb.op('dve', lambda e: e.reciprocal(out=den[:], in_=den[:]), reads=[den], writes=[den])
            kb.op('dve', lambda e: e.tensor_tensor(out=G[:], in0=GA[:], in1=den[:].unsqueeze(2).broadcast_to([128, GT, 16]),
                                                   op=ALU.mult), reads=[GA, den], writes=[G])
            if debug:
                out_toks.append(kb.dma('sp', gdbg[gi], G[:].rearrange("p t e -> p (t e)"), G, reads=[G]))
            for e_i in range(NE):
                si = gi * NE + e_i
                slot = si % 2
                if si + 1 < nsteps:
                    load_weights(si + 1)
                w1b, w3b, w2b = wb[slot]
                for tt in range(GT // 4):
                    hb = hT[0]
                    hv = hTv[0]
                    tsl = slice(tt * 512, (tt + 1) * 512)
                    for fc in range(8):
                        b1 = banks[fc % 2]
                        b3 = banks[2 + fc % 2]
                        sl_ = sil[fc % 2]
                        fsl = slice(fc * 128, (fc + 1) * 128)
                        kb.group('pe', [lambda e, c=c, b1=b1, fsl=fsl, w1b=w1b, tsl=tsl: e.matmul(b1[:], w1b[:, c, fsl], xT[:, c, tsl],
                                                                                 start=(c == 0), stop=(c == 7))
                                        for c in range(8)], reads=[w1b] + xTv[tt * 4:(tt + 1) * 4], writes=[b1])
                        kb.group('pe', [lambda e, c=c, b3=b3, fsl=fsl, w3b=w3b, tsl=tsl: e.matmul(b3[:], w3b[:, c, fsl], xT[:, c, tsl],
                                                                                 start=(c == 0), stop=(c == 7))
                                        for c in range(8)], reads=[w3b] + xTv[tt * 4:(tt + 1) * 4], writes=[b3])
                        kb.op('act', lambda e, b1=b1, sl_=sl_: e.activation(out=sl_[:], in_=b1[:], func=AF.Silu),
                              reads=[b1], writes=[sl_])
                        kb.op('dve', lambda e, b3=b3, sl_=sl_, fc=fc, hb=hb: e.tensor_tensor(out=hb[:, fc, :], in0=b3[:],
                                                                                          in1=sl_[:], op=ALU.mult),
                              reads=[b3, sl_], writes=[hv[fc]])
                    for t4 in range(4):
                        t = tt * 4 + t4
                        for oh in range(2):
                            yb = banks[4 + ybank % 4]
                            ybank += 1
                            osl = slice(oh * 512, (oh + 1) * 512)
                            kb.group('pe', [lambda e, fc=fc, yb=yb, t4=t4, osl=osl, hb=hb, w2b=w2b: e.matmul(
                                yb[:], hb[:, fc, t4 * 128:(t4 + 1) * 128], w2b[:, fc, osl], start=(fc == 0), stop=(fc == 7))
                                for fc in range(8)], reads=[w2b] + hv, writes=[yb])
                            av = accv[t][oh]
                            if e_i == 0:
                                kb.op('dve', lambda e, yb=yb, t=t, osl=osl, e_i=e_i: e.tensor_scalar(
                                    out=acc[:, t, osl], in0=yb[:], scalar1=G[:, t, e_i:e_i + 1], scalar2=None, op0=ALU.mult),
                                    reads=[yb, G], writes=[av])
                            else:
                                kb.op('dve', lambda e, yb=yb, t=t, osl=osl, e_i=e_i: e.scalar_tensor_tensor(
                                    out=acc[:, t, osl], in0=yb[:], scalar=G[:, t, e_i:e_i + 1], in1=acc[:, t, osl],
                                    op0=ALU.mult, op1=ALU.add), reads=[yb, G], writes=[av])
            for t in range(GT):
                tile = gi * GT + t
                xi = xin[t % 2]
                kb.dma('sp', xi[:], x[tile * 128:(tile + 1) * 128, :], xi, writes=[xi])
                out_toks.append(emit_ln(kb, L, xi, xi, [acc[:, t, 0:512], acc[:, t, 512:1024]], accv[t], g, b,
                                        y[tile * 128:(tile + 1) * 128, :]))
        kb.wait_only('sp', out_toks)
        kb.emit()
    return nc


BIGIDX = 16.0 * 1024 + 7.0
BIG2 = 65536.0


def build_moe_sparse(NT=32, C=768, NE=16, debug=False):
    nc = bass.Bass("TRN2", target_bir_lowering=False)
    T = NT * 128
    NS = C // 128
    NG = NT * 16
    x = nc.dram_tensor("x", [T, D], F32, kind="ExternalInput").ap()
    rw = nc.dram_tensor("rw", [D, 16], F32, kind="ExternalInput").ap()
    rbias = nc.dram_tensor("rbias", [128, NG], F32, kind="ExternalInput").ap()
    eoffd = nc.dram_tensor("eoff", [128, NG], F32, kind="ExternalInput").ap()
    w1 = nc.dram_tensor("w1", [16, D, D], F32, kind="ExternalInput").ap()
    w3 = nc.dram_tensor("w3", [16, D, D], F32, kind="ExternalInput").ap()
    w2 = nc.dram_tensor("w2", [16, D, D], F32, kind="ExternalInput").ap()
    lng = nc.dram_tensor("lng", [128, D], F32, kind="ExternalInput").ap()
    lnb = nc.dram_tensor("lnb", [128, D], F32, kind="ExternalInput").ap()
    y = nc.dram_tensor("y", [T, D], F32, kind="ExternalOutput").ap()
    xs = nc.dram_tensor("xs_scr", [16 * C, D], F32, kind="Internal").ap()
    ys = nc.dram_tensor("ys_scr", [16 * C, D], F32, kind="Internal").ap()
    if debug:
        dbg = nc.dram_tensor("dbg", [4, 128, NT], F32, kind="ExternalOutput").ap()
    with ExitStack() as es:
        kb = KB(nc, es)
        ident = kb.sb([128, 128], F32)
        make_ident(kb, ident)
        ltri = kb.sb([128, 128], F32)
        ones = kb.sb([128, 128], F32)
        kb.op('pool', lambda e: e.memset(ones[:], 1.0), writes=[ones])
        kb.op('pool', lambda e: e.memset(ltri[:], 1.0), writes=[ltri])
        kb.op('pool', lambda e: e.affine_select(out=ltri[:], in_=ltri[:], compare_op=ALU.is_gt, fill=0.0, base=0,
                                                pattern=[[1, 128]], channel_multiplier=-1), writes=[ltri])
        rws = kb.sb([128, 8, 16], F32)
        kb.dma('sp', rws[:], rw.rearrange("(c p) e -> p c e", p=128), rws, writes=[rws])
        rb = kb.sb([128, NG], F32)
        kb.dma('sp', rb[:], rbias[:, :], rb, writes=[rb])
        eoff = kb.sb([128, NG], F32)
        kb.dma('sp', eoff[:], eoffd[:, :], eoff, writes=[eoff])
        g = kb.sb([128, D], F32)
        b = kb.sb([128, D], F32)
        kb.dma('sp', g[:], lng[:, :], g, writes=[g])
        kb.dma('sp', b[:], lnb[:, :], b, writes=[b])
        L = LNBufs(kb, nbuf=1)
        xin = [kb.sb([128, D], F32) for _ in range(2)]
        x32 = [kb.sb([128, 8, 128], F32) for _ in range(2)]
        xT = kb.sb([128, 8, C], BF16)
        xTv = [kb.view() for _ in range(NS)]
        w13 = [[kb.sb([128, 8, D], BF16) for _ in range(2)] for _ in range(2)]
        w2b = kb.sb([128, 8, D], BF16)
        hT = kb.sb([128, 8, 512], BF16)
        hv = [kb.view() for _ in range(8)]
        sil = [kb.sb([128, 512], BF16) for _ in range(2)]
        ga = kb.sb([128, D], F32)
        gb_ = kb.sb([128, D], F32)
        banks = [kb.ps([128, 512], F32) for _ in range(8)]
        xsb = kb.view("xs")
        ysb = kb.view("ys")
        wsrc = [w1, w3]

        def load_w13(e):
            for j in range(2):
                kb.dma('pool', w13[e % 2][j][:], wsrc[j][e].rearrange("(c p) f -> p c f", p=128), w13[e % 2][j],
                       writes=[w13[e % 2][j]])

        def load_w2(e):
            kb.dma('pool', w2b[:], w2[e].rearrange("(c p) f -> p c f", p=128), w2b, writes=[w2b])

        if NE > 0:
            load_w13(0)
            load_w2(0)
        kb.op('dve', lambda e: e.memset(ga[:], 0.0), writes=[ga])
        kb.op('dve', lambda e: e.memset(gb_[:], 0.0), writes=[gb_])

        aff = kb.sb([128, NG], F32)
        affv = [kb.view() for _ in range(NT)]
        for t in range(NT):
            xi = xin[t % 2]
            x3 = x32[t % 2]
            kb.dma('sp', xi[:], x[t * 128:(t + 1) * 128, :], xi, writes=[xi])
            for hf in range(2):
                bk = banks[4 + hf]
                kb.group('pe', [lambda e, c=c, bk=bk, xi=xi: e.transpose(bk[:, (c % 4) * 128:(c % 4 + 1) * 128],
                                                                           xi[:, c * 128:(c + 1) * 128], ident[:])
                                for c in range(hf * 4, hf * 4 + 4)], reads=[xi, ident], writes=[bk])
                tk = kb.op('act', lambda e, bk=bk, hf=hf, x3=x3: e.copy(out=x3[:, hf * 4:(hf + 1) * 4, :],
                                                                         in_=bk[:].rearrange("p (c t) -> p c t", c=4)),
                           reads=[bk], writes=[x3] if hf == 0 else [], deps=x3.w if hf == 1 else ())
            x3.w = [tk]
            rbk = banks[6 + t % 2]
            kb.group('pe', [lambda e, c=c, x3=x3, rbk=rbk: e.matmul(rbk[:, 0:16], x3[:, c, :], rws[:, c, :],
                                                                   start=(c == 0), stop=(c == 7)) for c in range(8)],
                     reads=[x3, rws], writes=[rbk])
            kb.op('act', lambda e, t=t, rbk=rbk: e.activation(out=aff[:, t * 16:(t + 1) * 16], in_=rbk[:, 0:16], func=AF.Sigmoid),
                  reads=[rbk], writes=[affv[t]])
        S8 = kb.sb([128, NT * 4, 8], F32)
        Pt = kb.sb([128, NT * 4, 6], F32)
        gs = kb.sb([128, NT * 4], F32)
        gmax = kb.sb([128, NT], F32)
        ghot = kb.sb([128, NT * 4], F32)
        c1 = kb.sb([128, NT * 4, 4], F32)
        c2 = kb.sb([128, NT * 4, 4], F32)
        GA = kb.sb([128, NT, 16], F32)
        den = kb.sb([128, NT], F32)
        G = kb.sb([128, NT, 16], F32)
        aff4 = aff[:].rearrange("p (g j) -> p g j", j=4)

        def dv(fn, reads, writes):
            return kb.op('dve', fn, reads=reads, writes=writes)
        dv(lambda e: e.tensor_tensor(out=S8[:, :, 0:4], in0=aff4, in1=rb[:].rearrange("p (g j) -> p g j", j=4), op=ALU.add), affv + [rb], [S8])
        dv(lambda e: e.tensor_copy(out=S8[:, :, 4:8], in_=S8[:, :, 0:4]), [S8], [S8])
        for i, (a, bb) in enumerate([(0, 1), (0, 2), (0, 3), (1, 2), (1, 3), (2, 3)]):
            dv(lambda e, i=i, a=a, bb=bb: e.tensor_tensor(out=Pt[:, :, i], in0=S8[:, :, a], in1=S8[:, :, bb], op=ALU.add), [S8], [Pt])
        dv(lambda e: e.tensor_reduce(out=gs[:], in_=Pt[:], axis=AX.X, op=ALU.max), [Pt], [gs])
        dv(lambda e: e.tensor_reduce(out=gmax[:], in_=gs[:].rearrange("p (t g) -> p t g", g=4), axis=AX.X, op=ALU.max), [gs], [gmax])
        dv(lambda e: e.tensor_tensor(out=ghot[:].rearrange("p (t g) -> p t g", g=4), in0=gs[:].rearrange("p (t g) -> p t g", g=4),
                                     in1=gmax[:].unsqueeze(2).broadcast_to([128, NT, 4]), op=ALU.is_ge), [gs, gmax], [ghot])
        dv(lambda e: e.tensor_tensor(out=c1[:], in0=S8[:, :, 0:4], in1=S8[:, :, 1:5], op=ALU.is_gt), [S8], [c1])
        dv(lambda e: e.tensor_tensor(out=c2[:], in0=S8[:, :, 0:4], in1=S8[:, :, 2:6], op=ALU.is_gt), [S8], [c2])
        dv(lambda e: e.tensor_tensor(out=c1[:], in0=c1[:], in1=c2[:], op=ALU.add), [c1, c2], [c1])
        dv(lambda e: e.tensor_tensor(out=c2[:], in0=S8[:, :, 0:4], in1=S8[:, :, 3:7], op=ALU.is_gt), [S8], [c2])
        dv(lambda e: e.tensor_tensor(out=c1[:], in0=c1[:], in1=c2[:], op=ALU.add), [c1, c2], [c1])
        dv(lambda e: e.scalar_tensor_tensor(out=c1[:], in0=c1[:], scalar=2.0, in1=ghot[:].unsqueeze(2).broadcast_to([128, NT * 4, 4]),
                                            op0=ALU.is_ge, op1=ALU.mult), [c1, ghot], [c1])
        dv(lambda e: e.tensor_tensor(out=GA[:].rearrange("p t (g j) -> p (t g) j", j=4), in0=c1[:], in1=aff4, op=ALU.mult), [c1] + affv, [GA])
        dv(lambda e: e.tensor_reduce(out=den[:], in_=GA[:], axis=AX.X, op=ALU.add), [GA], [den])
        dv(lambda e: e.reciprocal(out=den[:], in_=den[:]), [den], [den])
        dv(lambda e: e.tensor_tensor(out=G[:], in0=GA[:], in1=den[:].unsqueeze(2).broadcast_to([128, NT, 16]), op=ALU.mult), [GA, den], [G])
        chs = c1
        chs3 = chs[:].rearrange("p (t g) j -> p t (g j)", g=4)
        CS = kb.sb([128, NT, 16], F32)
        dv(lambda e: e.memset(CS[:, 0, :], 0.0), [], [CS])
        for t in range(1, NT):
            dv(lambda e, t=t: e.tensor_tensor(out=CS[:, t, :], in0=CS[:, t - 1, :], in1=chs3[:, t - 1, :], op=ALU.add), [chs], [CS])
        rkb = banks[4]
        fns = []
        for t in range(NT):
            fns.append(lambda e, t=t: e.matmul(rkb[:, t * 16:(t + 1) * 16], ltri[:], chs3[:, t, :], start=True, stop=False))
            fns.append(lambda e, t=t: e.matmul(rkb[:, t * 16:(t + 1) * 16], ones[:], CS[:, t, :], start=False, stop=True))
        kb.group('pe', fns, reads=[ltri, ones, chs, CS], writes=[rkb])
        rank = kb.sb([128, NG], F32)
        sel = kb.sb([128, NG], F32)
        slotv = kb.sb([128, NT, 16], F32)
        slot2 = kb.sb([128, NT, 16], F32)
        Gv = S8
        Gv = kb.sb([128, NT, 16], F32)
        lo = kb.sb([128, NT], F32)
        hi = kb.sb([128, NT], F32)
        glo = kb.sb([128, NT], F32)
        ghi = kb.sb([128, NT], F32)
        lo_t = [kb.sb([128, 1], I32) for _ in range(NT)]
        hi_t = [kb.sb([128, 1], I32) for _ in range(NT)]
        chs_f = chs[:].rearrange("p a j -> p (a j)")
        dv(lambda e: e.tensor_copy(out=rank[:], in_=rkb[:, 0:NG]), [rkb], [rank])
        dv(lambda e: e.scalar_tensor_tensor(out=sel[:], in0=rank[:], scalar=float(C), in1=chs_f, op0=ALU.is_lt, op1=ALU.mult), [rank, chs], [sel])
        dv(lambda e: e.scalar_tensor_tensor(out=rank[:], in0=rank[:], scalar=-BIGIDX, in1=eoff[:], op0=ALU.add, op1=ALU.add), [rank, eoff], [rank])
        dv(lambda e: e.tensor_tensor(out=rank[:], in0=rank[:], in1=sel[:], op=ALU.mult), [rank, sel], [rank])
        dv(lambda e: e.tensor_scalar(out=slotv[:].rearrange("p t e -> p (t e)"), in0=rank[:], scalar1=BIGIDX, scalar2=None, op0=ALU.add), [rank], [slotv])
        dv(lambda e: e.tensor_tensor(out=Gv[:].rearrange("p t e -> p (t e)"), in0=G[:].rearrange("p t e -> p (t e)"), in1=sel[:], op=ALU.mult), [G, sel], [Gv])
        dv(lambda e: e.tensor_reduce(out=lo[:], in_=slotv[:], axis=AX.X, op=ALU.min), [slotv], [lo])
        dv(lambda e: e.tensor_tensor(out=slot2[:], in0=slotv[:], in1=lo[:].unsqueeze(2).broadcast_to([128, NT, 16]), op=ALU.is_equal), [slotv, lo], [slot2])
        dv(lambda e: e.tensor_tensor(out=GA[:], in0=Gv[:], in1=slot2[:], op=ALU.mult), [Gv, slot2], [GA])
        dv(lambda e: e.tensor_reduce(out=glo[:], in_=GA[:], axis=AX.X, op=ALU.add), [GA], [glo])
        dv(lambda e: e.scalar_tensor_tensor(out=slot2[:], in0=slot2[:], scalar=BIG2, in1=slotv[:], op0=ALU.mult, op1=ALU.add), [slot2, slotv], [slot2])
        dv(lambda e: e.tensor_reduce(out=hi[:], in_=slot2[:], axis=AX.X, op=ALU.min), [slot2], [hi])
        dv(lambda e: e.tensor_tensor(out=slot2[:], in0=slot2[:], in1=hi[:].unsqueeze(2).broadcast_to([128, NT, 16]), op=ALU.is_equal), [slot2, hi], [slot2])
        dv(lambda e: e.tensor_tensor(out=GA[:], in0=Gv[:], in1=slot2[:], op=ALU.mult), [Gv, slot2], [GA])
        dv(lambda e: e.tensor_reduce(out=ghi[:], in_=GA[:], axis=AX.X, op=ALU.add), [GA], [ghi])
        for t in range(NT):
            dv(lambda e, t=t: e.tensor_copy(out=lo_t[t][:], in_=lo[:, t:t + 1]), [lo], [lo_t[t]])
            dv(lambda e, t=t: e.tensor_copy(out=hi_t[t][:], in_=hi[:, t:t + 1]), [hi], [hi_t[t]])
        out_toks = []
        if debug:
            for k_, src in enumerate([lo, hi, glo, ghi]):
                out_toks.append(kb.dma('sp', dbg[k_], src[:], src, reads=[src]))
        bound = 16 * C - 1
        sc_toks = []
        for t in range(NT):
            xi = xin[t % 2]
            kb.dma('sp', xi[:], x[t * 128:(t + 1) * 128, :], xi, writes=[xi])
            kb.idma(xs[:, :], xi[:, :], lo_t[t][:, :], True, xsb, bound, reads=[xi, lo_t[t]], writes=[xsb])
            tk_s = kb.idma(xs[:, :], xi[:, :], hi_t[t][:, :], True, xsb, bound, reads=[xi, hi_t[t]], writes=[xsb])
            sc_toks.append(tk_s)
            if len(sc_toks) >= 3:
                kb.wait_only('pool', [sc_toks[-3]])
        ybank = 0
        chunks = []
        o_ = 0
        while o_ < C:
            wdt = min(512, C - o_)
            chunks.append((o_, wdt))
            o_ += wdt
        for e_i in range(NE):
            if e_i + 1 < NE:
                load_w13(e_i + 1)
            w1b, w3b = w13[e_i % 2]
            for s in range(NS):
                xi = xin[s % 2]
                r0 = e_i * C + s * 128
                kb.dma('sp', xi[:], xs[r0:r0 + 128, :], xi, reads=[xsb], writes=[xi])
                for hf in range(2):
                    bk = banks[4 + (ybank % 4)]
                    ybank += 1
                    kb.group('pe', [lambda e, c=c, bk=bk, xi=xi: e.transpose(bk[:, (c % 4) * 128:(c % 4 + 1) * 128],
                                                                               xi[:, c * 128:(c + 1) * 128], ident[:])
                                    for c in range(hf * 4, hf * 4 + 4)], reads=[xi, ident], writes=[bk])
                    if hf == 0:
                        ta = kb.op('act', lambda e, bk=bk, s=s: e.copy(out=xT[:, 0:4, s * 128:(s + 1) * 128],
                                                                       in_=bk[:].rearrange("p (c t) -> p c t", c=4)),
                                   reads=[bk], writes=[xTv[s]])
                    else:
                        tb = kb.op('dve', lambda e, bk=bk, s=s: e.tensor_copy(out=xT[:, 4:8, s * 128:(s + 1) * 128],
                                                                              in_=bk[:].rearrange("p (c t) -> p c t", c=4)),
                                   reads=[bk], deps=[ta])
                        xTv[s].w = [ta, tb]
            for (c0, wdt) in chunks:
                tsl = slice(c0, c0 + wdt)
                tv = xTv[c0 // 128:(c0 + wdt) // 128]
                for fc in range(8):
                    b1 = banks[fc % 2]
                    b3 = banks[2 + fc % 2]
                    sl_ = sil[fc % 2]
                    fsl = slice(fc * 128, (fc + 1) * 128)
                    kb.group('pe', [lambda e, c=c, b1=b1, fsl=fsl, w1b=w1b, tsl=tsl, wdt=wdt: e.matmul(
                        b1[:, 0:wdt], w1b[:, c, fsl], xT[:, c, tsl], start=(c == 0), stop=(c == 7)) for c in range(8)],
                        reads=[w1b] + tv, writes=[b1])
                    kb.group('pe', [lambda e, c=c, b3=b3, fsl=fsl, w3b=w3b, tsl=tsl, wdt=wdt: e.matmul(
                        b3[:, 0:wdt], w3b[:, c, fsl], xT[:, c, tsl], start=(c == 0), stop=(c == 7)) for c in range(8)],
                        reads=[w3b] + tv, writes=[b3])
                    kb.op('act', lambda e, b1=b1, sl_=sl_, wdt=wdt: e.activation(out=sl_[:, 0:wdt], in_=b1[:, 0:wdt], func=AF.Silu),
                          reads=[b1], writes=[sl_])
                    kb.op('dve', lambda e, b3=b3, sl_=sl_, fc=fc, wdt=wdt: e.tensor_tensor(out=hT[:, fc, 0:wdt], in0=b3[:, 0:wdt],
                                                                                           in1=sl_[:, 0:wdt], op=ALU.mult),
                          reads=[b3, sl_], writes=[hv[fc]])
                for t4 in range(wdt // 128):
                    s = c0 // 128 + t4
                    yst = x32[s % 2]
                    ysf = yst[:].rearrange("p c t -> p (c t)")
                    for oh in range(2):
                        yb = banks[4 + ybank % 4]
                        ybank += 1
                        osl = slice(oh * 512, (oh + 1) * 512)
                        kb.group('pe', [lambda e, fc=fc, yb=yb, t4=t4, osl=osl: e.matmul(
                            yb[:], hT[:, fc, t4 * 128:(t4 + 1) * 128], w2b[:, fc, osl], start=(fc == 0), stop=(fc == 7))
                            for fc in range(8)], reads=[w2b] + hv, writes=[yb])
                        if oh == 0:
                            t0_ = kb.op('act', lambda e, yb=yb, ysf=ysf, osl=osl: e.copy(out=ysf[:, osl], in_=yb[:]), reads=[yb], writes=[yst])
                        else:
                            t1_ = kb.op('dve', lambda e, yb=yb, ysf=ysf, osl=osl: e.tensor_copy(out=ysf[:, osl], in_=yb[:]), reads=[yb], deps=[t0_])
                            yst.w = [t0_, t1_]
                    r0 = e_i * C + s * 128
                    kb.dma('sp', ys[r0:r0 + 128, :], ysf, ysb, reads=[yst], writes=[ysb])
            if e_i + 1 < NE:
                load_w2(e_i + 1)
        acc = kb.sb([128, D], F32)
        accv = [kb.view(), kb.view()]
        for t in range(NT):
            xi = xin[t % 2]
            kb.dma('sp', xi[:], x[t * 128:(t + 1) * 128, :], xi, writes=[xi])
            kb.idma(ga[:, :], ys[:, :], lo_t[t][:, :], False, ga, bound, reads=[ysb, lo_t[t]], writes=[ga])
            kb.idma(gb_[:, :], ys[:, :], hi_t[t][:, :], False, gb_, bound, reads=[ysb, hi_t[t]], writes=[gb_])
            kb.op('act', lambda e, t=t: e.activation(out=acc[:], in_=ga[:], func=AF.Copy, scale=glo[:, t:t + 1]),
                  reads=[ga, glo], writes=[acc])
            for hf in range(2):
                sl = slice(hf * 512, (hf + 1) * 512)
                tk = kb.op('dve', lambda e, t=t, sl=sl: e.scalar_tensor_tensor(out=acc[:, sl], in0=gb_[:, sl], scalar=ghi[:, t:t + 1],
                                                                              in1=acc[:, sl], op0=ALU.mult, op1=ALU.add),
                           reads=[gb_, ghi, acc], writes=[accv[hf]])
            out_toks.append(emit_ln(kb, L, xi, xi, [acc[:, 0:512], acc[:, 512:1024]], accv, g, b, y[t * 128:(t + 1) * 128, :]))
            acc.r.update(accv[0].r)
            acc.r.update(accv[1].r)
        kb.wait_only('sp', out_toks)
        kb.emit()
    return nc


def pool_tables(first_is_start, last_is_end):
    Bm = np.zeros((3, 4, 128, 128), np.float32)
    Bh = np.zeros((3, 4, 16, 128), np.float32)
    for var in range(3):
        start_edge = (var == 0 and first_is_start)
        end_edge = (var == 2 and last_is_end)
        for g, w in enumerate((2, 4, 8, 16)):
            half = w // 2
            for t in range(128):
                lo, hi = t - half, t + half - 1
                if start_edge:
                    lo = max(lo, 0)
                if end_edge:
                    hi = min(hi, 127)
                cnt = hi - lo + 1
                for s in range(lo, hi + 1):
                    if 0 <= s < 128:
                        Bm[var, g, s, t] += 1.0 / cnt
                    elif s < 0:
                        Bh[var, g, 8 + s, t] += 1.0 / cnt
                    else:
                        Bh[var, g, 8 + (s - 128), t] += 1.0 / cnt
                Bm[var, g, t, t] -= 1.0
    return Bm, Bh


def build_pool(NT=32):
    nc = bass.Bass("TRN2", target_bir_lowering=False)
    T = NT * 128
    xp = nc.dram_tensor("xp", [T + 16, D], F32, kind="ExternalInput").ap()
    bm = nc.dram_tensor("bm", [128, 12 * 128], F32, kind="ExternalInput").ap()
    bh = nc.dram_tensor("bh", [16, 12 * 128], F32, kind="ExternalInput").ap()
    pw = nc.dram_tensor("pw", [4, 256, 256], F32, kind="ExternalInput").ap()
    psc = nc.dram_tensor("psc", [128, D], F32, kind="ExternalInput").ap()
    lng = nc.dram_tensor("lng", [128, D], F32, kind="ExternalInput").ap()
    lnb = nc.dram_tensor("lnb", [128, D], F32, kind="ExternalInput").ap()
    y = nc.dram_tensor("y", [T, D], F32, kind="ExternalOutput").ap()
    with ExitStack() as es:
        kb = KB(nc, es)
        g = kb.sb([128, D], F32)
        b = kb.sb([128, D], F32)
        kb.dma('sp', g[:], lng[:, :], g, writes=[g])
        kb.dma('sp', b[:], lnb[:, :], b, writes=[b])
        L = LNBufs(kb, nbuf=2)
        Bm = kb.sb([128, 12 * 128], BF16)
        Bh = kb.sb([16, 12 * 128], BF16)
        kb.dma('pool', Bm[:], bm[:, :], Bm, writes=[Bm])
        kb.dma('pool', Bh[:], bh[:, :], Bh, writes=[Bh])
        W32 = kb.sb([128, 4, 2, 256], F32)
        sc = kb.sb([128, D], F32)
        Wb = kb.sb([128, 4, 2, 256], BF16)
        kb.dma('sp', W32[:], pw.rearrange("g (hh p) e -> p g hh e", p=128), W32, writes=[W32])
        kb.dma('sp', sc[:], psc[:, :], sc, writes=[sc])
        kb.op('dve', lambda e: e.tensor_tensor(out=Wb[:], in0=W32[:],
                                               in1=sc[:].rearrange("p (g e) -> p g e", g=4).unsqueeze(2).broadcast_to([128, 4, 2, 256]),
                                               op=ALU.mult), reads=[W32, sc], writes=[Wb])
        xin = [kb.sb([128, D], F32) for _ in range(3)]
        xh = [kb.sb([16, D], F32) for _ in range(2)]
        xb = [kb.sb([128, D], BF16) for _ in range(2)]
        xhb = [kb.sb([16, D], BF16) for _ in range(2)]
        uT = [kb.sb([128, 8, 128], BF16) for _ in range(2)]
        uTa = [kb.view() for _ in range(2)]
        uTb = [kb.view() for _ in range(2)]
        pu = [[kb.ps([128, 512], F32) for _ in range(2)] for _ in range(2)]
        py = [[kb.ps([128, 512], F32) for _ in range(2)] for _ in range(2)]
        out_toks = []
        for i in range(NT):
            var = 0 if i == 0 else (2 if i == NT - 1 else 1)
            xi = xin[i % 3]
            xhi = xh[i % 2]
            xbi = xb[i % 2]
            xhbi = xhb[i % 2]
            u = uT[i % 2]
            ua, ub = uTa[i % 2], uTb[i % 2]
            pui = pu[i % 2]
            pyi = py[i % 2]
            kb.dma('sp', xi[:], xp[8 + i * 128: 8 + (i + 1) * 128, :], xi, writes=[xi])
            kb.dma('sp', xhi[0:8, :], xp[i * 128: i * 128 + 8, :], xhi, writes=[xhi])
            kb.dma('sp', xhi[8:16, :], xp[8 + (i + 1) * 128: 16 + (i + 1) * 128, :], xhi, writes=[xhi])
            kb.op('act', lambda e, xi=xi, xbi=xbi: e.copy(out=xbi[:], in_=xi[:]), reads=[xi], writes=[xbi])
            kb.op('dve', lambda e, xhi=xhi, xhbi=xhbi: e.tensor_copy(out=xhbi[:], in_=xhi[:]), reads=[xhi], writes=[xhbi])
            for hf in range(2):
                fns = []
                for fc in range(hf * 4, hf * 4 + 4):
                    col = (var * 4 + fc // 2) * 128
                    fns.append(lambda e, fc=fc, col=col, xbi=xbi, bk=pui[hf]: e.matmul(
                        bk[:, (fc % 4) * 128:(fc % 4 + 1) * 128], xbi[:, fc * 128:(fc + 1) * 128], Bm[:, col:col + 128],
                        start=True, stop=False))
                    fns.append(lambda e, fc=fc, col=col, xhbi=xhbi, bk=pui[hf]: e.matmul(
                        bk[:, (fc % 4) * 128:(fc % 4 + 1) * 128], xhbi[:, fc * 128:(fc + 1) * 128], Bh[:, col:col + 128],
                        start=False, stop=True))
                kb.group('pe', fns, reads=[xbi, xhbi, Bm, Bh], writes=[pui[hf]])
            kb.op('act', lambda e, u=u, bk=pui[0]: e.copy(out=u[:, 0:4, :], in_=bk[:].rearrange("p (c t) -> p c t", c=4)),
                  reads=[pui[0]], writes=[ua])
            kb.op('dve', lambda e, u=u, bk=pui[1]: e.tensor_copy(out=u[:, 4:8, :], in_=bk[:].rearrange("p (c t) -> p c t", c=4)),
                  reads=[pui[1]], writes=[ub])
            for bkidx in range(2):
                fns = []
                for gg in range(bkidx * 2, bkidx * 2 + 2):
                    for hh in range(2):
                        fns.append(lambda e, gg=gg, hh=hh, u=u, bk=pyi[bkidx]: e.matmul(
                            bk[:, (gg % 2) * 256:(gg % 2 + 1) * 256], u[:, gg * 2 + hh, :], Wb[:, gg, hh, :],
                            start=(hh == 0), stop=(hh == 1)))
                kb.group('pe', fns, reads=[ua if bkidx == 0 else ub, Wb], writes=[pyi[bkidx]])
            out_toks.append(emit_ln(kb, L, xi, xi, [pyi[0][:], pyi[1][:]], pyi, g, b, y[i * 128:(i + 1) * 128, :]))
        kb.wait_only('sp', out_toks)
        kb.emit()
    return nc


def build_qkv(NT=32):
    nc = bass.Bass("TRN2", target_bir_lowering=False)
    T = NT * 128
    x = nc.dram_tensor("x", [T, D], F32, kind="ExternalInput").ap()
    win = nc.dram_tensor("win", [D, 3 * D], F32, kind="ExternalInput").ap()
    cs = nc.dram_tensor("cs", [T, 64], F32, kind="ExternalInput").ap()
    qkv = nc.dram_tensor("qkv", [T, 3 * D], BF16, kind="ExternalOutput").ap()
    with ExitStack() as es:
        kb = KB(nc, es)
        ident = kb.sb([128, 128], F32)
        make_ident(kb, ident)
        wb = kb.sb([128, 8, 3 * D], BF16)
        wv = [kb.view() for _ in range(8)]
        for c in range(8):
            kb.dma('pool', wb[:, c, :], win[c * 128:(c + 1) * 128, :], wv[c], writes=[wv[c]])
        kb.op('pool', lambda e: e.tensor_scalar(out=wb[:, :, 0:D], in0=wb[:, :, 0:D], scalar1=0.125, scalar2=None, op0=ALU.mult),
              reads=[], writes=wv)
        xin = [kb.sb([128, D], F32) for _ in range(2)]
        cst = [kb.sb([128, 64], F32) for _ in range(2)]
        xT = [kb.sb([128, 8, 128], BF16) for _ in range(2)]
        kro = [kb.sb([128, 512], F32) for _ in range(2)]
        tm = [kb.sb([128, 8, 32], F32) for _ in range(4)]
        tp = [kb.sb([128, 8, 32], F32) for _ in range(4)]
        ost = [kb.sb([128, 3 * D], BF16) for _ in range(2)]
        ostv = [[kb.view() for _ in range(6)] for _ in range(2)]
        banks = [kb.ps([128, 512], F32) for _ in range(8)]
        nb = 0
        out_toks = []
        for i in range(NT):
            xi = xin[i % 2]
            ci = cst[i % 2]
            xt = xT[i % 2]
            oi = ost[i % 2]
            ov = ostv[i % 2]
            kr = kro[i % 2]
            kb.dma('sp', xi[:], x[i * 128:(i + 1) * 128, :], xi, writes=[xi])
            kb.dma('sp', ci[:], cs[i * 128:(i + 1) * 128, :], ci, writes=[ci])
            for hf in range(2):
                bk = banks[nb % 8]; nb += 1
                kb.group('pe', [lambda e, c=c, bk=bk, xi=xi: e.transpose(bk[:, (c % 4) * 128:(c % 4 + 1) * 128],
                                                                           xi[:, c * 128:(c + 1) * 128], ident[:])
                                for c in range(hf * 4, hf * 4 + 4)], reads=[xi, ident], writes=[bk])
                eng = 'act' if hf == 0 else 'dve'
                fn = (lambda e, bk=bk, hf=hf, xt=xt: e.copy(out=xt[:, hf * 4:(hf + 1) * 4, :], in_=bk[:].rearrange("p (c t) -> p c t", c=4))) \
                    if hf == 0 else (lambda e, bk=bk, hf=hf, xt=xt: e.tensor_copy(out=xt[:, hf * 4:(hf + 1) * 4, :], in_=bk[:].rearrange("p (c t) -> p c t", c=4)))
                if hf == 0:
                    t_a = kb.op(eng, fn, reads=[bk], writes=[xt])
                else:
                    t_b = kb.op(eng, fn, reads=[bk], deps=[t_a])
                    xt.w = [t_a, t_b]
            cosb = ci[:, 0:32].unsqueeze(1).broadcast_to([128, 8, 32])
            sinb = ci[:, 32:64].unsqueeze(1).broadcast_to([128, 8, 32])
            for blk in range(6):
                bk = banks[nb % 8]; nb += 1
                kb.group('pe', [lambda e, c=c, bk=bk, blk=blk, xt=xt: e.matmul(bk[:], xt[:, c, :], wb[:, c, blk * 512:(blk + 1) * 512],
                                                                               start=(c == 0), stop=(c == 7)) for c in range(8)],
                         reads=[xt] + wv, writes=[bk])
                osl = slice(blk * 512, (blk + 1) * 512)
                if blk in (0, 2):
                    if blk == 0:
                        eng, tt, src, sbuf = 'dve', tm, bk, bk
                    else:
                        kb.op('act', lambda e, bk=bk, kr=kr: e.copy(out=kr[:], in_=bk[:]), reads=[bk], writes=[kr])
                        eng, tt, src, sbuf = 'pool', tp, kr, kr
                    s4 = src[:].rearrange("p (h two j) -> p h two j", two=2, j=32)
                    o4 = oi[:, osl].rearrange("p (h two j) -> p h two j", two=2, j=32)
                    t1, t2 = s4[:, :, 0, :], s4[:, :, 1, :]
                    kb.op(eng, lambda e, t1=t1, tt=tt, cosb=cosb: e.tensor_tensor(out=tt[0][:], in0=t1, in1=cosb, op=ALU.mult), reads=[sbuf, ci], writes=[tt[0]])
                    kb.op(eng, lambda e, t2=t2, tt=tt, sinb=sinb: e.tensor_tensor(out=tt[1][:], in0=t2, in1=sinb, op=ALU.mult), reads=[sbuf, ci], writes=[tt[1]])
                    kb.op(eng, lambda e, t2=t2, tt=tt, cosb=cosb: e.tensor_tensor(out=tt[2][:], in0=t2, in1=cosb, op=ALU.mult), reads=[sbuf, ci], writes=[tt[2]])
                    kb.op(eng, lambda e, t1=t1, tt=tt, sinb=sinb: e.tensor_tensor(out=tt[3][:], in0=t1, in1=sinb, op=ALU.mult), reads=[sbuf, ci], writes=[tt[3]])
                    kb.op(eng, lambda e, tt=tt, o4=o4: e.tensor_tensor(out=o4[:, :, 0, :], in0=tt[0][:], in1=tt[1][:], op=ALU.subtract), reads=[tt[0], tt[1]], writes=[ov[blk]])
                    tk = kb.op(eng, lambda e, tt=tt, o4=o4: e.tensor_tensor(out=o4[:, :, 1, :], in0=tt[2][:], in1=tt[3][:], op=ALU.add), reads=[tt[2], tt[3]], deps=ov[blk].w)
                    ov[blk].w = ov[blk].w + [tk]
                else:
                    kb.op('act', lambda e, bk=bk, oi=oi, osl=osl: e.copy(out=oi[:, osl], in_=bk[:]), reads=[bk], writes=[ov[blk]])
            out_toks.append(kb.dma('sp', qkv[i * 128:(i + 1) * 128, :], oi[:], oi, reads=ov))
        kb.wait_only('sp', out_toks)
        kb.emit()
    return nc


def build_attn(NB=4):
    nc = bass.Bass("TRN2", target_bir_lowering=False)
    S = S_LEN
    qA, kA, vA, oA = [], [], [], []
    for di, d in enumerate(DILS):
        Ls = S // d
        nt = Ls // 128
        qA.append(nc.dram_tensor(f"qA{di}", [NB, 64, S], BF16, kind="ExternalInput").ap())
        kA.append(nc.dram_tensor(f"kA{di}", [NB, 64, d * (Ls + 128)], BF16, kind="ExternalInput").ap())
        vA.append(nc.dram_tensor(f"vA{di}", [NB, 128, d * (nt + 1) * 65], BF16, kind="ExternalInput").ap())
        oA.append(nc.dram_tensor(f"oA{di}", [NB, 128, 64 * 65], F32, kind="ExternalOutput").ap())
    qB = nc.dram_tensor("qB", [NB, 64, S], BF16, kind="ExternalInput").ap()
    kB = nc.dram_tensor("kB", [NB, 64, S], BF16, kind="ExternalInput").ap()
    vB = nc.dram_tensor("vB", [NB, 128, 64 * 65], BF16, kind="ExternalInput").ap()
    oB = nc.dram_tensor("oB", [NB, 128, 64 * 64], F32, kind="ExternalOutput").ap()
    ebias = nc.dram_tensor("ebias", [128, 25 * 128], F32, kind="ExternalInput").ap()
    maskd = nc.dram_tensor("maskd", [128, 256], F32, kind="ExternalInput").ap()
    with ExitStack() as es:
        kb = KB(nc, es)
        mask = kb.sb([128, 256], BF16)
        kb.dma('pool', mask[:], maskd[:, :], mask, writes=[mask])
        eb32 = kb.sb([128, 25 * 128], F32)
        E = kb.sb([128, 25 * 128], BF16)
        kb.dma('sp', eb32[:], ebias[:, :], eb32, writes=[eb32])
        kb.op('act', lambda e: e.activation(out=E[:], in_=eb32[:], func=AF.Exp), reads=[eb32], writes=[E])
        KMAX = 16 * (512 + 128)
        qT = [kb.sb([64, S], BF16) for _ in range(2)]
        kT = [kb.sb([64, KMAX], BF16) for _ in range(2)]
        V = [kb.sb([128, 80 * 65], BF16) for _ in range(2)]
        osb = [kb.sb([128, 64 * 65], F32) for _ in range(2)]
        pT = [kb.sb([128, 256], BF16) for _ in range(4)]
        rden = [kb.sb([128, 1], F32) for _ in range(2)]
        sbk = [kb.ps([128, 512], F32) for _ in range(3)]
        obk = [kb.ps([128, 512], F32) for _ in range(4)]
        stages = []
        for b in range(NB):
            for di in range(3):
                stages.append(('A', b, di))
            stages.append(('B', b, None))

        def load(si):
            kind, b, di = stages[si]
            sl = si % 2
            if kind == 'A':
                d = DILS[di]
                Ls = S // d
                nt = Ls // 128
                kb.dma('sp', qT[sl][:], qA[di][b], qT[sl], writes=[qT[sl]])
                kb.dma('sp', kT[sl][:, 0:d * (Ls + 128)], kA[di][b], kT[sl], writes=[kT[sl]])
                kb.dma('sp', V[sl][:, 0:d * (nt + 1) * 65], vA[di][b], V[sl], writes=[V[sl]])
            else:
                kb.dma('sp', qT[sl][:], qB[b], qT[sl], writes=[qT[sl]])
                kb.dma('sp', kT[sl][:, 0:S], kB[b], kT[sl], writes=[kT[sl]])
                kb.dma('sp', V[sl][:, 0:64 * 65], vB[b], V[sl], writes=[V[sl]])

        load(0)
        out_toks = []
        fronts, backs = [], []
        LOOK = 2

        def add_step(si, first, last, front_fn, back_fn):
            n = len(fronts)

            def front(n=n):
                front_fn(n)

            def back(n=n):
                if first and si + 1 < len(stages):
                    load(si + 1)
                back_fn(n)
                if last:
                    kind, b, di = stages[si]
                    ob = osb[si % 2]
                    if kind == 'A':
                        out_toks.append(kb.dma('sp', oA[di][b], ob[:], ob, reads=[ob]))
                    else:
                        out_toks.append(kb.dma('sp', oB[b], ob[:, 0:64 * 64], ob, reads=[ob]))
            fronts.append(front)
            backs.append(back)

        for si, (kind, b, di) in enumerate(stages):
            sl = si % 2
            q, k, v, ob = qT[sl], kT[sl], V[sl], osb[sl]
            if kind == 'A':
                d = DILS[di]
                Ls = S // d
                nt = Ls // 128
                for r in range(d):
                    for j in range(nt + 1):
                        q0 = 128 * (j - 1) if j >= 1 else 0
                        q1 = 128 * (j + 1) if j <= nt - 1 else 128 * nt
                        w = q1 - q0
                        moff = 128 if j == 0 else 0
                        koff = r * (Ls + 128) + 128 * j
                        vt = r * (nt + 1) + j
                        qa = r * Ls + q0

                        def front_fn(step, k=k, q=q, koff=koff, qa=qa, w=w, moff=moff):
                            sb_ = sbk[step % 3]
                            p = pT[step % 4]
                            kb.op('pe', lambda e: e.matmul(sb_[:, 0:w], k[:, koff:koff + 128], q[:, qa:qa + w], start=True, stop=True),
                                  reads=[k, q], writes=[sb_])
                            kb.op('act', lambda e: e.activation(out=p[:, 0:w], in_=sb_[:, 0:w], func=AF.Exp), reads=[sb_], writes=[p])
                            meng = 'dve' if step % 2 == 0 else 'pool'
                            kb.op(meng, lambda e: e.tensor_tensor(out=p[:, 0:w], in0=p[:, 0:w], in1=mask[:, moff:moff + w], op=ALU.mult),
                                  reads=[p, mask], writes=[p])

                        def back_fn(step, j=j, nt=nt, r=r, v=v, vt=vt, ob=ob):
                            p = pT[step % 4]
                            if j >= 1:
                                o_ = obk[(j - 1) % 4]
                                kb.op('pe', lambda e: e.matmul(o_[:, 0:65], p[:, 0:128], v[:, vt * 65:(vt + 1) * 65], start=False, stop=True),
                                      reads=[p, v], writes=[], deps=o_.w + list(o_.r.values()))
                                o_.w = [Tok(kb.sems['pe'], kb.cnt['pe'])]
                                blk = r * nt + (j - 1)
                                kb.op('dve', lambda e: e.tensor_copy(out=ob[:, blk * 65:(blk + 1) * 65], in_=o_[:, 0:65]),
                                      reads=[o_], writes=[], deps=list(ob.r.values()))
                                ob.w = [Tok(kb.sems['dve'], kb.cnt['dve'])]
                            if j <= nt - 1:
                                o2 = obk[j % 4]
                                pc = 128 if j >= 1 else 0
                                kb.op('pe', lambda e: e.matmul(o2[:, 0:65], p[:, pc:pc + 128], v[:, vt * 65:(vt + 1) * 65], start=True, stop=False),
                                      reads=[p, v], writes=[o2])
                        add_step(si, r == 0 and j == 0, r == d - 1 and j == nt, front_fn, back_fn)
            else:
                for m in range(64):
                    cls = {0: 0, 1: 1, 62: 3, 63: 4}.get(m, 2)
                    bt = min(max(m - 2, 0), 59)
                    for j in range(5):
                        tile = bt + j
                        ecol = (cls * 5 + j) * 128

                        def front_fn(step, k=k, q=q, tile=tile, m=m, ecol=ecol):
                            sb_ = sbk[step % 3]
                            p = pT[step % 4]
                            kb.op('pe', lambda e: e.matmul(sb_[:, 0:128], k[:, tile * 128:(tile + 1) * 128], q[:, m * 128:(m + 1) * 128],
                                                           start=True, stop=True), reads=[k, q], writes=[sb_])
                            kb.op('act', lambda e: e.activation(out=p[:, 0:128], in_=sb_[:, 0:128], func=AF.Exp), reads=[sb_], writes=[p])
                            meng = 'dve' if step % 2 == 0 else 'pool'
                            kb.op(meng, lambda e: e.tensor_tensor(out=p[:, 0:128], in0=p[:, 0:128], in1=E[:, ecol:ecol + 128], op=ALU.mult),
                                  reads=[p, E], writes=[p])

                        def back_fn(step, j=j, m=m, v=v, tile=tile, ob=ob):
                            p = pT[step % 4]
                            o_ = obk[m % 4]
                            if j == 0:
                                kb.op('pe', lambda e: e.matmul(o_[:, 0:65], p[:, 0:128], v[:, tile * 65:(tile + 1) * 65], start=True, stop=False),
                                      reads=[p, v], writes=[o_])
                            else:
                                kb.op('pe', lambda e: e.matmul(o_[:, 0:65], p[:, 0:128], v[:, tile * 65:(tile + 1) * 65], start=False, stop=(j == 4)),
                                      reads=[p, v], writes=[], deps=o_.w)
                                o_.w = [Tok(kb.sems['pe'], kb.cnt['pe'])]
                            if j == 4:
                                rd = rden[m % 2]
                                kb.op('dve', lambda e: e.reciprocal(out=rd[:], in_=o_[:, 64:65]), reads=[o_], writes=[rd])
                                kb.op('dve', lambda e: e.tensor_scalar(out=ob[:, m * 64:(m + 1) * 64], in0=o_[:, 0:64], scalar1=rd[:, 0:1],
                                                                       scalar2=None, op0=ALU.mult),
                                      reads=[o_, rd], writes=[], deps=list(ob.r.values()))
                                ob.w = [Tok(kb.sems['dve'], kb.cnt['dve'])]
                        add_step(si, m == 0 and j == 0, m == 63 and j == 4, front_fn, back_fn)
        nsteps = len(fronts)
        for n in range(min(LOOK, nsteps)):
            fronts[n]()
        for n in range(nsteps):
            if n + LOOK < nsteps:
                fronts[n + LOOK]()
            backs[n]()
        kb.wait_only('sp', out_toks)
        kb.emit()
    return nc


def build_oproj(NT=32):
    nc = bass.Bass("TRN2", target_bir_lowering=False)
    T = NT * 128
    x = nc.dram_tensor("x", [T, D], F32, kind="ExternalInput").ap()
    oa = nc.dram_tensor("oa", [3, T, 8 * 65], F32, kind="ExternalInput").ap()
    obd = nc.dram_tensor("ob", [T, 512], F32, kind="ExternalInput").ap()
    wout = nc.dram_tensor("wout", [D, D], F32, kind="ExternalInput").ap()
    lng = nc.dram_tensor("lng", [128, D], F32, kind="ExternalInput").ap()
    lnb = nc.dram_tensor("lnb", [128, D], F32, kind="ExternalInput").ap()
    y = nc.dram_tensor("y", [T, D], F32, kind="ExternalOutput").ap()
    with ExitStack() as es:
        kb = KB(nc, es)
        ident = kb.sb([128, 128], F32)
        make_ident(kb, ident)
        g = kb.sb([128, D], F32)
        b = kb.sb([128, D], F32)
        kb.dma('sp', g[:], lng[:, :], g, writes=[g])
        kb.dma('sp', b[:], lnb[:, :], b, writes=[b])
        L = LNBufs(kb, nbuf=2)
        wb = kb.sb([128, 8, D], BF16)
        kb.dma('pool', wb[:], wout.rearrange("(c p) f -> p c f", p=128), wb, writes=[wb])
        xin = [kb.sb([128, D], F32) for _ in range(3)]
        oat = [[kb.sb([128, 8, 65], F32) for _ in range(3)] for _ in range(2)]
        rd = [kb.sb([128, 8], F32) for _ in range(2)]
        ot = [kb.sb([128, D], F32) for _ in range(2)]
        ota = [kb.view() for _ in range(2)]
        otb = [kb.view() for _ in range(2)]
        oT = [kb.sb([128, 8, 128], BF16) for _ in range(2)]
        oTa = [kb.view() for _ in range(2)]
        oTb = [kb.view() for _ in range(2)]
        pt = [[kb.ps([128, 512], F32) for _ in range(2)] for _ in range(2)]
        py = [[kb.ps([128, 512], F32) for _ in range(2)] for _ in range(2)]
        out_toks = []
        for i in range(NT):
            xi = xin[i % 3]
            a3 = oat[i % 2]
            o = ot[i % 2]
            r_ = rd[i % 2]
            oTi = oT[i % 2]
            rows = slice(i * 128, (i + 1) * 128)
            kb.dma('sp', xi[:], x[rows, :], xi, writes=[xi])
            for br in range(3):
                kb.dma('sp', a3[br][:], oa[br, rows, :].rearrange("p (h c) -> p h c", c=65), a3[br], writes=[a3[br]])
            kb.dma('sp', o[:, 512:1024], obd[rows, :], otb[i % 2], writes=[otb[i % 2]])
            kb.op('dve', lambda e, a3=a3: e.tensor_tensor(out=a3[0][:], in0=a3[0][:], in1=a3[1][:], op=ALU.add), reads=[a3[1]], writes=[a3[0]])
            kb.op('dve', lambda e, a3=a3: e.tensor_tensor(out=a3[0][:], in0=a3[0][:], in1=a3[2][:], op=ALU.add), reads=[a3[2]], writes=[a3[0]])
            kb.op('dve', lambda e, a3=a3, r_=r_: e.reciprocal(out=r_[:], in_=a3[0][:, :, 64]), reads=[a3[0]], writes=[r_])
            kb.op('dve', lambda e, a3=a3, r_=r_, o=o: e.tensor_tensor(out=o[:, 0:512].rearrange("p (h c) -> p h c", c=64), in0=a3[0][:, :, 0:64],
                                                                   in1=r_[:].unsqueeze(2).broadcast_to([128, 8, 64]), op=ALU.mult),
                  reads=[a3[0], r_], writes=[ota[i % 2]])
            for hf in range(2):
                bk = pt[i % 2][hf]
                kb.group('pe', [lambda e, c=c, bk=bk, o=o: e.transpose(bk[:, (c % 4) * 128:(c % 4 + 1) * 128],
                                                                        o[:, c * 128:(c + 1) * 128], ident[:])
                                for c in range(hf * 4, hf * 4 + 4)], reads=[ota[i % 2] if hf == 0 else otb[i % 2], ident], writes=[bk])
                if hf == 0:
                    kb.op('act', lambda e, bk=bk, oTi=oTi: e.copy(out=oTi[:, 0:4, :], in_=bk[:].rearrange("p (c t) -> p c t", c=4)),
                          reads=[bk], writes=[oTa[i % 2]])
                else:
                    kb.op('dve', lambda e, bk=bk, oTi=oTi: e.tensor_copy(out=oTi[:, 4:8, :], in_=bk[:].rearrange("p (c t) -> p c t", c=4)),
                          reads=[bk], writes=[oTb[i % 2]])
            for oh in range(2):
                bk = py[i % 2][oh]
                kb.group('pe', [lambda e, c=c, bk=bk, oTi=oTi, oh=oh: e.matmul(bk[:], oTi[:, c, :], wb[:, c, oh * 512:(oh + 1) * 512],
                                                                               start=(c == 0), stop=(c == 7)) for c in range(8)],
                         reads=[oTa[i % 2], oTb[i % 2], wb], writes=[bk])
            out_toks.append(emit_ln(kb, L, xi, xi, [py[i % 2][0][:], py[i % 2][1][:]], py[i % 2], g, b, y[rows, :]))
        kb.wait_only('sp', out_toks)
        kb.emit()
    return nc

import ml_dtypes
BF = ml_dtypes.bfloat16
S_LEN = 8192
DILS = (1, 4, 16)


def rope_cs(pos):
    pos = pos.astype(np.float32)
    inv = (np.float32(10000.0) ** (-np.arange(0, 64, 2, dtype=np.float32) / np.float32(64))).astype(np.float32)
    ang = (pos[:, None] * inv[None, :]).astype(np.float32)
    return np.concatenate([np.cos(ang), np.sin(ang)], 1).astype(np.float32)


def attn_mask():
    p = np.arange(128)[:, None]
    a = np.arange(128)[None, :]
    return np.concatenate([(p <= a), (p >= a)], 1).astype(np.float32)


def na_bias_table(rpb_h):
    out = np.full((128, 25, 128), -30000.0, np.float32)
    kk = np.arange(128)
    qq = np.arange(128)
    for cls, m in enumerate((0, 1, 2, 62, 63)):
        base = min(max(2 * m - 4, 0), 118)
        qrow = 2 * m + qq // 64
        qcol = qq % 64
        rstart = np.clip(qrow - 4, 0, 120)
        cstart = np.clip(qcol - 8, 0, 48)
        for j in range(5):
            krow = base + 2 * j + kk // 64
            kcol = kk % 64
            ok = ((krow[:, None] >= rstart[None, :]) & (krow[:, None] < rstart[None, :] + 8)
                  & (kcol[:, None] >= cstart[None, :]) & (kcol[:, None] < cstart[None, :] + 16))
            roff = np.clip(krow[:, None] - qrow[None, :] + 7, 0, 14)
            coff = np.clip(kcol[:, None] - qcol[None, :], -15, 15) + 15
            vals = rpb_h[roff, coff]
            out[:, cls * 5 + j, :] = np.where(ok, vals, np.float32(-30000.0))
    return out.reshape(128, 25 * 128)


def attn_core_inputs(qkv, c, rpb_i):
    B, S = qkv.shape[0], qkv.shape[1]
    ha, hb = c, 8 + c
    im = {}
    q_h, k_h, v_h = qkv[:, :, 0, ha], qkv[:, :, 1, ha], qkv[:, :, 2, ha]
    one = np.ones((), BF)
    for di, d in enumerate(DILS):
        Ls = S // d
        nt = Ls // 128
        im[f"qA{di}"] = np.ascontiguousarray(q_h.reshape(B, Ls, d, 64).transpose(0, 3, 2, 1)).reshape(B, 64, d * Ls)
        kk = np.zeros((B, 64, d, Ls + 128), BF)
        kk[:, :, :, 64:64 + Ls] = k_h.reshape(B, Ls, d, 64).transpose(0, 3, 2, 1)
        im[f"kA{di}"] = kk.reshape(B, 64, d * (Ls + 128))
        vv = np.zeros((B, d, Ls + 128, 65), BF)
        vv[:, :, 64:64 + Ls, :64] = v_h.reshape(B, Ls, d, 64).transpose(0, 2, 1, 3)
        vv[:, :, 64:64 + Ls, 64] = one
        im[f"vA{di}"] = np.ascontiguousarray(vv.reshape(B, d, nt + 1, 128, 65).transpose(0, 3, 1, 2, 4)).reshape(B, 128, d * (nt + 1) * 65)
    im["qB"] = np.ascontiguousarray(qkv[:, :, 0, hb].transpose(0, 2, 1))
    im["kB"] = np.ascontiguousarray(qkv[:, :, 1, hb].transpose(0, 2, 1))
    vb = np.zeros((B, S, 65), BF)
    vb[:, :, :64] = qkv[:, :, 2, hb]
    vb[:, :, 64] = one
    im["vB"] = np.ascontiguousarray(vb.reshape(B, 64, 128, 65).transpose(0, 2, 1, 3)).reshape(B, 128, 64 * 65)
    im["ebias"] = na_bias_table(rpb_i[c])
    im["maskd"] = attn_mask()
    return im


def attn_core_outputs(results, B, S):
    oa = np.zeros((3, B, S, 8, 65), np.float32)
    ob = np.zeros((B, S, 8, 64), np.float32)
    for c, r in enumerate(results):
        for di, d in enumerate(DILS):
            Ls = S // d
            nt = Ls // 128
            a = r[f"oA{di}"].reshape(B, 128, d, nt, 65).transpose(0, 3, 1, 2, 4).reshape(B, S, 65)
            oa[di, :, :, c, :] = a
        ob[:, :, c, :] = r["oB"].reshape(B, 128, 64, 64).transpose(0, 2, 1, 3).reshape(B, S, 64)
    return oa.reshape(3, B, S, 8 * 65), ob.reshape(B, S, 512)


N_CORES = 8
_PROGS = {}


def _prog(name, fn):
    if name not in _PROGS:
        _PROGS[name] = fn()
    return _PROGS[name]


def _run(nc, in_maps):
    return run_bass_kernel_spmd(nc, in_maps, core_ids=list(range(N_CORES))).results


def _rep(v):
    return np.ascontiguousarray(np.broadcast_to(np.asarray(v, np.float32)[None], (128, 1024)))


def _attn_layer(xf, w_in, w_out, rpb_i, g, b, B, S):
    T = xf.shape[0] // N_CORES
    NT = T // 128
    halves = S // T
    nc1 = _prog('qkv', lambda: build_qkv(NT=NT))
    ims = []
    for c in range(N_CORES):
        p0 = (c % halves) * T
        ims.append({"x": xf[c * T:(c + 1) * T], "win": w_in, "cs": rope_cs(np.arange(p0, p0 + T))})
    r1 = _run(nc1, ims)
    qkv = np.concatenate([r["qkv"] for r in r1], 0).reshape(B, S, 3, 16, 64)
    nc2 = _prog('attn', lambda: build_attn(NB=B))
    r2 = _run(nc2, [attn_core_inputs(qkv, c, rpb_i) for c in range(N_CORES)])
    oa, ob = attn_core_outputs(r2, B, S)
    oa = oa.reshape(3, B * S, 8 * 65)
    ob = ob.reshape(B * S, 512)
    nc3 = _prog('oproj', lambda: build_oproj(NT=NT))
    ims = [{"x": xf[c * T:(c + 1) * T], "oa": np.ascontiguousarray(oa[:, c * T:(c + 1) * T]),
            "ob": np.ascontiguousarray(ob[c * T:(c + 1) * T]), "wout": w_out, "lng": _rep(g), "lnb": _rep(b)}
           for c in range(N_CORES)]
    r3 = _run(nc3, ims)
    return np.concatenate([r["y"] for r in r3], 0)


def _pool_layer(xf, pw, psc, g, b, B, S):
    T = xf.shape[0] // N_CORES
    NT = T // 128
    halves = S // T
    ncp = _prog('pool', lambda: build_pool(NT=NT))
    ims = []
    for c in range(N_CORES):
        h = c % halves
        xp = np.zeros((T + 16, 1024), np.float32)
        lo = c * T - (8 if h > 0 else 0)
        hi = (c + 1) * T + (8 if h < halves - 1 else 0)
        xp[8 - (c * T - lo): 8 + T + (hi - (c + 1) * T)] = xf[lo:hi]
        Bm, Bh = pool_tables(h == 0, h == halves - 1)
        ims.append({"xp": xp, "bm": np.ascontiguousarray(Bm.transpose(2, 0, 1, 3).reshape(128, 12 * 128)),
                    "bh": np.ascontiguousarray(Bh.transpose(2, 0, 1, 3).reshape(16, 12 * 128)),
                    "pw": pw, "psc": _rep(psc), "lng": _rep(g), "lnb": _rep(b)})
    r = _run(ncp, ims)
    return np.concatenate([q["y"] for q in r], 0)


MOE_CAP = 768


def _moe_layer(xf, rw, rbias, w1, w3, w2, g, b):
    T = xf.shape[0] // N_CORES
    NT = T // 128
    ncm = _prog('moe', lambda: build_moe_sparse(NT=NT, C=MOE_CAP, NE=16))
    rb = np.ascontiguousarray(np.broadcast_to(np.tile(np.asarray(rbias, np.float32), NT)[None], (128, NT * 16)))
    eoff = np.ascontiguousarray(np.broadcast_to(np.tile(np.arange(16, dtype=np.float32) * MOE_CAP, NT)[None], (128, NT * 16)))
    ims = [{"x": xf[c * T:(c + 1) * T], "rw": rw, "rbias": rb, "eoff": eoff, "w1": w1, "w3": w3, "w2": w2,
            "lng": _rep(g), "lnb": _rep(b)} for c in range(N_CORES)]
    r = _run(ncm, ims)
    return np.concatenate([q["y"] for q in r], 0)


def kernel(x, w_in, w_out, rpb, pool_w, pool_scale, router_w, router_bias, moe_w1, moe_w3, moe_w2, ln_g, ln_b):
    f = lambda a: np.ascontiguousarray(np.asarray(a, np.float32))
    x = f(x)
    B, S, Dm = x.shape
    xf = x.reshape(B * S, Dm)
    depth = moe_w1.shape[0]
    for layer in range(depth):
        i = layer // 2
        if layer % 2 == 0:
            xf = _attn_layer(xf, f(w_in[i]), f(w_out[i]), f(rpb[i]), ln_g[layer, 0], ln_b[layer, 0], B, S)
        else:
            xf = _pool_layer(xf, f(pool_w[i]), pool_scale[i], ln_g[layer, 0], ln_b[layer, 0], B, S)
        xf = _moe_layer(xf, f(router_w), router_bias, f(moe_w1[layer]), f(moe_w3[layer]), f(moe_w2[layer]),
                        ln_g[layer, 1], ln_b[layer, 1])
    return xf.reshape(B, S, Dm).astype(np.float32)
```

```python
import numpy as np
import concourse.bass as bass
import concourse.mybir as mybir
from concourse.bass_utils import run_bass_kernel_spmd
from contextlib import ExitStack

F32 = mybir.dt.float32
BF16 = mybir.dt.bfloat16
U32 = mybir.dt.uint32
I32 = mybir.dt.int32
AF = mybir.ActivationFunctionType
ALU = mybir.AluOpType
AX = mybir.AxisListType

ENGS = ['pe', 'act', 'dve', 'pool', 'sp']
ENG_ATTR = {'pe': 'tensor', 'act': 'scalar', 'dve': 'vector', 'pool': 'gpsimd', 'sp': 'sync'}


class Tok:
    __slots__ = ('sem', 'val')

    def __init__(self, sem, val):
        self.sem = sem
        self.val = val


class Buf:
    def __init__(self, kb, t, name):
        self.kb = kb
        self.t = t
        self.name = name
        self.w = []
        self.r = {}
        self._slot = None

    def __getitem__(self, k):
        return self.t[k]

    def slot(self):
        if self._slot is None:
            self._slot = self.kb.new_sem()
            self._slotval = 0
        return self._slot


class KB:
    def __init__(self, nc, es, same_eng_sync=True):
        self.nc = nc
        self.es = es
        self.ops = {e: [] for e in ENGS}
        self.nsem = 0
        self.sems = {e: self.new_sem() for e in ENGS}
        self.cnt = {e: 0 for e in ENGS}
        self.waited = {e: {} for e in ENGS}
        self.same_eng_sync = same_eng_sync
        self.nbuf = 0

    def new_sem(self):
        self.nsem += 1
        return self.es.enter_context(self.nc.semaphore(f"sem{self.nsem}"))

    def sb(self, shape, dtype, name=None):
        self.nbuf += 1
        name = name or f"sb{self.nbuf}"
        return Buf(self, self.es.enter_context(self.nc.sbuf_tensor(name, list(shape), dtype)), name)

    def ps(self, shape, dtype=F32, name=None):
        self.nbuf += 1
        name = name or f"ps{self.nbuf}"
        return Buf(self, self.es.enter_context(self.nc.psum_tensor(name, list(shape), dtype)), name)

    def view(self, name="v"):
        return Buf(self, None, name)

    def _waits(self, eng, deps):
        waits = []
        for d in deps:
            if d is None:
                continue
            if isinstance(d, (list, tuple)):
                waits += self._waits(eng, d)
                continue
            if (not self.same_eng_sync) and d.sem is self.sems[eng]:
                continue
            key = id(d.sem)
            if self.waited[eng].get(key, 0) >= d.val:
                continue
            self.waited[eng][key] = d.val
            waits.append((d.sem, d.val))
        return waits

    def _deps(self, reads, writes, deps):
        ds = list(deps)
        for b in reads:
            ds += b.w
        for b in writes:
            ds += b.w
            ds += list(b.r.values())
        return ds

    def _commit(self, tok, reads, writes):
        for b in writes:
            b.w = [tok]
            b.r = {}
        for b in reads:
            if b not in writes:
                o = b.r.get(id(tok.sem))
                if o is None or o.val < tok.val:
                    b.r[id(tok.sem)] = tok

    def op(self, eng, fn, reads=(), writes=(), deps=()):
        waits = self._waits(eng, self._deps(reads, writes, deps))
        self.cnt[eng] += 1
        self.ops[eng].append((waits, fn, (self.sems[eng], 1)))
        tok = Tok(self.sems[eng], self.cnt[eng])
        self._commit(tok, reads, writes)
        return tok

    def group(self, eng, fns, reads=(), writes=(), deps=()):
        waits = self._waits(eng, self._deps(reads, writes, deps))
        for i, fn in enumerate(fns):
            self.cnt[eng] += 1
            self.ops[eng].append((waits if i == 0 else [], fn, (self.sems[eng], 1)))
        tok = Tok(self.sems[eng], self.cnt[eng])
        self._commit(tok, reads, writes)
        return tok

    def dma(self, eng, out, in_, slotbuf, reads=(), writes=(), deps=(), **kw):
        sem = slotbuf.slot()
        ds = list(deps)
        for b in reads:
            ds += b.w
        for b in writes:
            ds += [t for t in b.w if t.sem is not sem]
            ds += list(b.r.values())
        waits = self._waits(eng, ds)
        slotbuf._slotval += 16
        self.ops[eng].append((waits, lambda e: e.dma_start(out=out, in_=in_, **kw), (sem, 16)))
        tok = Tok(sem, slotbuf._slotval)
        self._commit(tok, reads, writes)
        return tok

    def wait_only(self, eng, deps):
        waits = self._waits(eng, deps)
        if waits:
            self.ops[eng].append((waits, None, None))

    def emit(self):
        with self.nc.Block() as block:
            for e in ENGS:
                dec = getattr(block, ENG_ATTR[e])
                ops = self.ops[e]

                def body(eng, ops=ops):
                    for waits, fn, inc in ops:
                        for s, v in waits:
                            eng.wait_ge(s, v)
                        if fn is None:
                            continue
                        inst = fn(eng)
                        if inc is not None:
                            inst.then_inc(inc[0], inc[1])
                dec(body)


def _idma(self, out, in_, idx_ap, scatter, slotbuf, bound, reads=(), writes=(), deps=()):
    sem = slotbuf.slot()
    ds = list(deps)
    for b in reads:
        ds += b.w
    for b in writes:
        ds += [t for t in b.w if t.sem is not sem]
        ds += list(b.r.values())
    waits = self._waits('pool', ds)
    slotbuf._slotval += 16
    if not hasattr(self, '_bregs'):
        self._bregs = {}

    def breg(e):
        if bound not in self._bregs:
            self._bregs[bound] = e.to_reg(bound)
        return self._bregs[bound]
    if scatter:
        fn = lambda e: e.indirect_dma_start(out=out, out_offset=bass.IndirectOffsetOnAxis(ap=idx_ap, axis=0), in_=in_,
                                            in_offset=None, bounds_check=breg(e), oob_is_err=False)
    else:
        fn = lambda e: e.indirect_dma_start(out=out, out_offset=None, in_=in_,
                                            in_offset=bass.IndirectOffsetOnAxis(ap=idx_ap, axis=0), bounds_check=breg(e),
                                            oob_is_err=False)
    self.ops['pool'].append((waits, fn, (sem, 16)))
    tok = Tok(sem, slotbuf._slotval)
    self._commit(tok, reads, writes)
    return tok


KB.idma = _idma


D = 1024
ALPHA = 8.0 ** 0.25
LN_EPS = 1e-5


def make_ident(kb, ident):
    kb.op('pool', lambda e: e.memset(ident[:], 0.0), writes=[ident])
    kb.op('pool', lambda e: e.affine_select(out=ident[:], in_=ident[:], compare_op=ALU.not_equal, fill=1.0,
                                            base=0, pattern=[[-1, 128]], channel_multiplier=1), writes=[ident])


class LNBufs:
    def __init__(self, kb, nbuf=2):
        self.kb = kb
        self.n = nbuf
        self.i = 0
        self.z = [kb.sb([128, D], F32) for _ in range(nbuf)]
        self.o = [kb.sb([128, D], F32) for _ in range(nbuf)]
        self.st = [kb.sb([128, 12], F32) for _ in range(nbuf)]
        self.mv = [kb.sb([128, 2], F32) for _ in range(nbuf)]
        self.rs = [kb.sb([128, 1], F32) for _ in range(nbuf)]
        self.nb = [kb.sb([128, 1], F32) for _ in range(nbuf)]
        self.eps = kb.sb([128, 1], F32)
        kb.op('pool', lambda e: e.memset(self.eps[:], LN_EPS), writes=[self.eps])


def emit_ln(kb, L, xbuf, x_ap, h_aps, h_bufs, g, b, out_dram_ap, dma_eng='sp', b_eng='pool'):
    i = L.i
    L.i = (L.i + 1) % L.n
    z, o, st, mv, rs, nb = L.z[i], L.o[i], L.st[i], L.mv[i], L.rs[i], L.nb[i]
    for hf in range(2):
        sl = slice(hf * 512, (hf + 1) * 512)
        kb.op('dve', lambda e, sl=sl, hf=hf: e.scalar_tensor_tensor(out=z[:, sl], in0=x_ap[:, sl], scalar=ALPHA,
                                                                    in1=h_aps[hf], op0=ALU.mult, op1=ALU.add),
              reads=[xbuf, h_bufs[hf]], writes=[z] if hf == 0 else [], deps=z.w if hf == 1 else ())
    z.w = [Tok(kb.sems['dve'], kb.cnt['dve'])]
    for hf in range(2):
        sl = slice(hf * 512, (hf + 1) * 512)
        kb.op('dve', lambda e, sl=sl, hf=hf: e.bn_stats(out=st[:, hf * 6:(hf + 1) * 6], in_=z[:, sl]),
              reads=[z], writes=[st] if hf == 0 else [], deps=st.w if hf == 1 else ())
    st.w = [Tok(kb.sems['dve'], kb.cnt['dve'])]
    kb.op('dve', lambda e: e.bn_aggr(out=mv[:], in_=st[:]), reads=[st], writes=[mv])
    kb.op('act', lambda e: e.activation(out=rs[:], in_=mv[:, 1:2], func=AF.Sqrt, bias=L.eps[:], scale=1.0),
          reads=[mv, L.eps], writes=[rs])
    kb.op('dve', lambda e: e.reciprocal(out=rs[:], in_=rs[:]), reads=[rs], writes=[rs])
    kb.op('dve', lambda e: e.scalar_tensor_tensor(out=nb[:], in0=mv[:, 0:1], scalar=-1.0, in1=rs[:],
                                                  op0=ALU.mult, op1=ALU.mult), reads=[mv, rs], writes=[nb])
    kb.op('act', lambda e: e.activation(out=o[:], in_=z[:], func=AF.Identity, bias=nb[:], scale=rs[:]),
          reads=[z, rs, nb], writes=[o])
    kb.op('pool', lambda e: e.tensor_tensor(out=o[:], in0=o[:], in1=g[:], op=ALU.mult), reads=[o, g], writes=[o])
    kb.op(b_eng, lambda e: e.tensor_tensor(out=o[:], in0=o[:], in1=b[:], op=ALU.add), reads=[o, b], writes=[o])
    return kb.dma(dma_eng, out_dram_ap, o[:], o, reads=[o])


def build_moe(NT=32, GT=8, NE=16, debug=False):
    nc = bass.Bass("TRN2", target_bir_lowering=False)
    T = NT * 128
    x = nc.dram_tensor("x", [T, D], F32, kind="ExternalInput").ap()
    rw = nc.dram_tensor("rw", [D, 16], F32, kind="ExternalInput").ap()
    rbias = nc.dram_tensor("rbias", [128, GT * 16], F32, kind="ExternalInput").ap()
    w1 = nc.dram_tensor("w1", [16, D, D], F32, kind="ExternalInput").ap()
    w3 = nc.dram_tensor("w3", [16, D, D], F32, kind="ExternalInput").ap()
    w2 = nc.dram_tensor("w2", [16, D, D], F32, kind="ExternalInput").ap()
    lng = nc.dram_tensor("lng", [128, D], F32, kind="ExternalInput").ap()
    lnb = nc.dram_tensor("lnb", [128, D], F32, kind="ExternalInput").ap()
    y = nc.dram_tensor("y", [T, D], F32, kind="ExternalOutput").ap()
    if debug:
        gdbg = nc.dram_tensor("gdbg", [NT // GT, 128, GT * 16], F32, kind="ExternalOutput").ap()
    with ExitStack() as es:
        kb = KB(nc, es)
        ident = kb.sb([128, 128], F32)
        make_ident(kb, ident)
        rws = kb.sb([128, 8, 16], F32)
        kb.dma('sp', rws[:], rw.rearrange("(c p) e -> p c e", p=128), rws, writes=[rws])
        rb = kb.sb([128, GT * 16], F32)
        kb.dma('sp', rb[:], rbias[:, :], rb, writes=[rb])
        g = kb.sb([128, D], F32)
        b = kb.sb([128, D], F32)
        kb.dma('sp', g[:], lng[:, :], g, writes=[g])
        kb.dma('sp', b[:], lnb[:, :], b, writes=[b])
        L = LNBufs(kb, nbuf=1)

        xin = [kb.sb([128, D], F32) for _ in range(2)]
        xT32 = [kb.sb([128, 8, 128], F32) for _ in range(2)]
        xT = kb.sb([128, 8, GT * 128], BF16)
        xTv = [kb.view(f"xTv{t}") for t in range(GT)]
        acc = kb.sb([128, GT, D], F32)
        accv = [[kb.view() for _ in range(2)] for t in range(GT)]
        wb = [[kb.sb([128, 8, D], BF16) for _ in range(3)] for _ in range(2)]
        hT = [kb.sb([128, 8, 512], BF16) for _ in range(1)]
        hTv = [[kb.view() for _ in range(8)] for _ in range(1)]
        sil = [kb.sb([128, 512], BF16) for _ in range(2)]
        banks = [kb.ps([128, 512], F32) for _ in range(8)]
        NG = GT * 16
        aff = kb.sb([128, NG], F32)
        affv = [kb.view() for _ in range(GT)]
        S8 = kb.sb([128, GT * 4, 8], F32)
        Pt = kb.sb([128, GT * 4, 6], F32)
        gs = kb.sb([128, GT * 4], F32)
        gmax = kb.sb([128, GT], F32)
        ghot = kb.sb([128, GT * 4], F32)
        c1 = kb.sb([128, GT * 4, 4], F32)
        c2 = kb.sb([128, GT * 4, 4], F32)
        GA = kb.sb([128, GT, 16], F32)
        den = kb.sb([128, GT], F32)
        G = kb.sb([128, GT, 16], F32)

        wsrc = [w1, w3, w2]
        nsteps = (NT // GT) * NE
        step_list = [(gi, e) for gi in range(NT // GT) for e in range(NE)]

        def load_weights(si):
            gi, e = step_list[si]
            slot = si % 2
            for j in range(3):
                kb.dma('pool', wb[slot][j][:], wsrc[j][e].rearrange("(c p) f -> p c f", p=128), wb[slot][j],
                       writes=[wb[slot][j]])

        if nsteps > 0:
            load_weights(0)
        ybank = 0
        out_toks = []
        for gi in range(NT // GT):
            for t in range(GT):
                tile = gi * GT + t
                xi = xin[t % 2]
                x32 = xT32[t % 2]
                kb.dma('sp', xi[:], x[tile * 128:(tile + 1) * 128, :], xi, writes=[xi])
                for hf in range(2):
                    bk = banks[4 + hf]
                    kb.group('pe', [lambda e, c=c, bk=bk, xi=xi: e.transpose(bk[:, (c % 4) * 128:(c % 4 + 1) * 128],
                                                                               xi[:, c * 128:(c + 1) * 128], ident[:])
                                    for c in range(hf * 4, hf * 4 + 4)], reads=[xi, ident], writes=[bk])
                    if True:
                        kb.op('act', lambda e, bk=bk, hf=hf, x32=x32: e.copy(
                            out=x32[:, hf * 4:(hf + 1) * 4, :], in_=bk[:].rearrange("p (c t) -> p c t", c=4)),
                            reads=[bk], writes=[x32] if hf == 0 else [], deps=x32.w if hf == 1 else ())
                    kb.op('dve', lambda e, hf=hf, t=t, x32=x32: e.tensor_copy(
                        out=xT[:, hf * 4:(hf + 1) * 4, t * 128:(t + 1) * 128], in_=x32[:, hf * 4:(hf + 1) * 4, :]),
                        reads=[], writes=[xTv[t]] if hf == 0 else [], deps=list(xTv[t].w if hf == 1 else ()) + [Tok(kb.sems['act'], kb.cnt['act'])])
                x32.w = [Tok(kb.sems['act'], kb.cnt['act'])]
                xTv[t].w = [Tok(kb.sems['dve'], kb.cnt['dve'])]
                x32.r[id(kb.sems['dve'])] = Tok(kb.sems['dve'], kb.cnt['dve'])
                rbk = banks[6]
                kb.group('pe', [lambda e, c=c, x32=x32, rbk=rbk: e.matmul(rbk[:, 0:16], x32[:, c, :], rws[:, c, :],
                                                                         start=(c == 0), stop=(c == 7))
                                for c in range(8)], reads=[x32, rws], writes=[rbk])
                kb.op('act', lambda e, t=t, rbk=rbk: e.activation(out=aff[:, t * 16:(t + 1) * 16], in_=rbk[:, 0:16],
                                                                   func=AF.Sigmoid), reads=[rbk], writes=[affv[t]])
            S4 = S8[:, :, 0:4]
            aff4 = aff[:].rearrange("p (g j) -> p g j", j=4)
            kb.op('dve', lambda e: e.tensor_tensor(out=S8[:, :, 0:4], in0=aff4, in1=rb[:].rearrange("p (g j) -> p g j", j=4),
                                                   op=ALU.add), reads=affv + [rb], writes=[S8])
            kb.op('dve', lambda e: e.tensor_copy(out=S8[:, :, 4:8], in_=S8[:, :, 0:4]), reads=[S8], writes=[S8])
            pairs = [(0, 1), (0, 2), (0, 3), (1, 2), (1, 3), (2, 3)]
            for i, (a, bb) in enumerate(pairs):
                kb.op('dve', lambda e, i=i, a=a, bb=bb: e.tensor_tensor(out=Pt[:, :, i], in0=S8[:, :, a], in1=S8[:, :, bb],
                                                                         op=ALU.add), reads=[S8], writes=[Pt])
            kb.op('dve', lambda e: e.tensor_reduce(out=gs[:], in_=Pt[:], axis=AX.X, op=ALU.max), reads=[Pt], writes=[gs])
            kb.op('dve', lambda e: e.tensor_reduce(out=gmax[:], in_=gs[:].rearrange("p (t g) -> p t g", g=4), axis=AX.X,
                                                   op=ALU.max), reads=[gs], writes=[gmax])
            kb.op('dve', lambda e: e.tensor_tensor(out=ghot[:].rearrange("p (t g) -> p t g", g=4),
                                                   in0=gs[:].rearrange("p (t g) -> p t g", g=4),
                                                   in1=gmax[:].unsqueeze(2).broadcast_to([128, GT, 4]), op=ALU.is_ge),
                  reads=[gs, gmax], writes=[ghot])
            kb.op('dve', lambda e: e.tensor_tensor(out=c1[:], in0=S8[:, :, 0:4], in1=S8[:, :, 1:5], op=ALU.is_gt),
                  reads=[S8], writes=[c1])
            kb.op('dve', lambda e: e.tensor_tensor(out=c2[:], in0=S8[:, :, 0:4], in1=S8[:, :, 2:6], op=ALU.is_gt),
                  reads=[S8], writes=[c2])
            kb.op('dve', lambda e: e.tensor_tensor(out=c1[:], in0=c1[:], in1=c2[:], op=ALU.add), reads=[c1, c2], writes=[c1])
            kb.op('dve', lambda e: e.tensor_tensor(out=c2[:], in0=S8[:, :, 0:4], in1=S8[:, :, 3:7], op=ALU.is_gt),
                  reads=[S8], writes=[c2])
            kb.op('dve', lambda e: e.tensor_tensor(out=c1[:], in0=c1[:], in1=c2[:], op=ALU.add), reads=[c1, c2], writes=[c1])
            kb.op('dve', lambda e: e.scalar_tensor_tensor(out=c1[:], in0=c1[:], scalar=2.0,
                                                          in1=ghot[:].unsqueeze(2).broadcast_to([128, GT * 4, 4]),
                                                          op0=ALU.is_ge, op1=ALU.mult), reads=[c1, ghot], writes=[c1])
            kb.op('dve', lambda e: e.tensor_tensor(out=GA[:].rearrange("p t (g j) -> p (t g) j", j=4), in0=c1[:], in1=aff4,
                                                   op=ALU.mult), reads=[c1] + affv, writes=[GA])
            kb.op('dve', lambda e: e.tensor_reduce(out=den[:], in_=GA[:], axis=AX.X, op=ALU.add), reads=[GA], writes=[den])
            kb.op('dve', lambda e: e.reciprocal(out=den[:], in_=den[:]), reads=[den], writes=[den])
            kb.op('dve', lambda e: e.tensor_tensor(out=G[:], in0=GA[:], in1=den[:].unsqueeze(2).broadcast_to([128, GT, 16]),
                                                   op=ALU.mult), reads=[GA, den], writes=[G])
            if debug:
                out_toks.append(kb.dma('sp', gdbg[gi], G[:].rearrange("p t e -> p (t e)"), G, reads=[G]))
            for e_i in range(NE):
                si = gi * NE + e_i
                slot = si % 2
                if si + 1 < nsteps:
                    load_weights(si + 1)
                w1b, w3b, w2b = wb[slot]
                for tt in range(GT // 4):
                    hb = hT[0]
                    hv = hTv[0]
                    tsl = slice(tt * 512, (tt + 1) * 512)
                    for fc in range(8):
                        b1 = banks[fc % 2]
                        b3 = banks[2 + fc % 2]
                        sl_ = sil[fc % 2]
                        fsl = slice(fc * 128, (fc + 1) * 128)
                        kb.group('pe', [lambda e, c=c, b1=b1, fsl=fsl, w1b=w1b, tsl=tsl: e.matmul(b1[:], w1b[:, c, fsl], xT[:, c, tsl],
                                                                                 start=(c == 0), stop=(c == 7))
                                        for c in range(8)], reads=[w1b] + xTv[tt * 4:(tt + 1) * 4], writes=[b1])
                        kb.group('pe', [lambda e, c=c, b3=b3, fsl=fsl, w3b=w3b, tsl=tsl: e.matmul(b3[:], w3b[:, c, fsl], xT[:, c, tsl],
                                                                                 start=(c == 0), stop=(c == 7))
                                        for c in range(8)], reads=[w3b] + xTv[tt * 4:(tt + 1) * 4], writes=[b3])
                        kb.op('act', lambda e, b1=b1, sl_=sl_: e.activation(out=sl_[:], in_=b1[:], func=AF.Silu),
                              reads=[b1], writes=[sl_])
                        kb.op('dve', lambda e, b3=b3, sl_=sl_, fc=fc, hb=hb: e.tensor_tensor(out=hb[:, fc, :], in0=b3[:],
                                                                                          in1=sl_[:], op=ALU.mult),
                              reads=[b3, sl_], writes=[hv[fc]])
                    for t4 in range(4):
                        t = tt * 4 + t4
                        for oh in range(2):
                            yb = banks[4 + ybank % 4]
                            ybank += 1
                            osl = slice(oh * 512, (oh + 1) * 512)
                            kb.group('pe', [lambda e, fc=fc, yb=yb, t4=t4, osl=osl, hb=hb, w2b=w2b: e.matmul(
                                yb[:], hb[:, fc, t4 * 128:(t4 + 1) * 128], w2b[:, fc, osl], start=(fc == 0), stop=(fc == 7))
                                for fc in range(8)], reads=[w2b] + hv, writes=[yb])
                            av = accv[t][oh]
                            if e_i == 0:
                                kb.op('dve', lambda e, yb=yb, t=t, osl=osl, e_i=e_i: e.tensor_scalar(
                                    out=acc[:, t, osl], in0=yb[:], scalar1=G[:, t, e_i:e_i + 1], scalar2=None, op0=ALU.mult),
                                    reads=[yb, G], writes=[av])
                            else:
                                kb.op('dve', lambda e, yb=yb, t=t, osl=osl, e_i=e_i: e.scalar_tensor_tensor(
                                    out=acc[:, t, osl], in0=yb[:], scalar=G[:, t, e_i:e_i + 1], in1=acc[:, t, osl],
                                    op0=ALU.mult, op1=ALU.add), reads=[yb, G], writes=[av])
            for t in range(GT):
                tile = gi * GT + t
                xi = xin[t % 2]
                kb.dma('sp', xi[:], x[tile * 128:(tile + 1) * 128, :], xi, writes=[xi])
                out_toks.append(emit_ln(kb, L, xi, xi, [acc[:, t, 0:512], acc[:, t, 512:1024]], accv[t], g, b,
                                        y[tile * 128:(tile + 1) * 128, :]))
        kb.wait_only('sp', out_toks)
        kb.emit()
    return nc


BIGIDX = 16.0 * 1024 + 7.0
BIG2 = 65536.0


def build_moe_sparse(NT=32, C=768, NE=16, debug=False):
    nc = bass.Bass("TRN2", target_bir_lowering=False)
    T = NT * 128
    NS = C // 128
    NG = NT * 16
    x = nc.dram_tensor("x", [T, D], F32, kind="ExternalInput").ap()
    rw = nc.dram_tensor("rw", [D, 16], F32, kind="ExternalInput").ap()
    rbias = nc.dram_tensor("rbias", [128, NG], F32, kind="ExternalInput").ap()
    eoffd = nc.dram_tensor("eoff", [128, NG], F32, kind="ExternalInput").ap()
    w1 = nc.dram_tensor("w1", [16, D, D], F32, kind="ExternalInput").ap()
    w3 = nc.dram_tensor("w3", [16, D, D], F32, kind="ExternalInput").ap()
    w2 = nc.dram_tensor("w2", [16, D, D], F32, kind="ExternalInput").ap()
    lng = nc.dram_tensor("lng", [128, D], F32, kind="ExternalInput").ap()
    lnb = nc.dram_tensor("lnb", [128, D], F32, kind="ExternalInput").ap()
    y = nc.dram_tensor("y", [T, D], F32, kind="ExternalOutput").ap()
    xs = nc.dram_tensor("xs_scr", [16 * C, D], F32, kind="Internal").ap()
    ys = nc.dram_tensor("ys_scr", [16 * C, D], F32, kind="Internal").ap()
    if debug:
        dbg = nc.dram_tensor("dbg", [4, 128, NT], F32, kind="ExternalOutput").ap()
    with ExitStack() as es:
        kb = KB(nc, es)
        ident = kb.sb([128, 128], F32)
        make_ident(kb, ident)
        ltri = kb.sb([128, 128], F32)
        ones = kb.sb([128, 128], F32)
        kb.op('pool', lambda e: e.memset(ones[:], 1.0), writes=[ones])
        kb.op('pool', lambda e: e.memset(ltri[:], 1.0), writes=[ltri])
        kb.op('pool', lambda e: e.affine_select(out=ltri[:], in_=ltri[:], compare_op=ALU.is_gt, fill=0.0, base=0,
                                                pattern=[[1, 128]], channel_multiplier=-1), writes=[ltri])
        rws = kb.sb([128, 8, 16], F32)
        kb.dma('sp', rws[:], rw.rearrange("(c p) e -> p c e", p=128), rws, writes=[rws])
        rb = kb.sb([128, NG], F32)
        kb.dma('sp', rb[:], rbias[:, :], rb, writes=[rb])
        eoff = kb.sb([128, NG], F32)
        kb.dma('sp', eoff[:], eoffd[:, :], eoff, writes=[eoff])
        g = kb.sb([128, D], F32)
        b = kb.sb([128, D], F32)
        kb.dma('sp', g[:], lng[:, :], g, writes=[g])
        kb.dma('sp', b[:], lnb[:, :], b, writes=[b])
        L = LNBufs(kb, nbuf=1)
        xin = [kb.sb([128, D], F32) for _ in range(2)]
        x32 = [kb.sb([128, 8, 128], F32) for _ in range(2)]
        xT = kb.sb([128, 8, C], BF16)
        xTv = [kb.view() for _ in range(NS)]
        w13 = [[kb.sb([128, 8, D], BF16) for _ in range(2)] for _ in range(2)]
        w2b = kb.sb([128, 8, D], BF16)
        hT = kb.sb([128, 8, 512], BF16)
        hv = [kb.view() for _ in range(8)]
        sil = [kb.sb([128, 512], BF16) for _ in range(2)]
        banks = [kb.ps([128, 512], F32) for _ in range(8)]
        xsb = kb.view("xs")
        ysb = kb.view("ys")
        wsrc = [w1, w3]

        def load_w13(e):
            for j in range(2):
                kb.dma('pool', w13[e % 2][j][:], wsrc[j][e].rearrange("(c p) f -> p c f", p=128), w13[e % 2][j],
                       writes=[w13[e % 2][j]])

        def load_w2(e):
            kb.dma('pool', w2b[:], w2[e].rearrange("(c p) f -> p c f", p=128), w2b, writes=[w2b])

        if NE > 0:
            load_w13(0)
            load_w2(0)

        aff = kb.sb([128, NG], F32)
        affv = [kb.view() for _ in range(NT)]
        for t in range(NT):
            xi = xin[t % 2]
            x3 = x32[t % 2]
            kb.dma('sp', xi[:], x[t * 128:(t + 1) * 128, :], xi, writes=[xi])
            for hf in range(2):
                bk = banks[4 + hf]
                kb.group('pe', [lambda e, c=c, bk=bk, xi=xi: e.transpose(bk[:, (c % 4) * 128:(c % 4 + 1) * 128],
                                                                           xi[:, c * 128:(c + 1) * 128], ident[:])
                                for c in range(hf * 4, hf * 4 + 4)], reads=[xi, ident], writes=[bk])
                tk = kb.op('act', lambda e, bk=bk, hf=hf, x3=x3: e.copy(out=x3[:, hf * 4:(hf + 1) * 4, :],
                                                                         in_=bk[:].rearrange("p (c t) -> p c t", c=4)),
                           reads=[bk], writes=[x3] if hf == 0 else [], deps=x3.w if hf == 1 else ())
            x3.w = [tk]
            rbk = banks[6 + t % 2]
            kb.group('pe', [lambda e, c=c, x3=x3, rbk=rbk: e.matmul(rbk[:, 0:16], x3[:, c, :], rws[:, c, :],
                                                                   start=(c == 0), stop=(c == 7)) for c in range(8)],
                     reads=[x3, rws], writes=[rbk])
            kb.op('act', lambda e, t=t, rbk=rbk: e.activation(out=aff[:, t * 16:(t + 1) * 16], in_=rbk[:, 0:16], func=AF.Sigmoid),
                  reads=[rbk], writes=[affv[t]])
        S8 = kb.sb([128, NT * 4, 8], F32)
        Pt = kb.sb([128, NT * 4, 6], F32)
        gs = kb.sb([128, NT * 4], F32)
        gmax = kb.sb([128, NT], F32)
        ghot = kb.sb([128, NT * 4], F32)
        c1 = kb.sb([128, NT * 4, 4], F32)
        c2 = kb.sb([128, NT * 4, 4], F32)
        GA = kb.sb([128, NT, 16], F32)
        den = kb.sb([128, NT], F32)
        G = kb.sb([128, NT, 16], F32)
        aff4 = aff[:].rearrange("p (g j) -> p g j", j=4)

        def dv(fn, reads, writes):
            return kb.op('dve', fn, reads=reads, writes=writes)
        dv(lambda e: e.tensor_tensor(out=S8[:, :, 0:4], in0=aff4, in1=rb[:].rearrange("p (g j) -> p g j", j=4), op=ALU.add), affv + [rb], [S8])
        dv(lambda e: e.tensor_copy(out=S8[:, :, 4:8], in_=S8[:, :, 0:4]), [S8], [S8])
        for i, (a, bb) in enumerate([(0, 1), (0, 2), (0, 3), (1, 2), (1, 3), (2, 3)]):
            dv(lambda e, i=i, a=a, bb=bb: e.tensor_tensor(out=Pt[:, :, i], in0=S8[:, :, a], in1=S8[:, :, bb], op=ALU.add), [S8], [Pt])
        dv(lambda e: e.tensor_reduce(out=gs[:], in_=Pt[:], axis=AX.X, op=ALU.max), [Pt], [gs])
        dv(lambda e: e.tensor_reduce(out=gmax[:], in_=gs[:].rearrange("p (t g) -> p t g", g=4), axis=AX.X, op=ALU.max), [gs], [gmax])
        dv(lambda e: e.tensor_tensor(out=ghot[:].rearrange("p (t g) -> p t g", g=4), in0=gs[:].rearrange("p (t g) -> p t g", g=4),
                                     in1=gmax[:].unsqueeze(2).broadcast_to([128, NT, 4]), op=ALU.is_ge), [gs, gmax], [ghot])
        dv(lambda e: e.tensor_tensor(out=c1[:], in0=S8[:, :, 0:4], in1=S8[:, :, 1:5], op=ALU.is_gt), [S8], [c1])
        dv(lambda e: e.tensor_tensor(out=c2[:], in0=S8[:, :, 0:4], in1=S8[:, :, 2:6], op=ALU.is_gt), [S8], [c2])
        dv(lambda e: e.tensor_tensor(out=c1[:], in0=c1[:], in1=c2[:], op=ALU.add), [c1, c2], [c1])
        dv(lambda e: e.tensor_tensor(out=c2[:], in0=S8[:, :, 0:4], in1=S8[:, :, 3:7], op=ALU.is_gt), [S8], [c2])
        dv(lambda e: e.tensor_tensor(out=c1[:], in0=c1[:], in1=c2[:], op=ALU.add), [c1, c2], [c1])
        dv(lambda e: e.scalar_tensor_tensor(out=c1[:], in0=c1[:], scalar=2.0, in1=ghot[:].unsqueeze(2).broadcast_to([128, NT * 4, 4]),
                                            op0=ALU.is_ge, op1=ALU.mult), [c1, ghot], [c1])
        dv(lambda e: e.tensor_tensor(out=GA[:].rearrange("p t (g j) -> p (t g) j", j=4), in0=c1[:], in1=aff4, op=ALU.mult), [c1] + affv, [GA])
        dv(lambda e: e.tensor_reduce(out=den[:], in_=GA[:], axis=AX.X, op=ALU.add), [GA], [den])
        dv(lambda e: e.reciprocal(out=den[:], in_=den[:]), [den], [den])
        dv(lambda e: e.tensor_tensor(out=G[:], in0=GA[:], in1=den[:].unsqueeze(2).broadcast_to([128, NT, 16]), op=ALU.mult), [GA, den], [G])
        chs = c1
        chs3 = chs[:].rearrange("p (t g) j -> p t (g j)", g=4)
        CS = kb.sb([128, NT, 16], F32)
        dv(lambda e: e.memset(CS[:, 0, :], 0.0), [], [CS])
        for t in range(1, NT):
            dv(lambda e, t=t: e.tensor_tensor(out=CS[:, t, :], in0=CS[:, t - 1, :], in1=chs3[:, t - 1, :], op=ALU.add), [chs], [CS])
        rkb = banks[4]
        fns = []
        for t in range(NT):
            fns.append(lambda e, t=t: e.matmul(rkb[:, t * 16:(t + 1) * 16], ltri[:], chs3[:, t, :], start=True, stop=False))
            fns.append(lambda e, t=t: e.matmul(rkb[:, t * 16:(t + 1) * 16], ones[:], CS[:, t, :], start=False, stop=True))
        kb.group('pe', fns, reads=[ltri, ones, chs, CS], writes=[rkb])
        rank = kb.sb([128, NG], F32)
        sel = kb.sb([128, NG], F32)
        slotv = kb.sb([128, NT, 16], F32)
        slot2 = kb.sb([128, NT, 16], F32)
        Gv = S8
        Gv = kb.sb([128, NT, 16], F32)
        lo = kb.sb([128, NT], F32)
        hi = kb.sb([128, NT], F32)
        glo = kb.sb([128, NT], F32)
        ghi = kb.sb([128, NT], F32)
        lo_t = [kb.sb([128, 1], I32) for _ in range(NT)]
        hi_t = [kb.sb([128, 1], I32) for _ in range(NT)]
        chs_f = chs[:].rearrange("p a j -> p (a j)")
        dv(lambda e: e.tensor_copy(out=rank[:], in_=rkb[:, 0:NG]), [rkb], [rank])
        dv(lambda e: e.scalar_tensor_tensor(out=sel[:], in0=rank[:], scalar=float(C), in1=chs_f, op0=ALU.is_lt, op1=ALU.mult), [rank, chs], [sel])
        dv(lambda e: e.scalar_tensor_tensor(out=rank[:], in0=rank[:], scalar=-BIGIDX, in1=eoff[:], op0=ALU.add, op1=ALU.add), [rank, eoff], [rank])
        dv(lambda e: e.tensor_tensor(out=rank[:], in0=rank[:], in1=sel[:], op=ALU.mult), [rank, sel], [rank])
        dv(lambda e: e.tensor_scalar(out=slotv[:].rearrange("p t e -> p (t e)"), in0=rank[:], scalar1=BIGIDX, scalar2=None, op0=ALU.add), [rank], [slotv])
        dv(lambda e: e.tensor_tensor(out=Gv[:].rearrange("p t e -> p (t e)"), in0=G[:].rearrange("p t e -> p (t e)"), in1=sel[:], op=ALU.mult), [G, sel], [Gv])
        dv(lambda e: e.tensor_reduce(out=lo[:], in_=slotv[:], axis=AX.X, op=ALU.min), [slotv], [lo])
        dv(lambda e: e.tensor_tensor(out=slot2[:], in0=slotv[:], in1=lo[:].unsqueeze(2).broadcast_to([128, NT, 16]), op=ALU.is_equal), [slotv, lo], [slot2])
        dv(lambda e: e.tensor_tensor(out=GA[:], in0=Gv[:], in1=slot2[:], op=ALU.mult), [Gv, slot2], [GA])
        dv(lambda e: e.tensor_reduce(out=glo[:], in_=GA[:], axis=AX.X, op=ALU.add), [GA], [glo])
        dv(lambda e: e.scalar_tensor_tensor(out=slot2[:], in0=slot2[:], scalar=BIG2, in1=slotv[:], op0=ALU.mult, op1=ALU.add), [slot2, slotv], [slot2])
        dv(lambda e: e.tensor_reduce(out=hi[:], in_=slot2[:], axis=AX.X, op=ALU.min), [slot2], [hi])
        dv(lambda e: e.tensor_tensor(out=slot2[:], in0=slot2[:], in1=hi[:].unsqueeze(2).broadcast_to([128, NT, 16]), op=ALU.is_equal), [slot2, hi], [slot2])
        dv(lambda e: e.tensor_tensor(out=GA[:], in0=Gv[:], in1=slot2[:], op=ALU.mult), [Gv, slot2], [GA])
        dv(lambda e: e.tensor_reduce(out=ghi[:], in_=GA[:], axis=AX.X, op=ALU.add), [GA], [ghi])
        for t in range(NT):
            dv(lambda e, t=t: e.tensor_copy(out=lo_t[t][:], in_=lo[:, t:t + 1]), [lo], [lo_t[t]])
            dv(lambda e, t=t: e.tensor_copy(out=hi_t[t][:], in_=hi[:, t:t + 1]), [hi], [hi_t[t]])
        out_toks = []
        if debug:
            for k_, src in enumerate([lo, hi, glo, ghi]):
                out_toks.append(kb.dma('sp', dbg[k_], src[:], src, reads=[src]))
        bound = 16 * C - 1
        sc_toks = []
        for t in range(NT):
            xi = xin[t % 2]
            kb.dma('sp', xi[:], x[t * 128:(t + 1) * 128, :], xi, writes=[xi])
            kb.idma(xs[:, :], xi[:, :], lo_t[t][:, :], True, xsb, bound, reads=[xi, lo_t[t]], writes=[xsb])
            tk_s = kb.idma(xs[:, :], xi[:, :], hi_t[t][:, :], True, xsb, bound, reads=[xi, hi_t[t]], writes=[xsb])
            sc_toks.append(tk_s)
            if len(sc_toks) >= 3:
                kb.wait_only('pool', [sc_toks[-3]])
        ybank = 0
        chunks = []
        o_ = 0
        while o_ < C:
            wdt = min(512, C - o_)
            chunks.append((o_, wdt))
            o_ += wdt
        for e_i in range(NE):
            if e_i + 1 < NE:
                load_w13(e_i + 1)
            w1b, w3b = w13[e_i % 2]
            for s in range(NS):
                xi = xin[s % 2]
                r0 = e_i * C + s * 128
                kb.dma('sp', xi[:], xs[r0:r0 + 128, :], xi, reads=[xsb], writes=[xi])
                for hf in range(2):
                    bk = banks[4 + (ybank % 4)]
                    ybank += 1
                    kb.group('pe', [lambda e, c=c, bk=bk, xi=xi: e.transpose(bk[:, (c % 4) * 128:(c % 4 + 1) * 128],
                                                                               xi[:, c * 128:(c + 1) * 128], ident[:])
                                    for c in range(hf * 4, hf * 4 + 4)], reads=[xi, ident], writes=[bk])
                    if hf == 0:
                        ta = kb.op('act', lambda e, bk=bk, s=s: e.copy(out=xT[:, 0:4, s * 128:(s + 1) * 128],
                                                                       in_=bk[:].rearrange("p (c t) -> p c t", c=4)),
                                   reads=[bk], writes=[xTv[s]])
                    else:
                        tb = kb.op('dve', lambda e, bk=bk, s=s: e.tensor_copy(out=xT[:, 4:8, s * 128:(s + 1) * 128],
                                                                              in_=bk[:].rearrange("p (c t) -> p c t", c=4)),
                                   reads=[bk], deps=[ta])
                        xTv[s].w = [ta, tb]
            for (c0, wdt) in chunks:
                tsl = slice(c0, c0 + wdt)
                tv = xTv[c0 // 128:(c0 + wdt) // 128]
                for fc in range(8):
                    b1 = banks[fc % 2]
                    b3 = banks[2 + fc % 2]
                    sl_ = sil[fc % 2]
                    fsl = slice(fc * 128, (fc + 1) * 128)
                    kb.group('pe', [lambda e, c=c, b1=b1, fsl=fsl, w1b=w1b, tsl=tsl, wdt=wdt: e.matmul(
                        b1[:, 0:wdt], w1b[:, c, fsl], xT[:, c, tsl], start=(c == 0), stop=(c == 7)) for c in range(8)],
                        reads=[w1b] + tv, writes=[b1])
                    kb.group('pe', [lambda e, c=c, b3=b3, fsl=fsl, w3b=w3b, tsl=tsl, wdt=wdt: e.matmul(
                        b3[:, 0:wdt], w3b[:, c, fsl], xT[:, c, tsl], start=(c == 0), stop=(c == 7)) for c in range(8)],
                        reads=[w3b] + tv, writes=[b3])
                    kb.op('act', lambda e, b1=b1, sl_=sl_, wdt=wdt: e.activation(out=sl_[:, 0:wdt], in_=b1[:, 0:wdt], func=AF.Silu),
                          reads=[b1], writes=[sl_])
                    kb.op('dve', lambda e, b3=b3, sl_=sl_, fc=fc, wdt=wdt: e.tensor_tensor(out=hT[:, fc, 0:wdt], in0=b3[:, 0:wdt],
                                                                                           in1=sl_[:, 0:wdt], op=ALU.mult),
                          reads=[b3, sl_], writes=[hv[fc]])
                for t4 in range(wdt // 128):
                    s = c0 // 128 + t4
                    yst = x32[s % 2]
                    ysf = yst[:].rearrange("p c t -> p (c t)")
                    for oh in range(2):
                        yb = banks[4 + ybank % 4]
                        ybank += 1
                        osl = slice(oh * 512, (oh + 1) * 512)
                        kb.group('pe', [lambda e, fc=fc, yb=yb, t4=t4, osl=osl: e.matmul(
                            yb[:], hT[:, fc, t4 * 128:(t4 + 1) * 128], w2b[:, fc, osl], start=(fc == 0), stop=(fc == 7))
                            for fc in range(8)], reads=[w2b] + hv, writes=[yb])
                        if oh == 0:
                            t0_ = kb.op('act', lambda e, yb=yb, ysf=ysf, osl=osl: e.copy(out=ysf[:, osl], in_=yb[:]), reads=[yb], writes=[yst])
                        else:
                            t1_ = kb.op('dve', lambda e, yb=yb, ysf=ysf, osl=osl: e.tensor_copy(out=ysf[:, osl], in_=yb[:]), reads=[yb], deps=[t0_])
                            yst.w = [t0_, t1_]
                    r0 = e_i * C + s * 128
                    kb.dma('sp', ys[r0:r0 + 128, :], ysf, ysb, reads=[yst], writes=[ysb])
            if e_i + 1 < NE:
                load_w2(e_i + 1)
        pe_done = Tok(kb.sems['pe'], kb.cnt['pe'])
        tiles = []
        for sl_ in range(2):
            for j in range(2):
                flat = w13[sl_][j][:].rearrange("p c f -> p (c f)").bitcast(F32)
                for q_ in range(4):
                    bf_ = Buf(kb, flat[:, q_ * D:(q_ + 1) * D], f"alias{sl_}{j}{q_}")
                    bf_.r = {id(pe_done.sem): pe_done}
                    bf_.w = list(w13[sl_][j].w)
                    tiles.append(bf_)
        ga2, gb2, acc2, xin3 = tiles[0:2] + tiles[12:13], tiles[2:4] + tiles[13:14], tiles[4:6], tiles[6:8] + xin
        L3 = LNBufs.__new__(LNBufs)
        L3.kb, L3.n, L3.i = kb, 2, 0
        L3.z, L3.o = tiles[8:10], tiles[10:12]
        L3.st = [kb.sb([128, 12], F32) for _ in range(2)]
        L3.mv = [kb.sb([128, 2], F32) for _ in range(2)]
        L3.rs = [kb.sb([128, 1], F32) for _ in range(2)]
        L3.nb = [kb.sb([128, 1], F32) for _ in range(2)]
        L3.eps = L.eps
        for bf_ in ga2 + gb2:
            kb.op('dve', lambda e, bf_=bf_: e.memset(bf_[:], 0.0), writes=[bf_])
        def fetch(t):
            xi = xin3[t % 4]
            kb.dma('sp' if t % 2 == 0 else 'act', xi[:], x[t * 128:(t + 1) * 128, :], xi, writes=[xi])
            kb.idma(ga2[t % 3][:, :], ys[:, :], lo_t[t][:, :], False, ga2[t % 3], bound, reads=[ysb, lo_t[t]], writes=[ga2[t % 3]])
            kb.idma(gb2[t % 3][:, :], ys[:, :], hi_t[t][:, :], False, gb2[t % 3], bound, reads=[ysb, hi_t[t]], writes=[gb2[t % 3]])

        fetch(0)
        fetch(1)
        for t in range(NT):
            xi = xin3[t % 4]
            ga_, gb3, acc = ga2[t % 3], gb2[t % 3], acc2[t % 2]
            if t + 2 < NT:
                fetch(t + 2)
            kb.op('act', lambda e, t=t, acc=acc, ga_=ga_: e.activation(out=acc[:], in_=ga_[:], func=AF.Copy, scale=glo[:, t:t + 1]),
                  reads=[ga_, glo], writes=[acc])
            av = [kb.view(), kb.view()]
            for hf in range(2):
                sl = slice(hf * 512, (hf + 1) * 512)
                kb.op('dve', lambda e, t=t, sl=sl, acc=acc, gb3=gb3: e.scalar_tensor_tensor(
                    out=acc[:, sl], in0=gb3[:, sl], scalar=ghi[:, t:t + 1], in1=acc[:, sl], op0=ALU.mult, op1=ALU.add),
                    reads=[gb3, ghi, acc], writes=[av[hf]])
            out_toks.append(emit_ln(kb, L3, xi, xi, [acc[:, 0:512], acc[:, 512:1024]], av, g, b, y[t * 128:(t + 1) * 128, :],
                                    dma_eng='sp' if t % 2 == 0 else 'act', b_eng='dve'))
            acc.r.update(av[0].r)
            acc.r.update(av[1].r)
        kb.wait_only('sp', out_toks)
        kb.emit()
    return nc


def pool_tables(first_is_start, last_is_end):
    Bm = np.zeros((3, 4, 128, 128), np.float32)
    Bh = np.zeros((3, 4, 16, 128), np.float32)
    for var in range(3):
        start_edge = (var == 0 and first_is_start)
        end_edge = (var == 2 and last_is_end)
        for g, w in enumerate((2, 4, 8, 16)):
            half = w // 2
            for t in range(128):
                lo, hi = t - half, t + half - 1
                if start_edge:
                    lo = max(lo, 0)
                if end_edge:
                    hi = min(hi, 127)
                cnt = hi - lo + 1
                for s in range(lo, hi + 1):
                    if 0 <= s < 128:
                        Bm[var, g, s, t] += 1.0 / cnt
                    elif s < 0:
                        Bh[var, g, 8 + s, t] += 1.0 / cnt
                    else:
                        Bh[var, g, 8 + (s - 128), t] += 1.0 / cnt
                Bm[var, g, t, t] -= 1.0
    return Bm, Bh


def build_pool(NT=32):
    nc = bass.Bass("TRN2", target_bir_lowering=False)
    T = NT * 128
    xp = nc.dram_tensor("xp", [T + 16, D], F32, kind="ExternalInput").ap()
    bm = nc.dram_tensor("bm", [128, 12 * 128], F32, kind="ExternalInput").ap()
    bh = nc.dram_tensor("bh", [16, 12 * 128], F32, kind="ExternalInput").ap()
    pw = nc.dram_tensor("pw", [4, 256, 256], F32, kind="ExternalInput").ap()
    psc = nc.dram_tensor("psc", [128, D], F32, kind="ExternalInput").ap()
    lng = nc.dram_tensor("lng", [128, D], F32, kind="ExternalInput").ap()
    lnb = nc.dram_tensor("lnb", [128, D], F32, kind="ExternalInput").ap()
    y = nc.dram_tensor("y", [T, D], F32, kind="ExternalOutput").ap()
    with ExitStack() as es:
        kb = KB(nc, es)
        g = kb.sb([128, D], F32)
        b = kb.sb([128, D], F32)
        kb.dma('sp', g[:], lng[:, :], g, writes=[g])
        kb.dma('sp', b[:], lnb[:, :], b, writes=[b])
        L = LNBufs(kb, nbuf=2)
        Bm = kb.sb([128, 12 * 128], BF16)
        Bh = kb.sb([16, 12 * 128], BF16)
        kb.dma('pool', Bm[:], bm[:, :], Bm, writes=[Bm])
        kb.dma('pool', Bh[:], bh[:, :], Bh, writes=[Bh])
        W32 = kb.sb([128, 4, 2, 256], F32)
        sc = kb.sb([128, D], F32)
        Wb = kb.sb([128, 4, 2, 256], BF16)
        kb.dma('sp', W32[:], pw.rearrange("g (hh p) e -> p g hh e", p=128), W32, writes=[W32])
        kb.dma('sp', sc[:], psc[:, :], sc, writes=[sc])
        kb.op('dve', lambda e: e.tensor_tensor(out=Wb[:], in0=W32[:],
                                               in1=sc[:].rearrange("p (g e) -> p g e", g=4).unsqueeze(2).broadcast_to([128, 4, 2, 256]),
                                               op=ALU.mult), reads=[W32, sc], writes=[Wb])
        xin = [kb.sb([128, D], F32) for _ in range(3)]
        xh = [kb.sb([16, D], F32) for _ in range(2)]
        xb = [kb.sb([128, D], BF16) for _ in range(2)]
        xhb = [kb.sb([16, D], BF16) for _ in range(2)]
        uT = [kb.sb([128, 8, 128], BF16) for _ in range(2)]
        uTa = [kb.view() for _ in range(2)]
        uTb = [kb.view() for _ in range(2)]
        pu = [[kb.ps([128, 512], F32) for _ in range(2)] for _ in range(2)]
        py = [[kb.ps([128, 512], F32) for _ in range(2)] for _ in range(2)]
        out_toks = []
        for i in range(NT):
            var = 0 if i == 0 else (2 if i == NT - 1 else 1)
            xi = xin[i % 3]
            xhi = xh[i % 2]
            xbi = xb[i % 2]
            xhbi = xhb[i % 2]
            u = uT[i % 2]
            ua, ub = uTa[i % 2], uTb[i % 2]
            pui = pu[i % 2]
            pyi = py[i % 2]
            kb.dma('sp', xi[:], xp[8 + i * 128: 8 + (i + 1) * 128, :], xi, writes=[xi])
            kb.dma('sp', xhi[0:8, :], xp[i * 128: i * 128 + 8, :], xhi, writes=[xhi])
            kb.dma('sp', xhi[8:16, :], xp[8 + (i + 1) * 128: 16 + (i + 1) * 128, :], xhi, writes=[xhi])
            kb.op('act', lambda e, xi=xi, xbi=xbi: e.copy(out=xbi[:], in_=xi[:]), reads=[xi], writes=[xbi])
            kb.op('dve', lambda e, xhi=xhi, xhbi=xhbi: e.tensor_copy(out=xhbi[:], in_=xhi[:]), reads=[xhi], writes=[xhbi])
            for hf in range(2):
                fns = []
                for fc in range(hf * 4, hf * 4 + 4):
                    col = (var * 4 + fc // 2) * 128
                    fns.append(lambda e, fc=fc, col=col, xbi=xbi, bk=pui[hf]: e.matmul(
                        bk[:, (fc % 4) * 128:(fc % 4 + 1) * 128], xbi[:, fc * 128:(fc + 1) * 128], Bm[:, col:col + 128],
                        start=True, stop=False))
                    fns.append(lambda e, fc=fc, col=col, xhbi=xhbi, bk=pui[hf]: e.matmul(
                        bk[:, (fc % 4) * 128:(fc % 4 + 1) * 128], xhbi[:, fc * 128:(fc + 1) * 128], Bh[:, col:col + 128],
                        start=False, stop=True))
                kb.group('pe', fns, reads=[xbi, xhbi, Bm, Bh], writes=[pui[hf]])
            kb.op('act', lambda e, u=u, bk=pui[0]: e.copy(out=u[:, 0:4, :], in_=bk[:].rearrange("p (c t) -> p c t", c=4)),
                  reads=[pui[0]], writes=[ua])
            kb.op('dve', lambda e, u=u, bk=pui[1]: e.tensor_copy(out=u[:, 4:8, :], in_=bk[:].rearrange("p (c t) -> p c t", c=4)),
                  reads=[pui[1]], writes=[ub])
            for bkidx in range(2):
                fns = []
                for gg in range(bkidx * 2, bkidx * 2 + 2):
                    for hh in range(2):
                        fns.append(lambda e, gg=gg, hh=hh, u=u, bk=pyi[bkidx]: e.matmul(
                            bk[:, (gg % 2) * 256:(gg % 2 + 1) * 256], u[:, gg * 2 + hh, :], Wb[:, gg, hh, :],
                            start=(hh == 0), stop=(hh == 1)))
                kb.group('pe', fns, reads=[ua if bkidx == 0 else ub, Wb], writes=[pyi[bkidx]])
            out_toks.append(emit_ln(kb, L, xi, xi, [pyi[0][:], pyi[1][:]], pyi, g, b, y[i * 128:(i + 1) * 128, :]))
        kb.wait_only('sp', out_toks)
        kb.emit()
    return nc


def build_qkv(NT=32):
    nc = bass.Bass("TRN2", target_bir_lowering=False)
    T = NT * 128
    x = nc.dram_tensor("x", [T, D], F32, kind="ExternalInput").ap()
    win = nc.dram_tensor("win", [D, 3 * D], F32, kind="ExternalInput").ap()
    cs = nc.dram_tensor("cs", [T, 64], F32, kind="ExternalInput").ap()
    qkv = nc.dram_tensor("qkv", [T, 3 * D], BF16, kind="ExternalOutput").ap()
    with ExitStack() as es:
        kb = KB(nc, es)
        ident = kb.sb([128, 128], F32)
        make_ident(kb, ident)
        wb = kb.sb([128, 8, 3 * D], BF16)
        wv = [kb.view() for _ in range(8)]
        for c in range(8):
            kb.dma('pool', wb[:, c, :], win[c * 128:(c + 1) * 128, :], wv[c], writes=[wv[c]])
        kb.op('pool', lambda e: e.tensor_scalar(out=wb[:, :, 0:D], in0=wb[:, :, 0:D], scalar1=0.125, scalar2=None, op0=ALU.mult),
              reads=[], writes=wv)
        xin = [kb.sb([128, D], F32) for _ in range(2)]
        cst = [kb.sb([128, 64], F32) for _ in range(2)]
        xT = [kb.sb([128, 8, 128], BF16) for _ in range(2)]
        kro = [kb.sb([128, 512], F32) for _ in range(2)]
        tm = [kb.sb([128, 8, 32], F32) for _ in range(4)]
        tp = [kb.sb([128, 8, 32], F32) for _ in range(4)]
        ost = [kb.sb([128, 3 * D], BF16) for _ in range(2)]
        ostv = [[kb.view() for _ in range(6)] for _ in range(2)]
        banks = [kb.ps([128, 512], F32) for _ in range(8)]
        nb = 0
        out_toks = []
        for i in range(NT):
            xi = xin[i % 2]
            ci = cst[i % 2]
            xt = xT[i % 2]
            oi = ost[i % 2]
            ov = ostv[i % 2]
            kr = kro[i % 2]
            kb.dma('sp', xi[:], x[i * 128:(i + 1) * 128, :], xi, writes=[xi])
            kb.dma('sp', ci[:], cs[i * 128:(i + 1) * 128, :], ci, writes=[ci])
            for hf in range(2):
                bk = banks[nb % 8]; nb += 1
                kb.group('pe', [lambda e, c=c, bk=bk, xi=xi: e.transpose(bk[:, (c % 4) * 128:(c % 4 + 1) * 128],
                                                                           xi[:, c * 128:(c + 1) * 128], ident[:])
                                for c in range(hf * 4, hf * 4 + 4)], reads=[xi, ident], writes=[bk])
                eng = 'act' if hf == 0 else 'dve'
                fn = (lambda e, bk=bk, hf=hf, xt=xt: e.copy(out=xt[:, hf * 4:(hf + 1) * 4, :], in_=bk[:].rearrange("p (c t) -> p c t", c=4))) \
                    if hf == 0 else (lambda e, bk=bk, hf=hf, xt=xt: e.tensor_copy(out=xt[:, hf * 4:(hf + 1) * 4, :], in_=bk[:].rearrange("p (c t) -> p c t", c=4)))
                if hf == 0:
                    t_a = kb.op(eng, fn, reads=[bk], writes=[xt])
                else:
                    t_b = kb.op(eng, fn, reads=[bk], deps=[t_a])
                    xt.w = [t_a, t_b]
            cosb = ci[:, 0:32].unsqueeze(1).broadcast_to([128, 8, 32])
            sinb = ci[:, 32:64].unsqueeze(1).broadcast_to([128, 8, 32])
            for blk in range(6):
                bk = banks[nb % 8]; nb += 1
                kb.group('pe', [lambda e, c=c, bk=bk, blk=blk, xt=xt: e.matmul(bk[:], xt[:, c, :], wb[:, c, blk * 512:(blk + 1) * 512],
                                                                               start=(c == 0), stop=(c == 7)) for c in range(8)],
                         reads=[xt] + wv, writes=[bk])
                osl = slice(blk * 512, (blk + 1) * 512)
                if blk in (0, 2):
                    if blk == 0:
                        eng, tt, src, sbuf = 'dve', tm, bk, bk
                    else:
                        kb.op('act', lambda e, bk=bk, kr=kr: e.copy(out=kr[:], in_=bk[:]), reads=[bk], writes=[kr])
                        eng, tt, src, sbuf = 'pool', tp, kr, kr
                    s4 = src[:].rearrange("p (h two j) -> p h two j", two=2, j=32)
                    o4 = oi[:, osl].rearrange("p (h two j) -> p h two j", two=2, j=32)
                    t1, t2 = s4[:, :, 0, :], s4[:, :, 1, :]
                    kb.op(eng, lambda e, t1=t1, tt=tt, cosb=cosb: e.tensor_tensor(out=tt[0][:], in0=t1, in1=cosb, op=ALU.mult), reads=[sbuf, ci], writes=[tt[0]])
                    kb.op(eng, lambda e, t2=t2, tt=tt, sinb=sinb: e.tensor_tensor(out=tt[1][:], in0=t2, in1=sinb, op=ALU.mult), reads=[sbuf, ci], writes=[tt[1]])
                    kb.op(eng, lambda e, t2=t2, tt=tt, cosb=cosb: e.tensor_tensor(out=tt[2][:], in0=t2, in1=cosb, op=ALU.mult), reads=[sbuf, ci], writes=[tt[2]])
                    kb.op(eng, lambda e, t1=t1, tt=tt, sinb=sinb: e.tensor_tensor(out=tt[3][:], in0=t1, in1=sinb, op=ALU.mult), reads=[sbuf, ci], writes=[tt[3]])
                    kb.op(eng, lambda e, tt=tt, o4=o4: e.tensor_tensor(out=o4[:, :, 0, :], in0=tt[0][:], in1=tt[1][:], op=ALU.subtract), reads=[tt[0], tt[1]], writes=[ov[blk]])
                    tk = kb.op(eng, lambda e, tt=tt, o4=o4: e.tensor_tensor(out=o4[:, :, 1, :], in0=tt[2][:], in1=tt[3][:], op=ALU.add), reads=[tt[2], tt[3]], deps=ov[blk].w)
                    ov[blk].w = ov[blk].w + [tk]
                else:
                    kb.op('act', lambda e, bk=bk, oi=oi, osl=osl: e.copy(out=oi[:, osl], in_=bk[:]), reads=[bk], writes=[ov[blk]])
            out_toks.append(kb.dma('sp', qkv[i * 128:(i + 1) * 128, :], oi[:], oi, reads=ov))
        kb.wait_only('sp', out_toks)
        kb.emit()
    return nc


def build_attn(NB=4):
    nc = bass.Bass("TRN2", target_bir_lowering=False)
    S = S_LEN
    qA, kA, vA, oA = [], [], [], []
    for di, d in enumerate(DILS):
        Ls = S // d
        nt = Ls // 128
        qA.append(nc.dram_tensor(f"qA{di}", [NB, 64, S], BF16, kind="ExternalInput").ap())
        kA.append(nc.dram_tensor(f"kA{di}", [NB, 64, d * (Ls + 128)], BF16, kind="ExternalInput").ap())
        vA.append(nc.dram_tensor(f"vA{di}", [NB, 128, d * (nt + 1) * 65], BF16, kind="ExternalInput").ap())
        oA.append(nc.dram_tensor(f"oA{di}", [NB, 128, 64 * 65], F32, kind="ExternalOutput").ap())
    qB = nc.dram_tensor("qB", [NB, 64, S], BF16, kind="ExternalInput").ap()
    kB = nc.dram_tensor("kB", [NB, 64, S], BF16, kind="ExternalInput").ap()
    vB = nc.dram_tensor("vB", [NB, 128, 64 * 65], BF16, kind="ExternalInput").ap()
    oB = nc.dram_tensor("oB", [NB, 128, 64 * 64], F32, kind="ExternalOutput").ap()
    ebias = nc.dram_tensor("ebias", [128, 25 * 128], F32, kind="ExternalInput").ap()
    maskd = nc.dram_tensor("maskd", [128, 256], F32, kind="ExternalInput").ap()
    with ExitStack() as es:
        kb = KB(nc, es)
        mask = kb.sb([128, 256], BF16)
        kb.dma('pool', mask[:], maskd[:, :], mask, writes=[mask])
        eb32 = kb.sb([128, 25 * 128], F32)
        E = kb.sb([128, 25 * 128], BF16)
        kb.dma('sp', eb32[:], ebias[:, :], eb32, writes=[eb32])
        kb.op('act', lambda e: e.activation(out=E[:], in_=eb32[:], func=AF.Exp), reads=[eb32], writes=[E])
        KMAX = 16 * (512 + 128)
        qT = [kb.sb([64, S], BF16) for _ in range(2)]
        kT = [kb.sb([64, KMAX], BF16) for _ in range(2)]
        V = [kb.sb([128, 80 * 65], BF16) for _ in range(2)]
        osb = [kb.sb([128, 64 * 65], F32) for _ in range(2)]
        pT = [kb.sb([128, 256], BF16) for _ in range(6)]
        rden = [kb.sb([128, 1], F32) for _ in range(2)]
        sbk = [kb.ps([128, 512], F32) for _ in range(4)]
        obk = [kb.ps([128, 512], F32) for _ in range(4)]
        stages = []
        for b in range(NB):
            for di in range(3):
                stages.append(('A', b, di))
            stages.append(('B', b, None))

        def load(si):
            kind, b, di = stages[si]
            sl = si % 2
            if kind == 'A':
                d = DILS[di]
                Ls = S // d
                nt = Ls // 128
                kb.dma('sp', qT[sl][:], qA[di][b], qT[sl], writes=[qT[sl]])
                kb.dma('sp', kT[sl][:, 0:d * (Ls + 128)], kA[di][b], kT[sl], writes=[kT[sl]])
                kb.dma('sp', V[sl][:, 0:d * (nt + 1) * 65], vA[di][b], V[sl], writes=[V[sl]])
            else:
                kb.dma('sp', qT[sl][:], qB[b], qT[sl], writes=[qT[sl]])
                kb.dma('sp', kT[sl][:, 0:S], kB[b], kT[sl], writes=[kT[sl]])
                kb.dma('sp', V[sl][:, 0:64 * 65], vB[b], V[sl], writes=[V[sl]])

        load(0)
        out_toks = []
        fronts, backs = [], []
        LOOK = 3

        def add_step(si, first, last, front_fn, back_fn):
            n = len(fronts)

            def front(n=n):
                front_fn(n)

            def back(n=n):
                if first and si + 1 < len(stages):
                    load(si + 1)
                back_fn(n)
                if last:
                    kind, b, di = stages[si]
                    ob = osb[si % 2]
                    if kind == 'A':
                        out_toks.append(kb.dma('sp', oA[di][b], ob[:], ob, reads=[ob]))
                    else:
                        out_toks.append(kb.dma('sp', oB[b], ob[:, 0:64 * 64], ob, reads=[ob]))
            fronts.append(front)
            backs.append(back)

        for si, (kind, b, di) in enumerate(stages):
            sl = si % 2
            q, k, v, ob = qT[sl], kT[sl], V[sl], osb[sl]
            if kind == 'A':
                d = DILS[di]
                Ls = S // d
                nt = Ls // 128
                for r in range(d):
                    for j in range(nt + 1):
                        q0 = 128 * (j - 1) if j >= 1 else 0
                        q1 = 128 * (j + 1) if j <= nt - 1 else 128 * nt
                        w = q1 - q0
                        moff = 128 if j == 0 else 0
                        koff = r * (Ls + 128) + 128 * j
                        vt = r * (nt + 1) + j
                        qa = r * Ls + q0

                        def front_fn(step, k=k, q=q, koff=koff, qa=qa, w=w, moff=moff):
                            sb_ = sbk[step % 4]
                            p = pT[step % 6]
                            kb.op('pe', lambda e: e.matmul(sb_[:, 0:w], k[:, koff:koff + 128], q[:, qa:qa + w], start=True, stop=True),
                                  reads=[k, q], writes=[sb_])
                            kb.op('act', lambda e: e.activation(out=p[:, 0:w], in_=sb_[:, 0:w], func=AF.Exp), reads=[sb_], writes=[p])
                            meng = 'dve' if step % 2 == 0 else 'pool'
                            kb.op(meng, lambda e: e.tensor_tensor(out=p[:, 0:w], in0=p[:, 0:w], in1=mask[:, moff:moff + w], op=ALU.mult),
                                  reads=[p, mask], writes=[p])

                        def back_fn(step, j=j, nt=nt, r=r, v=v, vt=vt, ob=ob):
                            p = pT[step % 6]
                            if j >= 1:
                                o_ = obk[(j - 1) % 4]
                                kb.op('pe', lambda e: e.matmul(o_[:, 0:65], p[:, 0:128], v[:, vt * 65:(vt + 1) * 65], start=False, stop=True),
                                      reads=[p, v], writes=[], deps=o_.w + list(o_.r.values()))
                                o_.w = [Tok(kb.sems['pe'], kb.cnt['pe'])]
                                blk = r * nt + (j - 1)
                                kb.op('dve', lambda e: e.tensor_copy(out=ob[:, blk * 65:(blk + 1) * 65], in_=o_[:, 0:65]),
                                      reads=[o_], writes=[], deps=list(ob.r.values()))
                                ob.w = [Tok(kb.sems['dve'], kb.cnt['dve'])]
                            if j <= nt - 1:
                                o2 = obk[j % 4]
                                pc = 128 if j >= 1 else 0
                                kb.op('pe', lambda e: e.matmul(o2[:, 0:65], p[:, pc:pc + 128], v[:, vt * 65:(vt + 1) * 65], start=True, stop=False),
                                      reads=[p, v], writes=[o2])
                        add_step(si, r == 0 and j == 0, r == d - 1 and j == nt, front_fn, back_fn)
            else:
                for m in range(64):
                    cls = {0: 0, 1: 1, 62: 3, 63: 4}.get(m, 2)
                    bt = min(max(m - 2, 0), 59)
                    for j in range(5):
                        tile = bt + j
                        ecol = (cls * 5 + j) * 128

                        def front_fn(step, k=k, q=q, tile=tile, m=m, ecol=ecol):
                            sb_ = sbk[step % 4]
                            p = pT[step % 6]
                            kb.op('pe', lambda e: e.matmul(sb_[:, 0:128], k[:, tile * 128:(tile + 1) * 128], q[:, m * 128:(m + 1) * 128],
                                                           start=True, stop=True), reads=[k, q], writes=[sb_])
                            kb.op('act', lambda e: e.activation(out=p[:, 0:128], in_=sb_[:, 0:128], func=AF.Exp), reads=[sb_], writes=[p])
                            meng = 'dve' if step % 2 == 0 else 'pool'
                            kb.op(meng, lambda e: e.tensor_tensor(out=p[:, 0:128], in0=p[:, 0:128], in1=E[:, ecol:ecol + 128], op=ALU.mult),
                                  reads=[p, E], writes=[p])

                        def back_fn(step, j=j, m=m, v=v, tile=tile, ob=ob):
                            p = pT[step % 6]
                            o_ = obk[m % 4]
                            if j == 0:
                                kb.op('pe', lambda e: e.matmul(o_[:, 0:65], p[:, 0:128], v[:, tile * 65:(tile + 1) * 65], start=True, stop=False),
                                      reads=[p, v], writes=[o_])
                            else:
                                kb.op('pe', lambda e: e.matmul(o_[:, 0:65], p[:, 0:128], v[:, tile * 65:(tile + 1) * 65], start=False, stop=(j == 4)),
                                      reads=[p, v], writes=[], deps=o_.w)
                                o_.w = [Tok(kb.sems['pe'], kb.cnt['pe'])]
                            if j == 4:
                                rd = rden[m % 2]
                                kb.op('dve', lambda e: e.reciprocal(out=rd[:], in_=o_[:, 64:65]), reads=[o_], writes=[rd])
                                kb.op('dve', lambda e: e.tensor_scalar(out=ob[:, m * 64:(m + 1) * 64], in0=o_[:, 0:64], scalar1=rd[:, 0:1],
                                                                       scalar2=None, op0=ALU.mult),
                                      reads=[o_, rd], writes=[], deps=list(ob.r.values()))
                                ob.w = [Tok(kb.sems['dve'], kb.cnt['dve'])]
                        add_step(si, m == 0 and j == 0, m == 63 and j == 4, front_fn, back_fn)
        nsteps = len(fronts)
        for n in range(min(LOOK, nsteps)):
            fronts[n]()
        for n in range(nsteps):
            if n + LOOK < nsteps:
                fronts[n + LOOK]()
            backs[n]()
        kb.wait_only('sp', out_toks)
        kb.emit()
    return nc


def build_oproj(NT=32):
    nc = bass.Bass("TRN2", target_bir_lowering=False)
    T = NT * 128
    x = nc.dram_tensor("x", [T, D], F32, kind="ExternalInput").ap()
    oa = nc.dram_tensor("oa", [3, T, 8 * 65], F32, kind="ExternalInput").ap()
    obd = nc.dram_tensor("ob", [T, 512], F32, kind="ExternalInput").ap()
    wout = nc.dram_tensor("wout", [D, D], F32, kind="ExternalInput").ap()
    lng = nc.dram_tensor("lng", [128, D], F32, kind="ExternalInput").ap()
    lnb = nc.dram_tensor("lnb", [128, D], F32, kind="ExternalInput").ap()
    y = nc.dram_tensor("y", [T, D], F32, kind="ExternalOutput").ap()
    with ExitStack() as es:
        kb = KB(nc, es)
        ident = kb.sb([128, 128], F32)
        make_ident(kb, ident)
        g = kb.sb([128, D], F32)
        b = kb.sb([128, D], F32)
        kb.dma('sp', g[:], lng[:, :], g, writes=[g])
        kb.dma('sp', b[:], lnb[:, :], b, writes=[b])
        L = LNBufs(kb, nbuf=2)
        wb = kb.sb([128, 8, D], BF16)
        kb.dma('pool', wb[:], wout.rearrange("(c p) f -> p c f", p=128), wb, writes=[wb])
        xin = [kb.sb([128, D], F32) for _ in range(3)]
        oat = [[kb.sb([128, 8, 65], F32) for _ in range(3)] for _ in range(2)]
        rd = [kb.sb([128, 8], F32) for _ in range(2)]
        ot = [kb.sb([128, D], F32) for _ in range(2)]
        ota = [kb.view() for _ in range(2)]
        otb = [kb.view() for _ in range(2)]
        oT = [kb.sb([128, 8, 128], BF16) for _ in range(2)]
        oTa = [kb.view() for _ in range(2)]
        oTb = [kb.view() for _ in range(2)]
        pt = [[kb.ps([128, 512], F32) for _ in range(2)] for _ in range(2)]
        py = [[kb.ps([128, 512], F32) for _ in range(2)] for _ in range(2)]
        out_toks = []
        for i in range(NT):
            xi = xin[i % 3]
            a3 = oat[i % 2]
            o = ot[i % 2]
            r_ = rd[i % 2]
            oTi = oT[i % 2]
            rows = slice(i * 128, (i + 1) * 128)
            kb.dma('sp', xi[:], x[rows, :], xi, writes=[xi])
            for br in range(3):
                kb.dma('act' if br == 1 else 'sp', a3[br][:], oa[br, rows, :].rearrange("p (h c) -> p h c", c=65), a3[br], writes=[a3[br]])
            kb.dma('act', o[:, 512:1024], obd[rows, :], otb[i % 2], writes=[otb[i % 2]])
            kb.op('dve', lambda e, a3=a3: e.tensor_tensor(out=a3[0][:], in0=a3[0][:], in1=a3[1][:], op=ALU.add), reads=[a3[1]], writes=[a3[0]])
            kb.op('dve', lambda e, a3=a3: e.tensor_tensor(out=a3[0][:], in0=a3[0][:], in1=a3[2][:], op=ALU.add), reads=[a3[2]], writes=[a3[0]])
            kb.op('dve', lambda e, a3=a3, r_=r_: e.reciprocal(out=r_[:], in_=a3[0][:, :, 64]), reads=[a3[0]], writes=[r_])
            kb.op('dve', lambda e, a3=a3, r_=r_, o=o: e.tensor_tensor(out=o[:, 0:512].rearrange("p (h c) -> p h c", c=64), in0=a3[0][:, :, 0:64],
                                                                   in1=r_[:].unsqueeze(2).broadcast_to([128, 8, 64]), op=ALU.mult),
                  reads=[a3[0], r_], writes=[ota[i % 2]])
            for hf in range(2):
                bk = pt[i % 2][hf]
                kb.group('pe', [lambda e, c=c, bk=bk, o=o: e.transpose(bk[:, (c % 4) * 128:(c % 4 + 1) * 128],
                                                                        o[:, c * 128:(c + 1) * 128], ident[:])
                                for c in range(hf * 4, hf * 4 + 4)], reads=[ota[i % 2] if hf == 0 else otb[i % 2], ident], writes=[bk])
                if hf == 0:
                    kb.op('act', lambda e, bk=bk, oTi=oTi: e.copy(out=oTi[:, 0:4, :], in_=bk[:].rearrange("p (c t) -> p c t", c=4)),
                          reads=[bk], writes=[oTa[i % 2]])
                else:
                    kb.op('dve', lambda e, bk=bk, oTi=oTi: e.tensor_copy(out=oTi[:, 4:8, :], in_=bk[:].rearrange("p (c t) -> p c t", c=4)),
                          reads=[bk], writes=[oTb[i % 2]])
            for oh in range(2):
                bk = py[i % 2][oh]
                kb.group('pe', [lambda e, c=c, bk=bk, oTi=oTi, oh=oh: e.matmul(bk[:], oTi[:, c, :], wb[:, c, oh * 512:(oh + 1) * 512],
                                                                               start=(c == 0), stop=(c == 7)) for c in range(8)],
                         reads=[oTa[i % 2], oTb[i % 2], wb], writes=[bk])
            out_toks.append(emit_ln(kb, L, xi, xi, [py[i % 2][0][:], py[i % 2][1][:]], py[i % 2], g, b, y[rows, :]))
        kb.wait_only('sp', out_toks)
        kb.emit()
    return nc

import ml_dtypes
BF = ml_dtypes.bfloat16
S_LEN = 8192
DILS = (1, 4, 16)


def rope_cs(pos):
    pos = pos.astype(np.float32)
    inv = (np.float32(10000.0) ** (-np.arange(0, 64, 2, dtype=np.float32) / np.float32(64))).astype(np.float32)
    ang = (pos[:, None] * inv[None, :]).astype(np.float32)
    return np.concatenate([np.cos(ang), np.sin(ang)], 1).astype(np.float32)


def attn_mask():
    p = np.arange(128)[:, None]
    a = np.arange(128)[None, :]
    return np.concatenate([(p <= a), (p >= a)], 1).astype(np.float32)


def na_bias_table(rpb_h):
    out = np.full((128, 25, 128), -30000.0, np.float32)
    kk = np.arange(128)
    qq = np.arange(128)
    for cls, m in enumerate((0, 1, 2, 62, 63)):
        base = min(max(2 * m - 4, 0), 118)
        qrow = 2 * m + qq // 64
        qcol = qq % 64
        rstart = np.clip(qrow - 4, 0, 120)
        cstart = np.clip(qcol - 8, 0, 48)
        for j in range(5):
            krow = base + 2 * j + kk // 64
            kcol = kk % 64
            ok = ((krow[:, None] >= rstart[None, :]) & (krow[:, None] < rstart[None, :] + 8)
                  & (kcol[:, None] >= cstart[None, :]) & (kcol[:, None] < cstart[None, :] + 16))
            roff = np.clip(krow[:, None] - qrow[None, :] + 7, 0, 14)
            coff = np.clip(kcol[:, None] - qcol[None, :], -15, 15) + 15
            vals = rpb_h[roff, coff]
            out[:, cls * 5 + j, :] = np.where(ok, vals, np.float32(-30000.0))
    return out.reshape(128, 25 * 128)


def attn_core_inputs(qkv, c, rpb_i):
    B, S = qkv.shape[0], qkv.shape[1]
    ha, hb = c, 8 + c
    im = {}
    q_h, k_h, v_h = qkv[:, :, 0, ha], qkv[:, :, 1, ha], qkv[:, :, 2, ha]
    one = np.ones((), BF)
    for di, d in enumerate(DILS):
        Ls = S // d
        nt = Ls // 128
        im[f"qA{di}"] = np.ascontiguousarray(q_h.reshape(B, Ls, d, 64).transpose(0, 3, 2, 1)).reshape(B, 64, d * Ls)
        kk = np.zeros((B, 64, d, Ls + 128), BF)
        kk[:, :, :, 64:64 + Ls] = k_h.reshape(B, Ls, d, 64).transpose(0, 3, 2, 1)
        im[f"kA{di}"] = kk.reshape(B, 64, d * (Ls + 128))
        vv = np.zeros((B, d, Ls + 128, 65), BF)
        vv[:, :, 64:64 + Ls, :64] = v_h.reshape(B, Ls, d, 64).transpose(0, 2, 1, 3)
        vv[:, :, 64:64 + Ls, 64] = one
        im[f"vA{di}"] = np.ascontiguousarray(vv.reshape(B, d, nt + 1, 128, 65).transpose(0, 3, 1, 2, 4)).reshape(B, 128, d * (nt + 1) * 65)
    im["qB"] = np.ascontiguousarray(qkv[:, :, 0, hb].transpose(0, 2, 1))
    im["kB"] = np.ascontiguousarray(qkv[:, :, 1, hb].transpose(0, 2, 1))
    vb = np.zeros((B, S, 65), BF)
    vb[:, :, :64] = qkv[:, :, 2, hb]
    vb[:, :, 64] = one
    im["vB"] = np.ascontiguousarray(vb.reshape(B, 64, 128, 65).transpose(0, 2, 1, 3)).reshape(B, 128, 64 * 65)
    im["ebias"] = na_bias_table(rpb_i[c])
    im["maskd"] = attn_mask()
    return im


def attn_core_outputs(results, B, S):
    oa = np.zeros((3, B, S, 8, 65), np.float32)
    ob = np.zeros((B, S, 8, 64), np.float32)
    for c, r in enumerate(results):
        for di, d in enumerate(DILS):
            Ls = S // d
            nt = Ls // 128
            a = r[f"oA{di}"].reshape(B, 128, d, nt, 65).transpose(0, 3, 1, 2, 4).reshape(B, S, 65)
            oa[di, :, :, c, :] = a
        ob[:, :, c, :] = r["oB"].reshape(B, 128, 64, 64).transpose(0, 2, 1, 3).reshape(B, S, 64)
    return oa.reshape(3, B, S, 8 * 65), ob.reshape(B, S, 512)


N_CORES = 8
_PROGS = {}


def _prog(name, fn):
    if name not in _PROGS:
        _PROGS[name] = fn()
    return _PROGS[name]


def _run(nc, in_maps):
    return run_bass_kernel_spmd(nc, in_maps, core_ids=list(range(N_CORES))).results


def _rep(v):
    return np.ascontiguousarray(np.broadcast_to(np.asarray(v, np.float32)[None], (128, 1024)))


def _attn_layer(xf, w_in, w_out, rpb_i, g, b, B, S):
    T = xf.shape[0] // N_CORES
    NT = T // 128
    halves = S // T
    nc1 = _prog('qkv', lambda: build_qkv(NT=NT))
    ims = []
    for c in range(N_CORES):
        p0 = (c % halves) * T
        ims.append({"x": xf[c * T:(c + 1) * T], "win": w_in, "cs": rope_cs(np.arange(p0, p0 + T))})
    r1 = _run(nc1, ims)
    qkv = np.concatenate([r["qkv"] for r in r1], 0).reshape(B, S, 3, 16, 64)
    nc2 = _prog('attn', lambda: build_attn(NB=B))
    r2 = _run(nc2, [attn_core_inputs(qkv, c, rpb_i) for c in range(N_CORES)])
    oa, ob = attn_core_outputs(r2, B, S)
    oa = oa.reshape(3, B * S, 8 * 65)
    ob = ob.reshape(B * S, 512)
    nc3 = _prog('oproj', lambda: build_oproj(NT=NT))
    ims = [{"x": xf[c * T:(c + 1) * T], "oa": np.ascontiguousarray(oa[:, c * T:(c + 1) * T]),
            "ob": np.ascontiguousarray(ob[c * T:(c + 1) * T]), "wout": w_out, "lng": _rep(g), "lnb": _rep(b)}
           for c in range(N_CORES)]
    r3 = _run(nc3, ims)
    return np.concatenate([r["y"] for r in r3], 0)


def _pool_layer(xf, pw, psc, g, b, B, S):
    T = xf.shape[0] // N_CORES
    NT = T // 128
    halves = S // T
    ncp = _prog('pool', lambda: build_pool(NT=NT))
    ims = []
    for c in range(N_CORES):
        h = c % halves
        xp = np.zeros((T + 16, 1024), np.float32)
        lo = c * T - (8 if h > 0 else 0)
        hi = (c + 1) * T + (8 if h < halves - 1 else 0)
        xp[8 - (c * T - lo): 8 + T + (hi - (c + 1) * T)] = xf[lo:hi]
        Bm, Bh = pool_tables(h == 0, h == halves - 1)
        ims.append({"xp": xp, "bm": np.ascontiguousarray(Bm.transpose(2, 0, 1, 3).reshape(128, 12 * 128)),
                    "bh": np.ascontiguousarray(Bh.transpose(2, 0, 1, 3).reshape(16, 12 * 128)),
                    "pw": pw, "psc": _rep(psc), "lng": _rep(g), "lnb": _rep(b)})
    r = _run(ncp, ims)
    return np.concatenate([q["y"] for q in r], 0)


MOE_CAP = 768


def _moe_layer(xf, rw, rbias, w1, w3, w2, g, b):
    T = xf.shape[0] // N_CORES
    NT = T // 128
    ncm = _prog('moe', lambda: build_moe_sparse(NT=NT, C=MOE_CAP, NE=16))
    rb = np.ascontiguousarray(np.broadcast_to(np.tile(np.asarray(rbias, np.float32), NT)[None], (128, NT * 16)))
    eoff = np.ascontiguousarray(np.broadcast_to(np.tile(np.arange(16, dtype=np.float32) * MOE_CAP, NT)[None], (128, NT * 16)))
    ims = [{"x": xf[c * T:(c + 1) * T], "rw": rw, "rbias": rb, "eoff": eoff, "w1": w1, "w3": w3, "w2": w2,
            "lng": _rep(g), "lnb": _rep(b)} for c in range(N_CORES)]
    r = _run(ncm, ims)
    return np.concatenate([q["y"] for q in r], 0)


def kernel(x, w_in, w_out, rpb, pool_w, pool_scale, router_w, router_bias, moe_w1, moe_w3, moe_w2, ln_g, ln_b):
    f = lambda a: np.ascontiguousarray(np.asarray(a, np.float32))
    x = f(x)
    B, S, Dm = x.shape
    xf = x.reshape(B * S, Dm)
    depth = moe_w1.shape[0]
    for layer in range(depth):
        i = layer // 2
        if layer % 2 == 0:
            xf = _attn_layer(xf, f(w_in[i]), f(w_out[i]), f(rpb[i]), ln_g[layer, 0], ln_b[layer, 0], B, S)
        else:
            xf = _pool_layer(xf, f(pool_w[i]), pool_scale[i], ln_g[layer, 0], ln_b[layer, 0], B, S)
        xf = _moe_layer(xf, f(router_w), router_bias, f(moe_w1[layer]), f(moe_w3[layer]), f(moe_w2[layer]),
                        ln_g[layer, 1], ln_b[layer, 1])
    return xf.reshape(B, S, Dm).astype(np.float32)
```

```python
import numpy as np
import concourse.bass as bass
import concourse.mybir as mybir
from concourse.bass_utils import run_bass_kernel_spmd
from contextlib import ExitStack

F32 = mybir.dt.float32
BF16 = mybir.dt.bfloat16
U32 = mybir.dt.uint32
I32 = mybir.dt.int32
AF = mybir.ActivationFunctionType
ALU = mybir.AluOpType
AX = mybir.AxisListType

ENGS = ['pe', 'act', 'dve', 'pool', 'sp']
ENG_ATTR = {'pe': 'tensor', 'act': 'scalar', 'dve': 'vector', 'pool': 'gpsimd', 'sp': 'sync'}


class Tok:
    __slots__ = ('sem', 'val')

    def __init__(self, sem, val):
        self.sem = sem
        self.val = val


class Buf:
    def __init__(self, kb, t, name):
        self.kb = kb
        self.t = t
        self.name = name
        self.w = []
        self.r = {}
        self._slot = None

    def __getitem__(self, k):
        return self.t[k]

    def slot(self):
        if self._slot is None:
            self._slot = self.kb.new_sem()
            self._slotval = 0
        return self._slot


class KB:
    def __init__(self, nc, es, same_eng_sync=True):
        self.nc = nc
        self.es = es
        self.ops = {e: [] for e in ENGS}
        self.nsem = 0
        self.sems = {e: self.new_sem() for e in ENGS}
        self.cnt = {e: 0 for e in ENGS}
        self.waited = {e: {} for e in ENGS}
        self.same_eng_sync = same_eng_sync
        self.nbuf = 0

    def new_sem(self):
        self.nsem += 1
        return self.es.enter_context(self.nc.semaphore(f"sem{self.nsem}"))

    def sb(self, shape, dtype, name=None):
        self.nbuf += 1
        name = name or f"sb{self.nbuf}"
        return Buf(self, self.es.enter_context(self.nc.sbuf_tensor(name, list(shape), dtype)), name)

    def ps(self, shape, dtype=F32, name=None):
        self.nbuf += 1
        name = name or f"ps{self.nbuf}"
        return Buf(self, self.es.enter_context(self.nc.psum_tensor(name, list(shape), dtype)), name)

    def view(self, name="v"):
        return Buf(self, None, name)

    def _waits(self, eng, deps):
        waits = []
        for d in deps:
            if d is None:
                continue
            if isinstance(d, (list, tuple)):
                waits += self._waits(eng, d)
                continue
            if (not self.same_eng_sync) and d.sem is self.sems[eng]:
                continue
            key = id(d.sem)
            if self.waited[eng].get(key, 0) >= d.val:
                continue
            self.waited[eng][key] = d.val
            waits.append((d.sem, d.val))
        return waits

    def _deps(self, reads, writes, deps):
        ds = list(deps)
        for b in reads:
            ds += b.w
        for b in writes:
            ds += b.w
            ds += list(b.r.values())
        return ds

    def _commit(self, tok, reads, writes):
        for b in writes:
            b.w = [tok]
            b.r = {}
        for b in reads:
            if b not in writes:
                o = b.r.get(id(tok.sem))
                if o is None or o.val < tok.val:
                    b.r[id(tok.sem)] = tok

    def op(self, eng, fn, reads=(), writes=(), deps=()):
        waits = self._waits(eng, self._deps(reads, writes, deps))
        self.cnt[eng] += 1
        self.ops[eng].append((waits, fn, (self.sems[eng], 1)))
        tok = Tok(self.sems[eng], self.cnt[eng])
        self._commit(tok, reads, writes)
        return tok

    def group(self, eng, fns, reads=(), writes=(), deps=()):
        waits = self._waits(eng, self._deps(reads, writes, deps))
        for i, fn in enumerate(fns):
            self.cnt[eng] += 1
            self.ops[eng].append((waits if i == 0 else [], fn, (self.sems[eng], 1)))
        tok = Tok(self.sems[eng], self.cnt[eng])
        self._commit(tok, reads, writes)
        return tok

    def dma(self, eng, out, in_, slotbuf, reads=(), writes=(), deps=(), **kw):
        sem = slotbuf.slot()
        ds = list(deps)
        for b in reads:
            ds += b.w
        for b in writes:
            ds += [t for t in b.w if t.sem is not sem]
            ds += list(b.r.values())
        waits = self._waits(eng, ds)
        slotbuf._slotval += 16
        self.ops[eng].append((waits, lambda e: e.dma_start(out=out, in_=in_, **kw), (sem, 16)))
        tok = Tok(sem, slotbuf._slotval)
        self._commit(tok, reads, writes)
        return tok

    def wait_only(self, eng, deps):
        waits = self._waits(eng, deps)
        if waits:
            self.ops[eng].append((waits, None, None))

    def emit(self):
        with self.nc.Block() as block:
            for e in ENGS:
                dec = getattr(block, ENG_ATTR[e])
                ops = self.ops[e]

                def body(eng, ops=ops):
                    for waits, fn, inc in ops:
                        for s, v in waits:
                            eng.wait_ge(s, v)
                        if fn is None:
                            continue
                        inst = fn(eng)
                        if inc is not None:
                            inst.then_inc(inc[0], inc[1])
                dec(body)


def _idma(self, out, in_, idx_ap, scatter, slotbuf, bound, reads=(), writes=(), deps=()):
    sem = slotbuf.slot()
    ds = list(deps)
    for b in reads:
        ds += b.w
    for b in writes:
        ds += [t for t in b.w if t.sem is not sem]
        ds += list(b.r.values())
    waits = self._waits('pool', ds)
    slotbuf._slotval += 16
    if not hasattr(self, '_bregs'):
        self._bregs = {}

    def breg(e):
        if bound not in self._bregs:
            self._bregs[bound] = e.to_reg(bound)
        return self._bregs[bound]
    if scatter:
        fn = lambda e: e.indirect_dma_start(out=out, out_offset=bass.IndirectOffsetOnAxis(ap=idx_ap, axis=0), in_=in_,
                                            in_offset=None, bounds_check=breg(e), oob_is_err=False)
    else:
        fn = lambda e: e.indirect_dma_start(out=out, out_offset=None, in_=in_,
                                            in_offset=bass.IndirectOffsetOnAxis(ap=idx_ap, axis=0), bounds_check=breg(e),
                                            oob_is_err=False)
    self.ops['pool'].append((waits, fn, (sem, 16)))
    tok = Tok(sem, slotbuf._slotval)
    self._commit(tok, reads, writes)
    return tok


KB.idma = _idma


D = 1024
ALPHA = 8.0 ** 0.25
LN_EPS = 1e-5


def make_ident(kb, ident):
    kb.op('pool', lambda e: e.memset(ident[:], 0.0), writes=[ident])
    kb.op('pool', lambda e: e.affine_select(out=ident[:], in_=ident[:], compare_op=ALU.not_equal, fill=1.0,
                                            base=0, pattern=[[-1, 128]], channel_multiplier=1), writes=[ident])


class LNBufs:
    def __init__(self, kb, nbuf=2):
        self.kb = kb
        self.n = nbuf
        self.i = 0
        self.z = [kb.sb([128, D], F32) for _ in range(nbuf)]
        self.o = [kb.sb([128, D], F32) for _ in range(nbuf)]
        self.st = [kb.sb([128, 12], F32) for _ in range(nbuf)]
        self.mv = [kb.sb([128, 2], F32) for _ in range(nbuf)]
        self.rs = [kb.sb([128, 1], F32) for _ in range(nbuf)]
        self.nb = [kb.sb([128, 1], F32) for _ in range(nbuf)]
        self.eps = kb.sb([128, 1], F32)
        kb.op('pool', lambda e: e.memset(self.eps[:], LN_EPS), writes=[self.eps])


def emit_ln1(kb, L, xbuf, x_ap, h_aps, h_bufs):
    i = L.i
    L.i = (L.i + 1) % L.n
    z, st, mv, rs, nb = L.z[i], L.st[i], L.mv[i], L.rs[i], L.nb[i]
    for hf in range(2):
        sl = slice(hf * 512, (hf + 1) * 512)
        kb.op('dve', lambda e, sl=sl, hf=hf: e.scalar_tensor_tensor(out=z[:, sl], in0=x_ap[:, sl], scalar=ALPHA,
                                                                    in1=h_aps[hf], op0=ALU.mult, op1=ALU.add),
              reads=[xbuf, h_bufs[hf]], writes=[z] if hf == 0 else [], deps=z.w if hf == 1 else ())
    z.w = [Tok(kb.sems['dve'], kb.cnt['dve'])]
    for hf in range(2):
        sl = slice(hf * 512, (hf + 1) * 512)
        kb.op('dve', lambda e, sl=sl, hf=hf: e.bn_stats(out=st[:, hf * 6:(hf + 1) * 6], in_=z[:, sl]),
              reads=[z], writes=[st] if hf == 0 else [], deps=st.w if hf == 1 else ())
    st.w = [Tok(kb.sems['dve'], kb.cnt['dve'])]
    kb.op('dve', lambda e: e.bn_aggr(out=mv[:], in_=st[:]), reads=[st], writes=[mv])
    kb.op('act', lambda e: e.activation(out=rs[:], in_=mv[:, 1:2], func=AF.Sqrt, bias=L.eps[:], scale=1.0),
          reads=[mv, L.eps], writes=[rs])
    kb.op('dve', lambda e: e.reciprocal(out=rs[:], in_=rs[:]), reads=[rs], writes=[rs])
    kb.op('dve', lambda e: e.scalar_tensor_tensor(out=nb[:], in0=mv[:, 0:1], scalar=-1.0, in1=rs[:],
                                                  op0=ALU.mult, op1=ALU.mult), reads=[mv, rs], writes=[nb])
    return i


def emit_ln2(kb, L, i, g, b, out_dram_ap, dma_eng='sp', b_eng='pool'):
    z, o, rs, nb = L.z[i], L.o[i], L.rs[i], L.nb[i]
    kb.op('act', lambda e: e.activation(out=o[:], in_=z[:], func=AF.Identity, bias=nb[:], scale=rs[:]),
          reads=[z, rs, nb], writes=[o])
    kb.op('pool', lambda e: e.tensor_tensor(out=o[:], in0=o[:], in1=g[:], op=ALU.mult), reads=[o, g], writes=[o])
    kb.op(b_eng, lambda e: e.tensor_tensor(out=o[:], in0=o[:], in1=b[:], op=ALU.add), reads=[o, b], writes=[o])
    return kb.dma(dma_eng, out_dram_ap, o[:], o, reads=[o])


def emit_ln(kb, L, xbuf, x_ap, h_aps, h_bufs, g, b, out_dram_ap, dma_eng='sp', b_eng='pool'):
    i = emit_ln1(kb, L, xbuf, x_ap, h_aps, h_bufs)
    return emit_ln2(kb, L, i, g, b, out_dram_ap, dma_eng, b_eng)


def skewed(n, front, ln1, ln2):
    toks = []
    slots = {}
    for s in range(n + 2):
        if s < n:
            front(s)
        if 1 <= s <= n:
            slots[s - 1] = ln1(s - 1)
        if s >= 2:
            toks.append(ln2(s - 2, slots.pop(s - 2)))
    return toks


def build_moe(NT=32, GT=8, NE=16, debug=False):
    nc = bass.Bass("TRN2", target_bir_lowering=False)
    T = NT * 128
    x = nc.dram_tensor("x", [T, D], F32, kind="ExternalInput").ap()
    rw = nc.dram_tensor("rw", [D, 16], F32, kind="ExternalInput").ap()
    rbias = nc.dram_tensor("rbias", [128, GT * 16], F32, kind="ExternalInput").ap()
    w1 = nc.dram_tensor("w1", [16, D, D], F32, kind="ExternalInput").ap()
    w3 = nc.dram_tensor("w3", [16, D, D], F32, kind="ExternalInput").ap()
    w2 = nc.dram_tensor("w2", [16, D, D], F32, kind="ExternalInput").ap()
    lng = nc.dram_tensor("lng", [128, D], F32, kind="ExternalInput").ap()
    lnb = nc.dram_tensor("lnb", [128, D], F32, kind="ExternalInput").ap()
    y = nc.dram_tensor("y", [T, D], F32, kind="ExternalOutput").ap()
    if debug:
        gdbg = nc.dram_tensor("gdbg", [NT // GT, 128, GT * 16], F32, kind="ExternalOutput").ap()
    with ExitStack() as es:
        kb = KB(nc, es)
        ident = kb.sb([128, 128], F32)
        make_ident(kb, ident)
        rws = kb.sb([128, 8, 16], F32)
        kb.dma('sp', rws[:], rw.rearrange("(c p) e -> p c e", p=128), rws, writes=[rws])
        rb = kb.sb([128, GT * 16], F32)
        kb.dma('sp', rb[:], rbias[:, :], rb, writes=[rb])
        g = kb.sb([128, D], F32)
        b = kb.sb([128, D], F32)
        kb.dma('sp', g[:], lng[:, :], g, writes=[g])
        kb.dma('sp', b[:], lnb[:, :], b, writes=[b])
        L = LNBufs(kb, nbuf=1)

        xin = [kb.sb([128, D], F32) for _ in range(2)]
        xT32 = [kb.sb([128, 8, 128], F32) for _ in range(2)]
        xT = kb.sb([128, 8, GT * 128], BF16)
        xTv = [kb.view(f"xTv{t}") for t in range(GT)]
        acc = kb.sb([128, GT, D], F32)
        accv = [[kb.view() for _ in range(2)] for t in range(GT)]
        wb = [[kb.sb([128, 8, D], BF16) for _ in range(3)] for _ in range(2)]
        hT = [kb.sb([128, 8, 512], BF16) for _ in range(1)]
        hTv = [[kb.view() for _ in range(8)] for _ in range(1)]
        sil = [kb.sb([128, 512], BF16) for _ in range(2)]
        banks = [kb.ps([128, 512], F32) for _ in range(8)]
        NG = GT * 16
        aff = kb.sb([128, NG], F32)
        affv = [kb.view() for _ in range(GT)]
        S8 = kb.sb([128, GT * 4, 8], F32)
        Pt = kb.sb([128, GT * 4, 6], F32)
        gs = kb.sb([128, GT * 4], F32)
        gmax = kb.sb([128, GT], F32)
        ghot = kb.sb([128, GT * 4], F32)
        c1 = kb.sb([128, GT * 4, 4], F32)
        c2 = kb.sb([128, GT * 4, 4], F32)
        GA = kb.sb([128, GT, 16], F32)
        den = kb.sb([128, GT], F32)
        G = kb.sb([128, GT, 16], F32)

        wsrc = [w1, w3, w2]
        nsteps = (NT // GT) * NE
        step_list = [(gi, e) for gi in range(NT // GT) for e in range(NE)]

        def load_weights(si):
            gi, e = step_list[si]
            slot = si % 2
            for j in range(3):
                kb.dma('pool', wb[slot][j][:], wsrc[j][e].rearrange("(c p) f -> p c f", p=128), wb[slot][j],
                       writes=[wb[slot][j]])

        if nsteps > 0:
            load_weights(0)
        ybank = 0
        out_toks = []
        for gi in range(NT // GT):
            for t in range(GT):
                tile = gi * GT + t
                xi = xin[t % 2]
                x32 = xT32[t % 2]
                kb.dma('sp', xi[:], x[tile * 128:(tile + 1) * 128, :], xi, writes=[xi])
                for hf in range(2):
                    bk = banks[4 + hf]
                    kb.group('pe', [lambda e, c=c, bk=bk, xi=xi: e.transpose(bk[:, (c % 4) * 128:(c % 4 + 1) * 128],
                                                                               xi[:, c * 128:(c + 1) * 128], ident[:])
                                    for c in range(hf * 4, hf * 4 + 4)], reads=[xi, ident], writes=[bk])
                    if True:
                        kb.op('act', lambda e, bk=bk, hf=hf, x32=x32: e.copy(
                            out=x32[:, hf * 4:(hf + 1) * 4, :], in_=bk[:].rearrange("p (c t) -> p c t", c=4)),
                            reads=[bk], writes=[x32] if hf == 0 else [], deps=x32.w if hf == 1 else ())
                    kb.op('dve', lambda e, hf=hf, t=t, x32=x32: e.tensor_copy(
                        out=xT[:, hf * 4:(hf + 1) * 4, t * 128:(t + 1) * 128], in_=x32[:, hf * 4:(hf + 1) * 4, :]),
                        reads=[], writes=[xTv[t]] if hf == 0 else [], deps=list(xTv[t].w if hf == 1 else ()) + [Tok(kb.sems['act'], kb.cnt['act'])])
                x32.w = [Tok(kb.sems['act'], kb.cnt['act'])]
                xTv[t].w = [Tok(kb.sems['dve'], kb.cnt['dve'])]
                x32.r[id(kb.sems['dve'])] = Tok(kb.sems['dve'], kb.cnt['dve'])
                rbk = banks[6]
                kb.group('pe', [lambda e, c=c, x32=x32, rbk=rbk: e.matmul(rbk[:, 0:16], x32[:, c, :], rws[:, c, :],
                                                                         start=(c == 0), stop=(c == 7))
                                for c in range(8)], reads=[x32, rws], writes=[rbk])
                kb.op('act', lambda e, t=t, rbk=rbk: e.activation(out=aff[:, t * 16:(t + 1) * 16], in_=rbk[:, 0:16],
                                                                   func=AF.Sigmoid), reads=[rbk], writes=[affv[t]])
            S4 = S8[:, :, 0:4]
            aff4 = aff[:].rearrange("p (g j) -> p g j", j=4)
            kb.op('dve', lambda e: e.tensor_tensor(out=S8[:, :, 0:4], in0=aff4, in1=rb[:].rearrange("p (g j) -> p g j", j=4),
                                                   op=ALU.add), reads=affv + [rb], writes=[S8])
            kb.op('dve', lambda e: e.tensor_copy(out=S8[:, :, 4:8], in_=S8[:, :, 0:4]), reads=[S8], writes=[S8])
            pairs = [(0, 1), (0, 2), (0, 3), (1, 2), (1, 3), (2, 3)]
            for i, (a, bb) in enumerate(pairs):
                kb.op('dve', lambda e, i=i, a=a, bb=bb: e.tensor_tensor(out=Pt[:, :, i], in0=S8[:, :, a], in1=S8[:, :, bb],
                                                                         op=ALU.add), reads=[S8], writes=[Pt])
            kb.op('dve', lambda e: e.tensor_reduce(out=gs[:], in_=Pt[:], axis=AX.X, op=ALU.max), reads=[Pt], writes=[gs])
            kb.op('dve', lambda e: e.tensor_reduce(out=gmax[:], in_=gs[:].rearrange("p (t g) -> p t g", g=4), axis=AX.X,
                                                   op=ALU.max), reads=[gs], writes=[gmax])
            kb.op('dve', lambda e: e.tensor_tensor(out=ghot[:].rearrange("p (t g) -> p t g", g=4),
                                                   in0=gs[:].rearrange("p (t g) -> p t g", g=4),
                                                   in1=gmax[:].unsqueeze(2).broadcast_to([128, GT, 4]), op=ALU.is_ge),
                  reads=[gs, gmax], writes=[ghot])
            kb.op('dve', lambda e: e.tensor_tensor(out=c1[:], in0=S8[:, :, 0:4], in1=S8[:, :, 1:5], op=ALU.is_gt),
                  reads=[S8], writes=[c1])
            kb.op('dve', lambda e: e.tensor_tensor(out=c2[:], in0=S8[:, :, 0:4], in1=S8[:, :, 2:6], op=ALU.is_gt),
                  reads=[S8], writes=[c2])
            kb.op('dve', lambda e: e.tensor_tensor(out=c1[:], in0=c1[:], in1=c2[:], op=ALU.add), reads=[c1, c2], writes=[c1])
            kb.op('dve', lambda e: e.tensor_tensor(out=c2[:], in0=S8[:, :, 0:4], in1=S8[:, :, 3:7], op=ALU.is_gt),
                  reads=[S8], writes=[c2])
            kb.op('dve', lambda e: e.tensor_tensor(out=c1[:], in0=c1[:], in1=c2[:], op=ALU.add), reads=[c1, c2], writes=[c1])
            kb.op('dve', lambda e: e.scalar_tensor_tensor(out=c1[:], in0=c1[:], scalar=2.0,
                                                          in1=ghot[:].unsqueeze(2).broadcast_to([128, GT * 4, 4]),
                                                          op0=ALU.is_ge, op1=ALU.mult), reads=[c1, ghot], writes=[c1])
            kb.op('dve', lambda e: e.tensor_tensor(out=GA[:].rearrange("p t (g j) -> p (t g) j", j=4), in0=c1[:], in1=aff4,
                                                   op=ALU.mult), reads=[c1] + affv, writes=[GA])
            kb.op('dve', lambda e: e.tensor_reduce(out=den[:], in_=GA[:], axis=AX.X, op=ALU.add), reads=[GA], writes=[den])
            kb.op('dve', lambda e: e.reciprocal(out=den[:], in_=den[:]), reads=[den], writes=[den])
            kb.op('dve', lambda e: e.tensor_tensor(out=G[:], in0=GA[:], in1=den[:].unsqueeze(2).broadcast_to([128, GT, 16]),
                                                   op=ALU.mult), reads=[GA, den], writes=[G])
            if debug:
                out_toks.append(kb.dma('sp', gdbg[gi], G[:].rearrange("p t e -> p (t e)"), G, reads=[G]))
            for e_i in range(NE):
                si = gi * NE + e_i
                slot = si % 2
                if si + 1 < nsteps:
                    load_weights(si + 1)
                w1b, w3b, w2b = wb[slot]
                for tt in range(GT // 4):
                    hb = hT[0]
                    hv = hTv[0]
                    tsl = slice(tt * 512, (tt + 1) * 512)
                    for fc in range(8):
                        b1 = banks[fc % 2]
                        b3 = banks[2 + fc % 2]
                        sl_ = sil[fc % 2]
                        fsl = slice(fc * 128, (fc + 1) * 128)
                        kb.group('pe', [lambda e, c=c, b1=b1, fsl=fsl, w1b=w1b, tsl=tsl: e.matmul(b1[:], w1b[:, c, fsl], xT[:, c, tsl],
                                                                                 start=(c == 0), stop=(c == 7))
                                        for c in range(8)], reads=[w1b] + xTv[tt * 4:(tt + 1) * 4], writes=[b1])
                        kb.group('pe', [lambda e, c=c, b3=b3, fsl=fsl, w3b=w3b, tsl=tsl: e.matmul(b3[:], w3b[:, c, fsl], xT[:, c, tsl],
                                                                                 start=(c == 0), stop=(c == 7))
                                        for c in range(8)], reads=[w3b] + xTv[tt * 4:(tt + 1) * 4], writes=[b3])
                        kb.op('act', lambda e, b1=b1, sl_=sl_: e.activation(out=sl_[:], in_=b1[:], func=AF.Silu),
                              reads=[b1], writes=[sl_])
                        kb.op('dve', lambda e, b3=b3, sl_=sl_, fc=fc, hb=hb: e.tensor_tensor(out=hb[:, fc, :], in0=b3[:],
                                                                                          in1=sl_[:], op=ALU.mult),
                              reads=[b3, sl_], writes=[hv[fc]])
                    for t4 in range(4):
                        t = tt * 4 + t4
                        for oh in range(2):
                            yb = banks[4 + ybank % 4]
                            ybank += 1
                            osl = slice(oh * 512, (oh + 1) * 512)
                            kb.group('pe', [lambda e, fc=fc, yb=yb, t4=t4, osl=osl, hb=hb, w2b=w2b: e.matmul(
                                yb[:], hb[:, fc, t4 * 128:(t4 + 1) * 128], w2b[:, fc, osl], start=(fc == 0), stop=(fc == 7))
                                for fc in range(8)], reads=[w2b] + hv, writes=[yb])
                            av = accv[t][oh]
                            if e_i == 0:
                                kb.op('dve', lambda e, yb=yb, t=t, osl=osl, e_i=e_i: e.tensor_scalar(
                                    out=acc[:, t, osl], in0=yb[:], scalar1=G[:, t, e_i:e_i + 1], scalar2=None, op0=ALU.mult),
                                    reads=[yb, G], writes=[av])
                            else:
                                kb.op('dve', lambda e, yb=yb, t=t, osl=osl, e_i=e_i: e.scalar_tensor_tensor(
                                    out=acc[:, t, osl], in0=yb[:], scalar=G[:, t, e_i:e_i + 1], in1=acc[:, t, osl],
                                    op0=ALU.mult, op1=ALU.add), reads=[yb, G], writes=[av])
            for t in range(GT):
                tile = gi * GT + t
                xi = xin[t % 2]
                kb.dma('sp', xi[:], x[tile * 128:(tile + 1) * 128, :], xi, writes=[xi])
                out_toks.append(emit_ln(kb, L, xi, xi, [acc[:, t, 0:512], acc[:, t, 512:1024]], accv[t], g, b,
                                        y[tile * 128:(tile + 1) * 128, :]))
        kb.wait_only('sp', out_toks)
        kb.emit()
    return nc


BIGIDX = 16.0 * 1024 + 7.0
BIG2 = 65536.0


def build_moe_sparse(NT=32, C=768, NE=16, debug=False):
    nc = bass.Bass("TRN2", target_bir_lowering=False)
    T = NT * 128
    NS = C // 128
    NG = NT * 16
    x = nc.dram_tensor("x", [T, D], F32, kind="ExternalInput").ap()
    rw = nc.dram_tensor("rw", [D, 16], F32, kind="ExternalInput").ap()
    rbias = nc.dram_tensor("rbias", [128, NG], F32, kind="ExternalInput").ap()
    eoffd = nc.dram_tensor("eoff", [128, NG], F32, kind="ExternalInput").ap()
    w1 = nc.dram_tensor("w1", [16, D, D], F32, kind="ExternalInput").ap()
    w3 = nc.dram_tensor("w3", [16, D, D], F32, kind="ExternalInput").ap()
    w2 = nc.dram_tensor("w2", [16, D, D], F32, kind="ExternalInput").ap()
    lng = nc.dram_tensor("lng", [128, D], F32, kind="ExternalInput").ap()
    lnb = nc.dram_tensor("lnb", [128, D], F32, kind="ExternalInput").ap()
    y = nc.dram_tensor("y", [T, D], F32, kind="ExternalOutput").ap()
    xs = nc.dram_tensor("xs_scr", [16 * C, D], F32, kind="Internal").ap()
    ys = nc.dram_tensor("ys_scr", [16 * C, D], F32, kind="Internal").ap()
    if debug:
        dbg = nc.dram_tensor("dbg", [4, 128, NT], F32, kind="ExternalOutput").ap()
    with ExitStack() as es:
        kb = KB(nc, es)
        ident = kb.sb([128, 128], F32)
        make_ident(kb, ident)
        ltri = kb.sb([128, 128], F32)
        ones = kb.sb([128, 128], F32)
        kb.op('pool', lambda e: e.memset(ones[:], 1.0), writes=[ones])
        kb.op('pool', lambda e: e.memset(ltri[:], 1.0), writes=[ltri])
        kb.op('pool', lambda e: e.affine_select(out=ltri[:], in_=ltri[:], compare_op=ALU.is_gt, fill=0.0, base=0,
                                                pattern=[[1, 128]], channel_multiplier=-1), writes=[ltri])
        rws = kb.sb([128, 8, 16], F32)
        kb.dma('sp', rws[:], rw.rearrange("(c p) e -> p c e", p=128), rws, writes=[rws])
        rb = kb.sb([128, NG], F32)
        kb.dma('sp', rb[:], rbias[:, :], rb, writes=[rb])
        eoff = kb.sb([128, NG], F32)
        kb.dma('sp', eoff[:], eoffd[:, :], eoff, writes=[eoff])
        g = kb.sb([128, D], F32)
        b = kb.sb([128, D], F32)
        kb.dma('sp', g[:], lng[:, :], g, writes=[g])
        kb.dma('sp', b[:], lnb[:, :], b, writes=[b])
        L = LNBufs(kb, nbuf=1)
        xin = [kb.sb([128, D], F32) for _ in range(2)]
        x32 = [kb.sb([128, 8, 128], F32) for _ in range(2)]
        xT = kb.sb([128, 8, C], BF16)
        xTv = [kb.view() for _ in range(NS)]
        w13 = [[kb.sb([128, 8, D], BF16) for _ in range(2)] for _ in range(2)]
        w2b = kb.sb([128, 8, D], BF16)
        hT = kb.sb([128, 8, 512], BF16)
        hv = [kb.view() for _ in range(8)]
        sil = [kb.sb([128, 512], BF16) for _ in range(2)]
        banks = [kb.ps([128, 512], F32) for _ in range(8)]
        xsb = kb.view("xs")
        ysb = kb.view("ys")
        wsrc = [w1, w3]

        def load_w13(e):
            for j in range(2):
                kb.dma('pool', w13[e % 2][j][:], wsrc[j][e].rearrange("(c p) f -> p c f", p=128), w13[e % 2][j],
                       writes=[w13[e % 2][j]])

        def load_w2(e):
            kb.dma('pool', w2b[:], w2[e].rearrange("(c p) f -> p c f", p=128), w2b, writes=[w2b])

        if NE > 0:
            load_w13(0)
            load_w2(0)

        aff = kb.sb([128, NG], F32)
        affv = [kb.view() for _ in range(NT)]
        for t in range(NT):
            xi = xin[t % 2]
            x3 = x32[t % 2]
            kb.dma('sp', xi[:], x[t * 128:(t + 1) * 128, :], xi, writes=[xi])
            for hf in range(2):
                bk = banks[4 + hf]
                kb.group('pe', [lambda e, c=c, bk=bk, xi=xi: e.transpose(bk[:, (c % 4) * 128:(c % 4 + 1) * 128],
                                                                           xi[:, c * 128:(c + 1) * 128], ident[:])
                                for c in range(hf * 4, hf * 4 + 4)], reads=[xi, ident], writes=[bk])
                tk = kb.op('act', lambda e, bk=bk, hf=hf, x3=x3: e.copy(out=x3[:, hf * 4:(hf + 1) * 4, :],
                                                                         in_=bk[:].rearrange("p (c t) -> p c t", c=4)),
                           reads=[bk], writes=[x3] if hf == 0 else [], deps=x3.w if hf == 1 else ())
            x3.w = [tk]
            rbk = banks[6 + t % 2]
            kb.group('pe', [lambda e, c=c, x3=x3, rbk=rbk: e.matmul(rbk[:, 0:16], x3[:, c, :], rws[:, c, :],
                                                                   start=(c == 0), stop=(c == 7)) for c in range(8)],
                     reads=[x3, rws], writes=[rbk])
            kb.op('act', lambda e, t=t, rbk=rbk: e.activation(out=aff[:, t * 16:(t + 1) * 16], in_=rbk[:, 0:16], func=AF.Sigmoid),
                  reads=[rbk], writes=[affv[t]])
        S8 = kb.sb([128, NT * 4, 8], F32)
        Pt = kb.sb([128, NT * 4, 6], F32)
        gs = kb.sb([128, NT * 4], F32)
        gmax = kb.sb([128, NT], F32)
        ghot = kb.sb([128, NT * 4], F32)
        c1 = kb.sb([128, NT * 4, 4], F32)
        c2 = kb.sb([128, NT * 4, 4], F32)
        GA = kb.sb([128, NT, 16], F32)
        den = kb.sb([128, NT], F32)
        G = kb.sb([128, NT, 16], F32)
        aff4 = aff[:].rearrange("p (g j) -> p g j", j=4)

        def dv(fn, reads, writes):
            return kb.op('dve', fn, reads=reads, writes=writes)
        dv(lambda e: e.tensor_tensor(out=S8[:, :, 0:4], in0=aff4, in1=rb[:].rearrange("p (g j) -> p g j", j=4), op=ALU.add), affv + [rb], [S8])
        dv(lambda e: e.tensor_copy(out=S8[:, :, 4:8], in_=S8[:, :, 0:4]), [S8], [S8])
        for i, (a, bb) in enumerate([(0, 1), (0, 2), (0, 3), (1, 2), (1, 3), (2, 3)]):
            dv(lambda e, i=i, a=a, bb=bb: e.tensor_tensor(out=Pt[:, :, i], in0=S8[:, :, a], in1=S8[:, :, bb], op=ALU.add), [S8], [Pt])
        dv(lambda e: e.tensor_reduce(out=gs[:], in_=Pt[:], axis=AX.X, op=ALU.max), [Pt], [gs])
        dv(lambda e: e.tensor_reduce(out=gmax[:], in_=gs[:].rearrange("p (t g) -> p t g", g=4), axis=AX.X, op=ALU.max), [gs], [gmax])
        dv(lambda e: e.tensor_tensor(out=ghot[:].rearrange("p (t g) -> p t g", g=4), in0=gs[:].rearrange("p (t g) -> p t g", g=4),
                                     in1=gmax[:].unsqueeze(2).broadcast_to([128, NT, 4]), op=ALU.is_ge), [gs, gmax], [ghot])
        dv(lambda e: e.tensor_tensor(out=c1[:], in0=S8[:, :, 0:4], in1=S8[:, :, 1:5], op=ALU.is_gt), [S8], [c1])
        dv(lambda e: e.tensor_tensor(out=c2[:], in0=S8[:, :, 0:4], in1=S8[:, :, 2:6], op=ALU.is_gt), [S8], [c2])
        dv(lambda e: e.tensor_tensor(out=c1[:], in0=c1[:], in1=c2[:], op=ALU.add), [c1, c2], [c1])
        dv(lambda e: e.tensor_tensor(out=c2[:], in0=S8[:, :, 0:4], in1=S8[:, :, 3:7], op=ALU.is_gt), [S8], [c2])
        dv(lambda e: e.tensor_tensor(out=c1[:], in0=c1[:], in1=c2[:], op=ALU.add), [c1, c2], [c1])
        dv(lambda e: e.scalar_tensor_tensor(out=c1[:], in0=c1[:], scalar=2.0, in1=ghot[:].unsqueeze(2).broadcast_to([128, NT * 4, 4]),
                                            op0=ALU.is_ge, op1=ALU.mult), [c1, ghot], [c1])
        dv(lambda e: e.tensor_tensor(out=GA[:].rearrange("p t (g j) -> p (t g) j", j=4), in0=c1[:], in1=aff4, op=ALU.mult), [c1] + affv, [GA])
        dv(lambda e: e.tensor_reduce(out=den[:], in_=GA[:], axis=AX.X, op=ALU.add), [GA], [den])
        dv(lambda e: e.reciprocal(out=den[:], in_=den[:]), [den], [den])
        dv(lambda e: e.tensor_tensor(out=G[:], in0=GA[:], in1=den[:].unsqueeze(2).broadcast_to([128, NT, 16]), op=ALU.mult), [GA, den], [G])
        chs = c1
        chs3 = chs[:].rearrange("p (t g) j -> p t (g j)", g=4)
        CS = kb.sb([128, NT, 16], F32)
        dv(lambda e: e.memset(CS[:, 0, :], 0.0), [], [CS])
        for t in range(1, NT):
            dv(lambda e, t=t: e.tensor_tensor(out=CS[:, t, :], in0=CS[:, t - 1, :], in1=chs3[:, t - 1, :], op=ALU.add), [chs], [CS])
        rkb = banks[4]
        fns = []
        for t in range(NT):
            fns.append(lambda e, t=t: e.matmul(rkb[:, t * 16:(t + 1) * 16], ltri[:], chs3[:, t, :], start=True, stop=False))
            fns.append(lambda e, t=t: e.matmul(rkb[:, t * 16:(t + 1) * 16], ones[:], CS[:, t, :], start=False, stop=True))
        kb.group('pe', fns, reads=[ltri, ones, chs, CS], writes=[rkb])
        rank = kb.sb([128, NG], F32)
        sel = kb.sb([128, NG], F32)
        slotv = kb.sb([128, NT, 16], F32)
        slot2 = kb.sb([128, NT, 16], F32)
        Gv = S8
        Gv = kb.sb([128, NT, 16], F32)
        lo = kb.sb([128, NT], F32)
        hi = kb.sb([128, NT], F32)
        glo = kb.sb([128, NT], F32)
        ghi = kb.sb([128, NT], F32)
        lo_t = [kb.sb([128, 1], I32) for _ in range(NT)]
        hi_t = [kb.sb([128, 1], I32) for _ in range(NT)]
        chs_f = chs[:].rearrange("p a j -> p (a j)")
        dv(lambda e: e.tensor_copy(out=rank[:], in_=rkb[:, 0:NG]), [rkb], [rank])
        dv(lambda e: e.scalar_tensor_tensor(out=sel[:], in0=rank[:], scalar=float(C), in1=chs_f, op0=ALU.is_lt, op1=ALU.mult), [rank, chs], [sel])
        dv(lambda e: e.scalar_tensor_tensor(out=rank[:], in0=rank[:], scalar=-BIGIDX, in1=eoff[:], op0=ALU.add, op1=ALU.add), [rank, eoff], [rank])
        dv(lambda e: e.tensor_tensor(out=rank[:], in0=rank[:], in1=sel[:], op=ALU.mult), [rank, sel], [rank])
        dv(lambda e: e.tensor_scalar(out=slotv[:].rearrange("p t e -> p (t e)"), in0=rank[:], scalar1=BIGIDX, scalar2=None, op0=ALU.add), [rank], [slotv])
        dv(lambda e: e.tensor_tensor(out=Gv[:].rearrange("p t e -> p (t e)"), in0=G[:].rearrange("p t e -> p (t e)"), in1=sel[:], op=ALU.mult), [G, sel], [Gv])
        dv(lambda e: e.tensor_reduce(out=lo[:], in_=slotv[:], axis=AX.X, op=ALU.min), [slotv], [lo])
        dv(lambda e: e.tensor_tensor(out=slot2[:], in0=slotv[:], in1=lo[:].unsqueeze(2).broadcast_to([128, NT, 16]), op=ALU.is_equal), [slotv, lo], [slot2])
        dv(lambda e: e.tensor_tensor(out=GA[:], in0=Gv[:], in1=slot2[:], op=ALU.mult), [Gv, slot2], [GA])
        dv(lambda e: e.tensor_reduce(out=glo[:], in_=GA[:], axis=AX.X, op=ALU.add), [GA], [glo])
        dv(lambda e: e.scalar_tensor_tensor(out=slot2[:], in0=slot2[:], scalar=BIG2, in1=slotv[:], op0=ALU.mult, op1=ALU.add), [slot2, slotv], [slot2])
        dv(lambda e: e.tensor_reduce(out=hi[:], in_=slot2[:], axis=AX.X, op=ALU.min), [slot2], [hi])
        dv(lambda e: e.tensor_tensor(out=slot2[:], in0=slot2[:], in1=hi[:].unsqueeze(2).broadcast_to([128, NT, 16]), op=ALU.is_equal), [slot2, hi], [slot2])
        dv(lambda e: e.tensor_tensor(out=GA[:], in0=Gv[:], in1=slot2[:], op=ALU.mult), [Gv, slot2], [GA])
        dv(lambda e: e.tensor_reduce(out=ghi[:], in_=GA[:], axis=AX.X, op=ALU.add), [GA], [ghi])
        for t in range(NT):
            dv(lambda e, t=t: e.tensor_copy(out=lo_t[t][:], in_=lo[:, t:t + 1]), [lo], [lo_t[t]])
            dv(lambda e, t=t: e.tensor_copy(out=hi_t[t][:], in_=hi[:, t:t + 1]), [hi], [hi_t[t]])
        out_toks = []
        if debug:
            for k_, src in enumerate([lo, hi, glo, ghi]):
                out_toks.append(kb.dma('sp', dbg[k_], src[:], src, reads=[src]))
        bound = 16 * C - 1
        sc_toks = []
        for t in range(NT):
            xi = xin[t % 2]
            kb.dma('sp', xi[:], x[t * 128:(t + 1) * 128, :], xi, writes=[xi])
            kb.idma(xs[:, :], xi[:, :], lo_t[t][:, :], True, xsb, bound, reads=[xi, lo_t[t]], writes=[xsb])
            tk_s = kb.idma(xs[:, :], xi[:, :], hi_t[t][:, :], True, xsb, bound, reads=[xi, hi_t[t]], writes=[xsb])
            sc_toks.append(tk_s)
            if len(sc_toks) >= 3:
                kb.wait_only('pool', [sc_toks[-3]])
        ybank = 0
        chunks = []
        o_ = 0
        while o_ < C:
            wdt = min(512, C - o_)
            chunks.append((o_, wdt))
            o_ += wdt
        for e_i in range(NE):
            if e_i + 1 < NE:
                load_w13(e_i + 1)
            w1b, w3b = w13[e_i % 2]
            for s in range(NS):
                xi = xin[s % 2]
                r0 = e_i * C + s * 128
                kb.dma('sp', xi[:], xs[r0:r0 + 128, :], xi, reads=[xsb], writes=[xi])
                for hf in range(2):
                    bk = banks[4 + (ybank % 4)]
                    ybank += 1
                    kb.group('pe', [lambda e, c=c, bk=bk, xi=xi: e.transpose(bk[:, (c % 4) * 128:(c % 4 + 1) * 128],
                                                                               xi[:, c * 128:(c + 1) * 128], ident[:])
                                    for c in range(hf * 4, hf * 4 + 4)], reads=[xi, ident], writes=[bk])
                    if hf == 0:
                        ta = kb.op('act', lambda e, bk=bk, s=s: e.copy(out=xT[:, 0:4, s * 128:(s + 1) * 128],
                                                                       in_=bk[:].rearrange("p (c t) -> p c t", c=4)),
                                   reads=[bk], writes=[xTv[s]])
                    else:
                        tb = kb.op('dve', lambda e, bk=bk, s=s: e.tensor_copy(out=xT[:, 4:8, s * 128:(s + 1) * 128],
                                                                              in_=bk[:].rearrange("p (c t) -> p c t", c=4)),
                                   reads=[bk], deps=[ta])
                        xTv[s].w = [ta, tb]
            for (c0, wdt) in chunks:
                tsl = slice(c0, c0 + wdt)
                tv = xTv[c0 // 128:(c0 + wdt) // 128]
                for fc in range(8):
                    b1 = banks[fc % 2]
                    b3 = banks[2 + fc % 2]
                    sl_ = sil[fc % 2]
                    fsl = slice(fc * 128, (fc + 1) * 128)
                    kb.group('pe', [lambda e, c=c, b1=b1, fsl=fsl, w1b=w1b, tsl=tsl, wdt=wdt: e.matmul(
                        b1[:, 0:wdt], w1b[:, c, fsl], xT[:, c, tsl], start=(c == 0), stop=(c == 7)) for c in range(8)],
                        reads=[w1b] + tv, writes=[b1])
                    kb.group('pe', [lambda e, c=c, b3=b3, fsl=fsl, w3b=w3b, tsl=tsl, wdt=wdt: e.matmul(
                        b3[:, 0:wdt], w3b[:, c, fsl], xT[:, c, tsl], start=(c == 0), stop=(c == 7)) for c in range(8)],
                        reads=[w3b] + tv, writes=[b3])
                    kb.op('act', lambda e, b1=b1, sl_=sl_, wdt=wdt: e.activation(out=sl_[:, 0:wdt], in_=b1[:, 0:wdt], func=AF.Silu),
                          reads=[b1], writes=[sl_])
                    kb.op('dve', lambda e, b3=b3, sl_=sl_, fc=fc, wdt=wdt: e.tensor_tensor(out=hT[:, fc, 0:wdt], in0=b3[:, 0:wdt],
                                                                                           in1=sl_[:, 0:wdt], op=ALU.mult),
                          reads=[b3, sl_], writes=[hv[fc]])
                for t4 in range(wdt // 128):
                    s = c0 // 128 + t4
                    yst = x32[s % 2]
                    ysf = yst[:].rearrange("p c t -> p (c t)")
                    for oh in range(2):
                        yb = banks[4 + ybank % 4]
                        ybank += 1
                        osl = slice(oh * 512, (oh + 1) * 512)
                        kb.group('pe', [lambda e, fc=fc, yb=yb, t4=t4, osl=osl: e.matmul(
                            yb[:], hT[:, fc, t4 * 128:(t4 + 1) * 128], w2b[:, fc, osl], start=(fc == 0), stop=(fc == 7))
                            for fc in range(8)], reads=[w2b] + hv, writes=[yb])
                        if oh == 0:
                            t0_ = kb.op('act', lambda e, yb=yb, ysf=ysf, osl=osl: e.copy(out=ysf[:, osl], in_=yb[:]), reads=[yb], writes=[yst])
                        else:
                            t1_ = kb.op('dve', lambda e, yb=yb, ysf=ysf, osl=osl: e.tensor_copy(out=ysf[:, osl], in_=yb[:]), reads=[yb], deps=[t0_])
                            yst.w = [t0_, t1_]
                    r0 = e_i * C + s * 128
                    kb.dma('act', ys[r0:r0 + 128, :], ysf, ysb, reads=[yst], writes=[ysb])
            if e_i + 1 < NE:
                load_w2(e_i + 1)
        pe_done = Tok(kb.sems['pe'], kb.cnt['pe'])
        tiles = []
        for sl_ in range(2):
            for j in range(2):
                flat = w13[sl_][j][:].rearrange("p c f -> p (c f)").bitcast(F32)
                for q_ in range(4):
                    bf_ = Buf(kb, flat[:, q_ * D:(q_ + 1) * D], f"alias{sl_}{j}{q_}")
                    bf_.r = {id(pe_done.sem): pe_done}
                    bf_.w = list(w13[sl_][j].w)
                    tiles.append(bf_)
        ga2, gb2, acc2, xin3 = tiles[0:2] + tiles[12:13], tiles[2:4] + tiles[13:14], tiles[4:6], tiles[6:8] + xin
        L3 = LNBufs.__new__(LNBufs)
        L3.kb, L3.n, L3.i = kb, 2, 0
        L3.z, L3.o = tiles[8:10], tiles[10:12]
        L3.st = [kb.sb([128, 12], F32) for _ in range(2)]
        L3.mv = [kb.sb([128, 2], F32) for _ in range(2)]
        L3.rs = [kb.sb([128, 1], F32) for _ in range(2)]
        L3.nb = [kb.sb([128, 1], F32) for _ in range(2)]
        L3.eps = L.eps
        for bf_ in ga2 + gb2:
            kb.op('dve', lambda e, bf_=bf_: e.memset(bf_[:], 0.0), writes=[bf_])
        def fetch(t):
            xi = xin3[t % 4]
            kb.dma('sp' if t % 2 == 0 else 'act', xi[:], x[t * 128:(t + 1) * 128, :], xi, writes=[xi])
            kb.idma(ga2[t % 3][:, :], ys[:, :], lo_t[t][:, :], False, ga2[t % 3], bound, reads=[ysb, lo_t[t]], writes=[ga2[t % 3]])
            kb.idma(gb2[t % 3][:, :], ys[:, :], hi_t[t][:, :], False, gb2[t % 3], bound, reads=[ysb, hi_t[t]], writes=[gb2[t % 3]])

        fetch(0)
        fetch(1)
        avs = {}

        def front3(t):
            ga_, gb3, acc = ga2[t % 3], gb2[t % 3], acc2[t % 2]
            if t + 2 < NT:
                fetch(t + 2)
            kb.op('act', lambda e: e.activation(out=acc[:], in_=ga_[:], func=AF.Copy, scale=glo[:, t:t + 1]),
                  reads=[ga_, glo], writes=[acc])
            av = [kb.view(), kb.view()]
            for hf in range(2):
                sl = slice(hf * 512, (hf + 1) * 512)
                kb.op('dve', lambda e, sl=sl: e.scalar_tensor_tensor(
                    out=acc[:, sl], in0=gb3[:, sl], scalar=ghi[:, t:t + 1], in1=acc[:, sl], op0=ALU.mult, op1=ALU.add),
                    reads=[gb3, ghi, acc], writes=[av[hf]])
            avs[t] = av

        def ln1_3(t):
            xi, acc, av = xin3[t % 4], acc2[t % 2], avs.pop(t)
            slot = emit_ln1(kb, L3, xi, xi, [acc[:, 0:512], acc[:, 512:1024]], av)
            acc.r.update(av[0].r)
            acc.r.update(av[1].r)
            return slot

        def ln2_3(t, slot):
            return emit_ln2(kb, L3, slot, g, b, y[t * 128:(t + 1) * 128, :], dma_eng='sp' if t % 2 == 0 else 'act', b_eng='dve')
        out_toks += skewed(NT, front3, ln1_3, ln2_3)
        kb.wait_only('sp', out_toks)
        kb.emit()
    return nc


def pool_tables(first_is_start, last_is_end):
    Bm = np.zeros((3, 4, 128, 128), np.float32)
    Bh = np.zeros((3, 4, 16, 128), np.float32)
    for var in range(3):
        start_edge = (var == 0 and first_is_start)
        end_edge = (var == 2 and last_is_end)
        for g, w in enumerate((2, 4, 8, 16)):
            half = w // 2
            for t in range(128):
                lo, hi = t - half, t + half - 1
                if start_edge:
                    lo = max(lo, 0)
                if end_edge:
                    hi = min(hi, 127)
                cnt = hi - lo + 1
                for s in range(lo, hi + 1):
                    if 0 <= s < 128:
                        Bm[var, g, s, t] += 1.0 / cnt
                    elif s < 0:
                        Bh[var, g, 8 + s, t] += 1.0 / cnt
                    else:
                        Bh[var, g, 8 + (s - 128), t] += 1.0 / cnt
                Bm[var, g, t, t] -= 1.0
    return Bm, Bh


def build_pool(NT=32):
    nc = bass.Bass("TRN2", target_bir_lowering=False)
    T = NT * 128
    xp = nc.dram_tensor("xp", [T + 16, D], F32, kind="ExternalInput").ap()
    bm = nc.dram_tensor("bm", [128, 12 * 128], F32, kind="ExternalInput").ap()
    bh = nc.dram_tensor("bh", [16, 12 * 128], F32, kind="ExternalInput").ap()
    pw = nc.dram_tensor("pw", [4, 256, 256], F32, kind="ExternalInput").ap()
    psc = nc.dram_tensor("psc", [128, D], F32, kind="ExternalInput").ap()
    lng = nc.dram_tensor("lng", [128, D], F32, kind="ExternalInput").ap()
    lnb = nc.dram_tensor("lnb", [128, D], F32, kind="ExternalInput").ap()
    y = nc.dram_tensor("y", [T, D], F32, kind="ExternalOutput").ap()
    with ExitStack() as es:
        kb = KB(nc, es)
        g = kb.sb([128, D], F32)
        b = kb.sb([128, D], F32)
        kb.dma('sp', g[:], lng[:, :], g, writes=[g])
        kb.dma('sp', b[:], lnb[:, :], b, writes=[b])
        L = LNBufs(kb, nbuf=2)
        Bm = kb.sb([128, 12 * 128], BF16)
        Bh = kb.sb([16, 12 * 128], BF16)
        kb.dma('pool', Bm[:], bm[:, :], Bm, writes=[Bm])
        kb.dma('pool', Bh[:], bh[:, :], Bh, writes=[Bh])
        W32 = kb.sb([128, 4, 2, 256], F32)
        sc = kb.sb([128, D], F32)
        Wb = kb.sb([128, 4, 2, 256], BF16)
        kb.dma('sp', W32[:], pw.rearrange("g (hh p) e -> p g hh e", p=128), W32, writes=[W32])
        kb.dma('sp', sc[:], psc[:, :], sc, writes=[sc])
        kb.op('dve', lambda e: e.tensor_tensor(out=Wb[:], in0=W32[:],
                                               in1=sc[:].rearrange("p (g e) -> p g e", g=4).unsqueeze(2).broadcast_to([128, 4, 2, 256]),
                                               op=ALU.mult), reads=[W32, sc], writes=[Wb])
        xin = [kb.sb([128, D], F32) for _ in range(3)]
        xh = [kb.sb([16, D], F32) for _ in range(2)]
        xb = [kb.sb([128, D], BF16) for _ in range(2)]
        xhb = [kb.sb([16, D], BF16) for _ in range(2)]
        uT = [kb.sb([128, 8, 128], BF16) for _ in range(2)]
        uTa = [kb.view() for _ in range(2)]
        uTb = [kb.view() for _ in range(2)]
        pu = [[kb.ps([128, 512], F32) for _ in range(2)] for _ in range(2)]
        py = [[kb.ps([128, 512], F32) for _ in range(2)] for _ in range(2)]
        def front(i):
            var = 0 if i == 0 else (2 if i == NT - 1 else 1)
            xi = xin[i % 3]
            xhi = xh[i % 2]
            xbi = xb[i % 2]
            xhbi = xhb[i % 2]
            u = uT[i % 2]
            ua, ub = uTa[i % 2], uTb[i % 2]
            pui = pu[i % 2]
            pyi = py[i % 2]
            kb.dma('sp', xi[:], xp[8 + i * 128: 8 + (i + 1) * 128, :], xi, writes=[xi])
            kb.dma('sp', xhi[0:8, :], xp[i * 128: i * 128 + 8, :], xhi, writes=[xhi])
            kb.dma('sp', xhi[8:16, :], xp[8 + (i + 1) * 128: 16 + (i + 1) * 128, :], xhi, writes=[xhi])
            kb.op('act', lambda e, xi=xi, xbi=xbi: e.copy(out=xbi[:], in_=xi[:]), reads=[xi], writes=[xbi])
            kb.op('dve', lambda e, xhi=xhi, xhbi=xhbi: e.tensor_copy(out=xhbi[:], in_=xhi[:]), reads=[xhi], writes=[xhbi])
            for hf in range(2):
                fns = []
                for fc in range(hf * 4, hf * 4 + 4):
                    col = (var * 4 + fc // 2) * 128
                    fns.append(lambda e, fc=fc, col=col, xbi=xbi, bk=pui[hf]: e.matmul(
                        bk[:, (fc % 4) * 128:(fc % 4 + 1) * 128], xbi[:, fc * 128:(fc + 1) * 128], Bm[:, col:col + 128],
                        start=True, stop=False))
                    fns.append(lambda e, fc=fc, col=col, xhbi=xhbi, bk=pui[hf]: e.matmul(
                        bk[:, (fc % 4) * 128:(fc % 4 + 1) * 128], xhbi[:, fc * 128:(fc + 1) * 128], Bh[:, col:col + 128],
                        start=False, stop=True))
                kb.group('pe', fns, reads=[xbi, xhbi, Bm, Bh], writes=[pui[hf]])
            kb.op('act', lambda e, u=u, bk=pui[0]: e.copy(out=u[:, 0:4, :], in_=bk[:].rearrange("p (c t) -> p c t", c=4)),
                  reads=[pui[0]], writes=[ua])
            kb.op('dve', lambda e, u=u, bk=pui[1]: e.tensor_copy(out=u[:, 4:8, :], in_=bk[:].rearrange("p (c t) -> p c t", c=4)),
                  reads=[pui[1]], writes=[ub])
            for bkidx in range(2):
                fns = []
                for gg in range(bkidx * 2, bkidx * 2 + 2):
                    for hh in range(2):
                        fns.append(lambda e, gg=gg, hh=hh, u=u, bk=pyi[bkidx]: e.matmul(
                            bk[:, (gg % 2) * 256:(gg % 2 + 1) * 256], u[:, gg * 2 + hh, :], Wb[:, gg, hh, :],
                            start=(hh == 0), stop=(hh == 1)))
                kb.group('pe', fns, reads=[ua if bkidx == 0 else ub, Wb], writes=[pyi[bkidx]])

        def ln1(i):
            xi, pyi = xin[i % 3], py[i % 2]
            return emit_ln1(kb, L, xi, xi, [pyi[0][:], pyi[1][:]], pyi)

        def ln2(i, slot):
            return emit_ln2(kb, L, slot, g, b, y[i * 128:(i + 1) * 128, :], dma_eng='sp' if i % 2 == 0 else 'act')
        out_toks = skewed(NT, front, ln1, ln2)
        kb.wait_only('sp', out_toks)
        kb.emit()
    return nc


def build_qkv(NT=32):
    nc = bass.Bass("TRN2", target_bir_lowering=False)
    T = NT * 128
    x = nc.dram_tensor("x", [T, D], F32, kind="ExternalInput").ap()
    win = nc.dram_tensor("win", [D, 3 * D], F32, kind="ExternalInput").ap()
    cs = nc.dram_tensor("cs", [T, 64], F32, kind="ExternalInput").ap()
    qkv = nc.dram_tensor("qkv", [T, 3 * D], BF16, kind="ExternalOutput").ap()
    with ExitStack() as es:
        kb = KB(nc, es)
        ident = kb.sb([128, 128], F32)
        make_ident(kb, ident)
        wb = kb.sb([128, 8, 3 * D], BF16)
        wv = [kb.view() for _ in range(8)]
        for c in range(8):
            kb.dma('pool', wb[:, c, :], win[c * 128:(c + 1) * 128, :], wv[c], writes=[wv[c]])
        kb.op('pool', lambda e: e.tensor_scalar(out=wb[:, :, 0:D], in0=wb[:, :, 0:D], scalar1=0.125, scalar2=None, op0=ALU.mult),
              reads=[], writes=wv)
        xin = [kb.sb([128, D], F32) for _ in range(2)]
        cst = [kb.sb([128, 64], F32) for _ in range(2)]
        xT = [kb.sb([128, 8, 128], BF16) for _ in range(2)]
        kro = [kb.sb([128, 512], F32) for _ in range(2)]
        tm = [kb.sb([128, 8, 32], F32) for _ in range(4)]
        tp = [kb.sb([128, 8, 32], F32) for _ in range(4)]
        ost = [kb.sb([128, 3 * D], BF16) for _ in range(2)]
        ostv = [[kb.view() for _ in range(6)] for _ in range(2)]
        banks = [kb.ps([128, 512], F32) for _ in range(8)]
        nb = 0
        out_toks = []
        for i in range(NT):
            xi = xin[i % 2]
            ci = cst[i % 2]
            xt = xT[i % 2]
            oi = ost[i % 2]
            ov = ostv[i % 2]
            kr = kro[i % 2]
            kb.dma('sp', xi[:], x[i * 128:(i + 1) * 128, :], xi, writes=[xi])
            kb.dma('sp', ci[:], cs[i * 128:(i + 1) * 128, :], ci, writes=[ci])
            for hf in range(2):
                bk = banks[nb % 8]; nb += 1
                kb.group('pe', [lambda e, c=c, bk=bk, xi=xi: e.transpose(bk[:, (c % 4) * 128:(c % 4 + 1) * 128],
                                                                           xi[:, c * 128:(c + 1) * 128], ident[:])
                                for c in range(hf * 4, hf * 4 + 4)], reads=[xi, ident], writes=[bk])
                eng = 'act' if hf == 0 else 'dve'
                fn = (lambda e, bk=bk, hf=hf, xt=xt: e.copy(out=xt[:, hf * 4:(hf + 1) * 4, :], in_=bk[:].rearrange("p (c t) -> p c t", c=4))) \
                    if hf == 0 else (lambda e, bk=bk, hf=hf, xt=xt: e.tensor_copy(out=xt[:, hf * 4:(hf + 1) * 4, :], in_=bk[:].rearrange("p (c t) -> p c t", c=4)))
                if hf == 0:
                    t_a = kb.op(eng, fn, reads=[bk], writes=[xt])
                else:
                    t_b = kb.op(eng, fn, reads=[bk], deps=[t_a])
                    xt.w = [t_a, t_b]
            cosb = ci[:, 0:32].unsqueeze(1).broadcast_to([128, 8, 32])
            sinb = ci[:, 32:64].unsqueeze(1).broadcast_to([128, 8, 32])
            for blk in range(6):
                bk = banks[nb % 8]; nb += 1
                kb.group('pe', [lambda e, c=c, bk=bk, blk=blk, xt=xt: e.matmul(bk[:], xt[:, c, :], wb[:, c, blk * 512:(blk + 1) * 512],
                                                                               start=(c == 0), stop=(c == 7)) for c in range(8)],
                         reads=[xt] + wv, writes=[bk])
                osl = slice(blk * 512, (blk + 1) * 512)
                if blk in (0, 2):
                    if blk == 0:
                        eng, tt, src, sbuf = 'dve', tm, bk, bk
                    else:
                        kb.op('act', lambda e, bk=bk, kr=kr: e.copy(out=kr[:], in_=bk[:]), reads=[bk], writes=[kr])
                        eng, tt, src, sbuf = 'pool', tp, kr, kr
                    s4 = src[:].rearrange("p (h two j) -> p h two j", two=2, j=32)
                    o4 = oi[:, osl].rearrange("p (h two j) -> p h two j", two=2, j=32)
                    t1, t2 = s4[:, :, 0, :], s4[:, :, 1, :]
                    kb.op(eng, lambda e, t1=t1, tt=tt, cosb=cosb: e.tensor_tensor(out=tt[0][:], in0=t1, in1=cosb, op=ALU.mult), reads=[sbuf, ci], writes=[tt[0]])
                    kb.op(eng, lambda e, t2=t2, tt=tt, sinb=sinb: e.tensor_tensor(out=tt[1][:], in0=t2, in1=sinb, op=ALU.mult), reads=[sbuf, ci], writes=[tt[1]])
                    kb.op(eng, lambda e, t2=t2, tt=tt, cosb=cosb: e.tensor_tensor(out=tt[2][:], in0=t2, in1=cosb, op=ALU.mult), reads=[sbuf, ci], writes=[tt[2]])
                    kb.op(eng, lambda e, t1=t1, tt=tt, sinb=sinb: e.tensor_tensor(out=tt[3][:], in0=t1, in1=sinb, op=ALU.mult), reads=[sbuf, ci], writes=[tt[3]])
                    kb.op(eng, lambda e, tt=tt, o4=o4: e.tensor_tensor(out=o4[:, :, 0, :], in0=tt[0][:], in1=tt[1][:], op=ALU.subtract), reads=[tt[0], tt[1]], writes=[ov[blk]])
                    tk = kb.op(eng, lambda e, tt=tt, o4=o4: e.tensor_tensor(out=o4[:, :, 1, :], in0=tt[2][:], in1=tt[3][:], op=ALU.add), reads=[tt[2], tt[3]], deps=ov[blk].w)
                    ov[blk].w = ov[blk].w + [tk]
                else:
                    kb.op('act', lambda e, bk=bk, oi=oi, osl=osl: e.copy(out=oi[:, osl], in_=bk[:]), reads=[bk], writes=[ov[blk]])
            out_toks.append(kb.dma('act', qkv[i * 128:(i + 1) * 128, :], oi[:], oi, reads=ov))
        kb.wait_only('sp', out_toks)
        kb.emit()
    return nc


def build_attn(NB=4):
    nc = bass.Bass("TRN2", target_bir_lowering=False)
    S = S_LEN
    qA, kA, vA, oA = [], [], [], []
    for di, d in enumerate(DILS):
        Ls = S // d
        nt = Ls // 128
        qA.append(nc.dram_tensor(f"qA{di}", [NB, 64, S], BF16, kind="ExternalInput").ap())
        kA.append(nc.dram_tensor(f"kA{di}", [NB, 64, d * (Ls + 128)], BF16, kind="ExternalInput").ap())
        vA.append(nc.dram_tensor(f"vA{di}", [NB, 128, d * (nt + 1) * 65], BF16, kind="ExternalInput").ap())
        oA.append(nc.dram_tensor(f"oA{di}", [NB, 128, 64 * 65], F32, kind="ExternalOutput").ap())
    qB = nc.dram_tensor("qB", [NB, 64, S], BF16, kind="ExternalInput").ap()
    kB = nc.dram_tensor("kB", [NB, 64, S], BF16, kind="ExternalInput").ap()
    vB = nc.dram_tensor("vB", [NB, 128, 64 * 65], BF16, kind="ExternalInput").ap()
    oB = nc.dram_tensor("oB", [NB, 128, 64 * 64], F32, kind="ExternalOutput").ap()
    ebias = nc.dram_tensor("ebias", [128, 25 * 128], F32, kind="ExternalInput").ap()
    maskd = nc.dram_tensor("maskd", [128, 256], F32, kind="ExternalInput").ap()
    with ExitStack() as es:
        kb = KB(nc, es)
        mask = kb.sb([128, 256], BF16)
        kb.dma('pool', mask[:], maskd[:, :], mask, writes=[mask])
        eb32 = kb.sb([128, 25 * 128], F32)
        E = kb.sb([128, 25 * 128], BF16)
        kb.dma('sp', eb32[:], ebias[:, :], eb32, writes=[eb32])
        kb.op('act', lambda e: e.activation(out=E[:], in_=eb32[:], func=AF.Exp), reads=[eb32], writes=[E])
        KMAX = 16 * (512 + 128)
        qT = [kb.sb([64, S], BF16) for _ in range(2)]
        kT = [kb.sb([64, KMAX], BF16) for _ in range(2)]
        V = [kb.sb([128, 80 * 65], BF16) for _ in range(2)]
        osb = [kb.sb([128, 64 * 65], F32) for _ in range(2)]
        pT = [kb.sb([128, 256], BF16) for _ in range(6)]
        rden = [kb.sb([128, 1], F32) for _ in range(2)]
        sbk = [kb.ps([128, 512], F32) for _ in range(4)]
        obk = [kb.ps([128, 512], F32) for _ in range(4)]
        stages = []
        for b in range(NB):
            for di in range(3):
                stages.append(('A', b, di))
            stages.append(('B', b, None))

        def load(si):
            kind, b, di = stages[si]
            sl = si % 2
            if kind == 'A':
                d = DILS[di]
                Ls = S // d
                nt = Ls // 128
                kb.dma('sp', qT[sl][:], qA[di][b], qT[sl], writes=[qT[sl]])
                kb.dma('sp', kT[sl][:, 0:d * (Ls + 128)], kA[di][b], kT[sl], writes=[kT[sl]])
                kb.dma('sp', V[sl][:, 0:d * (nt + 1) * 65], vA[di][b], V[sl], writes=[V[sl]])
            else:
                kb.dma('sp', qT[sl][:], qB[b], qT[sl], writes=[qT[sl]])
                kb.dma('sp', kT[sl][:, 0:S], kB[b], kT[sl], writes=[kT[sl]])
                kb.dma('sp', V[sl][:, 0:64 * 65], vB[b], V[sl], writes=[V[sl]])

        load(0)
        out_toks = []
        fronts, backs = [], []
        LOOK = 3

        def add_step(si, first, last, front_fn, back_fn):
            n = len(fronts)

            def front(n=n):
                front_fn(n)

            def back(n=n):
                if first and si + 1 < len(stages):
                    load(si + 1)
                back_fn(n)
                if last:
                    kind, b, di = stages[si]
                    ob = osb[si % 2]
                    if kind == 'A':
                        out_toks.append(kb.dma('sp', oA[di][b], ob[:], ob, reads=[ob]))
                    else:
                        out_toks.append(kb.dma('sp', oB[b], ob[:, 0:64 * 64], ob, reads=[ob]))
            fronts.append(front)
            backs.append(back)

        for si, (kind, b, di) in enumerate(stages):
            sl = si % 2
            q, k, v, ob = qT[sl], kT[sl], V[sl], osb[sl]
            if kind == 'A':
                d = DILS[di]
                Ls = S // d
                nt = Ls // 128
                for r in range(d):
                    for j in range(nt + 1):
                        q0 = 128 * (j - 1) if j >= 1 else 0
                        q1 = 128 * (j + 1) if j <= nt - 1 else 128 * nt
                        w = q1 - q0
                        moff = 128 if j == 0 else 0
                        koff = r * (Ls + 128) + 128 * j
                        vt = r * (nt + 1) + j
                        qa = r * Ls + q0

                        def front_fn(step, k=k, q=q, koff=koff, qa=qa, w=w, moff=moff):
                            sb_ = sbk[step % 4]
                            p = pT[step % 6]
                            kb.op('pe', lambda e: e.matmul(sb_[:, 0:w], k[:, koff:koff + 128], q[:, qa:qa + w], start=True, stop=True),
                                  reads=[k, q], writes=[sb_])
                            kb.op('act', lambda e: e.activation(out=p[:, 0:w], in_=sb_[:, 0:w], func=AF.Exp), reads=[sb_], writes=[p])
                            meng = 'dve' if step % 2 == 0 else 'pool'
                            kb.op(meng, lambda e: e.tensor_tensor(out=p[:, 0:w], in0=p[:, 0:w], in1=mask[:, moff:moff + w], op=ALU.mult),
                                  reads=[p, mask], writes=[p])

                        def back_fn(step, j=j, nt=nt, r=r, v=v, vt=vt, ob=ob):
                            p = pT[step % 6]
                            if j >= 1:
                                o_ = obk[(j - 1) % 4]
                                kb.op('pe', lambda e: e.matmul(o_[:, 0:65], p[:, 0:128], v[:, vt * 65:(vt + 1) * 65], start=False, stop=True),
                                      reads=[p, v], writes=[], deps=o_.w + list(o_.r.values()))
                                o_.w = [Tok(kb.sems['pe'], kb.cnt['pe'])]
                                blk = r * nt + (j - 1)
                                kb.op('dve', lambda e: e.tensor_copy(out=ob[:, blk * 65:(blk + 1) * 65], in_=o_[:, 0:65]),
                                      reads=[o_], writes=[], deps=list(ob.r.values()))
                                ob.w = [Tok(kb.sems['dve'], kb.cnt['dve'])]
                            if j <= nt - 1:
                                o2 = obk[j % 4]
                                pc = 128 if j >= 1 else 0
                                kb.op('pe', lambda e: e.matmul(o2[:, 0:65], p[:, pc:pc + 128], v[:, vt * 65:(vt + 1) * 65], start=True, stop=False),
                                      reads=[p, v], writes=[o2])
                        add_step(si, r == 0 and j == 0, r == d - 1 and j == nt, front_fn, back_fn)
            else:
                for m in range(64):
                    cls = {0: 0, 1: 1, 62: 3, 63: 4}.get(m, 2)
                    bt = min(max(m - 2, 0), 59)
                    for j in range(5):
                        tile = bt + j
                        ecol = (cls * 5 + j) * 128

                        def front_fn(step, k=k, q=q, tile=tile, m=m, ecol=ecol):
                            sb_ = sbk[step % 4]
                            p = pT[step % 6]
                            kb.op('pe', lambda e: e.matmul(sb_[:, 0:128], k[:, tile * 128:(tile + 1) * 128], q[:, m * 128:(m + 1) * 128],
                                                           start=True, stop=True), reads=[k, q], writes=[sb_])
                            kb.op('act', lambda e: e.activation(out=p[:, 0:128], in_=sb_[:, 0:128], func=AF.Exp), reads=[sb_], writes=[p])
                            meng = 'dve' if step % 2 == 0 else 'pool'
                            kb.op(meng, lambda e: e.tensor_tensor(out=p[:, 0:128], in0=p[:, 0:128], in1=E[:, ecol:ecol + 128], op=ALU.mult),
                                  reads=[p, E], writes=[p])

                        def back_fn(step, j=j, m=m, v=v, tile=tile, ob=ob):
                            p = pT[step % 6]
                            o_ = obk[m % 4]
                            if j == 0:
                                kb.op('pe', lambda e: e.matmul(o_[:, 0:65], p[:, 0:128], v[:, tile * 65:(tile + 1) * 65], start=True, stop=False),
                                      reads=[p, v], writes=[o_])
                            else:
                                kb.op('pe', lambda e: e.matmul(o_[:, 0:65], p[:, 0:128], v[:, tile * 65:(tile + 1) * 65], start=False, stop=(j == 4)),
                                      reads=[p, v], writes=[], deps=o_.w)
                                o_.w = [Tok(kb.sems['pe'], kb.cnt['pe'])]
                            if j == 4:
                                rd = rden[m % 2]
                                kb.op('dve', lambda e: e.reciprocal(out=rd[:], in_=o_[:, 64:65]), reads=[o_], writes=[rd])
                                kb.op('dve', lambda e: e.tensor_scalar(out=ob[:, m * 64:(m + 1) * 64], in0=o_[:, 0:64], scalar1=rd[:, 0:1],
                                                                       scalar2=None, op0=ALU.mult),
                                      reads=[o_, rd], writes=[], deps=list(ob.r.values()))
                                ob.w = [Tok(kb.sems['dve'], kb.cnt['dve'])]
                        add_step(si, m == 0 and j == 0, m == 63 and j == 4, front_fn, back_fn)
        nsteps = len(fronts)
        for n in range(min(LOOK, nsteps)):
            fronts[n]()
        for n in range(nsteps):
            if n + LOOK < nsteps:
                fronts[n + LOOK]()
            backs[n]()
        kb.wait_only('sp', out_toks)
        kb.emit()
    return nc


def build_oproj(NT=32):
    nc = bass.Bass("TRN2", target_bir_lowering=False)
    T = NT * 128
    x = nc.dram_tensor("x", [T, D], F32, kind="ExternalInput").ap()
    oa = nc.dram_tensor("oa", [3, T, 8 * 65], F32, kind="ExternalInput").ap()
    obd = nc.dram_tensor("ob", [T, 512], F32, kind="ExternalInput").ap()
    wout = nc.dram_tensor("wout", [D, D], F32, kind="ExternalInput").ap()
    lng = nc.dram_tensor("lng", [128, D], F32, kind="ExternalInput").ap()
    lnb = nc.dram_tensor("lnb", [128, D], F32, kind="ExternalInput").ap()
    y = nc.dram_tensor("y", [T, D], F32, kind="ExternalOutput").ap()
    with ExitStack() as es:
        kb = KB(nc, es)
        ident = kb.sb([128, 128], F32)
        make_ident(kb, ident)
        g = kb.sb([128, D], F32)
        b = kb.sb([128, D], F32)
        kb.dma('sp', g[:], lng[:, :], g, writes=[g])
        kb.dma('sp', b[:], lnb[:, :], b, writes=[b])
        L = LNBufs(kb, nbuf=2)
        wb = kb.sb([128, 8, D], BF16)
        kb.dma('pool', wb[:], wout.rearrange("(c p) f -> p c f", p=128), wb, writes=[wb])
        xin = [kb.sb([128, D], F32) for _ in range(3)]
        oat = [[kb.sb([128, 8, 65], F32) for _ in range(3)] for _ in range(2)]
        rd = [kb.sb([128, 8], F32) for _ in range(2)]
        ot = [kb.sb([128, D], F32) for _ in range(2)]
        ota = [kb.view() for _ in range(2)]
        otb = [kb.view() for _ in range(2)]
        oT = [kb.sb([128, 8, 128], BF16) for _ in range(2)]
        oTa = [kb.view() for _ in range(2)]
        oTb = [kb.view() for _ in range(2)]
        pt = [[kb.ps([128, 512], F32) for _ in range(2)] for _ in range(2)]
        py = [[kb.ps([128, 512], F32) for _ in range(2)] for _ in range(2)]
        def front(i):
            xi = xin[i % 3]
            a3 = oat[i % 2]
            o = ot[i % 2]
            r_ = rd[i % 2]
            oTi = oT[i % 2]
            rows = slice(i * 128, (i + 1) * 128)
            kb.dma('sp', xi[:], x[rows, :], xi, writes=[xi])
            for br in range(3):
                kb.dma('act' if br == 1 else 'sp', a3[br][:], oa[br, rows, :].rearrange("p (h c) -> p h c", c=65), a3[br], writes=[a3[br]])
            kb.dma('act', o[:, 512:1024], obd[rows, :], otb[i % 2], writes=[otb[i % 2]])
            kb.op('dve', lambda e, a3=a3: e.tensor_tensor(out=a3[0][:], in0=a3[0][:], in1=a3[1][:], op=ALU.add), reads=[a3[1]], writes=[a3[0]])
            kb.op('dve', lambda e, a3=a3: e.tensor_tensor(out=a3[0][:], in0=a3[0][:], in1=a3[2][:], op=ALU.add), reads=[a3[2]], writes=[a3[0]])
            kb.op('dve', lambda e, a3=a3, r_=r_: e.reciprocal(out=r_[:], in_=a3[0][:, :, 64]), reads=[a3[0]], writes=[r_])
            kb.op('dve', lambda e, a3=a3, r_=r_, o=o: e.tensor_tensor(out=o[:, 0:512].rearrange("p (h c) -> p h c", c=64), in0=a3[0][:, :, 0:64],
                                                                   in1=r_[:].unsqueeze(2).broadcast_to([128, 8, 64]), op=ALU.mult),
                  reads=[a3[0], r_], writes=[ota[i % 2]])
            for hf in range(2):
                bk = pt[i % 2][hf]
                kb.group('pe', [lambda e, c=c, bk=bk, o=o: e.transpose(bk[:, (c % 4) * 128:(c % 4 + 1) * 128],
                                                                        o[:, c * 128:(c + 1) * 128], ident[:])
                                for c in range(hf * 4, hf * 4 + 4)], reads=[ota[i % 2] if hf == 0 else otb[i % 2], ident], writes=[bk])
                if hf == 0:
                    kb.op('act', lambda e, bk=bk, oTi=oTi: e.copy(out=oTi[:, 0:4, :], in_=bk[:].rearrange("p (c t) -> p c t", c=4)),
                          reads=[bk], writes=[oTa[i % 2]])
                else:
                    kb.op('dve', lambda e, bk=bk, oTi=oTi: e.tensor_copy(out=oTi[:, 4:8, :], in_=bk[:].rearrange("p (c t) -> p c t", c=4)),
                          reads=[bk], writes=[oTb[i % 2]])
            for oh in range(2):
                bk = py[i % 2][oh]
                kb.group('pe', [lambda e, c=c, bk=bk, oTi=oTi, oh=oh: e.matmul(bk[:], oTi[:, c, :], wb[:, c, oh * 512:(oh + 1) * 512],
                                                                               start=(c == 0), stop=(c == 7)) for c in range(8)],
                         reads=[oTa[i % 2], oTb[i % 2], wb], writes=[bk])

        def ln1(i):
            xi = xin[i % 3]
            return emit_ln1(kb, L, xi, xi, [py[i % 2][0][:], py[i % 2][1][:]], py[i % 2])

        def ln2(i, slot):
            return emit_ln2(kb, L, slot, g, b, y[i * 128:(i + 1) * 128, :], dma_eng='sp' if i % 2 == 0 else 'act')
        out_toks = skewed(NT, front, ln1, ln2)
        kb.wait_only('sp', out_toks)
        kb.emit()
    return nc

import ml_dtypes
BF = ml_dtypes.bfloat16
S_LEN = 8192
DILS = (1, 4, 16)


def rope_cs(pos):
    pos = pos.astype(np.float32)
    inv = (np.float32(10000.0) ** (-np.arange(0, 64, 2, dtype=np.float32) / np.float32(64))).astype(np.float32)
    ang = (pos[:, None] * inv[None, :]).astype(np.float32)
    return np.concatenate([np.cos(ang), np.sin(ang)], 1).astype(np.float32)


def attn_mask():
    p = np.arange(128)[:, None]
    a = np.arange(128)[None, :]
    return np.concatenate([(p <= a), (p >= a)], 1).astype(np.float32)


def na_bias_table(rpb_h):
    out = np.full((128, 25, 128), -30000.0, np.float32)
    kk = np.arange(128)
    qq = np.arange(128)
    for cls, m in enumerate((0, 1, 2, 62, 63)):
        base = min(max(2 * m - 4, 0), 118)
        qrow = 2 * m + qq // 64
        qcol = qq % 64
        rstart = np.clip(qrow - 4, 0, 120)
        cstart = np.clip(qcol - 8, 0, 48)
        for j in range(5):
            krow = base + 2 * j + kk // 64
            kcol = kk % 64
            ok = ((krow[:, None] >= rstart[None, :]) & (krow[:, None] < rstart[None, :] + 8)
                  & (kcol[:, None] >= cstart[None, :]) & (kcol[:, None] < cstart[None, :] + 16))
            roff = np.clip(krow[:, None] - qrow[None, :] + 7, 0, 14)
            coff = np.clip(kcol[:, None] - qcol[None, :], -15, 15) + 15
            vals = rpb_h[roff, coff]
            out[:, cls * 5 + j, :] = np.where(ok, vals, np.float32(-30000.0))
    return out.reshape(128, 25 * 128)


def attn_core_inputs(qkv, c, rpb_i):
    B, S = qkv.shape[0], qkv.shape[1]
    ha, hb = c, 8 + c
    im = {}
    q_h, k_h, v_h = qkv[:, :, 0, ha], qkv[:, :, 1, ha], qkv[:, :, 2, ha]
    one = np.ones((), BF)
    for di, d in enumerate(DILS):
        Ls = S // d
        nt = Ls // 128
        im[f"qA{di}"] = np.ascontiguousarray(q_h.reshape(B, Ls, d, 64).transpose(0, 3, 2, 1)).reshape(B, 64, d * Ls)
        kk = np.zeros((B, 64, d, Ls + 128), BF)
        kk[:, :, :, 64:64 + Ls] = k_h.reshape(B, Ls, d, 64).transpose(0, 3, 2, 1)
        im[f"kA{di}"] = kk.reshape(B, 64, d * (Ls + 128))
        vv = np.zeros((B, d, Ls + 128, 65), BF)
        vv[:, :, 64:64 + Ls, :64] = v_h.reshape(B, Ls, d, 64).transpose(0, 2, 1, 3)
        vv[:, :, 64:64 + Ls, 64] = one
        im[f"vA{di}"] = np.ascontiguousarray(vv.reshape(B, d, nt + 1, 128, 65).transpose(0, 3, 1, 2, 4)).reshape(B, 128, d * (nt + 1) * 65)
    im["qB"] = np.ascontiguousarray(qkv[:, :, 0, hb].transpose(0, 2, 1))
    im["kB"] = np.ascontiguousarray(qkv[:, :, 1, hb].transpose(0, 2, 1))
    vb = np.zeros((B, S, 65), BF)
    vb[:, :, :64] = qkv[:, :, 2, hb]
    vb[:, :, 64] = one
    im["vB"] = np.ascontiguousarray(vb.reshape(B, 64, 128, 65).transpose(0, 2, 1, 3)).reshape(B, 128, 64 * 65)
    im["ebias"] = na_bias_table(rpb_i[c])
    im["maskd"] = attn_mask()
    return im


def attn_core_outputs(results, B, S):
    oa = np.zeros((3, B, S, 8, 65), np.float32)
    ob = np.zeros((B, S, 8, 64), np.float32)
    for c, r in enumerate(results):
        for di, d in enumerate(DILS):
            Ls = S // d
            nt = Ls // 128
            a = r[f"oA{di}"].reshape(B, 128, d, nt, 65).transpose(0, 3, 1, 2, 4).reshape(B, S, 65)
            oa[di, :, :, c, :] = a
        ob[:, :, c, :] = r["oB"].reshape(B, 128, 64, 64).transpose(0, 2, 1, 3).reshape(B, S, 64)
    return oa.reshape(3, B, S, 8 * 65), ob.reshape(B, S, 512)


N_CORES = 8
_PROGS = {}


def _prog(name, fn):
    if name not in _PROGS:
        _PROGS[name] = fn()
    return _PROGS[name]


def _run(nc, in_maps):
    return run_bass_kernel_spmd(nc, in_maps, core_ids=list(range(N_CORES))).results


def _rep(v):
    return np.ascontiguousarray(np.broadcast_to(np.asarray(v, np.float32)[None], (128, 1024)))


def _attn_layer(xf, w_in, w_out, rpb_i, g, b, B, S):
    T = xf.shape[0] // N_CORES
    NT = T // 128
    halves = S // T
    nc1 = _prog('qkv', lambda: build_qkv(NT=NT))
    ims = []
    for c in range(N_CORES):
        p0 = (c % halves) * T
        ims.append({"x": xf[c * T:(c + 1) * T], "win": w_in, "cs": rope_cs(np.arange(p0, p0 + T))})
    r1 = _run(nc1, ims)
    qkv = np.concatenate([r["qkv"] for r in r1], 0).reshape(B, S, 3, 16, 64)
    nc2 = _prog('attn', lambda: build_attn(NB=B))
    r2 = _run(nc2, [attn_core_inputs(qkv, c, rpb_i) for c in range(N_CORES)])
    oa, ob = attn_core_outputs(r2, B, S)
    oa = oa.reshape(3, B * S, 8 * 65)
    ob = ob.reshape(B * S, 512)
    nc3 = _prog('oproj', lambda: build_oproj(NT=NT))
    ims = [{"x": xf[c * T:(c + 1) * T], "oa": np.ascontiguousarray(oa[:, c * T:(c + 1) * T]),
            "ob": np.ascontiguousarray(ob[c * T:(c + 1) * T]), "wout": w_out, "lng": _rep(g), "lnb": _rep(b)}
           for c in range(N_CORES)]
    r3 = _run(nc3, ims)
    return np.concatenate([r["y"] for r in r3], 0)


def _pool_layer(xf, pw, psc, g, b, B, S):
    T = xf.shape[0] // N_CORES
    NT = T // 128
    halves = S // T
    ncp = _prog('pool', lambda: build_pool(NT=NT))
    ims = []
    for c in range(N_CORES):
        h = c % halves
        xp = np.zeros((T + 16, 1024), np.float32)
        lo = c * T - (8 if h > 0 else 0)
        hi = (c + 1) * T + (8 if h < halves - 1 else 0)
        xp[8 - (c * T - lo): 8 + T + (hi - (c + 1) * T)] = xf[lo:hi]
        Bm, Bh = pool_tables(h == 0, h == halves - 1)
        ims.append({"xp": xp, "bm": np.ascontiguousarray(Bm.transpose(2, 0, 1, 3).reshape(128, 12 * 128)),
                    "bh": np.ascontiguousarray(Bh.transpose(2, 0, 1, 3).reshape(16, 12 * 128)),
                    "pw": pw, "psc": _rep(psc), "lng": _rep(g), "lnb": _rep(b)})
    r = _run(ncp, ims)
    return np.concatenate([q["y"] for q in r], 0)


MOE_CAP = 768


def _moe_layer(xf, rw, rbias, w1, w3, w2, g, b):
    T = xf.shape[0] // N_CORES
    NT = T // 128
    ncm = _prog('moe', lambda: build_moe_sparse(NT=NT, C=MOE_CAP, NE=16))
    rb = np.ascontiguousarray(np.broadcast_to(np.tile(np.asarray(rbias, np.float32), NT)[None], (128, NT * 16)))
    eoff = np.ascontiguousarray(np.broadcast_to(np.tile(np.arange(16, dtype=np.float32) * MOE_CAP, NT)[None], (128, NT * 16)))
    ims = [{"x": xf[c * T:(c + 1) * T], "rw": rw, "rbias": rb, "eoff": eoff, "w1": w1, "w3": w3, "w2": w2,
            "lng": _rep(g), "lnb": _rep(b)} for c in range(N_CORES)]
    r = _run(ncm, ims)
    return np.concatenate([q["y"] for q in r], 0)


def kernel(x, w_in, w_out, rpb, pool_w, pool_scale, router_w, router_bias, moe_w1, moe_w3, moe_w2, ln_g, ln_b):
    f = lambda a: np.ascontiguousarray(np.asarray(a, np.float32))
    x = f(x)
    B, S, Dm = x.shape
    xf = x.reshape(B * S, Dm)
    depth = moe_w1.shape[0]
    for layer in range(depth):
        i = layer // 2
        if layer % 2 == 0:
            xf = _attn_layer(xf, f(w_in[i]), f(w_out[i]), f(rpb[i]), ln_g[layer, 0], ln_b[layer, 0], B, S)
        else:
            xf = _pool_layer(xf, f(pool_w[i]), pool_scale[i], ln_g[layer, 0], ln_b[layer, 0], B, S)
        xf = _moe_layer(xf, f(router_w), router_bias, f(moe_w1[layer]), f(moe_w3[layer]), f(moe_w2[layer]),
                        ln_g[layer, 1], ln_b[layer, 1])
    return xf.reshape(B, S, Dm).astype(np.float32)
```

```python
import numpy as np
import concourse.bass as bass
import concourse.mybir as mybir
from concourse.bass_utils import run_bass_kernel_spmd
from contextlib import ExitStack

F32 = mybir.dt.float32
BF16 = mybir.dt.bfloat16
U32 = mybir.dt.uint32
I32 = mybir.dt.int32
AF = mybir.ActivationFunctionType
ALU = mybir.AluOpType
AX = mybir.AxisListType

ENGS = ['pe', 'act', 'dve', 'pool', 'sp']
ENG_ATTR = {'pe': 'tensor', 'act': 'scalar', 'dve': 'vector', 'pool': 'gpsimd', 'sp': 'sync'}


class Tok:
    __slots__ = ('sem', 'val')

    def __init__(self, sem, val):
        self.sem = sem
        self.val = val


class Buf:
    def __init__(self, kb, t, name):
        self.kb = kb
        self.t = t
        self.name = name
        self.w = []
        self.r = {}
        self._slot = None

    def __getitem__(self, k):
        return self.t[k]

    def slot(self):
        if self._slot is None:
            self._slot = self.kb.new_sem()
            self._slotval = 0
        return self._slot


class KB:
    def __init__(self, nc, es, same_eng_sync=True):
        self.nc = nc
        self.es = es
        self.ops = {e: [] for e in ENGS}
        self.nsem = 0
        self.sems = {e: self.new_sem() for e in ENGS}
        self.cnt = {e: 0 for e in ENGS}
        self.waited = {e: {} for e in ENGS}
        self.same_eng_sync = same_eng_sync
        self.nbuf = 0

    def new_sem(self):
        self.nsem += 1
        return self.es.enter_context(self.nc.semaphore(f"sem{self.nsem}"))

    def sb(self, shape, dtype, name=None):
        self.nbuf += 1
        name = name or f"sb{self.nbuf}"
        return Buf(self, self.es.enter_context(self.nc.sbuf_tensor(name, list(shape), dtype)), name)

    def ps(self, shape, dtype=F32, name=None):
        self.nbuf += 1
        name = name or f"ps{self.nbuf}"
        return Buf(self, self.es.enter_context(self.nc.psum_tensor(name, list(shape), dtype)), name)

    def view(self, name="v"):
        return Buf(self, None, name)

    def _waits(self, eng, deps):
        waits = []
        for d in deps:
            if d is None:
                continue
            if isinstance(d, (list, tuple)):
                waits += self._waits(eng, d)
                continue
            if (not self.same_eng_sync) and d.sem is self.sems[eng]:
                continue
            key = id(d.sem)
            if self.waited[eng].get(key, 0) >= d.val:
                continue
            self.waited[eng][key] = d.val
            waits.append((d.sem, d.val))
        return waits

    def _deps(self, reads, writes, deps):
        ds = list(deps)
        for b in reads:
            ds += b.w
        for b in writes:
            ds += b.w
            ds += list(b.r.values())
        return ds

    def _commit(self, tok, reads, writes):
        for b in writes:
            b.w = [tok]
            b.r = {}
        for b in reads:
            if b not in writes:
                o = b.r.get(id(tok.sem))
                if o is None or o.val < tok.val:
                    b.r[id(tok.sem)] = tok

    def op(self, eng, fn, reads=(), writes=(), deps=()):
        waits = self._waits(eng, self._deps(reads, writes, deps))
        self.cnt[eng] += 1
        self.ops[eng].append((waits, fn, (self.sems[eng], 1)))
        tok = Tok(self.sems[eng], self.cnt[eng])
        self._commit(tok, reads, writes)
        return tok

    def group(self, eng, fns, reads=(), writes=(), deps=()):
        waits = self._waits(eng, self._deps(reads, writes, deps))
        for i, fn in enumerate(fns):
            self.cnt[eng] += 1
            self.ops[eng].append((waits if i == 0 else [], fn, (self.sems[eng], 1)))
        tok = Tok(self.sems[eng], self.cnt[eng])
        self._commit(tok, reads, writes)
        return tok

    def dma(self, eng, out, in_, slotbuf, reads=(), writes=(), deps=(), **kw):
        sem = slotbuf.slot()
        ds = list(deps)
        for b in reads:
            ds += b.w
        for b in writes:
            ds += [t for t in b.w if t.sem is not sem]
            ds += list(b.r.values())
        waits = self._waits(eng, ds)
        slotbuf._slotval += 16
        self.ops[eng].append((waits, lambda e: e.dma_start(out=out, in_=in_, **kw), (sem, 16)))
        tok = Tok(sem, slotbuf._slotval)
        self._commit(tok, reads, writes)
        return tok

    def wait_only(self, eng, deps):
        waits = self._waits(eng, deps)
        if waits:
            self.ops[eng].append((waits, None, None))

    def emit(self):
        with self.nc.Block() as block:
            for e in ENGS:
                dec = getattr(block, ENG_ATTR[e])
                ops = self.ops[e]

                def body(eng, ops=ops):
                    for waits, fn, inc in ops:
                        for s, v in waits:
                            eng.wait_ge(s, v)
                        if fn is None:
                            continue
                        inst = fn(eng)
                        if inc is not None:
                            inst.then_inc(inc[0], inc[1])
                dec(body)


def _idma(self, out, in_, idx_ap, scatter, slotbuf, bound, reads=(), writes=(), deps=()):
    sem = slotbuf.slot()
    ds = list(deps)
    for b in reads:
        ds += b.w
    for b in writes:
        ds += [t for t in b.w if t.sem is not sem]
        ds += list(b.r.values())
    waits = self._waits('pool', ds)
    slotbuf._slotval += 16
    if not hasattr(self, '_bregs'):
        self._bregs = {}

    def breg(e):
        if bound not in self._bregs:
            self._bregs[bound] = e.to_reg(bound)
        return self._bregs[bound]
    if scatter:
        fn = lambda e: e.indirect_dma_start(out=out, out_offset=bass.IndirectOffsetOnAxis(ap=idx_ap, axis=0), in_=in_,
                                            in_offset=None, bounds_check=breg(e), oob_is_err=False)
    else:
        fn = lambda e: e.indirect_dma_start(out=out, out_offset=None, in_=in_,
                                            in_offset=bass.IndirectOffsetOnAxis(ap=idx_ap, axis=0), bounds_check=breg(e),
                                            oob_is_err=False)
    self.ops['pool'].append((waits, fn, (sem, 16)))
    tok = Tok(sem, slotbuf._slotval)
    self._commit(tok, reads, writes)
    return tok


KB.idma = _idma


D = 1024
ALPHA = 8.0 ** 0.25
LN_EPS = 1e-5


def make_ident(kb, ident):
    kb.op('pool', lambda e: e.memset(ident[:], 0.0), writes=[ident])
    kb.op('pool', lambda e: e.affine_select(out=ident[:], in_=ident[:], compare_op=ALU.not_equal, fill=1.0,
                                            base=0, pattern=[[-1, 128]], channel_multiplier=1), writes=[ident])


class LNBufs:
    def __init__(self, kb, nbuf=2):
        self.kb = kb
        self.n = nbuf
        self.i = 0
        self.z = [kb.sb([128, D], F32) for _ in range(nbuf)]
        self.o = [kb.sb([128, D], F32) for _ in range(nbuf)]
        self.st = [kb.sb([128, 12], F32) for _ in range(nbuf)]
        self.mv = [kb.sb([128, 2], F32) for _ in range(nbuf)]
        self.rs = [kb.sb([128, 1], F32) for _ in range(nbuf)]
        self.nb = [kb.sb([128, 1], F32) for _ in range(nbuf)]
        self.eps = kb.sb([128, 1], F32)
        kb.op('pool', lambda e: e.memset(self.eps[:], LN_EPS), writes=[self.eps])


def emit_ln1(kb, L, xbuf, x_ap, h_aps, h_bufs):
    i = L.i
    L.i = (L.i + 1) % L.n
    z, st, mv, rs, nb = L.z[i], L.st[i], L.mv[i], L.rs[i], L.nb[i]
    for hf in range(2):
        sl = slice(hf * 512, (hf + 1) * 512)
        kb.op('dve', lambda e, sl=sl, hf=hf: e.scalar_tensor_tensor(out=z[:, sl], in0=x_ap[:, sl], scalar=ALPHA,
                                                                    in1=h_aps[hf], op0=ALU.mult, op1=ALU.add),
              reads=[xbuf, h_bufs[hf]], writes=[z] if hf == 0 else [], deps=z.w if hf == 1 else ())
    z.w = [Tok(kb.sems['dve'], kb.cnt['dve'])]
    for hf in range(2):
        sl = slice(hf * 512, (hf + 1) * 512)
        kb.op('dve', lambda e, sl=sl, hf=hf: e.bn_stats(out=st[:, hf * 6:(hf + 1) * 6], in_=z[:, sl]),
              reads=[z], writes=[st] if hf == 0 else [], deps=st.w if hf == 1 else ())
    st.w = [Tok(kb.sems['dve'], kb.cnt['dve'])]
    kb.op('dve', lambda e: e.bn_aggr(out=mv[:], in_=st[:]), reads=[st], writes=[mv])
    kb.op('act', lambda e: e.activation(out=rs[:], in_=mv[:, 1:2], func=AF.Sqrt, bias=L.eps[:], scale=1.0),
          reads=[mv, L.eps], writes=[rs])
    kb.op('dve', lambda e: e.reciprocal(out=rs[:], in_=rs[:]), reads=[rs], writes=[rs])
    kb.op('dve', lambda e: e.scalar_tensor_tensor(out=nb[:], in0=mv[:, 0:1], scalar=-1.0, in1=rs[:],
                                                  op0=ALU.mult, op1=ALU.mult), reads=[mv, rs], writes=[nb])
    return i


def emit_ln2(kb, L, i, g, b, out_dram_ap, dma_eng='sp', b_eng='pool'):
    z, o, rs, nb = L.z[i], L.o[i], L.rs[i], L.nb[i]
    kb.op('act', lambda e: e.activation(out=o[:], in_=z[:], func=AF.Identity, bias=nb[:], scale=rs[:]),
          reads=[z, rs, nb], writes=[o])
    kb.op('pool', lambda e: e.tensor_tensor(out=o[:], in0=o[:], in1=g[:], op=ALU.mult), reads=[o, g], writes=[o])
    kb.op(b_eng, lambda e: e.tensor_tensor(out=o[:], in0=o[:], in1=b[:], op=ALU.add), reads=[o, b], writes=[o])
    return kb.dma(dma_eng, out_dram_ap, o[:], o, reads=[o])


def emit_ln(kb, L, xbuf, x_ap, h_aps, h_bufs, g, b, out_dram_ap, dma_eng='sp', b_eng='pool'):
    i = emit_ln1(kb, L, xbuf, x_ap, h_aps, h_bufs)
    return emit_ln2(kb, L, i, g, b, out_dram_ap, dma_eng, b_eng)


def skewed(n, front, ln1, ln2):
    toks = []
    slots = {}
    for s in range(n + 2):
        if s < n:
            front(s)
        if 1 <= s <= n:
            slots[s - 1] = ln1(s - 1)
        if s >= 2:
            toks.append(ln2(s - 2, slots.pop(s - 2)))
    return toks


def build_moe(NT=32, GT=8, NE=16, debug=False):
    nc = bass.Bass("TRN2", target_bir_lowering=False)
    T = NT * 128
    x = nc.dram_tensor("x", [T, D], F32, kind="ExternalInput").ap()
    rw = nc.dram_tensor("rw", [D, 16], F32, kind="ExternalInput").ap()
    rbias = nc.dram_tensor("rbias", [128, GT * 16], F32, kind="ExternalInput").ap()
    w1 = nc.dram_tensor("w1", [16, D, D], F32, kind="ExternalInput").ap()
    w3 = nc.dram_tensor("w3", [16, D, D], F32, kind="ExternalInput").ap()
    w2 = nc.dram_tensor("w2", [16, D, D], F32, kind="ExternalInput").ap()
    lng = nc.dram_tensor("lng", [128, D], F32, kind="ExternalInput").ap()
    lnb = nc.dram_tensor("lnb", [128, D], F32, kind="ExternalInput").ap()
    y = nc.dram_tensor("y", [T, D], F32, kind="ExternalOutput").ap()
    if debug:
        gdbg = nc.dram_tensor("gdbg", [NT // GT, 128, GT * 16], F32, kind="ExternalOutput").ap()
    with ExitStack() as es:
        kb = KB(nc, es)
        ident = kb.sb([128, 128], F32)
        make_ident(kb, ident)
        rws = kb.sb([128, 8, 16], F32)
        kb.dma('sp', rws[:], rw.rearrange("(c p) e -> p c e", p=128), rws, writes=[rws])
        rb = kb.sb([128, GT * 16], F32)
        kb.dma('sp', rb[:], rbias[:, :], rb, writes=[rb])
        g = kb.sb([128, D], F32)
        b = kb.sb([128, D], F32)
        kb.dma('sp', g[:], lng[:, :], g, writes=[g])
        kb.dma('sp', b[:], lnb[:, :], b, writes=[b])
        L = LNBufs(kb, nbuf=1)

        xin = [kb.sb([128, D], F32) for _ in range(2)]
        xT32 = [kb.sb([128, 8, 128], F32) for _ in range(2)]
        xT = kb.sb([128, 8, GT * 128], BF16)
        xTv = [kb.view(f"xTv{t}") for t in range(GT)]
        acc = kb.sb([128, GT, D], F32)
        accv = [[kb.view() for _ in range(2)] for t in range(GT)]
        wb = [[kb.sb([128, 8, D], BF16) for _ in range(3)] for _ in range(2)]
        hT = [kb.sb([128, 8, 512], BF16) for _ in range(1)]
        hTv = [[kb.view() for _ in range(8)] for _ in range(1)]
        sil = [kb.sb([128, 512], BF16) for _ in range(2)]
        banks = [kb.ps([128, 512], F32) for _ in range(8)]
        NG = GT * 16
        aff = kb.sb([128, NG], F32)
        affv = [kb.view() for _ in range(GT)]
        S8 = kb.sb([128, GT * 4, 8], F32)
        Pt = kb.sb([128, GT * 4, 6], F32)
        gs = kb.sb([128, GT * 4], F32)
        gmax = kb.sb([128, GT], F32)
        ghot = kb.sb([128, GT * 4], F32)
        c1 = kb.sb([128, GT * 4, 4], F32)
        c2 = kb.sb([128, GT * 4, 4], F32)
        GA = kb.sb([128, GT, 16], F32)
        den = kb.sb([128, GT], F32)
        G = kb.sb([128, GT, 16], F32)

        wsrc = [w1, w3, w2]
        nsteps = (NT // GT) * NE
        step_list = [(gi, e) for gi in range(NT // GT) for e in range(NE)]

        def load_weights(si):
            gi, e = step_list[si]
            slot = si % 2
            for j in range(3):
                kb.dma('pool', wb[slot][j][:], wsrc[j][e].rearrange("(c p) f -> p c f", p=128), wb[slot][j],
                       writes=[wb[slot][j]])

        if nsteps > 0:
            load_weights(0)
        ybank = 0
        out_toks = []
        for gi in range(NT // GT):
            for t in range(GT):
                tile = gi * GT + t
                xi = xin[t % 2]
                x32 = xT32[t % 2]
                kb.dma('sp', xi[:], x[tile * 128:(tile + 1) * 128, :], xi, writes=[xi])
                for hf in range(2):
                    bk = banks[4 + hf]
                    kb.group('pe', [lambda e, c=c, bk=bk, xi=xi: e.transpose(bk[:, (c % 4) * 128:(c % 4 + 1) * 128],
                                                                               xi[:, c * 128:(c + 1) * 128], ident[:])
                                    for c in range(hf * 4, hf * 4 + 4)], reads=[xi, ident], writes=[bk])
                    if True:
                        kb.op('act', lambda e, bk=bk, hf=hf, x32=x32: e.copy(
                            out=x32[:, hf * 4:(hf + 1) * 4, :], in_=bk[:].rearrange("p (c t) -> p c t", c=4)),
                            reads=[bk], writes=[x32] if hf == 0 else [], deps=x32.w if hf == 1 else ())
                    kb.op('dve', lambda e, hf=hf, t=t, x32=x32: e.tensor_copy(
                        out=xT[:, hf * 4:(hf + 1) * 4, t * 128:(t + 1) * 128], in_=x32[:, hf * 4:(hf + 1) * 4, :]),
                        reads=[], writes=[xTv[t]] if hf == 0 else [], deps=list(xTv[t].w if hf == 1 else ()) + [Tok(kb.sems['act'], kb.cnt['act'])])
                x32.w = [Tok(kb.sems['act'], kb.cnt['act'])]
                xTv[t].w = [Tok(kb.sems['dve'], kb.cnt['dve'])]
                x32.r[id(kb.sems['dve'])] = Tok(kb.sems['dve'], kb.cnt['dve'])
                rbk = banks[6]
                kb.group('pe', [lambda e, c=c, x32=x32, rbk=rbk: e.matmul(rbk[:, 0:16], x32[:, c, :], rws[:, c, :],
                                                                         start=(c == 0), stop=(c == 7))
                                for c in range(8)], reads=[x32, rws], writes=[rbk])
                kb.op('act', lambda e, t=t, rbk=rbk: e.activation(out=aff[:, t * 16:(t + 1) * 16], in_=rbk[:, 0:16],
                                                                   func=AF.Sigmoid), reads=[rbk], writes=[affv[t]])
            S4 = S8[:, :, 0:4]
            aff4 = aff[:].rearrange("p (g j) -> p g j", j=4)
            kb.op('dve', lambda e: e.tensor_tensor(out=S8[:, :, 0:4], in0=aff4, in1=rb[:].rearrange("p (g j) -> p g j", j=4),
                                                   op=ALU.add), reads=affv + [rb], writes=[S8])
            kb.op('dve', lambda e: e.tensor_copy(out=S8[:, :, 4:8], in_=S8[:, :, 0:4]), reads=[S8], writes=[S8])
            pairs = [(0, 1), (0, 2), (0, 3), (1, 2), (1, 3), (2, 3)]
            for i, (a, bb) in enumerate(pairs):
                kb.op('dve', lambda e, i=i, a=a, bb=bb: e.tensor_tensor(out=Pt[:, :, i], in0=S8[:, :, a], in1=S8[:, :, bb],
                                                                         op=ALU.add), reads=[S8], writes=[Pt])
            kb.op('dve', lambda e: e.tensor_reduce(out=gs[:], in_=Pt[:], axis=AX.X, op=ALU.max), reads=[Pt], writes=[gs])
            kb.op('dve', lambda e: e.tensor_reduce(out=gmax[:], in_=gs[:].rearrange("p (t g) -> p t g", g=4), axis=AX.X,
                                                   op=ALU.max), reads=[gs], writes=[gmax])
            kb.op('dve', lambda e: e.tensor_tensor(out=ghot[:].rearrange("p (t g) -> p t g", g=4),
                                                   in0=gs[:].rearrange("p (t g) -> p t g", g=4),
                                                   in1=gmax[:].unsqueeze(2).broadcast_to([128, GT, 4]), op=ALU.is_ge),
                  reads=[gs, gmax], writes=[ghot])
            kb.op('dve', lambda e: e.tensor_tensor(out=c1[:], in0=S8[:, :, 0:4], in1=S8[:, :, 1:5], op=ALU.is_gt),
                  reads=[S8], writes=[c1])
            kb.op('dve', lambda e: e.tensor_tensor(out=c2[:], in0=S8[:, :, 0:4], in1=S8[:, :, 2:6], op=ALU.is_gt),
                  reads=[S8], writes=[c2])
            kb.op('dve', lambda e: e.tensor_tensor(out=c1[:], in0=c1[:], in1=c2[:], op=ALU.add), reads=[c1, c2], writes=[c1])
            kb.op('dve', lambda e: e.tensor_tensor(out=c2[:], in0=S8[:, :, 0:4], in1=S8[:, :, 3:7], op=ALU.is_gt),
                  reads=[S8], writes=[c2])
            kb.op('dve', lambda e: e.tensor_tensor(out=c1[:], in0=c1[:], in1=c2[:], op=ALU.add), reads=[c1, c2], writes=[c1])
            kb.op('dve', lambda e: e.scalar_tensor_tensor(out=c1[:], in0=c1[:], scalar=2.0,
                                                          in1=ghot[:].unsqueeze(2).broadcast_to([128, GT * 4, 4]),
                                                          op0=ALU.is_ge, op1=ALU.mult), reads=[c1, ghot], writes=[c1])
            kb.op('dve', lambda e: e.tensor_tensor(out=GA[:].rearrange("p t (g j) -> p (t g) j", j=4), in0=c1[:], in1=aff4,
                                                   op=ALU.mult), reads=[c1] + affv, writes=[GA])
            kb.op('dve', lambda e: e.tensor_reduce(out=den[:], in_=GA[:], axis=AX.X, op=ALU.add), reads=[GA], writes=[den])
            kb.op('dve', lambda e: e.reciprocal(out=den[:], in_=den[:]), reads=[den], writes=[den])
            kb.op('dve', lambda e: e.tensor_tensor(out=G[:], in0=GA[:], in1=den[:].unsqueeze(2).broadcast_to([128, GT, 16]),
                                                   op=ALU.mult), reads=[GA, den], writes=[G])
            if debug:
                out_toks.append(kb.dma('sp', gdbg[gi], G[:].rearrange("p t e -> p (t e)"), G, reads=[G]))
            for e_i in range(NE):
                si = gi * NE + e_i
                slot = si % 2
                if si + 1 < nsteps:
                    load_weights(si + 1)
                w1b, w3b, w2b = wb[slot]
                for tt in range(GT // 4):
                    hb = hT[0]
                    hv = hTv[0]
                    tsl = slice(tt * 512, (tt + 1) * 512)
                    for fc in range(8):
                        b1 = banks[fc % 2]
                        b3 = banks[2 + fc % 2]
                        sl_ = sil[fc % 2]
                        fsl = slice(fc * 128, (fc + 1) * 128)
                        kb.group('pe', [lambda e, c=c, b1=b1, fsl=fsl, w1b=w1b, tsl=tsl: e.matmul(b1[:], w1b[:, c, fsl], xT[:, c, tsl],
                                                                                 start=(c == 0), stop=(c == 7))
                                        for c in range(8)], reads=[w1b] + xTv[tt * 4:(tt + 1) * 4], writes=[b1])
                        kb.group('pe', [lambda e, c=c, b3=b3, fsl=fsl, w3b=w3b, tsl=tsl: e.matmul(b3[:], w3b[:, c, fsl], xT[:, c, tsl],
                                                                                 start=(c == 0), stop=(c == 7))
                                        for c in range(8)], reads=[w3b] + xTv[tt * 4:(tt + 1) * 4], writes=[b3])
                        kb.op('act', lambda e, b1=b1, sl_=sl_: e.activation(out=sl_[:], in_=b1[:], func=AF.Silu),
                              reads=[b1], writes=[sl_])
                        kb.op('dve', lambda e, b3=b3, sl_=sl_, fc=fc, hb=hb: e.tensor_tensor(out=hb[:, fc, :], in0=b3[:],
                                                                                          in1=sl_[:], op=ALU.mult),
                              reads=[b3, sl_], writes=[hv[fc]])
                    for t4 in range(4):
                        t = tt * 4 + t4
                        for oh in range(2):
                            yb = banks[4 + ybank % 4]
                            ybank += 1
                            osl = slice(oh * 512, (oh + 1) * 512)
                            kb.group('pe', [lambda e, fc=fc, yb=yb, t4=t4, osl=osl, hb=hb, w2b=w2b: e.matmul(
                                yb[:], hb[:, fc, t4 * 128:(t4 + 1) * 128], w2b[:, fc, osl], start=(fc == 0), stop=(fc == 7))
                                for fc in range(8)], reads=[w2b] + hv, writes=[yb])
                            av = accv[t][oh]
                            if e_i == 0:
                                kb.op('dve', lambda e, yb=yb, t=t, osl=osl, e_i=e_i: e.tensor_scalar(
                                    out=acc[:, t, osl], in0=yb[:], scalar1=G[:, t, e_i:e_i + 1], scalar2=None, op0=ALU.mult),
                                    reads=[yb, G], writes=[av])
                            else:
                                kb.op('dve', lambda e, yb=yb, t=t, osl=osl, e_i=e_i: e.scalar_tensor_tensor(
                                    out=acc[:, t, osl], in0=yb[:], scalar=G[:, t, e_i:e_i + 1], in1=acc[:, t, osl],
                                    op0=ALU.mult, op1=ALU.add), reads=[yb, G], writes=[av])
            for t in range(GT):
                tile = gi * GT + t
                xi = xin[t % 2]
                kb.dma('sp', xi[:], x[tile * 128:(tile + 1) * 128, :], xi, writes=[xi])
                out_toks.append(emit_ln(kb, L, xi, xi, [acc[:, t, 0:512], acc[:, t, 512:1024]], accv[t], g, b,
                                        y[tile * 128:(tile + 1) * 128, :]))
        kb.wait_only('sp', out_toks)
        kb.emit()
    return nc


BIGIDX = 16.0 * 1024 + 7.0
BIG2 = 65536.0


def build_moe_sparse(NT=32, C=768, NE=16, debug=False):
    nc = bass.Bass("TRN2", target_bir_lowering=False)
    T = NT * 128
    NS = C // 128
    NG = NT * 16
    x = nc.dram_tensor("x", [T, D], F32, kind="ExternalInput").ap()
    rw = nc.dram_tensor("rw", [D, 16], F32, kind="ExternalInput").ap()
    rbias = nc.dram_tensor("rbias", [128, NG], F32, kind="ExternalInput").ap()
    eoffd = nc.dram_tensor("eoff", [128, NG], F32, kind="ExternalInput").ap()
    w1 = nc.dram_tensor("w1", [16, D, D], F32, kind="ExternalInput").ap()
    w3 = nc.dram_tensor("w3", [16, D, D], F32, kind="ExternalInput").ap()
    w2 = nc.dram_tensor("w2", [16, D, D], F32, kind="ExternalInput").ap()
    lng = nc.dram_tensor("lng", [128, D], F32, kind="ExternalInput").ap()
    lnb = nc.dram_tensor("lnb", [128, D], F32, kind="ExternalInput").ap()
    y = nc.dram_tensor("y", [T, D], F32, kind="ExternalOutput").ap()
    xs = nc.dram_tensor("xs_scr", [16 * C, D], F32, kind="Internal").ap()
    ys = nc.dram_tensor("ys_scr", [16 * C, D], F32, kind="Internal").ap()
    if debug:
        dbg = nc.dram_tensor("dbg", [4, 128, NT], F32, kind="ExternalOutput").ap()
    with ExitStack() as es:
        kb = KB(nc, es)
        ident = kb.sb([128, 128], F32)
        make_ident(kb, ident)
        ltri = kb.sb([128, 128], F32)
        ones = kb.sb([128, 128], F32)
        kb.op('pool', lambda e: e.memset(ones[:], 1.0), writes=[ones])
        kb.op('pool', lambda e: e.memset(ltri[:], 1.0), writes=[ltri])
        kb.op('pool', lambda e: e.affine_select(out=ltri[:], in_=ltri[:], compare_op=ALU.is_gt, fill=0.0, base=0,
                                                pattern=[[1, 128]], channel_multiplier=-1), writes=[ltri])
        rws = kb.sb([128, 8, 16], F32)
        kb.dma('sp', rws[:], rw.rearrange("(c p) e -> p c e", p=128), rws, writes=[rws])
        rb = kb.sb([128, NG], F32)
        kb.dma('sp', rb[:], rbias[:, :], rb, writes=[rb])
        eoff = kb.sb([128, NG], F32)
        kb.dma('sp', eoff[:], eoffd[:, :], eoff, writes=[eoff])
        g = kb.sb([128, D], F32)
        b = kb.sb([128, D], F32)
        kb.dma('sp', g[:], lng[:, :], g, writes=[g])
        kb.dma('sp', b[:], lnb[:, :], b, writes=[b])
        L = LNBufs(kb, nbuf=1)
        xin = [kb.sb([128, D], F32) for _ in range(2)]
        x32 = [kb.sb([128, 8, 128], F32) for _ in range(2)]
        xTs = [kb.sb([128, 8, C], BF16) for _ in range(2)]
        xTvs = [[kb.view() for _ in range(NS)] for _ in range(2)]
        xin6 = xin + [kb.sb([128, D], F32) for _ in range(max(0, NS - 2))]
        w13 = [[kb.sb([128, 8, D], BF16) for _ in range(2)] for _ in range(2)]
        w2b = kb.sb([128, 8, D], BF16)
        hT = kb.sb([128, 8, 512], BF16)
        hv = [kb.view() for _ in range(8)]
        sil = [kb.sb([128, 512], BF16) for _ in range(2)]
        banks = [kb.ps([128, 512], F32) for _ in range(8)]
        xsb = kb.view("xs")
        ysb = kb.view("ys")
        wsrc = [w1, w3]

        def load_w13(e):
            for j in range(2):
                kb.dma('pool', w13[e % 2][j][:], wsrc[j][e].rearrange("(c p) f -> p c f", p=128), w13[e % 2][j],
                       writes=[w13[e % 2][j]])

        def load_w2(e):
            kb.dma('pool', w2b[:], w2[e].rearrange("(c p) f -> p c f", p=128), w2b, writes=[w2b])

        if NE > 0:
            load_w13(0)
            load_w2(0)

        aff = kb.sb([128, NG], F32)
        affv = [kb.view() for _ in range(NT)]
        for t in range(NT):
            xi = xin[t % 2]
            x3 = x32[t % 2]
            kb.dma('sp', xi[:], x[t * 128:(t + 1) * 128, :], xi, writes=[xi])
            for hf in range(2):
                bk = banks[4 + hf]
                kb.group('pe', [lambda e, c=c, bk=bk, xi=xi: e.transpose(bk[:, (c % 4) * 128:(c % 4 + 1) * 128],
                                                                           xi[:, c * 128:(c + 1) * 128], ident[:])
                                for c in range(hf * 4, hf * 4 + 4)], reads=[xi, ident], writes=[bk])
                tk = kb.op('act', lambda e, bk=bk, hf=hf, x3=x3: e.copy(out=x3[:, hf * 4:(hf + 1) * 4, :],
                                                                         in_=bk[:].rearrange("p (c t) -> p c t", c=4)),
                           reads=[bk], writes=[x3] if hf == 0 else [], deps=x3.w if hf == 1 else ())
            x3.w = [tk]
            rbk = banks[6 + t % 2]
            kb.group('pe', [lambda e, c=c, x3=x3, rbk=rbk: e.matmul(rbk[:, 0:16], x3[:, c, :], rws[:, c, :],
                                                                   start=(c == 0), stop=(c == 7)) for c in range(8)],
                     reads=[x3, rws], writes=[rbk])
            kb.op('act', lambda e, t=t, rbk=rbk: e.activation(out=aff[:, t * 16:(t + 1) * 16], in_=rbk[:, 0:16], func=AF.Sigmoid),
                  reads=[rbk], writes=[affv[t]])
        S8 = kb.sb([128, NT * 4, 8], F32)
        Pt = kb.sb([128, NT * 4, 6], F32)
        gs = kb.sb([128, NT * 4], F32)
        gmax = kb.sb([128, NT], F32)
        ghot = kb.sb([128, NT * 4], F32)
        c1 = kb.sb([128, NT * 4, 4], F32)
        c2 = kb.sb([128, NT * 4, 4], F32)
        GA = kb.sb([128, NT, 16], F32)
        den = kb.sb([128, NT], F32)
        G = kb.sb([128, NT, 16], F32)
        aff4 = aff[:].rearrange("p (g j) -> p g j", j=4)

        def dv(fn, reads, writes):
            return kb.op('dve', fn, reads=reads, writes=writes)
        dv(lambda e: e.tensor_tensor(out=S8[:, :, 0:4], in0=aff4, in1=rb[:].rearrange("p (g j) -> p g j", j=4), op=ALU.add), affv + [rb], [S8])
        dv(lambda e: e.tensor_copy(out=S8[:, :, 4:8], in_=S8[:, :, 0:4]), [S8], [S8])
        for i, (a, bb) in enumerate([(0, 1), (0, 2), (0, 3), (1, 2), (1, 3), (2, 3)]):
            dv(lambda e, i=i, a=a, bb=bb: e.tensor_tensor(out=Pt[:, :, i], in0=S8[:, :, a], in1=S8[:, :, bb], op=ALU.add), [S8], [Pt])
        dv(lambda e: e.tensor_reduce(out=gs[:], in_=Pt[:], axis=AX.X, op=ALU.max), [Pt], [gs])
        dv(lambda e: e.tensor_reduce(out=gmax[:], in_=gs[:].rearrange("p (t g) -> p t g", g=4), axis=AX.X, op=ALU.max), [gs], [gmax])
        dv(lambda e: e.tensor_tensor(out=ghot[:].rearrange("p (t g) -> p t g", g=4), in0=gs[:].rearrange("p (t g) -> p t g", g=4),
                                     in1=gmax[:].unsqueeze(2).broadcast_to([128, NT, 4]), op=ALU.is_ge), [gs, gmax], [ghot])
        dv(lambda e: e.tensor_tensor(out=c1[:], in0=S8[:, :, 0:4], in1=S8[:, :, 1:5], op=ALU.is_gt), [S8], [c1])
        dv(lambda e: e.tensor_tensor(out=c2[:], in0=S8[:, :, 0:4], in1=S8[:, :, 2:6], op=ALU.is_gt), [S8], [c2])
        dv(lambda e: e.tensor_tensor(out=c1[:], in0=c1[:], in1=c2[:], op=ALU.add), [c1, c2], [c1])
        dv(lambda e: e.tensor_tensor(out=c2[:], in0=S8[:, :, 0:4], in1=S8[:, :, 3:7], op=ALU.is_gt), [S8], [c2])
        dv(lambda e: e.tensor_tensor(out=c1[:], in0=c1[:], in1=c2[:], op=ALU.add), [c1, c2], [c1])
        dv(lambda e: e.scalar_tensor_tensor(out=c1[:], in0=c1[:], scalar=2.0, in1=ghot[:].unsqueeze(2).broadcast_to([128, NT * 4, 4]),
                                            op0=ALU.is_ge, op1=ALU.mult), [c1, ghot], [c1])
        dv(lambda e: e.tensor_tensor(out=GA[:].rearrange("p t (g j) -> p (t g) j", j=4), in0=c1[:], in1=aff4, op=ALU.mult), [c1] + affv, [GA])
        dv(lambda e: e.tensor_reduce(out=den[:], in_=GA[:], axis=AX.X, op=ALU.add), [GA], [den])
        dv(lambda e: e.reciprocal(out=den[:], in_=den[:]), [den], [den])
        dv(lambda e: e.tensor_tensor(out=G[:], in0=GA[:], in1=den[:].unsqueeze(2).broadcast_to([128, NT, 16]), op=ALU.mult), [GA, den], [G])
        chs = c1
        chs3 = chs[:].rearrange("p (t g) j -> p t (g j)", g=4)
        CS = kb.sb([128, NT, 16], F32)
        dv(lambda e: e.memset(CS[:, 0, :], 0.0), [], [CS])
        for t in range(1, NT):
            dv(lambda e, t=t: e.tensor_tensor(out=CS[:, t, :], in0=CS[:, t - 1, :], in1=chs3[:, t - 1, :], op=ALU.add), [chs], [CS])
        rkb = banks[4]
        fns = []
        for t in range(NT):
            fns.append(lambda e, t=t: e.matmul(rkb[:, t * 16:(t + 1) * 16], ltri[:], chs3[:, t, :], start=True, stop=False))
            fns.append(lambda e, t=t: e.matmul(rkb[:, t * 16:(t + 1) * 16], ones[:], CS[:, t, :], start=False, stop=True))
        kb.group('pe', fns, reads=[ltri, ones, chs, CS], writes=[rkb])
        rank = kb.sb([128, NG], F32)
        sel = kb.sb([128, NG], F32)
        slotv = kb.sb([128, NT, 16], F32)
        slot2 = kb.sb([128, NT, 16], F32)
        Gv = S8
        Gv = kb.sb([128, NT, 16], F32)
        lo = kb.sb([128, NT], F32)
        hi = kb.sb([128, NT], F32)
        glo = kb.sb([128, NT], F32)
        ghi = kb.sb([128, NT], F32)
        lo_t = [kb.sb([128, 1], I32) for _ in range(NT)]
        hi_t = [kb.sb([128, 1], I32) for _ in range(NT)]
        chs_f = chs[:].rearrange("p a j -> p (a j)")
        dv(lambda e: e.tensor_copy(out=rank[:], in_=rkb[:, 0:NG]), [rkb], [rank])
        dv(lambda e: e.scalar_tensor_tensor(out=sel[:], in0=rank[:], scalar=float(C), in1=chs_f, op0=ALU.is_lt, op1=ALU.mult), [rank, chs], [sel])
        dv(lambda e: e.scalar_tensor_tensor(out=rank[:], in0=rank[:], scalar=-BIGIDX, in1=eoff[:], op0=ALU.add, op1=ALU.add), [rank, eoff], [rank])
        dv(lambda e: e.tensor_tensor(out=rank[:], in0=rank[:], in1=sel[:], op=ALU.mult), [rank, sel], [rank])
        dv(lambda e: e.tensor_scalar(out=slotv[:].rearrange("p t e -> p (t e)"), in0=rank[:], scalar1=BIGIDX, scalar2=None, op0=ALU.add), [rank], [slotv])
        dv(lambda e: e.tensor_tensor(out=Gv[:].rearrange("p t e -> p (t e)"), in0=G[:].rearrange("p t e -> p (t e)"), in1=sel[:], op=ALU.mult), [G, sel], [Gv])
        dv(lambda e: e.tensor_reduce(out=lo[:], in_=slotv[:], axis=AX.X, op=ALU.min), [slotv], [lo])
        dv(lambda e: e.tensor_tensor(out=slot2[:], in0=slotv[:], in1=lo[:].unsqueeze(2).broadcast_to([128, NT, 16]), op=ALU.is_equal), [slotv, lo], [slot2])
        dv(lambda e: e.tensor_tensor(out=GA[:], in0=Gv[:], in1=slot2[:], op=ALU.mult), [Gv, slot2], [GA])
        dv(lambda e: e.tensor_reduce(out=glo[:], in_=GA[:], axis=AX.X, op=ALU.add), [GA], [glo])
        dv(lambda e: e.scalar_tensor_tensor(out=slot2[:], in0=slot2[:], scalar=BIG2, in1=slotv[:], op0=ALU.mult, op1=ALU.add), [slot2, slotv], [slot2])
        dv(lambda e: e.tensor_reduce(out=hi[:], in_=slot2[:], axis=AX.X, op=ALU.min), [slot2], [hi])
        dv(lambda e: e.tensor_tensor(out=slot2[:], in0=slot2[:], in1=hi[:].unsqueeze(2).broadcast_to([128, NT, 16]), op=ALU.is_equal), [slot2, hi], [slot2])
        dv(lambda e: e.tensor_tensor(out=GA[:], in0=Gv[:], in1=slot2[:], op=ALU.mult), [Gv, slot2], [GA])
        dv(lambda e: e.tensor_reduce(out=ghi[:], in_=GA[:], axis=AX.X, op=ALU.add), [GA], [ghi])
        for t in range(NT):
            dv(lambda e, t=t: e.tensor_copy(out=lo_t[t][:], in_=lo[:, t:t + 1]), [lo], [lo_t[t]])
            dv(lambda e, t=t: e.tensor_copy(out=hi_t[t][:], in_=hi[:, t:t + 1]), [hi], [hi_t[t]])
        out_toks = []
        if debug:
            for k_, src in enumerate([lo, hi, glo, ghi]):
                out_toks.append(kb.dma('sp', dbg[k_], src[:], src, reads=[src]))
        bound = 16 * C - 1
        sc_toks = []
        for t in range(NT):
            xi = xin[t % 2]
            kb.dma('sp', xi[:], x[t * 128:(t + 1) * 128, :], xi, writes=[xi])
            kb.idma(xs[:, :], xi[:, :], lo_t[t][:, :], True, xsb, bound, reads=[xi, lo_t[t]], writes=[xsb])
            tk_s = kb.idma(xs[:, :], xi[:, :], hi_t[t][:, :], True, xsb, bound, reads=[xi, hi_t[t]], writes=[xsb])
            sc_toks.append(tk_s)
            if len(sc_toks) >= 3:
                kb.wait_only('pool', [sc_toks[-3]])
        ybank = 0
        chunks = []
        o_ = 0
        while o_ < C:
            wdt = min(512, C - o_)
            chunks.append((o_, wdt))
            o_ += wdt
        ybank_box = [0]

        def prep_load(e):
            for s in range(NS):
                r0 = e * C + s * 128
                kb.dma('sp', xin6[s][:], xs[r0:r0 + 128, :], xin6[s], reads=[xsb], writes=[xin6[s]])

        def prep_tr(e):
            xT_, xTv_ = xTs[e % 2], xTvs[e % 2]
            for s in range(NS):
                xi = xin6[s]
                for hf in range(2):
                    bk = banks[4 + (ybank_box[0] % 4)]
                    ybank_box[0] += 1
                    kb.group('pe', [lambda e_, c=c, bk=bk, xi=xi: e_.transpose(bk[:, (c % 4) * 128:(c % 4 + 1) * 128],
                                                                                xi[:, c * 128:(c + 1) * 128], ident[:])
                                    for c in range(hf * 4, hf * 4 + 4)], reads=[xi, ident], writes=[bk])
                    if hf == 0:
                        ta = kb.op('act', lambda e_, bk=bk, s=s, xT_=xT_: e_.copy(out=xT_[:, 0:4, s * 128:(s + 1) * 128],
                                                                                   in_=bk[:].rearrange("p (c t) -> p c t", c=4)),
                                   reads=[bk], writes=[xTv_[s]])
                    else:
                        tb = kb.op('dve', lambda e_, bk=bk, s=s, xT_=xT_: e_.tensor_copy(out=xT_[:, 4:8, s * 128:(s + 1) * 128],
                                                                                          in_=bk[:].rearrange("p (c t) -> p c t", c=4)),
                                   reads=[bk], deps=[ta])
                        xTv_[s].w = [ta, tb]

        if NE > 0:
            prep_load(0)
            prep_tr(0)
        for e_i in range(NE):
            if e_i + 1 < NE:
                load_w13(e_i + 1)
                prep_load(e_i + 1)
            w1b, w3b = w13[e_i % 2]
            xT, xTv = xTs[e_i % 2], xTvs[e_i % 2]
            for ci_, (c0, wdt) in enumerate(chunks):
                tsl = slice(c0, c0 + wdt)
                tv = xTv[c0 // 128:(c0 + wdt) // 128]
                for fc in range(8):
                    b1 = banks[fc % 2]
                    b3 = banks[2 + fc % 2]
                    sl_ = sil[fc % 2]
                    fsl = slice(fc * 128, (fc + 1) * 128)
                    kb.group('pe', [lambda e, c=c, b1=b1, fsl=fsl, w1b=w1b, tsl=tsl, wdt=wdt, xT=xT: e.matmul(
                        b1[:, 0:wdt], w1b[:, c, fsl], xT[:, c, tsl], start=(c == 0), stop=(c == 7)) for c in range(8)],
                        reads=[w1b] + tv, writes=[b1])
                    kb.group('pe', [lambda e, c=c, b3=b3, fsl=fsl, w3b=w3b, tsl=tsl, wdt=wdt, xT=xT: e.matmul(
                        b3[:, 0:wdt], w3b[:, c, fsl], xT[:, c, tsl], start=(c == 0), stop=(c == 7)) for c in range(8)],
                        reads=[w3b] + tv, writes=[b3])
                    kb.op('act', lambda e, b1=b1, sl_=sl_, wdt=wdt: e.activation(out=sl_[:, 0:wdt], in_=b1[:, 0:wdt], func=AF.Silu),
                          reads=[b1], writes=[sl_])
                    kb.op('dve', lambda e, b3=b3, sl_=sl_, fc=fc, wdt=wdt: e.tensor_tensor(out=hT[:, fc, 0:wdt], in0=b3[:, 0:wdt],
                                                                                           in1=sl_[:, 0:wdt], op=ALU.mult),
                          reads=[b3, sl_], writes=[hv[fc]])
                for t4 in range(wdt // 128):
                    s = c0 // 128 + t4
                    yst = x32[s % 2]
                    ysf = yst[:].rearrange("p c t -> p (c t)")
                    for oh in range(2):
                        yb = banks[4 + ybank_box[0] % 4]
                        ybank_box[0] += 1
                        osl = slice(oh * 512, (oh + 1) * 512)
                        kb.group('pe', [lambda e, fc=fc, yb=yb, t4=t4, osl=osl: e.matmul(
                            yb[:], hT[:, fc, t4 * 128:(t4 + 1) * 128], w2b[:, fc, osl], start=(fc == 0), stop=(fc == 7))
                            for fc in range(8)], reads=[w2b] + hv, writes=[yb])
                        if oh == 0:
                            t0_ = kb.op('act', lambda e, yb=yb, ysf=ysf, osl=osl: e.copy(out=ysf[:, osl], in_=yb[:]), reads=[yb], writes=[yst])
                        else:
                            t1_ = kb.op('dve', lambda e, yb=yb, ysf=ysf, osl=osl: e.tensor_copy(out=ysf[:, osl], in_=yb[:]), reads=[yb], deps=[t0_])
                            yst.w = [t0_, t1_]
                    r0 = e_i * C + s * 128
                    kb.dma('act', ys[r0:r0 + 128, :], ysf, ysb, reads=[yst], writes=[ysb])
                if ci_ == 0 and e_i + 1 < NE:
                    prep_tr(e_i + 1)
            if e_i + 1 < NE:
                load_w2(e_i + 1)
        pe_done = Tok(kb.sems['pe'], kb.cnt['pe'])
        tiles = []
        for sl_ in range(2):
            for j in range(2):
                flat = w13[sl_][j][:].rearrange("p c f -> p (c f)").bitcast(F32)
                for q_ in range(4):
                    bf_ = Buf(kb, flat[:, q_ * D:(q_ + 1) * D], f"alias{sl_}{j}{q_}")
                    bf_.r = {id(pe_done.sem): pe_done}
                    bf_.w = list(w13[sl_][j].w)
                    tiles.append(bf_)
        ga2, gb2, acc2, xin3 = tiles[0:2] + tiles[12:13], tiles[2:4] + tiles[13:14], tiles[4:6], tiles[6:8] + xin
        L3 = LNBufs.__new__(LNBufs)
        L3.kb, L3.n, L3.i = kb, 2, 0
        L3.z, L3.o = tiles[8:10], tiles[10:12]
        L3.st = [kb.sb([128, 12], F32) for _ in range(2)]
        L3.mv = [kb.sb([128, 2], F32) for _ in range(2)]
        L3.rs = [kb.sb([128, 1], F32) for _ in range(2)]
        L3.nb = [kb.sb([128, 1], F32) for _ in range(2)]
        L3.eps = L.eps
        for bf_ in ga2 + gb2:
            kb.op('dve', lambda e, bf_=bf_: e.memset(bf_[:], 0.0), writes=[bf_])
        def fetch(t):
            xi = xin3[t % 4]
            kb.dma('sp' if t % 2 == 0 else 'act', xi[:], x[t * 128:(t + 1) * 128, :], xi, writes=[xi])
            kb.idma(ga2[t % 3][:, :], ys[:, :], lo_t[t][:, :], False, ga2[t % 3], bound, reads=[ysb, lo_t[t]], writes=[ga2[t % 3]])
            kb.idma(gb2[t % 3][:, :], ys[:, :], hi_t[t][:, :], False, gb2[t % 3], bound, reads=[ysb, hi_t[t]], writes=[gb2[t % 3]])

        fetch(0)
        fetch(1)
        avs = {}

        def front3(t):
            ga_, gb3, acc = ga2[t % 3], gb2[t % 3], acc2[t % 2]
            if t + 2 < NT:
                fetch(t + 2)
            kb.op('act', lambda e: e.activation(out=acc[:], in_=ga_[:], func=AF.Copy, scale=glo[:, t:t + 1]),
                  reads=[ga_, glo], writes=[acc])
            av = [kb.view(), kb.view()]
            for hf in range(2):
                sl = slice(hf * 512, (hf + 1) * 512)
                kb.op('dve', lambda e, sl=sl: e.scalar_tensor_tensor(
                    out=acc[:, sl], in0=gb3[:, sl], scalar=ghi[:, t:t + 1], in1=acc[:, sl], op0=ALU.mult, op1=ALU.add),
                    reads=[gb3, ghi, acc], writes=[av[hf]])
            avs[t] = av

        def ln1_3(t):
            xi, acc, av = xin3[t % 4], acc2[t % 2], avs.pop(t)
            slot = emit_ln1(kb, L3, xi, xi, [acc[:, 0:512], acc[:, 512:1024]], av)
            acc.r.update(av[0].r)
            acc.r.update(av[1].r)
            return slot

        def ln2_3(t, slot):
            return emit_ln2(kb, L3, slot, g, b, y[t * 128:(t + 1) * 128, :], dma_eng='sp' if t % 2 == 0 else 'act', b_eng='dve')
        out_toks += skewed(NT, front3, ln1_3, ln2_3)
        kb.wait_only('sp', out_toks)
        kb.emit()
    return nc


def pool_tables(first_is_start, last_is_end):
    Bm = np.zeros((3, 4, 128, 128), np.float32)
    Bh = np.zeros((3, 4, 16, 128), np.float32)
    for var in range(3):
        start_edge = (var == 0 and first_is_start)
        end_edge = (var == 2 and last_is_end)
        for g, w in enumerate((2, 4, 8, 16)):
            half = w // 2
            for t in range(128):
                lo, hi = t - half, t + half - 1
                if start_edge:
                    lo = max(lo, 0)
                if end_edge:
                    hi = min(hi, 127)
                cnt = hi - lo + 1
                for s in range(lo, hi + 1):
                    if 0 <= s < 128:
                        Bm[var, g, s, t] += 1.0 / cnt
                    elif s < 0:
                        Bh[var, g, 8 + s, t] += 1.0 / cnt
                    else:
                        Bh[var, g, 8 + (s - 128), t] += 1.0 / cnt
                Bm[var, g, t, t] -= 1.0
    return Bm, Bh


def build_pool(NT=32):
    nc = bass.Bass("TRN2", target_bir_lowering=False)
    T = NT * 128
    xp = nc.dram_tensor("xp", [T + 16, D], F32, kind="ExternalInput").ap()
    bm = nc.dram_tensor("bm", [128, 12 * 128], F32, kind="ExternalInput").ap()
    bh = nc.dram_tensor("bh", [16, 12 * 128], F32, kind="ExternalInput").ap()
    pw = nc.dram_tensor("pw", [4, 256, 256], F32, kind="ExternalInput").ap()
    psc = nc.dram_tensor("psc", [128, D], F32, kind="ExternalInput").ap()
    lng = nc.dram_tensor("lng", [128, D], F32, kind="ExternalInput").ap()
    lnb = nc.dram_tensor("lnb", [128, D], F32, kind="ExternalInput").ap()
    y = nc.dram_tensor("y", [T, D], F32, kind="ExternalOutput").ap()
    with ExitStack() as es:
        kb = KB(nc, es)
        g = kb.sb([128, D], F32)
        b = kb.sb([128, D], F32)
        kb.dma('sp', g[:], lng[:, :], g, writes=[g])
        kb.dma('sp', b[:], lnb[:, :], b, writes=[b])
        L = LNBufs(kb, nbuf=2)
        Bm = kb.sb([128, 12 * 128], BF16)
        Bh = kb.sb([16, 12 * 128], BF16)
        kb.dma('pool', Bm[:], bm[:, :], Bm, writes=[Bm])
        kb.dma('pool', Bh[:], bh[:, :], Bh, writes=[Bh])
        W32 = kb.sb([128, 4, 2, 256], F32)
        sc = kb.sb([128, D], F32)
        Wb = kb.sb([128, 4, 2, 256], BF16)
        kb.dma('sp', W32[:], pw.rearrange("g (hh p) e -> p g hh e", p=128), W32, writes=[W32])
        kb.dma('sp', sc[:], psc[:, :], sc, writes=[sc])
        kb.op('dve', lambda e: e.tensor_tensor(out=Wb[:], in0=W32[:],
                                               in1=sc[:].rearrange("p (g e) -> p g e", g=4).unsqueeze(2).broadcast_to([128, 4, 2, 256]),
                                               op=ALU.mult), reads=[W32, sc], writes=[Wb])
        xin = [kb.sb([128, D], F32) for _ in range(3)]
        xh = [kb.sb([16, D], F32) for _ in range(2)]
        xb = [kb.sb([128, D], BF16) for _ in range(2)]
        xhb = [kb.sb([16, D], BF16) for _ in range(2)]
        uT = [kb.sb([128, 8, 128], BF16) for _ in range(2)]
        uTa = [kb.view() for _ in range(2)]
        uTb = [kb.view() for _ in range(2)]
        pu = [[kb.ps([128, 512], F32) for _ in range(2)] for _ in range(2)]
        py = [[kb.ps([128, 512], F32) for _ in range(2)] for _ in range(2)]
        def front(i):
            var = 0 if i == 0 else (2 if i == NT - 1 else 1)
            xi = xin[i % 3]
            xhi = xh[i % 2]
            xbi = xb[i % 2]
            xhbi = xhb[i % 2]
            u = uT[i % 2]
            ua, ub = uTa[i % 2], uTb[i % 2]
            pui = pu[i % 2]
            pyi = py[i % 2]
            kb.dma('sp', xi[:], xp[8 + i * 128: 8 + (i + 1) * 128, :], xi, writes=[xi])
            kb.dma('sp', xhi[0:8, :], xp[i * 128: i * 128 + 8, :], xhi, writes=[xhi])
            kb.dma('sp', xhi[8:16, :], xp[8 + (i + 1) * 128: 16 + (i + 1) * 128, :], xhi, writes=[xhi])
            kb.op('act', lambda e, xi=xi, xbi=xbi: e.copy(out=xbi[:], in_=xi[:]), reads=[xi], writes=[xbi])
            kb.op('dve', lambda e, xhi=xhi, xhbi=xhbi: e.tensor_copy(out=xhbi[:], in_=xhi[:]), reads=[xhi], writes=[xhbi])
            for hf in range(2):
                fns = []
                for fc in range(hf * 4, hf * 4 + 4):
                    col = (var * 4 + fc // 2) * 128
                    fns.append(lambda e, fc=fc, col=col, xbi=xbi, bk=pui[hf]: e.matmul(
                        bk[:, (fc % 4) * 128:(fc % 4 + 1) * 128], xbi[:, fc * 128:(fc + 1) * 128], Bm[:, col:col + 128],
                        start=True, stop=False))
                    fns.append(lambda e, fc=fc, col=col, xhbi=xhbi, bk=pui[hf]: e.matmul(
                        bk[:, (fc % 4) * 128:(fc % 4 + 1) * 128], xhbi[:, fc * 128:(fc + 1) * 128], Bh[:, col:col + 128],
                        start=False, stop=True))
                kb.group('pe', fns, reads=[xbi, xhbi, Bm, Bh], writes=[pui[hf]])
            kb.op('act', lambda e, u=u, bk=pui[0]: e.copy(out=u[:, 0:4, :], in_=bk[:].rearrange("p (c t) -> p c t", c=4)),
                  reads=[pui[0]], writes=[ua])
            kb.op('dve', lambda e, u=u, bk=pui[1]: e.tensor_copy(out=u[:, 4:8, :], in_=bk[:].rearrange("p (c t) -> p c t", c=4)),
                  reads=[pui[1]], writes=[ub])
            for bkidx in range(2):
                fns = []
                for gg in range(bkidx * 2, bkidx * 2 + 2):
                    for hh in range(2):
                        fns.append(lambda e, gg=gg, hh=hh, u=u, bk=pyi[bkidx]: e.matmul(
                            bk[:, (gg % 2) * 256:(gg % 2 + 1) * 256], u[:, gg * 2 + hh, :], Wb[:, gg, hh, :],
                            start=(hh == 0), stop=(hh == 1)))
                kb.group('pe', fns, reads=[ua if bkidx == 0 else ub, Wb], writes=[pyi[bkidx]])

        def ln1(i):
            xi, pyi = xin[i % 3], py[i % 2]
            return emit_ln1(kb, L, xi, xi, [pyi[0][:], pyi[1][:]], pyi)

        def ln2(i, slot):
            return emit_ln2(kb, L, slot, g, b, y[i * 128:(i + 1) * 128, :], dma_eng='sp' if i % 2 == 0 else 'act')
        out_toks = skewed(NT, front, ln1, ln2)
        kb.wait_only('sp', out_toks)
        kb.emit()
    return nc


def build_qkv(NT=32):
    nc = bass.Bass("TRN2", target_bir_lowering=False)
    T = NT * 128
    x = nc.dram_tensor("x", [T, D], F32, kind="ExternalInput").ap()
    win = nc.dram_tensor("win", [D, 3 * D], F32, kind="ExternalInput").ap()
    cs = nc.dram_tensor("cs", [T, 64], F32, kind="ExternalInput").ap()
    qkv = nc.dram_tensor("qkv", [T, 3 * D], BF16, kind="ExternalOutput").ap()
    with ExitStack() as es:
        kb = KB(nc, es)
        ident = kb.sb([128, 128], F32)
        make_ident(kb, ident)
        wb = kb.sb([128, 8, 3 * D], BF16)
        wv = [kb.view() for _ in range(8)]
        for c in range(8):
            kb.dma('pool', wb[:, c, :], win[c * 128:(c + 1) * 128, :], wv[c], writes=[wv[c]])
        kb.op('pool', lambda e: e.tensor_scalar(out=wb[:, :, 0:D], in0=wb[:, :, 0:D], scalar1=0.125, scalar2=None, op0=ALU.mult),
              reads=[], writes=wv)
        xin = [kb.sb([128, D], F32) for _ in range(2)]
        cst = [kb.sb([128, 64], F32) for _ in range(2)]
        xT = [kb.sb([128, 8, 128], BF16) for _ in range(2)]
        kro = [kb.sb([128, 512], F32) for _ in range(2)]
        tm = [kb.sb([128, 8, 32], F32) for _ in range(4)]
        tp = [kb.sb([128, 8, 32], F32) for _ in range(4)]
        ost = [kb.sb([128, 3 * D], BF16) for _ in range(2)]
        ostv = [[kb.view() for _ in range(6)] for _ in range(2)]
        banks = [kb.ps([128, 512], F32) for _ in range(8)]
        nb = 0
        out_toks = []
        for i in range(NT):
            xi = xin[i % 2]
            ci = cst[i % 2]
            xt = xT[i % 2]
            oi = ost[i % 2]
            ov = ostv[i % 2]
            kr = kro[i % 2]
            kb.dma('sp', xi[:], x[i * 128:(i + 1) * 128, :], xi, writes=[xi])
            kb.dma('sp', ci[:], cs[i * 128:(i + 1) * 128, :], ci, writes=[ci])
            for hf in range(2):
                bk = banks[nb % 8]; nb += 1
                kb.group('pe', [lambda e, c=c, bk=bk, xi=xi: e.transpose(bk[:, (c % 4) * 128:(c % 4 + 1) * 128],
                                                                           xi[:, c * 128:(c + 1) * 128], ident[:])
                                for c in range(hf * 4, hf * 4 + 4)], reads=[xi, ident], writes=[bk])
                eng = 'act' if hf == 0 else 'dve'
                fn = (lambda e, bk=bk, hf=hf, xt=xt: e.copy(out=xt[:, hf * 4:(hf + 1) * 4, :], in_=bk[:].rearrange("p (c t) -> p c t", c=4))) \
                    if hf == 0 else (lambda e, bk=bk, hf=hf, xt=xt: e.tensor_copy(out=xt[:, hf * 4:(hf + 1) * 4, :], in_=bk[:].rearrange("p (c t) -> p c t", c=4)))
                if hf == 0:
                    t_a = kb.op(eng, fn, reads=[bk], writes=[xt])
                else:
                    t_b = kb.op(eng, fn, reads=[bk], deps=[t_a])
                    xt.w = [t_a, t_b]
            cosb = ci[:, 0:32].unsqueeze(1).broadcast_to([128, 8, 32])
            sinb = ci[:, 32:64].unsqueeze(1).broadcast_to([128, 8, 32])
            for blk in range(6):
                bk = banks[nb % 8]; nb += 1
                kb.group('pe', [lambda e, c=c, bk=bk, blk=blk, xt=xt: e.matmul(bk[:], xt[:, c, :], wb[:, c, blk * 512:(blk + 1) * 512],
                                                                               start=(c == 0), stop=(c == 7)) for c in range(8)],
                         reads=[xt] + wv, writes=[bk])
                osl = slice(blk * 512, (blk + 1) * 512)
                if blk in (0, 2):
                    if blk == 0:
                        eng, tt, src, sbuf = 'dve', tm, bk, bk
                    else:
                        kb.op('act', lambda e, bk=bk, kr=kr: e.copy(out=kr[:], in_=bk[:]), reads=[bk], writes=[kr])
                        eng, tt, src, sbuf = 'pool', tp, kr, kr
                    s4 = src[:].rearrange("p (h two j) -> p h two j", two=2, j=32)
                    o4 = oi[:, osl].rearrange("p (h two j) -> p h two j", two=2, j=32)
                    t1, t2 = s4[:, :, 0, :], s4[:, :, 1, :]
                    kb.op(eng, lambda e, t1=t1, tt=tt, cosb=cosb: e.tensor_tensor(out=tt[0][:], in0=t1, in1=cosb, op=ALU.mult), reads=[sbuf, ci], writes=[tt[0]])
                    kb.op(eng, lambda e, t2=t2, tt=tt, sinb=sinb: e.tensor_tensor(out=tt[1][:], in0=t2, in1=sinb, op=ALU.mult), reads=[sbuf, ci], writes=[tt[1]])
                    kb.op(eng, lambda e, t2=t2, tt=tt, cosb=cosb: e.tensor_tensor(out=tt[2][:], in0=t2, in1=cosb, op=ALU.mult), reads=[sbuf, ci], writes=[tt[2]])
                    kb.op(eng, lambda e, t1=t1, tt=tt, sinb=sinb: e.tensor_tensor(out=tt[3][:], in0=t1, in1=sinb, op=ALU.mult), reads=[sbuf, ci], writes=[tt[3]])
                    kb.op(eng, lambda e, tt=tt, o4=o4: e.tensor_tensor(out=o4[:, :, 0, :], in0=tt[0][:], in1=tt[1][:], op=ALU.subtract), reads=[tt[0], tt[1]], writes=[ov[blk]])
                    tk = kb.op(eng, lambda e, tt=tt, o4=o4: e.tensor_tensor(out=o4[:, :, 1, :], in0=tt[2][:], in1=tt[3][:], op=ALU.add), reads=[tt[2], tt[3]], deps=ov[blk].w)
                    ov[blk].w = ov[blk].w + [tk]
                else:
                    kb.op('act', lambda e, bk=bk, oi=oi, osl=osl: e.copy(out=oi[:, osl], in_=bk[:]), reads=[bk], writes=[ov[blk]])
            out_toks.append(kb.dma('act', qkv[i * 128:(i + 1) * 128, :], oi[:], oi, reads=ov))
        kb.wait_only('sp', out_toks)
        kb.emit()
    return nc


def build_attn(NB=4):
    nc = bass.Bass("TRN2", target_bir_lowering=False)
    S = S_LEN
    qA, kA, vA, oA = [], [], [], []
    for di, d in enumerate(DILS):
        Ls = S // d
        nt = Ls // 128
        qA.append(nc.dram_tensor(f"qA{di}", [NB, 64, S], BF16, kind="ExternalInput").ap())
        kA.append(nc.dram_tensor(f"kA{di}", [NB, 64, d * (Ls + 128)], BF16, kind="ExternalInput").ap())
        vA.append(nc.dram_tensor(f"vA{di}", [NB, 128, d * (nt + 1) * 65], BF16, kind="ExternalInput").ap())
        oA.append(nc.dram_tensor(f"oA{di}", [NB, 128, 64 * 65], F32, kind="ExternalOutput").ap())
    qB = nc.dram_tensor("qB", [NB, 64, S], BF16, kind="ExternalInput").ap()
    kB = nc.dram_tensor("kB", [NB, 64, S], BF16, kind="ExternalInput").ap()
    vB = nc.dram_tensor("vB", [NB, 128, 64 * 65], BF16, kind="ExternalInput").ap()
    oB = nc.dram_tensor("oB", [NB, 128, 64 * 64], F32, kind="ExternalOutput").ap()
    ebias = nc.dram_tensor("ebias", [128, 25 * 128], F32, kind="ExternalInput").ap()
    maskd = nc.dram_tensor("maskd", [128, 256], F32, kind="ExternalInput").ap()
    with ExitStack() as es:
        kb = KB(nc, es)
        mask = kb.sb([128, 256], BF16)
        kb.dma('pool', mask[:], maskd[:, :], mask, writes=[mask])
        eb32 = kb.sb([128, 25 * 128], F32)
        E = kb.sb([128, 25 * 128], BF16)
        kb.dma('sp', eb32[:], ebias[:, :], eb32, writes=[eb32])
        kb.op('act', lambda e: e.activation(out=E[:], in_=eb32[:], func=AF.Exp), reads=[eb32], writes=[E])
        KMAX = 16 * (512 + 128)
        qT = [kb.sb([64, S], BF16) for _ in range(2)]
        kT = [kb.sb([64, KMAX], BF16) for _ in range(2)]
        V = [kb.sb([128, 80 * 65], BF16) for _ in range(2)]
        osb = [kb.sb([128, 64 * 65], F32) for _ in range(2)]
        pT = [kb.sb([128, 256], BF16) for _ in range(6)]
        rden = [kb.sb([128, 1], F32) for _ in range(2)]
        sbk = [kb.ps([128, 512], F32) for _ in range(4)]
        obk = [kb.ps([128, 512], F32) for _ in range(4)]
        stages = []
        for b in range(NB):
            for di in range(3):
                stages.append(('A', b, di))
            stages.append(('B', b, None))

        def load(si):
            kind, b, di = stages[si]
            sl = si % 2
            if kind == 'A':
                d = DILS[di]
                Ls = S // d
                nt = Ls // 128
                kb.dma('sp', qT[sl][:], qA[di][b], qT[sl], writes=[qT[sl]])
                kb.dma('sp', kT[sl][:, 0:d * (Ls + 128)], kA[di][b], kT[sl], writes=[kT[sl]])
                kb.dma('sp', V[sl][:, 0:d * (nt + 1) * 65], vA[di][b], V[sl], writes=[V[sl]])
            else:
                kb.dma('sp', qT[sl][:], qB[b], qT[sl], writes=[qT[sl]])
                kb.dma('sp', kT[sl][:, 0:S], kB[b], kT[sl], writes=[kT[sl]])
                kb.dma('sp', V[sl][:, 0:64 * 65], vB[b], V[sl], writes=[V[sl]])

        load(0)
        out_toks = []
        fronts, backs = [], []
        LOOK = 3

        def add_step(si, first, last, front_fn, back_fn):
            n = len(fronts)

            def front(n=n):
                front_fn(n)

            def back(n=n):
                if first and si + 1 < len(stages):
                    load(si + 1)
                back_fn(n)
                if last:
                    kind, b, di = stages[si]
                    ob = osb[si % 2]
                    if kind == 'A':
                        out_toks.append(kb.dma('sp', oA[di][b], ob[:], ob, reads=[ob]))
                    else:
                        out_toks.append(kb.dma('sp', oB[b], ob[:, 0:64 * 64], ob, reads=[ob]))
            fronts.append(front)
            backs.append(back)

        for si, (kind, b, di) in enumerate(stages):
            sl = si % 2
            q, k, v, ob = qT[sl], kT[sl], V[sl], osb[sl]
            if kind == 'A':
                d = DILS[di]
                Ls = S // d
                nt = Ls // 128
                for r in range(d):
                    for j in range(nt + 1):
                        q0 = 128 * (j - 1) if j >= 1 else 0
                        q1 = 128 * (j + 1) if j <= nt - 1 else 128 * nt
                        w = q1 - q0
                        moff = 128 if j == 0 else 0
                        koff = r * (Ls + 128) + 128 * j
                        vt = r * (nt + 1) + j
                        qa = r * Ls + q0

                        def front_fn(step, k=k, q=q, koff=koff, qa=qa, w=w, moff=moff):
                            sb_ = sbk[step % 4]
                            p = pT[step % 6]
                            kb.op('pe', lambda e: e.matmul(sb_[:, 0:w], k[:, koff:koff + 128], q[:, qa:qa + w], start=True, stop=True),
                                  reads=[k, q], writes=[sb_])
                            kb.op('act', lambda e: e.activation(out=p[:, 0:w], in_=sb_[:, 0:w], func=AF.Exp), reads=[sb_], writes=[p])
                            meng = 'dve' if step % 2 == 0 else 'pool'
                            kb.op(meng, lambda e: e.tensor_tensor(out=p[:, 0:w], in0=p[:, 0:w], in1=mask[:, moff:moff + w], op=ALU.mult),
                                  reads=[p, mask], writes=[p])

                        def back_fn(step, j=j, nt=nt, r=r, v=v, vt=vt, ob=ob):
                            p = pT[step % 6]
                            if j >= 1:
                                o_ = obk[(j - 1) % 4]
                                kb.op('pe', lambda e: e.matmul(o_[:, 0:65], p[:, 0:128], v[:, vt * 65:(vt + 1) * 65], start=False, stop=True),
                                      reads=[p, v], writes=[], deps=o_.w + list(o_.r.values()))
                                o_.w = [Tok(kb.sems['pe'], kb.cnt['pe'])]
                                blk = r * nt + (j - 1)
                                kb.op('dve', lambda e: e.tensor_copy(out=ob[:, blk * 65:(blk + 1) * 65], in_=o_[:, 0:65]),
                                      reads=[o_], writes=[], deps=list(ob.r.values()))
                                ob.w = [Tok(kb.sems['dve'], kb.cnt['dve'])]
                            if j <= nt - 1:
                                o2 = obk[j % 4]
                                pc = 128 if j >= 1 else 0
                                kb.op('pe', lambda e: e.matmul(o2[:, 0:65], p[:, pc:pc + 128], v[:, vt * 65:(vt + 1) * 65], start=True, stop=False),
                                      reads=[p, v], writes=[o2])
                        add_step(si, r == 0 and j == 0, r == d - 1 and j == nt, front_fn, back_fn)
            else:
                for m in range(64):
                    cls = {0: 0, 1: 1, 62: 3, 63: 4}.get(m, 2)
                    bt = min(max(m - 2, 0), 59)
                    for j in range(5):
                        tile = bt + j
                        ecol = (cls * 5 + j) * 128

                        def front_fn(step, k=k, q=q, tile=tile, m=m, ecol=ecol):
                            sb_ = sbk[step % 4]
                            p = pT[step % 6]
                            kb.op('pe', lambda e: e.matmul(sb_[:, 0:128], k[:, tile * 128:(tile + 1) * 128], q[:, m * 128:(m + 1) * 128],
                                                           start=True, stop=True), reads=[k, q], writes=[sb_])
                            kb.op('act', lambda e: e.activation(out=p[:, 0:128], in_=sb_[:, 0:128], func=AF.Exp), reads=[sb_], writes=[p])
                            meng = 'dve' if step % 2 == 0 else 'pool'
                            kb.op(meng, lambda e: e.tensor_tensor(out=p[:, 0:128], in0=p[:, 0:128], in1=E[:, ecol:ecol + 128], op=ALU.mult),
                                  reads=[p, E], writes=[p])

                        def back_fn(step, j=j, m=m, v=v, tile=tile, ob=ob):
                            p = pT[step % 6]
                            o_ = obk[m % 4]
                            if j == 0:
                                kb.op('pe', lambda e: e.matmul(o_[:, 0:65], p[:, 0:128], v[:, tile * 65:(tile + 1) * 65], start=True, stop=False),
                                      reads=[p, v], writes=[o_])
                            else:
                                kb.op('pe', lambda e: e.matmul(o_[:, 0:65], p[:, 0:128], v[:, tile * 65:(tile + 1) * 65], start=False, stop=(j == 4)),
                                      reads=[p, v], writes=[], deps=o_.w)
                                o_.w = [Tok(kb.sems['pe'], kb.cnt['pe'])]
                            if j == 4:
                                rd = rden[m % 2]
                                kb.op('dve', lambda e: e.reciprocal(out=rd[:], in_=o_[:, 64:65]), reads=[o_], writes=[rd])
                                kb.op('dve', lambda e: e.tensor_scalar(out=ob[:, m * 64:(m + 1) * 64], in0=o_[:, 0:64], scalar1=rd[:, 0:1],
                                                                       scalar2=None, op0=ALU.mult),
                                      reads=[o_, rd], writes=[], deps=list(ob.r.values()))
                                ob.w = [Tok(kb.sems['dve'], kb.cnt['dve'])]
                        add_step(si, m == 0 and j == 0, m == 63 and j == 4, front_fn, back_fn)
        nsteps = len(fronts)
        for n in range(min(LOOK, nsteps)):
            fronts[n]()
        for n in range(nsteps):
            if n + LOOK < nsteps:
                fronts[n + LOOK]()
            backs[n]()
        kb.wait_only('sp', out_toks)
        kb.emit()
    return nc


def build_oproj(NT=32):
    nc = bass.Bass("TRN2", target_bir_lowering=False)
    T = NT * 128
    x = nc.dram_tensor("x", [T, D], F32, kind="ExternalInput").ap()
    oa = nc.dram_tensor("oa", [3, T, 8 * 65], F32, kind="ExternalInput").ap()
    obd = nc.dram_tensor("ob", [T, 512], F32, kind="ExternalInput").ap()
    wout = nc.dram_tensor("wout", [D, D], F32, kind="ExternalInput").ap()
    lng = nc.dram_tensor("lng", [128, D], F32, kind="ExternalInput").ap()
    lnb = nc.dram_tensor("lnb", [128, D], F32, kind="ExternalInput").ap()
    y = nc.dram_tensor("y", [T, D], F32, kind="ExternalOutput").ap()
    with ExitStack() as es:
        kb = KB(nc, es)
        ident = kb.sb([128, 128], F32)
        make_ident(kb, ident)
        g = kb.sb([128, D], F32)
        b = kb.sb([128, D], F32)
        kb.dma('sp', g[:], lng[:, :], g, writes=[g])
        kb.dma('sp', b[:], lnb[:, :], b, writes=[b])
        L = LNBufs(kb, nbuf=2)
        wb = kb.sb([128, 8, D], BF16)
        kb.dma('pool', wb[:], wout.rearrange("(c p) f -> p c f", p=128), wb, writes=[wb])
        xin = [kb.sb([128, D], F32) for _ in range(3)]
        oat = [[kb.sb([128, 8, 65], F32) for _ in range(3)] for _ in range(2)]
        rd = [kb.sb([128, 8], F32) for _ in range(2)]
        ot = [kb.sb([128, D], F32) for _ in range(2)]
        ota = [kb.view() for _ in range(2)]
        otb = [kb.view() for _ in range(2)]
        oT = [kb.sb([128, 8, 128], BF16) for _ in range(2)]
        oTa = [kb.view() for _ in range(2)]
        oTb = [kb.view() for _ in range(2)]
        pt = [[kb.ps([128, 512], F32) for _ in range(2)] for _ in range(2)]
        py = [[kb.ps([128, 512], F32) for _ in range(2)] for _ in range(2)]
        def front(i):
            xi = xin[i % 3]
            a3 = oat[i % 2]
            o = ot[i % 2]
            r_ = rd[i % 2]
            oTi = oT[i % 2]
            rows = slice(i * 128, (i + 1) * 128)
            kb.dma('sp', xi[:], x[rows, :], xi, writes=[xi])
            for br in range(3):
                kb.dma('act' if br == 1 else 'sp', a3[br][:], oa[br, rows, :].rearrange("p (h c) -> p h c", c=65), a3[br], writes=[a3[br]])
            kb.dma('act', o[:, 512:1024], obd[rows, :], otb[i % 2], writes=[otb[i % 2]])
            kb.op('dve', lambda e, a3=a3: e.tensor_tensor(out=a3[0][:], in0=a3[0][:], in1=a3[1][:], op=ALU.add), reads=[a3[1]], writes=[a3[0]])
            kb.op('dve', lambda e, a3=a3: e.tensor_tensor(out=a3[0][:], in0=a3[0][:], in1=a3[2][:], op=ALU.add), reads=[a3[2]], writes=[a3[0]])
            kb.op('dve', lambda e, a3=a3, r_=r_: e.reciprocal(out=r_[:], in_=a3[0][:, :, 64]), reads=[a3[0]], writes=[r_])
            kb.op('dve', lambda e, a3=a3, r_=r_, o=o: e.tensor_tensor(out=o[:, 0:512].rearrange("p (h c) -> p h c", c=64), in0=a3[0][:, :, 0:64],
                                                                   in1=r_[:].unsqueeze(2).broadcast_to([128, 8, 64]), op=ALU.mult),
                  reads=[a3[0], r_], writes=[ota[i % 2]])
            for hf in range(2):
                bk = pt[i % 2][hf]
                kb.group('pe', [lambda e, c=c, bk=bk, o=o: e.transpose(bk[:, (c % 4) * 128:(c % 4 + 1) * 128],
                                                                        o[:, c * 128:(c + 1) * 128], ident[:])
                                for c in range(hf * 4, hf * 4 + 4)], reads=[ota[i % 2] if hf == 0 else otb[i % 2], ident], writes=[bk])
                if hf == 0:
                    kb.op('act', lambda e, bk=bk, oTi=oTi: e.copy(out=oTi[:, 0:4, :], in_=bk[:].rearrange("p (c t) -> p c t", c=4)),
                          reads=[bk], writes=[oTa[i % 2]])
                else:
                    kb.op('dve', lambda e, bk=bk, oTi=oTi: e.tensor_copy(out=oTi[:, 4:8, :], in_=bk[:].rearrange("p (c t) -> p c t", c=4)),
                          reads=[bk], writes=[oTb[i % 2]])
            for oh in range(2):
                bk = py[i % 2][oh]
                kb.group('pe', [lambda e, c=c, bk=bk, oTi=oTi, oh=oh: e.matmul(bk[:], oTi[:, c, :], wb[:, c, oh * 512:(oh + 1) * 512],
                                                                               start=(c == 0), stop=(c == 7)) for c in range(8)],
                         reads=[oTa[i % 2], oTb[i % 2], wb], writes=[bk])

        def ln1(i):
            xi = xin[i % 3]
            return emit_ln1(kb, L, xi, xi, [py[i % 2][0][:], py[i % 2][1][:]], py[i % 2])

        def ln2(i, slot):
            return emit_ln2(kb, L, slot, g, b, y[i * 128:(i + 1) * 128, :], dma_eng='sp' if i % 2 == 0 else 'act')
        out_toks = skewed(NT, front, ln1, ln2)
        kb.wait_only('sp', out_toks)
        kb.emit()
    return nc

import ml_dtypes
BF = ml_dtypes.bfloat16
S_LEN = 8192
DILS = (1, 4, 16)


def rope_cs(pos):
    pos = pos.astype(np.float32)
    inv = (np.float32(10000.0) ** (-np.arange(0, 64, 2, dtype=np.float32) / np.float32(64))).astype(np.float32)
    ang = (pos[:, None] * inv[None, :]).astype(np.float32)
    return np.concatenate([np.cos(ang), np.sin(ang)], 1).astype(np.float32)


def attn_mask():
    p = np.arange(128)[:, None]
    a = np.arange(128)[None, :]
    return np.concatenate([(p <= a), (p >= a)], 1).astype(np.float32)


def na_bias_table(rpb_h):
    out = np.full((128, 25, 128), -30000.0, np.float32)
    kk = np.arange(128)
    qq = np.arange(128)
    for cls, m in enumerate((0, 1, 2, 62, 63)):
        base = min(max(2 * m - 4, 0), 118)
        qrow = 2 * m + qq // 64
        qcol = qq % 64
        rstart = np.clip(qrow - 4, 0, 120)
        cstart = np.clip(qcol - 8, 0, 48)
        for j in range(5):
            krow = base + 2 * j + kk // 64
            kcol = kk % 64
            ok = ((krow[:, None] >= rstart[None, :]) & (krow[:, None] < rstart[None, :] + 8)
                  & (kcol[:, None] >= cstart[None, :]) & (kcol[:, None] < cstart[None, :] + 16))
            roff = np.clip(krow[:, None] - qrow[None, :] + 7, 0, 14)
            coff = np.clip(kcol[:, None] - qcol[None, :], -15, 15) + 15
            vals = rpb_h[roff, coff]
            out[:, cls * 5 + j, :] = np.where(ok, vals, np.float32(-30000.0))
    return out.reshape(128, 25 * 128)


def attn_core_inputs(qkv, c, rpb_i):
    B, S = qkv.shape[0], qkv.shape[1]
    ha, hb = c, 8 + c
    im = {}
    q_h, k_h, v_h = qkv[:, :, 0, ha], qkv[:, :, 1, ha], qkv[:, :, 2, ha]
    one = np.ones((), BF)
    for di, d in enumerate(DILS):
        Ls = S // d
        nt = Ls // 128
        im[f"qA{di}"] = np.ascontiguousarray(q_h.reshape(B, Ls, d, 64).transpose(0, 3, 2, 1)).reshape(B, 64, d * Ls)
        kk = np.zeros((B, 64, d, Ls + 128), BF)
        kk[:, :, :, 64:64 + Ls] = k_h.reshape(B, Ls, d, 64).transpose(0, 3, 2, 1)
        im[f"kA{di}"] = kk.reshape(B, 64, d * (Ls + 128))
        vv = np.zeros((B, d, Ls + 128, 65), BF)
        vv[:, :, 64:64 + Ls, :64] = v_h.reshape(B, Ls, d, 64).transpose(0, 2, 1, 3)
        vv[:, :, 64:64 + Ls, 64] = one
        im[f"vA{di}"] = np.ascontiguousarray(vv.reshape(B, d, nt + 1, 128, 65).transpose(0, 3, 1, 2, 4)).reshape(B, 128, d * (nt + 1) * 65)
    im["qB"] = np.ascontiguousarray(qkv[:, :, 0, hb].transpose(0, 2, 1))
    im["kB"] = np.ascontiguousarray(qkv[:, :, 1, hb].transpose(0, 2, 1))
    vb = np.zeros((B, S, 65), BF)
    vb[:, :, :64] = qkv[:, :, 2, hb]
    vb[:, :, 64] = one
    im["vB"] = np.ascontiguousarray(vb.reshape(B, 64, 128, 65).transpose(0, 2, 1, 3)).reshape(B, 128, 64 * 65)
    im["ebias"] = na_bias_table(rpb_i[c])
    im["maskd"] = attn_mask()
    return im


def attn_core_outputs(results, B, S):
    oa = np.zeros((3, B, S, 8, 65), np.float32)
    ob = np.zeros((B, S, 8, 64), np.float32)
    for c, r in enumerate(results):
        for di, d in enumerate(DILS):
            Ls = S // d
            nt = Ls // 128
            a = r[f"oA{di}"].reshape(B, 128, d, nt, 65).transpose(0, 3, 1, 2, 4).reshape(B, S, 65)
            oa[di, :, :, c, :] = a
        ob[:, :, c, :] = r["oB"].reshape(B, 128, 64, 64).transpose(0, 2, 1, 3).reshape(B, S, 64)
    return oa.reshape(3, B, S, 8 * 65), ob.reshape(B, S, 512)


N_CORES = 8
_PROGS = {}


def _prog(name, fn):
    if name not in _PROGS:
        _PROGS[name] = fn()
    return _PROGS[name]


def _run(nc, in_maps):
    return run_bass_kernel_spmd(nc, in_maps, core_ids=list(range(N_CORES))).results


def _rep(v):
    return np.ascontiguousarray(np.broadcast_to(np.asarray(v, np.float32)[None], (128, 1024)))


def _attn_layer(xf, w_in, w_out, rpb_i, g, b, B, S):
    T = xf.shape[0] // N_CORES
    NT = T // 128
    halves = S // T
    nc1 = _prog('qkv', lambda: build_qkv(NT=NT))
    ims = []
    for c in range(N_CORES):
        p0 = (c % halves) * T
        ims.append({"x": xf[c * T:(c + 1) * T], "win": w_in, "cs": rope_cs(np.arange(p0, p0 + T))})
    r1 = _run(nc1, ims)
    qkv = np.concatenate([r["qkv"] for r in r1], 0).reshape(B, S, 3, 16, 64)
    nc2 = _prog('attn', lambda: build_attn(NB=B))
    r2 = _run(nc2, [attn_core_inputs(qkv, c, rpb_i) for c in range(N_CORES)])
    oa, ob = attn_core_outputs(r2, B, S)
    oa = oa.reshape(3, B * S, 8 * 65)
    ob = ob.reshape(B * S, 512)
    nc3 = _prog('oproj', lambda: build_oproj(NT=NT))
    ims = [{"x": xf[c * T:(c + 1) * T], "oa": np.ascontiguousarray(oa[:, c * T:(c + 1) * T]),
            "ob": np.ascontiguousarray(ob[c * T:(c + 1) * T]), "wout": w_out, "lng": _rep(g), "lnb": _rep(b)}
           for c in range(N_CORES)]
    r3 = _run(nc3, ims)
    return np.concatenate([r["y"] for r in r3], 0)


def _pool_layer(xf, pw, psc, g, b, B, S):
    T = xf.shape[0] // N_CORES
    NT = T // 128
    halves = S // T
    ncp = _prog('pool', lambda: build_pool(NT=NT))
    ims = []
    for c in range(N_CORES):
        h = c % halves
        xp = np.zeros((T + 16, 1024), np.float32)
        lo = c * T - (8 if h > 0 else 0)
        hi = (c + 1) * T + (8 if h < halves - 1 else 0)
        xp[8 - (c * T - lo): 8 + T + (hi - (c + 1) * T)] = xf[lo:hi]
        Bm, Bh = pool_tables(h == 0, h == halves - 1)
        ims.append({"xp": xp, "bm": np.ascontiguousarray(Bm.transpose(2, 0, 1, 3).reshape(128, 12 * 128)),
                    "bh": np.ascontiguousarray(Bh.transpose(2, 0, 1, 3).reshape(16, 12 * 128)),
                    "pw": pw, "psc": _rep(psc), "lng": _rep(g), "lnb": _rep(b)})
    r = _run(ncp, ims)
    return np.concatenate([q["y"] for q in r], 0)


MOE_CAP = 768


def _moe_layer(xf, rw, rbias, w1, w3, w2, g, b):
    T = xf.shape[0] // N_CORES
    NT = T // 128
    ncm = _prog('moe', lambda: build_moe_sparse(NT=NT, C=MOE_CAP, NE=16))
    rb = np.ascontiguousarray(np.broadcast_to(np.tile(np.asarray(rbias, np.float32), NT)[None], (128, NT * 16)))
    eoff = np.ascontiguousarray(np.broadcast_to(np.tile(np.arange(16, dtype=np.float32) * MOE_CAP, NT)[None], (128, NT * 16)))
    ims = [{"x": xf[c * T:(c + 1) * T], "rw": rw, "rbias": rb, "eoff": eoff, "w1": w1, "w3": w3, "w2": w2,
            "lng": _rep(g), "lnb": _rep(b)} for c in range(N_CORES)]
    r = _run(ncm, ims)
    return np.concatenate([q["y"] for q in r], 0)


def kernel(x, w_in, w_out, rpb, pool_w, pool_scale, router_w, router_bias, moe_w1, moe_w3, moe_w2, ln_g, ln_b):
    f = lambda a: np.ascontiguousarray(np.asarray(a, np.float32))
    x = f(x)
    B, S, Dm = x.shape
    xf = x.reshape(B * S, Dm)
    depth = moe_w1.shape[0]
    for layer in range(depth):
        i = layer // 2
        if layer % 2 == 0:
            xf = _attn_layer(xf, f(w_in[i]), f(w_out[i]), f(rpb[i]), ln_g[layer, 0], ln_b[layer, 0], B, S)
        else:
            xf = _pool_layer(xf, f(pool_w[i]), pool_scale[i], ln_g[layer, 0], ln_b[layer, 0], B, S)
        xf = _moe_layer(xf, f(router_w), router_bias, f(moe_w1[layer]), f(moe_w3[layer]), f(moe_w2[layer]),
                        ln_g[layer, 1], ln_b[layer, 1])
    return xf.reshape(B, S, Dm).astype(np.float32)
```

```python
import numpy as np
import concourse.bass as bass
import concourse.mybir as mybir
from concourse.bass_utils import run_bass_kernel_spmd
from contextlib import ExitStack

F32 = mybir.dt.float32
BF16 = mybir.dt.bfloat16
U32 = mybir.dt.uint32
I32 = mybir.dt.int32
AF = mybir.ActivationFunctionType
ALU = mybir.AluOpType
AX = mybir.AxisListType

ENGS = ['pe', 'act', 'dve', 'pool', 'sp']
ENG_ATTR = {'pe': 'tensor', 'act': 'scalar', 'dve': 'vector', 'pool': 'gpsimd', 'sp': 'sync'}


class Tok:
    __slots__ = ('sem', 'val')

    def __init__(self, sem, val):
        self.sem = sem
        self.val = val


class Buf:
    def __init__(self, kb, t, name):
        self.kb = kb
        self.t = t
        self.name = name
        self.w = []
        self.r = {}
        self._slot = None

    def __getitem__(self, k):
        return self.t[k]

    def slot(self):
        if self._slot is None:
            self._slot = self.kb.new_sem()
            self._slotval = 0
        return self._slot


class KB:
    def __init__(self, nc, es, same_eng_sync=True):
        self.nc = nc
        self.es = es
        self.ops = {e: [] for e in ENGS}
        self.nsem = 0
        self.sems = {e: self.new_sem() for e in ENGS}
        self.cnt = {e: 0 for e in ENGS}
        self.waited = {e: {} for e in ENGS}
        self.same_eng_sync = same_eng_sync
        self.nbuf = 0

    def new_sem(self):
        self.nsem += 1
        return self.es.enter_context(self.nc.semaphore(f"sem{self.nsem}"))

    def sb(self, shape, dtype, name=None):
        self.nbuf += 1
        name = name or f"sb{self.nbuf}"
        return Buf(self, self.es.enter_context(self.nc.sbuf_tensor(name, list(shape), dtype)), name)

    def ps(self, shape, dtype=F32, name=None):
        self.nbuf += 1
        name = name or f"ps{self.nbuf}"
        return Buf(self, self.es.enter_context(self.nc.psum_tensor(name, list(shape), dtype)), name)

    def view(self, name="v"):
        return Buf(self, None, name)

    def _waits(self, eng, deps):
        waits = []
        for d in deps:
            if d is None:
                continue
            if isinstance(d, (list, tuple)):
                waits += self._waits(eng, d)
                continue
            if (not self.same_eng_sync) and d.sem is self.sems[eng]:
                continue
            key = id(d.sem)
            if self.waited[eng].get(key, 0) >= d.val:
                continue
            self.waited[eng][key] = d.val
            waits.append((d.sem, d.val))
        return waits

    def _deps(self, reads, writes, deps):
        ds = list(deps)
        for b in reads:
            ds += b.w
        for b in writes:
            ds += b.w
            ds += list(b.r.values())
        return ds

    def _commit(self, tok, reads, writes):
        for b in writes:
            b.w = [tok]
            b.r = {}
        for b in reads:
            if b not in writes:
                o = b.r.get(id(tok.sem))
                if o is None or o.val < tok.val:
                    b.r[id(tok.sem)] = tok

    def op(self, eng, fn, reads=(), writes=(), deps=()):
        waits = self._waits(eng, self._deps(reads, writes, deps))
        self.cnt[eng] += 1
        self.ops[eng].append((waits, fn, (self.sems[eng], 1)))
        tok = Tok(self.sems[eng], self.cnt[eng])
        self._commit(tok, reads, writes)
        return tok

    def group(self, eng, fns, reads=(), writes=(), deps=()):
        waits = self._waits(eng, self._deps(reads, writes, deps))
        for i, fn in enumerate(fns):
            self.cnt[eng] += 1
            self.ops[eng].append((waits if i == 0 else [], fn, (self.sems[eng], 1)))
        tok = Tok(self.sems[eng], self.cnt[eng])
        self._commit(tok, reads, writes)
        return tok

    def dma(self, eng, out, in_, slotbuf, reads=(), writes=(), deps=(), group=False, **kw):
        sem = slotbuf.slot()
        ds = list(deps)
        if not group and slotbuf._slotval > 0:
            ds.append(Tok(sem, slotbuf._slotval))
        for b in reads:
            ds += b.w
        for b in writes:
            ds += [t for t in b.w if t.sem is not sem]
            ds += list(b.r.values())
        waits = self._waits(eng, ds)
        slotbuf._slotval += 16
        self.ops[eng].append((waits, lambda e: e.dma_start(out=out, in_=in_, **kw), (sem, 16)))
        tok = Tok(sem, slotbuf._slotval)
        self._commit(tok, reads, writes)
        return tok

    def wait_only(self, eng, deps):
        waits = self._waits(eng, deps)
        if waits:
            self.ops[eng].append((waits, None, None))

    def emit(self):
        with self.nc.Block() as block:
            for e in ENGS:
                dec = getattr(block, ENG_ATTR[e])
                ops = self.ops[e]

                def body(eng, ops=ops):
                    for waits, fn, inc in ops:
                        for s, v in waits:
                            eng.wait_ge(s, v)
                        if fn is None:
                            continue
                        inst = fn(eng)
                        if inc is not None:
                            inst.then_inc(inc[0], inc[1])
                dec(body)


def _idma(self, out, in_, idx_ap, scatter, slotbuf, bound, reads=(), writes=(), deps=(), group=False):
    sem = slotbuf.slot()
    ds = list(deps)
    if not group and slotbuf._slotval > 0:
        ds.append(Tok(sem, slotbuf._slotval))
    for b in reads:
        ds += b.w
    for b in writes:
        ds += [t for t in b.w if t.sem is not sem]
        ds += list(b.r.values())
    waits = self._waits('pool', ds)
    slotbuf._slotval += 16
    if not hasattr(self, '_bregs'):
        self._bregs = {}

    def breg(e):
        if bound not in self._bregs:
            self._bregs[bound] = e.to_reg(bound)
        return self._bregs[bound]
    if scatter:
        fn = lambda e: e.indirect_dma_start(out=out, out_offset=bass.IndirectOffsetOnAxis(ap=idx_ap, axis=0), in_=in_,
                                            in_offset=None, bounds_check=breg(e), oob_is_err=False)
    else:
        fn = lambda e: e.indirect_dma_start(out=out, out_offset=None, in_=in_,
                                            in_offset=bass.IndirectOffsetOnAxis(ap=idx_ap, axis=0), bounds_check=breg(e),
                                            oob_is_err=False)
    self.ops['pool'].append((waits, fn, (sem, 16)))
    tok = Tok(sem, slotbuf._slotval)
    self._commit(tok, reads, writes)
    return tok


KB.idma = _idma


D = 1024
ALPHA = 8.0 ** 0.25
LN_EPS = 1e-5


def make_ident(kb, ident):
    kb.op('pool', lambda e: e.memset(ident[:], 0.0), writes=[ident])
    kb.op('pool', lambda e: e.affine_select(out=ident[:], in_=ident[:], compare_op=ALU.not_equal, fill=1.0,
                                            base=0, pattern=[[-1, 128]], channel_multiplier=1), writes=[ident])


class LNBufs:
    def __init__(self, kb, nbuf=2):
        self.kb = kb
        self.n = nbuf
        self.i = 0
        self.z = [kb.sb([128, D], F32) for _ in range(nbuf)]
        self.o = [kb.sb([128, D], F32) for _ in range(nbuf)]
        self.st = [kb.sb([128, 12], F32) for _ in range(nbuf)]
        self.mv = [kb.sb([128, 2], F32) for _ in range(nbuf)]
        self.rs = [kb.sb([128, 1], F32) for _ in range(nbuf)]
        self.nb = [kb.sb([128, 1], F32) for _ in range(nbuf)]
        self.eps = kb.sb([128, 1], F32)
        kb.op('pool', lambda e: e.memset(self.eps[:], LN_EPS), writes=[self.eps])


def emit_ln1(kb, L, xbuf, x_ap, h_aps, h_bufs):
    i = L.i
    L.i = (L.i + 1) % L.n
    z, st, mv, rs, nb = L.z[i], L.st[i], L.mv[i], L.rs[i], L.nb[i]
    for hf in range(2):
        sl = slice(hf * 512, (hf + 1) * 512)
        kb.op('dve', lambda e, sl=sl, hf=hf: e.scalar_tensor_tensor(out=z[:, sl], in0=x_ap[:, sl], scalar=ALPHA,
                                                                    in1=h_aps[hf], op0=ALU.mult, op1=ALU.add),
              reads=[xbuf, h_bufs[hf]], writes=[z] if hf == 0 else [], deps=z.w if hf == 1 else ())
    z.w = [Tok(kb.sems['dve'], kb.cnt['dve'])]
    for hf in range(2):
        sl = slice(hf * 512, (hf + 1) * 512)
        kb.op('dve', lambda e, sl=sl, hf=hf: e.bn_stats(out=st[:, hf * 6:(hf + 1) * 6], in_=z[:, sl]),
              reads=[z], writes=[st] if hf == 0 else [], deps=st.w if hf == 1 else ())
    st.w = [Tok(kb.sems['dve'], kb.cnt['dve'])]
    kb.op('dve', lambda e: e.bn_aggr(out=mv[:], in_=st[:]), reads=[st], writes=[mv])
    kb.op('act', lambda e: e.activation(out=rs[:], in_=mv[:, 1:2], func=AF.Sqrt, bias=L.eps[:], scale=1.0),
          reads=[mv, L.eps], writes=[rs])
    kb.op('dve', lambda e: e.reciprocal(out=rs[:], in_=rs[:]), reads=[rs], writes=[rs])
    kb.op('dve', lambda e: e.scalar_tensor_tensor(out=nb[:], in0=mv[:, 0:1], scalar=-1.0, in1=rs[:],
                                                  op0=ALU.mult, op1=ALU.mult), reads=[mv, rs], writes=[nb])
    return i


def emit_ln2(kb, L, i, g, b, out_dram_ap, dma_eng='sp', b_eng='pool'):
    z, o, rs, nb = L.z[i], L.o[i], L.rs[i], L.nb[i]
    kb.op('act', lambda e: e.activation(out=o[:], in_=z[:], func=AF.Identity, bias=nb[:], scale=rs[:]),
          reads=[z, rs, nb], writes=[o])
    kb.op('pool', lambda e: e.tensor_tensor(out=o[:], in0=o[:], in1=g[:], op=ALU.mult), reads=[o, g], writes=[o])
    kb.op(b_eng, lambda e: e.tensor_tensor(out=o[:], in0=o[:], in1=b[:], op=ALU.add), reads=[o, b], writes=[o])
    return kb.dma(dma_eng, out_dram_ap, o[:], o, reads=[o])


def emit_ln(kb, L, xbuf, x_ap, h_aps, h_bufs, g, b, out_dram_ap, dma_eng='sp', b_eng='pool'):
    i = emit_ln1(kb, L, xbuf, x_ap, h_aps, h_bufs)
    return emit_ln2(kb, L, i, g, b, out_dram_ap, dma_eng, b_eng)


def skewed(n, front, ln1, ln2):
    toks = []
    slots = {}
    for s in range(n + 2):
        if s < n:
            front(s)
        if 1 <= s <= n:
            slots[s - 1] = ln1(s - 1)
        if s >= 2:
            toks.append(ln2(s - 2, slots.pop(s - 2)))
    return toks


def build_moe(NT=32, GT=8, NE=16, debug=False):
    nc = bass.Bass("TRN2", target_bir_lowering=False)
    T = NT * 128
    x = nc.dram_tensor("x", [T, D], F32, kind="ExternalInput").ap()
    rw = nc.dram_tensor("rw", [D, 16], F32, kind="ExternalInput").ap()
    rbias = nc.dram_tensor("rbias", [128, GT * 16], F32, kind="ExternalInput").ap()
    w1 = nc.dram_tensor("w1", [16, D, D], F32, kind="ExternalInput").ap()
    w3 = nc.dram_tensor("w3", [16, D, D], F32, kind="ExternalInput").ap()
    w2 = nc.dram_tensor("w2", [16, D, D], F32, kind="ExternalInput").ap()
    lng = nc.dram_tensor("lng", [128, D], F32, kind="ExternalInput").ap()
    lnb = nc.dram_tensor("lnb", [128, D], F32, kind="ExternalInput").ap()
    y = nc.dram_tensor("y", [T, D], F32, kind="ExternalOutput").ap()
    if debug:
        gdbg = nc.dram_tensor("gdbg", [NT // GT, 128, GT * 16], F32, kind="ExternalOutput").ap()
    with ExitStack() as es:
        kb = KB(nc, es)
        ident = kb.sb([128, 128], F32)
        make_ident(kb, ident)
        rws = kb.sb([128, 8, 16], F32)
        kb.dma('sp', rws[:], rw.rearrange("(c p) e -> p c e", p=128), rws, writes=[rws])
        rb = kb.sb([128, GT * 16], F32)
        kb.dma('sp', rb[:], rbias[:, :], rb, writes=[rb])
        g = kb.sb([128, D], F32)
        b = kb.sb([128, D], F32)
        kb.dma('sp', g[:], lng[:, :], g, writes=[g])
        kb.dma('sp', b[:], lnb[:, :], b, writes=[b])
        L = LNBufs(kb, nbuf=1)

        xin = [kb.sb([128, D], F32) for _ in range(2)]
        xT32 = [kb.sb([128, 8, 128], F32) for _ in range(2)]
        xT = kb.sb([128, 8, GT * 128], BF16)
        xTv = [kb.view(f"xTv{t}") for t in range(GT)]
        acc = kb.sb([128, GT, D], F32)
        accv = [[kb.view() for _ in range(2)] for t in range(GT)]
        wb = [[kb.sb([128, 8, D], BF16) for _ in range(3)] for _ in range(2)]
        hT = [kb.sb([128, 8, 512], BF16) for _ in range(1)]
        hTv = [[kb.view() for _ in range(8)] for _ in range(1)]
        sil = [kb.sb([128, 512], BF16) for _ in range(2)]
        banks = [kb.ps([128, 512], F32) for _ in range(8)]
        NG = GT * 16
        aff = kb.sb([128, NG], F32)
        affv = [kb.view() for _ in range(GT)]
        S8 = kb.sb([128, GT * 4, 8], F32)
        Pt = kb.sb([128, GT * 4, 6], F32)
        gs = kb.sb([128, GT * 4], F32)
        gmax = kb.sb([128, GT], F32)
        ghot = kb.sb([128, GT * 4], F32)
        c1 = kb.sb([128, GT * 4, 4], F32)
        c2 = kb.sb([128, GT * 4, 4], F32)
        GA = kb.sb([128, GT, 16], F32)
        den = kb.sb([128, GT], F32)
        G = kb.sb([128, GT, 16], F32)

        wsrc = [w1, w3, w2]
        nsteps = (NT // GT) * NE
        step_list = [(gi, e) for gi in range(NT // GT) for e in range(NE)]

        def load_weights(si):
            gi, e = step_list[si]
            slot = si % 2
            for j in range(3):
                kb.dma('pool', wb[slot][j][:], wsrc[j][e].rearrange("(c p) f -> p c f", p=128), wb[slot][j],
                       writes=[wb[slot][j]])

        if nsteps > 0:
            load_weights(0)
        ybank = 0
        out_toks = []
        for gi in range(NT // GT):
            for t in range(GT):
                tile = gi * GT + t
                xi = xin[t % 2]
                x32 = xT32[t % 2]
                kb.dma('sp', xi[:], x[tile * 128:(tile + 1) * 128, :], xi, writes=[xi])
                for hf in range(2):
                    bk = banks[4 + hf]
                    kb.group('pe', [lambda e, c=c, bk=bk, xi=xi: e.transpose(bk[:, (c % 4) * 128:(c % 4 + 1) * 128],
                                                                               xi[:, c * 128:(c + 1) * 128], ident[:])
                                    for c in range(hf * 4, hf * 4 + 4)], reads=[xi, ident], writes=[bk])
                    if True:
                        kb.op('act', lambda e, bk=bk, hf=hf, x32=x32: e.copy(
                            out=x32[:, hf * 4:(hf + 1) * 4, :], in_=bk[:].rearrange("p (c t) -> p c t", c=4)),
                            reads=[bk], writes=[x32] if hf == 0 else [], deps=x32.w if hf == 1 else ())
                    kb.op('dve', lambda e, hf=hf, t=t, x32=x32: e.tensor_copy(
                        out=xT[:, hf * 4:(hf + 1) * 4, t * 128:(t + 1) * 128], in_=x32[:, hf * 4:(hf + 1) * 4, :]),
                        reads=[], writes=[xTv[t]] if hf == 0 else [], deps=list(xTv[t].w if hf == 1 else ()) + [Tok(kb.sems['act'], kb.cnt['act'])])
                x32.w = [Tok(kb.sems['act'], kb.cnt['act'])]
                xTv[t].w = [Tok(kb.sems['dve'], kb.cnt['dve'])]
                x32.r[id(kb.sems['dve'])] = Tok(kb.sems['dve'], kb.cnt['dve'])
                rbk = banks[6]
                kb.group('pe', [lambda e, c=c, x32=x32, rbk=rbk: e.matmul(rbk[:, 0:16], x32[:, c, :], rws[:, c, :],
                                                                         start=(c == 0), stop=(c == 7))
                                for c in range(8)], reads=[x32, rws], writes=[rbk])
                kb.op('act', lambda e, t=t, rbk=rbk: e.activation(out=aff[:, t * 16:(t + 1) * 16], in_=rbk[:, 0:16],
                                                                   func=AF.Sigmoid), reads=[rbk], writes=[affv[t]])
            S4 = S8[:, :, 0:4]
            aff4 = aff[:].rearrange("p (g j) -> p g j", j=4)
            kb.op('dve', lambda e: e.tensor_tensor(out=S8[:, :, 0:4], in0=aff4, in1=rb[:].rearrange("p (g j) -> p g j", j=4),
                                                   op=ALU.add), reads=affv + [rb], writes=[S8])
            kb.op('dve', lambda e: e.tensor_copy(out=S8[:, :, 4:8], in_=S8[:, :, 0:4]), reads=[S8], writes=[S8])
            pairs = [(0, 1), (0, 2), (0, 3), (1, 2), (1, 3), (2, 3)]
            for i, (a, bb) in enumerate(pairs):
                kb.op('dve', lambda e, i=i, a=a, bb=bb: e.tensor_tensor(out=Pt[:, :, i], in0=S8[:, :, a], in1=S8[:, :, bb],
                                                                         op=ALU.add), reads=[S8], writes=[Pt])
            kb.op('dve', lambda e: e.tensor_reduce(out=gs[:], in_=Pt[:], axis=AX.X, op=ALU.max), reads=[Pt], writes=[gs])
            kb.op('dve', lambda e: e.tensor_reduce(out=gmax[:], in_=gs[:].rearrange("p (t g) -> p t g", g=4), axis=AX.X,
                                                   op=ALU.max), reads=[gs], writes=[gmax])
            kb.op('dve', lambda e: e.tensor_tensor(out=ghot[:].rearrange("p (t g) -> p t g", g=4),
                                                   in0=gs[:].rearrange("p (t g) -> p t g", g=4),
                                                   in1=gmax[:].unsqueeze(2).broadcast_to([128, GT, 4]), op=ALU.is_ge),
                  reads=[gs, gmax], writes=[ghot])
            kb.op('dve', lambda e: e.tensor_tensor(out=c1[:], in0=S8[:, :, 0:4], in1=S8[:, :, 1:5], op=ALU.is_gt),
                  reads=[S8], writes=[c1])
            kb.op('dve', lambda e: e.tensor_tensor(out=c2[:], in0=S8[:, :, 0:4], in1=S8[:, :, 2:6], op=ALU.is_gt),
                  reads=[S8], writes=[c2])
            kb.op('dve', lambda e: e.tensor_tensor(out=c1[:], in0=c1[:], in1=c2[:], op=ALU.add), reads=[c1, c2], writes=[c1])
            kb.op('dve', lambda e: e.tensor_tensor(out=c2[:], in0=S8[:, :, 0:4], in1=S8[:, :, 3:7], op=ALU.is_gt),
                  reads=[S8], writes=[c2])
            kb.op('dve', lambda e: e.tensor_tensor(out=c1[:], in0=c1[:], in1=c2[:], op=ALU.add), reads=[c1, c2], writes=[c1])
            kb.op('dve', lambda e: e.scalar_tensor_tensor(out=c1[:], in0=c1[:], scalar=2.0,
                                                          in1=ghot[:].unsqueeze(2).broadcast_to([128, GT * 4, 4]),
                                                          op0=ALU.is_ge, op1=ALU.mult), reads=[c1, ghot], writes=[c1])
            kb.op('dve', lambda e: e.tensor_tensor(out=GA[:].rearrange("p t (g j) -> p (t g) j", j=4), in0=c1[:], in1=aff4,
                                                   op=ALU.mult), reads=[c1] + affv, writes=[GA])
            kb.op('dve', lambda e: e.tensor_reduce(out=den[:], in_=GA[:], axis=AX.X, op=ALU.add), reads=[GA], writes=[den])
            kb.op('dve', lambda e: e.reciprocal(out=den[:], in_=den[:]), reads=[den], writes=[den])
            kb.op('dve', lambda e: e.tensor_tensor(out=G[:], in0=GA[:], in1=den[:].unsqueeze(2).broadcast_to([128, GT, 16]),
                                                   op=ALU.mult), reads=[GA, den], writes=[G])
            if debug:
                out_toks.append(kb.dma('sp', gdbg[gi], G[:].rearrange("p t e -> p (t e)"), G, reads=[G]))
            for e_i in range(NE):
                si = gi * NE + e_i
                slot = si % 2
                if si + 1 < nsteps:
                    load_weights(si + 1)
                w1b, w3b, w2b = wb[slot]
                for tt in range(GT // 4):
                    hb = hT[0]
                    hv = hTv[0]
                    tsl = slice(tt * 512, (tt + 1) * 512)
                    for fc in range(8):
                        b1 = banks[fc % 2]
                        b3 = banks[2 + fc % 2]
                        sl_ = sil[fc % 2]
                        fsl = slice(fc * 128, (fc + 1) * 128)
                        kb.group('pe', [lambda e, c=c, b1=b1, fsl=fsl, w1b=w1b, tsl=tsl: e.matmul(b1[:], w1b[:, c, fsl], xT[:, c, tsl],
                                                                                 start=(c == 0), stop=(c == 7))
                                        for c in range(8)], reads=[w1b] + xTv[tt * 4:(tt + 1) * 4], writes=[b1])
                        kb.group('pe', [lambda e, c=c, b3=b3, fsl=fsl, w3b=w3b, tsl=tsl: e.matmul(b3[:], w3b[:, c, fsl], xT[:, c, tsl],
                                                                                 start=(c == 0), stop=(c == 7))
                                        for c in range(8)], reads=[w3b] + xTv[tt * 4:(tt + 1) * 4], writes=[b3])
                        kb.op('act', lambda e, b1=b1, sl_=sl_: e.activation(out=sl_[:], in_=b1[:], func=AF.Silu),
                              reads=[b1], writes=[sl_])
                        kb.op('dve', lambda e, b3=b3, sl_=sl_, fc=fc, hb=hb: e.tensor_tensor(out=hb[:, fc, :], in0=b3[:],
                                                                                          in1=sl_[:], op=ALU.mult),
                              reads=[b3, sl_], writes=[hv[fc]])
                    for t4 in range(4):
                        t = tt * 4 + t4
                        for oh in range(2):
                            yb = banks[4 + ybank % 4]
                            ybank += 1
                            osl = slice(oh * 512, (oh + 1) * 512)
                            kb.group('pe', [lambda e, fc=fc, yb=yb, t4=t4, osl=osl, hb=hb, w2b=w2b: e.matmul(
                                yb[:], hb[:, fc, t4 * 128:(t4 + 1) * 128], w2b[:, fc, osl], start=(fc == 0), stop=(fc == 7))
                                for fc in range(8)], reads=[w2b] + hv, writes=[yb])
                            av = accv[t][oh]
                            if e_i == 0:
                                kb.op('dve', lambda e, yb=yb, t=t, osl=osl, e_i=e_i: e.tensor_scalar(
                                    out=acc[:, t, osl], in0=yb[:], scalar1=G[:, t, e_i:e_i + 1], scalar2=None, op0=ALU.mult),
                                    reads=[yb, G], writes=[av])
                            else:
                                kb.op('dve', lambda e, yb=yb, t=t, osl=osl, e_i=e_i: e.scalar_tensor_tensor(
                                    out=acc[:, t, osl], in0=yb[:], scalar=G[:, t, e_i:e_i + 1], in1=acc[:, t, osl],
                                    op0=ALU.mult, op1=ALU.add), reads=[yb, G], writes=[av])
            for t in range(GT):
                tile = gi * GT + t
                xi = xin[t % 2]
                kb.dma('sp', xi[:], x[tile * 128:(tile + 1) * 128, :], xi, writes=[xi])
                out_toks.append(emit_ln(kb, L, xi, xi, [acc[:, t, 0:512], acc[:, t, 512:1024]], accv[t], g, b,
                                        y[tile * 128:(tile + 1) * 128, :]))
        kb.wait_only('sp', out_toks)
        kb.emit()
    return nc


BIGIDX = 16.0 * 1024 + 7.0
BIG2 = 65536.0


def build_moe_sparse(NT=32, C=768, NE=16, debug=False):
    nc = bass.Bass("TRN2", target_bir_lowering=False)
    T = NT * 128
    NS = C // 128
    NG = NT * 16
    x = nc.dram_tensor("x", [T, D], F32, kind="ExternalInput").ap()
    rw = nc.dram_tensor("rw", [D, 16], F32, kind="ExternalInput").ap()
    rbias = nc.dram_tensor("rbias", [128, NG], F32, kind="ExternalInput").ap()
    eoffd = nc.dram_tensor("eoff", [128, NG], F32, kind="ExternalInput").ap()
    w1 = nc.dram_tensor("w1", [16, D, D], F32, kind="ExternalInput").ap()
    w3 = nc.dram_tensor("w3", [16, D, D], F32, kind="ExternalInput").ap()
    w2 = nc.dram_tensor("w2", [16, D, D], F32, kind="ExternalInput").ap()
    lng = nc.dram_tensor("lng", [128, D], F32, kind="ExternalInput").ap()
    lnb = nc.dram_tensor("lnb", [128, D], F32, kind="ExternalInput").ap()
    y = nc.dram_tensor("y", [T, D], F32, kind="ExternalOutput").ap()
    xs = nc.dram_tensor("xs_scr", [16 * C, D], F32, kind="Internal").ap()
    ys = nc.dram_tensor("ys_scr", [16 * C, D], F32, kind="Internal").ap()
    if debug:
        dbg = nc.dram_tensor("dbg", [4, 128, NT], F32, kind="ExternalOutput").ap()
    with ExitStack() as es:
        kb = KB(nc, es)
        ident = kb.sb([128, 128], F32)
        make_ident(kb, ident)
        ltri = kb.sb([128, 128], F32)
        ones = kb.sb([128, 128], F32)
        kb.op('pool', lambda e: e.memset(ones[:], 1.0), writes=[ones])
        kb.op('pool', lambda e: e.memset(ltri[:], 1.0), writes=[ltri])
        kb.op('pool', lambda e: e.affine_select(out=ltri[:], in_=ltri[:], compare_op=ALU.is_gt, fill=0.0, base=0,
                                                pattern=[[1, 128]], channel_multiplier=-1), writes=[ltri])
        rws = kb.sb([128, 8, 16], F32)
        kb.dma('sp', rws[:], rw.rearrange("(c p) e -> p c e", p=128), rws, writes=[rws])
        rb = kb.sb([128, NG], F32)
        kb.dma('sp', rb[:], rbias[:, :], rb, writes=[rb])
        eoff = kb.sb([128, NG], F32)
        kb.dma('sp', eoff[:], eoffd[:, :], eoff, writes=[eoff])
        g = kb.sb([128, D], F32)
        b = kb.sb([128, D], F32)
        kb.dma('sp', g[:], lng[:, :], g, writes=[g])
        kb.dma('sp', b[:], lnb[:, :], b, writes=[b])
        L = LNBufs(kb, nbuf=1)
        xin = [kb.sb([128, D], F32) for _ in range(2)]
        x32 = [kb.sb([128, 8, 128], F32) for _ in range(2)]
        xTs = [kb.sb([128, 8, C], BF16) for _ in range(2)]
        xTvs = [[kb.view() for _ in range(NS)] for _ in range(2)]
        xin6 = xin + [kb.sb([128, D], F32) for _ in range(max(0, NS - 2))]
        w13 = [[kb.sb([128, 8, D], BF16) for _ in range(2)] for _ in range(2)]
        w2b = kb.sb([128, 8, D], BF16)
        hT = kb.sb([128, 8, 512], BF16)
        hv = [kb.view() for _ in range(8)]
        sil = [kb.sb([128, 512], BF16) for _ in range(2)]
        banks = [kb.ps([128, 512], F32) for _ in range(8)]
        xsb = kb.view("xs")
        ysb = kb.view("ys")
        wsrc = [w1, w3]

        def load_w13(e):
            for j in range(2):
                kb.dma('pool', w13[e % 2][j][:], wsrc[j][e].rearrange("(c p) f -> p c f", p=128), w13[e % 2][j],
                       writes=[w13[e % 2][j]])

        def load_w2(e):
            kb.dma('pool', w2b[:], w2[e].rearrange("(c p) f -> p c f", p=128), w2b, writes=[w2b])

        if NE > 0:
            load_w13(0)
            load_w2(0)

        aff = kb.sb([128, NG], F32)
        affv = [kb.view() for _ in range(NT)]
        for t in range(NT):
            xi = xin6[t % len(xin6)]
            x3 = x32[t % 2]
            kb.dma('sp' if t % 2 == 0 else 'act', xi[:], x[t * 128:(t + 1) * 128, :], xi, writes=[xi])
            for hf in range(2):
                bk = banks[4 + hf]
                kb.group('pe', [lambda e, c=c, bk=bk, xi=xi: e.transpose(bk[:, (c % 4) * 128:(c % 4 + 1) * 128],
                                                                           xi[:, c * 128:(c + 1) * 128], ident[:])
                                for c in range(hf * 4, hf * 4 + 4)], reads=[xi, ident], writes=[bk])
                tk = kb.op('act', lambda e, bk=bk, hf=hf, x3=x3: e.copy(out=x3[:, hf * 4:(hf + 1) * 4, :],
                                                                         in_=bk[:].rearrange("p (c t) -> p c t", c=4)),
                           reads=[bk], writes=[x3] if hf == 0 else [], deps=x3.w if hf == 1 else ())
            x3.w = [tk]
            rbk = banks[6 + t % 2]
            kb.group('pe', [lambda e, c=c, x3=x3, rbk=rbk: e.matmul(rbk[:, 0:16], x3[:, c, :], rws[:, c, :],
                                                                   start=(c == 0), stop=(c == 7)) for c in range(8)],
                     reads=[x3, rws], writes=[rbk])
            kb.op('act', lambda e, t=t, rbk=rbk: e.activation(out=aff[:, t * 16:(t + 1) * 16], in_=rbk[:, 0:16], func=AF.Sigmoid),
                  reads=[rbk], writes=[affv[t]])
        S8 = kb.sb([128, NT * 4, 8], F32)
        Pt = kb.sb([128, NT * 4, 6], F32)
        gs = kb.sb([128, NT * 4], F32)
        gmax = kb.sb([128, NT], F32)
        ghot = kb.sb([128, NT * 4], F32)
        c1 = kb.sb([128, NT * 4, 4], F32)
        c2 = kb.sb([128, NT * 4, 4], F32)
        GA = kb.sb([128, NT, 16], F32)
        den = kb.sb([128, NT], F32)
        G = kb.sb([128, NT, 16], F32)
        aff4 = aff[:].rearrange("p (g j) -> p g j", j=4)

        def dv(fn, reads, writes):
            return kb.op('dve', fn, reads=reads, writes=writes)
        dv(lambda e: e.tensor_tensor(out=S8[:, :, 0:4], in0=aff4, in1=rb[:].rearrange("p (g j) -> p g j", j=4), op=ALU.add), affv + [rb], [S8])
        dv(lambda e: e.tensor_copy(out=S8[:, :, 4:8], in_=S8[:, :, 0:4]), [S8], [S8])
        for i, (a, bb) in enumerate([(0, 1), (0, 2), (0, 3), (1, 2), (1, 3), (2, 3)]):
            dv(lambda e, i=i, a=a, bb=bb: e.tensor_tensor(out=Pt[:, :, i], in0=S8[:, :, a], in1=S8[:, :, bb], op=ALU.add), [S8], [Pt])
        dv(lambda e: e.tensor_reduce(out=gs[:], in_=Pt[:], axis=AX.X, op=ALU.max), [Pt], [gs])
        dv(lambda e: e.tensor_reduce(out=gmax[:], in_=gs[:].rearrange("p (t g) -> p t g", g=4), axis=AX.X, op=ALU.max), [gs], [gmax])
        dv(lambda e: e.tensor_tensor(out=ghot[:].rearrange("p (t g) -> p t g", g=4), in0=gs[:].rearrange("p (t g) -> p t g", g=4),
                                     in1=gmax[:].unsqueeze(2).broadcast_to([128, NT, 4]), op=ALU.is_ge), [gs, gmax], [ghot])
        dv(lambda e: e.tensor_tensor(out=c1[:], in0=S8[:, :, 0:4], in1=S8[:, :, 1:5], op=ALU.is_gt), [S8], [c1])
        dv(lambda e: e.tensor_tensor(out=c2[:], in0=S8[:, :, 0:4], in1=S8[:, :, 2:6], op=ALU.is_gt), [S8], [c2])
        dv(lambda e: e.tensor_tensor(out=c1[:], in0=c1[:], in1=c2[:], op=ALU.add), [c1, c2], [c1])
        dv(lambda e: e.tensor_tensor(out=c2[:], in0=S8[:, :, 0:4], in1=S8[:, :, 3:7], op=ALU.is_gt), [S8], [c2])
        dv(lambda e: e.tensor_tensor(out=c1[:], in0=c1[:], in1=c2[:], op=ALU.add), [c1, c2], [c1])
        dv(lambda e: e.scalar_tensor_tensor(out=c1[:], in0=c1[:], scalar=2.0, in1=ghot[:].unsqueeze(2).broadcast_to([128, NT * 4, 4]),
                                            op0=ALU.is_ge, op1=ALU.mult), [c1, ghot], [c1])
        dv(lambda e: e.tensor_tensor(out=GA[:].rearrange("p t (g j) -> p (t g) j", j=4), in0=c1[:], in1=aff4, op=ALU.mult), [c1] + affv, [GA])
        dv(lambda e: e.tensor_reduce(out=den[:], in_=GA[:], axis=AX.X, op=ALU.add), [GA], [den])
        dv(lambda e: e.reciprocal(out=den[:], in_=den[:]), [den], [den])
        dv(lambda e: e.tensor_tensor(out=G[:], in0=GA[:], in1=den[:].unsqueeze(2).broadcast_to([128, NT, 16]), op=ALU.mult), [GA, den], [G])
        chs = c1
        chs3 = chs[:].rearrange("p (t g) j -> p t (g j)", g=4)
        CS = kb.sb([128, NT, 16], F32)
        dv(lambda e: e.memset(CS[:, 0, :], 0.0), [], [CS])
        for t in range(1, NT):
            dv(lambda e, t=t: e.tensor_tensor(out=CS[:, t, :], in0=CS[:, t - 1, :], in1=chs3[:, t - 1, :], op=ALU.add), [chs], [CS])
        rkb = banks[4]
        fns = []
        for t in range(NT):
            fns.append(lambda e, t=t: e.matmul(rkb[:, t * 16:(t + 1) * 16], ltri[:], chs3[:, t, :], start=True, stop=False))
            fns.append(lambda e, t=t: e.matmul(rkb[:, t * 16:(t + 1) * 16], ones[:], CS[:, t, :], start=False, stop=True))
        kb.group('pe', fns, reads=[ltri, ones, chs, CS], writes=[rkb])
        rank = kb.sb([128, NG], F32)
        sel = kb.sb([128, NG], F32)
        slotv = kb.sb([128, NT, 16], F32)
        slot2 = kb.sb([128, NT, 16], F32)
        Gv = S8
        Gv = kb.sb([128, NT, 16], F32)
        lo = kb.sb([128, NT], F32)
        hi = kb.sb([128, NT], F32)
        glo = kb.sb([128, NT], F32)
        ghi = kb.sb([128, NT], F32)
        lo_t = [kb.sb([128, 1], I32) for _ in range(NT)]
        hi_t = [kb.sb([128, 1], I32) for _ in range(NT)]
        chs_f = chs[:].rearrange("p a j -> p (a j)")
        dv(lambda e: e.tensor_copy(out=rank[:], in_=rkb[:, 0:NG]), [rkb], [rank])
        dv(lambda e: e.scalar_tensor_tensor(out=sel[:], in0=rank[:], scalar=float(C), in1=chs_f, op0=ALU.is_lt, op1=ALU.mult), [rank, chs], [sel])
        dv(lambda e: e.scalar_tensor_tensor(out=rank[:], in0=rank[:], scalar=-BIGIDX, in1=eoff[:], op0=ALU.add, op1=ALU.add), [rank, eoff], [rank])
        dv(lambda e: e.tensor_tensor(out=rank[:], in0=rank[:], in1=sel[:], op=ALU.mult), [rank, sel], [rank])
        dv(lambda e: e.tensor_scalar(out=slotv[:].rearrange("p t e -> p (t e)"), in0=rank[:], scalar1=BIGIDX, scalar2=None, op0=ALU.add), [rank], [slotv])
        dv(lambda e: e.tensor_tensor(out=Gv[:].rearrange("p t e -> p (t e)"), in0=G[:].rearrange("p t e -> p (t e)"), in1=sel[:], op=ALU.mult), [G, sel], [Gv])
        dv(lambda e: e.tensor_reduce(out=lo[:], in_=slotv[:], axis=AX.X, op=ALU.min), [slotv], [lo])
        dv(lambda e: e.tensor_tensor(out=slot2[:], in0=slotv[:], in1=lo[:].unsqueeze(2).broadcast_to([128, NT, 16]), op=ALU.is_equal), [slotv, lo], [slot2])
        dv(lambda e: e.tensor_tensor(out=GA[:], in0=Gv[:], in1=slot2[:], op=ALU.mult), [Gv, slot2], [GA])
        dv(lambda e: e.tensor_reduce(out=glo[:], in_=GA[:], axis=AX.X, op=ALU.add), [GA], [glo])
        dv(lambda e: e.scalar_tensor_tensor(out=slot2[:], in0=slot2[:], scalar=BIG2, in1=slotv[:], op0=ALU.mult, op1=ALU.add), [slot2, slotv], [slot2])
        dv(lambda e: e.tensor_reduce(out=hi[:], in_=slot2[:], axis=AX.X, op=ALU.min), [slot2], [hi])
        dv(lambda e: e.tensor_tensor(out=slot2[:], in0=slot2[:], in1=hi[:].unsqueeze(2).broadcast_to([128, NT, 16]), op=ALU.is_equal), [slot2, hi], [slot2])
        dv(lambda e: e.tensor_tensor(out=GA[:], in0=Gv[:], in1=slot2[:], op=ALU.mult), [Gv, slot2], [GA])
        dv(lambda e: e.tensor_reduce(out=ghi[:], in_=GA[:], axis=AX.X, op=ALU.add), [GA], [ghi])
        for t in range(NT):
            dv(lambda e, t=t: e.tensor_copy(out=lo_t[t][:], in_=lo[:, t:t + 1]), [lo], [lo_t[t]])
            dv(lambda e, t=t: e.tensor_copy(out=hi_t[t][:], in_=hi[:, t:t + 1]), [hi], [hi_t[t]])
        out_toks = []
        if debug:
            for k_, src in enumerate([lo, hi, glo, ghi]):
                out_toks.append(kb.dma('sp', dbg[k_], src[:], src, reads=[src]))
        bound = 16 * C - 1
        sc_toks = []
        for t in range(NT):
            xi = xin6[t % len(xin6)]
            kb.dma('sp' if t % 2 == 0 else 'act', xi[:], x[t * 128:(t + 1) * 128, :], xi, writes=[xi])
            kb.idma(xs[:, :], xi[:, :], lo_t[t][:, :], True, xsb, bound, reads=[xi, lo_t[t]], writes=[xsb], group=True)
            tk_s = kb.idma(xs[:, :], xi[:, :], hi_t[t][:, :], True, xsb, bound, reads=[xi, hi_t[t]], writes=[xsb], group=True)
        ybank = 0
        chunks = []
        o_ = 0
        while o_ < C:
            wdt = min(512, C - o_)
            chunks.append((o_, wdt))
            o_ += wdt
        ybank_box = [0]

        def prep_load(e):
            for s in range(NS):
                r0 = e * C + s * 128
                kb.dma('sp', xin6[s][:], xs[r0:r0 + 128, :], xin6[s], reads=[xsb], writes=[xin6[s]])

        def prep_tr(e):
            xT_, xTv_ = xTs[e % 2], xTvs[e % 2]
            for s in range(NS):
                xi = xin6[s]
                for hf in range(2):
                    bk = banks[4 + (ybank_box[0] % 4)]
                    ybank_box[0] += 1
                    kb.group('pe', [lambda e_, c=c, bk=bk, xi=xi: e_.transpose(bk[:, (c % 4) * 128:(c % 4 + 1) * 128],
                                                                                xi[:, c * 128:(c + 1) * 128], ident[:])
                                    for c in range(hf * 4, hf * 4 + 4)], reads=[xi, ident], writes=[bk])
                    if hf == 0:
                        ta = kb.op('act', lambda e_, bk=bk, s=s, xT_=xT_: e_.copy(out=xT_[:, 0:4, s * 128:(s + 1) * 128],
                                                                                   in_=bk[:].rearrange("p (c t) -> p c t", c=4)),
                                   reads=[bk], writes=[xTv_[s]])
                    else:
                        tb = kb.op('dve', lambda e_, bk=bk, s=s, xT_=xT_: e_.tensor_copy(out=xT_[:, 4:8, s * 128:(s + 1) * 128],
                                                                                          in_=bk[:].rearrange("p (c t) -> p c t", c=4)),
                                   reads=[bk], deps=[ta])
                        xTv_[s].w = [ta, tb]

        if NE > 0:
            prep_load(0)
            prep_tr(0)
        for e_i in range(NE):
            if e_i + 1 < NE:
                load_w13(e_i + 1)
                prep_load(e_i + 1)
            w1b, w3b = w13[e_i % 2]
            xT, xTv = xTs[e_i % 2], xTvs[e_i % 2]
            for ci_, (c0, wdt) in enumerate(chunks):
                tsl = slice(c0, c0 + wdt)
                tv = xTv[c0 // 128:(c0 + wdt) // 128]
                for fc in range(8):
                    b1 = banks[fc % 2]
                    b3 = banks[2 + fc % 2]
                    sl_ = sil[fc % 2]
                    fsl = slice(fc * 128, (fc + 1) * 128)
                    kb.group('pe', [lambda e, c=c, b1=b1, fsl=fsl, w1b=w1b, tsl=tsl, wdt=wdt, xT=xT: e.matmul(
                        b1[:, 0:wdt], w1b[:, c, fsl], xT[:, c, tsl], start=(c == 0), stop=(c == 7)) for c in range(8)],
                        reads=[w1b] + tv, writes=[b1])
                    kb.group('pe', [lambda e, c=c, b3=b3, fsl=fsl, w3b=w3b, tsl=tsl, wdt=wdt, xT=xT: e.matmul(
                        b3[:, 0:wdt], w3b[:, c, fsl], xT[:, c, tsl], start=(c == 0), stop=(c == 7)) for c in range(8)],
                        reads=[w3b] + tv, writes=[b3])
                    kb.op('act', lambda e, b1=b1, sl_=sl_, wdt=wdt: e.activation(out=sl_[:, 0:wdt], in_=b1[:, 0:wdt], func=AF.Silu),
                          reads=[b1], writes=[sl_])
                    kb.op('dve', lambda e, b3=b3, sl_=sl_, fc=fc, wdt=wdt: e.tensor_tensor(out=hT[:, fc, 0:wdt], in0=b3[:, 0:wdt],
                                                                                           in1=sl_[:, 0:wdt], op=ALU.mult),
                          reads=[b3, sl_], writes=[hv[fc]])
                for t4 in range(wdt // 128):
                    s = c0 // 128 + t4
                    yst = x32[s % 2]
                    ysf = yst[:].rearrange("p c t -> p (c t)")
                    for oh in range(2):
                        yb = banks[4 + ybank_box[0] % 4]
                        ybank_box[0] += 1
                        osl = slice(oh * 512, (oh + 1) * 512)
                        kb.group('pe', [lambda e, fc=fc, yb=yb, t4=t4, osl=osl: e.matmul(
                            yb[:], hT[:, fc, t4 * 128:(t4 + 1) * 128], w2b[:, fc, osl], start=(fc == 0), stop=(fc == 7))
                            for fc in range(8)], reads=[w2b] + hv, writes=[yb])
                        if oh == 0:
                            t0_ = kb.op('act', lambda e, yb=yb, ysf=ysf, osl=osl: e.copy(out=ysf[:, osl], in_=yb[:]), reads=[yb], writes=[yst])
                        else:
                            t1_ = kb.op('dve', lambda e, yb=yb, ysf=ysf, osl=osl: e.tensor_copy(out=ysf[:, osl], in_=yb[:]), reads=[yb], deps=[t0_])
                            yst.w = [t0_, t1_]
                    r0 = e_i * C + s * 128
                    kb.dma('act', ys[r0:r0 + 128, :], ysf, ysb, reads=[yst], writes=[ysb], group=True)
                if ci_ == 0 and e_i + 1 < NE:
                    prep_tr(e_i + 1)
            if e_i + 1 < NE:
                load_w2(e_i + 1)
        pe_done = Tok(kb.sems['pe'], kb.cnt['pe'])
        tiles = []
        for sl_ in range(2):
            for j in range(2):
                flat = w13[sl_][j][:].rearrange("p c f -> p (c f)").bitcast(F32)
                for q_ in range(4):
                    bf_ = Buf(kb, flat[:, q_ * D:(q_ + 1) * D], f"alias{sl_}{j}{q_}")
                    bf_.r = {id(pe_done.sem): pe_done}
                    bf_.w = list(w13[sl_][j].w)
                    tiles.append(bf_)
        ga2, gb2, acc2, xin3 = tiles[0:2] + tiles[12:13], tiles[2:4] + tiles[13:14], tiles[4:6], tiles[6:8] + xin
        L3 = LNBufs.__new__(LNBufs)
        L3.kb, L3.n, L3.i = kb, 2, 0
        L3.z, L3.o = tiles[8:10], tiles[10:12]
        L3.st = [kb.sb([128, 12], F32) for _ in range(2)]
        L3.mv = [kb.sb([128, 2], F32) for _ in range(2)]
        L3.rs = [kb.sb([128, 1], F32) for _ in range(2)]
        L3.nb = [kb.sb([128, 1], F32) for _ in range(2)]
        L3.eps = L.eps
        for bf_ in ga2 + gb2:
            kb.op('dve', lambda e, bf_=bf_: e.memset(bf_[:], 0.0), writes=[bf_])
        def fetch(t):
            xi = xin3[t % 4]
            kb.dma('sp' if t % 2 == 0 else 'act', xi[:], x[t * 128:(t + 1) * 128, :], xi, writes=[xi])
            kb.idma(ga2[t % 3][:, :], ys[:, :], lo_t[t][:, :], False, ga2[t % 3], bound, reads=[ysb, lo_t[t]], writes=[ga2[t % 3]])
            kb.idma(gb2[t % 3][:, :], ys[:, :], hi_t[t][:, :], False, gb2[t % 3], bound, reads=[ysb, hi_t[t]], writes=[gb2[t % 3]])

        fetch(0)
        fetch(1)
        avs = {}

        def front3(t):
            ga_, gb3, acc = ga2[t % 3], gb2[t % 3], acc2[t % 2]
            if t + 2 < NT:
                fetch(t + 2)
            kb.op('act', lambda e: e.activation(out=acc[:], in_=ga_[:], func=AF.Copy, scale=glo[:, t:t + 1]),
                  reads=[ga_, glo], writes=[acc])
            av = [kb.view(), kb.view()]
            for hf in range(2):
                sl = slice(hf * 512, (hf + 1) * 512)
                kb.op('dve', lambda e, sl=sl: e.scalar_tensor_tensor(
                    out=acc[:, sl], in0=gb3[:, sl], scalar=ghi[:, t:t + 1], in1=acc[:, sl], op0=ALU.mult, op1=ALU.add),
                    reads=[gb3, ghi, acc], writes=[av[hf]])
            avs[t] = av

        def ln1_3(t):
            xi, acc, av = xin3[t % 4], acc2[t % 2], avs.pop(t)
            slot = emit_ln1(kb, L3, xi, xi, [acc[:, 0:512], acc[:, 512:1024]], av)
            acc.r.update(av[0].r)
            acc.r.update(av[1].r)
            return slot

        def ln2_3(t, slot):
            return emit_ln2(kb, L3, slot, g, b, y[t * 128:(t + 1) * 128, :], dma_eng='sp' if t % 2 == 0 else 'act', b_eng='dve')
        out_toks += skewed(NT, front3, ln1_3, ln2_3)
        kb.wait_only('sp', out_toks)
        kb.emit()
    return nc


def pool_tables(first_is_start, last_is_end):
    Bm = np.zeros((3, 4, 128, 128), np.float32)
    Bh = np.zeros((3, 4, 16, 128), np.float32)
    for var in range(3):
        start_edge = (var == 0 and first_is_start)
        end_edge = (var == 2 and last_is_end)
        for g, w in enumerate((2, 4, 8, 16)):
            half = w // 2
            for t in range(128):
                lo, hi = t - half, t + half - 1
                if start_edge:
                    lo = max(lo, 0)
                if end_edge:
                    hi = min(hi, 127)
                cnt = hi - lo + 1
                for s in range(lo, hi + 1):
                    if 0 <= s < 128:
                        Bm[var, g, s, t] += 1.0 / cnt
                    elif s < 0:
                        Bh[var, g, 8 + s, t] += 1.0 / cnt
                    else:
                        Bh[var, g, 8 + (s - 128), t] += 1.0 / cnt
                Bm[var, g, t, t] -= 1.0
    return Bm, Bh


def build_pool(NT=32):
    nc = bass.Bass("TRN2", target_bir_lowering=False)
    T = NT * 128
    xp = nc.dram_tensor("xp", [T + 16, D], F32, kind="ExternalInput").ap()
    bm = nc.dram_tensor("bm", [128, 12 * 128], F32, kind="ExternalInput").ap()
    bh = nc.dram_tensor("bh", [16, 12 * 128], F32, kind="ExternalInput").ap()
    pw = nc.dram_tensor("pw", [4, 256, 256], F32, kind="ExternalInput").ap()
    psc = nc.dram_tensor("psc", [128, D], F32, kind="ExternalInput").ap()
    lng = nc.dram_tensor("lng", [128, D], F32, kind="ExternalInput").ap()
    lnb = nc.dram_tensor("lnb", [128, D], F32, kind="ExternalInput").ap()
    y = nc.dram_tensor("y", [T, D], F32, kind="ExternalOutput").ap()
    with ExitStack() as es:
        kb = KB(nc, es)
        g = kb.sb([128, D], F32)
        b = kb.sb([128, D], F32)
        kb.dma('sp', g[:], lng[:, :], g, writes=[g])
        kb.dma('sp', b[:], lnb[:, :], b, writes=[b])
        L = LNBufs(kb, nbuf=2)
        Bm = kb.sb([128, 12 * 128], BF16)
        Bh = kb.sb([16, 12 * 128], BF16)
        kb.dma('pool', Bm[:], bm[:, :], Bm, writes=[Bm])
        kb.dma('pool', Bh[:], bh[:, :], Bh, writes=[Bh])
        W32 = kb.sb([128, 4, 2, 256], F32)
        sc = kb.sb([128, D], F32)
        Wb = kb.sb([128, 4, 2, 256], BF16)
        kb.dma('sp', W32[:], pw.rearrange("g (hh p) e -> p g hh e", p=128), W32, writes=[W32])
        kb.dma('sp', sc[:], psc[:, :], sc, writes=[sc])
        kb.op('dve', lambda e: e.tensor_tensor(out=Wb[:], in0=W32[:],
                                               in1=sc[:].rearrange("p (g e) -> p g e", g=4).unsqueeze(2).broadcast_to([128, 4, 2, 256]),
                                               op=ALU.mult), reads=[W32, sc], writes=[Wb])
        xin = [kb.sb([128, D], F32) for _ in range(3)]
        xh = [kb.sb([16, D], F32) for _ in range(2)]
        xb = [kb.sb([128, D], BF16) for _ in range(2)]
        xhb = [kb.sb([16, D], BF16) for _ in range(2)]
        uT = [kb.sb([128, 8, 128], BF16) for _ in range(2)]
        uTa = [kb.view() for _ in range(2)]
        uTb = [kb.view() for _ in range(2)]
        pu = [[kb.ps([128, 512], F32) for _ in range(2)] for _ in range(2)]
        py = [[kb.ps([128, 512], F32) for _ in range(2)] for _ in range(2)]
        def front(i):
            var = 0 if i == 0 else (2 if i == NT - 1 else 1)
            xi = xin[i % 3]
            xhi = xh[i % 2]
            xbi = xb[i % 2]
            xhbi = xhb[i % 2]
            u = uT[i % 2]
            ua, ub = uTa[i % 2], uTb[i % 2]
            pui = pu[i % 2]
            pyi = py[i % 2]
            kb.dma('sp', xi[:], xp[8 + i * 128: 8 + (i + 1) * 128, :], xi, writes=[xi])
            kb.dma('sp', xhi[0:8, :], xp[i * 128: i * 128 + 8, :], xhi, writes=[xhi])
            kb.dma('sp', xhi[8:16, :], xp[8 + (i + 1) * 128: 16 + (i + 1) * 128, :], xhi, writes=[xhi], group=True)
            kb.op('act', lambda e, xi=xi, xbi=xbi: e.copy(out=xbi[:], in_=xi[:]), reads=[xi], writes=[xbi])
            kb.op('dve', lambda e, xhi=xhi, xhbi=xhbi: e.tensor_copy(out=xhbi[:], in_=xhi[:]), reads=[xhi], writes=[xhbi])
            for hf in range(2):
                fns = []
                for fc in range(hf * 4, hf * 4 + 4):
                    col = (var * 4 + fc // 2) * 128
                    fns.append(lambda e, fc=fc, col=col, xbi=xbi, bk=pui[hf]: e.matmul(
                        bk[:, (fc % 4) * 128:(fc % 4 + 1) * 128], xbi[:, fc * 128:(fc + 1) * 128], Bm[:, col:col + 128],
                        start=True, stop=False))
                    fns.append(lambda e, fc=fc, col=col, xhbi=xhbi, bk=pui[hf]: e.matmul(
                        bk[:, (fc % 4) * 128:(fc % 4 + 1) * 128], xhbi[:, fc * 128:(fc + 1) * 128], Bh[:, col:col + 128],
                        start=False, stop=True))
                kb.group('pe', fns, reads=[xbi, xhbi, Bm, Bh], writes=[pui[hf]])
            kb.op('act', lambda e, u=u, bk=pui[0]: e.copy(out=u[:, 0:4, :], in_=bk[:].rearrange("p (c t) -> p c t", c=4)),
                  reads=[pui[0]], writes=[ua])
            kb.op('dve', lambda e, u=u, bk=pui[1]: e.tensor_copy(out=u[:, 4:8, :], in_=bk[:].rearrange("p (c t) -> p c t", c=4)),
                  reads=[pui[1]], writes=[ub])
            for bkidx in range(2):
                fns = []
                for gg in range(bkidx * 2, bkidx * 2 + 2):
                    for hh in range(2):
                        fns.append(lambda e, gg=gg, hh=hh, u=u, bk=pyi[bkidx]: e.matmul(
                            bk[:, (gg % 2) * 256:(gg % 2 + 1) * 256], u[:, gg * 2 + hh, :], Wb[:, gg, hh, :],
                            start=(hh == 0), stop=(hh == 1)))
                kb.group('pe', fns, reads=[ua if bkidx == 0 else ub, Wb], writes=[pyi[bkidx]])

        def ln1(i):
            xi, pyi = xin[i % 3], py[i % 2]
            return emit_ln1(kb, L, xi, xi, [pyi[0][:], pyi[1][:]], pyi)

        def ln2(i, slot):
            return emit_ln2(kb, L, slot, g, b, y[i * 128:(i + 1) * 128, :], dma_eng='sp' if i % 2 == 0 else 'act')
        out_toks = skewed(NT, front, ln1, ln2)
        kb.wait_only('sp', out_toks)
        kb.emit()
    return nc


def build_qkv(NT=32):
    nc = bass.Bass("TRN2", target_bir_lowering=False)
    T = NT * 128
    x = nc.dram_tensor("x", [T, D], F32, kind="ExternalInput").ap()
    win = nc.dram_tensor("win", [D, 3 * D], F32, kind="ExternalInput").ap()
    cs = nc.dram_tensor("cs", [T, 64], F32, kind="ExternalInput").ap()
    qkv = nc.dram_tensor("qkv", [T, 3 * D], BF16, kind="ExternalOutput").ap()
    with ExitStack() as es:
        kb = KB(nc, es)
        ident = kb.sb([128, 128], F32)
        make_ident(kb, ident)
        wb = kb.sb([128, 8, 3 * D], BF16)
        wv = [kb.view() for _ in range(8)]
        for c in range(8):
            kb.dma('pool', wb[:, c, :], win[c * 128:(c + 1) * 128, :], wv[c], writes=[wv[c]])
        kb.op('pool', lambda e: e.tensor_scalar(out=wb[:, :, 0:D], in0=wb[:, :, 0:D], scalar1=0.125, scalar2=None, op0=ALU.mult),
              reads=[], writes=wv)
        xin = [kb.sb([128, D], F32) for _ in range(2)]
        cst = [kb.sb([128, 64], F32) for _ in range(2)]
        xT = [kb.sb([128, 8, 128], BF16) for _ in range(2)]
        kro = [kb.sb([128, 512], F32) for _ in range(2)]
        tm = [kb.sb([128, 8, 32], F32) for _ in range(4)]
        tp = [kb.sb([128, 8, 32], F32) for _ in range(4)]
        ost = [kb.sb([128, 3 * D], BF16) for _ in range(2)]
        ostv = [[kb.view() for _ in range(6)] for _ in range(2)]
        banks = [kb.ps([128, 512], F32) for _ in range(8)]
        nb = 0
        out_toks = []
        for i in range(NT):
            xi = xin[i % 2]
            ci = cst[i % 2]
            xt = xT[i % 2]
            oi = ost[i % 2]
            ov = ostv[i % 2]
            kr = kro[i % 2]
            kb.dma('sp', xi[:], x[i * 128:(i + 1) * 128, :], xi, writes=[xi])
            kb.dma('sp', ci[:], cs[i * 128:(i + 1) * 128, :], ci, writes=[ci])
            for hf in range(2):
                bk = banks[nb % 8]; nb += 1
                kb.group('pe', [lambda e, c=c, bk=bk, xi=xi: e.transpose(bk[:, (c % 4) * 128:(c % 4 + 1) * 128],
                                                                           xi[:, c * 128:(c + 1) * 128], ident[:])
                                for c in range(hf * 4, hf * 4 + 4)], reads=[xi, ident], writes=[bk])
                eng = 'act' if hf == 0 else 'dve'
                fn = (lambda e, bk=bk, hf=hf, xt=xt: e.copy(out=xt[:, hf * 4:(hf + 1) * 4, :], in_=bk[:].rearrange("p (c t) -> p c t", c=4))) \
                    if hf == 0 else (lambda e, bk=bk, hf=hf, xt=xt: e.tensor_copy(out=xt[:, hf * 4:(hf + 1) * 4, :], in_=bk[:].rearrange("p (c t) -> p c t", c=4)))
                if hf == 0:
                    t_a = kb.op(eng, fn, reads=[bk], writes=[xt])
                else:
                    t_b = kb.op(eng, fn, reads=[bk], deps=[t_a])
                    xt.w = [t_a, t_b]
            cosb = ci[:, 0:32].unsqueeze(1).broadcast_to([128, 8, 32])
            sinb = ci[:, 32:64].unsqueeze(1).broadcast_to([128, 8, 32])
            for blk in range(6):
                bk = banks[nb % 8]; nb += 1
                kb.group('pe', [lambda e, c=c, bk=bk, blk=blk, xt=xt: e.matmul(bk[:], xt[:, c, :], wb[:, c, blk * 512:(blk + 1) * 512],
                                                                               start=(c == 0), stop=(c == 7)) for c in range(8)],
                         reads=[xt] + wv, writes=[bk])
                osl = slice(blk * 512, (blk + 1) * 512)
                if blk in (0, 2):
                    if blk == 0:
                        eng, tt, src, sbuf = 'dve', tm, bk, bk
                    else:
                        kb.op('act', lambda e, bk=bk, kr=kr: e.copy(out=kr[:], in_=bk[:]), reads=[bk], writes=[kr])
                        eng, tt, src, sbuf = 'pool', tp, kr, kr
                    s4 = src[:].rearrange("p (h two j) -> p h two j", two=2, j=32)
                    o4 = oi[:, osl].rearrange("p (h two j) -> p h two j", two=2, j=32)
                    t1, t2 = s4[:, :, 0, :], s4[:, :, 1, :]
                    kb.op(eng, lambda e, t1=t1, tt=tt, cosb=cosb: e.tensor_tensor(out=tt[0][:], in0=t1, in1=cosb, op=ALU.mult), reads=[sbuf, ci], writes=[tt[0]])
                    kb.op(eng, lambda e, t2=t2, tt=tt, sinb=sinb: e.tensor_tensor(out=tt[1][:], in0=t2, in1=sinb, op=ALU.mult), reads=[sbuf, ci], writes=[tt[1]])
                    kb.op(eng, lambda e, t2=t2, tt=tt, cosb=cosb: e.tensor_tensor(out=tt[2][:], in0=t2, in1=cosb, op=ALU.mult), reads=[sbuf, ci], writes=[tt[2]])
                    kb.op(eng, lambda e, t1=t1, tt=tt, sinb=sinb: e.tensor_tensor(out=tt[3][:], in0=t1, in1=sinb, op=ALU.mult), reads=[sbuf, ci], writes=[tt[3]])
                    kb.op(eng, lambda e, tt=tt, o4=o4: e.tensor_tensor(out=o4[:, :, 0, :], in0=tt[0][:], in1=tt[1][:], op=ALU.subtract), reads=[tt[0], tt[1]], writes=[ov[blk]])
                    tk = kb.op(eng, lambda e, tt=tt, o4=o4: e.tensor_tensor(out=o4[:, :, 1, :], in0=tt[2][:], in1=tt[3][:], op=ALU.add), reads=[tt[2], tt[3]], deps=ov[blk].w)
                    ov[blk].w = ov[blk].w + [tk]
                else:
                    kb.op('act', lambda e, bk=bk, oi=oi, osl=osl: e.copy(out=oi[:, osl], in_=bk[:]), reads=[bk], writes=[ov[blk]])
            out_toks.append(kb.dma('act', qkv[i * 128:(i + 1) * 128, :], oi[:], oi, reads=ov))
        kb.wait_only('sp', out_toks)
        kb.emit()
    return nc


def build_attn(NB=4):
    nc = bass.Bass("TRN2", target_bir_lowering=False)
    S = S_LEN
    qA, kA, vA, oA = [], [], [], []
    for di, d in enumerate(DILS):
        Ls = S // d
        nt = Ls // 128
        qA.append(nc.dram_tensor(f"qA{di}", [NB, 64, S], BF16, kind="ExternalInput").ap())
        kA.append(nc.dram_tensor(f"kA{di}", [NB, 64, d * (Ls + 128)], BF16, kind="ExternalInput").ap())
        vA.append(nc.dram_tensor(f"vA{di}", [NB, 128, d * (nt + 1) * 65], BF16, kind="ExternalInput").ap())
        oA.append(nc.dram_tensor(f"oA{di}", [NB, 128, 64 * 65], F32, kind="ExternalOutput").ap())
    qB = nc.dram_tensor("qB", [NB, 64, S], BF16, kind="ExternalInput").ap()
    kB = nc.dram_tensor("kB", [NB, 64, S], BF16, kind="ExternalInput").ap()
    vB = nc.dram_tensor("vB", [NB, 128, 64 * 65], BF16, kind="ExternalInput").ap()
    oB = nc.dram_tensor("oB", [NB, 128, 64 * 64], F32, kind="ExternalOutput").ap()
    ebias = nc.dram_tensor("ebias", [128, 25 * 128], F32, kind="ExternalInput").ap()
    maskd = nc.dram_tensor("maskd", [128, 256], F32, kind="ExternalInput").ap()
    with ExitStack() as es:
        kb = KB(nc, es)
        mask = kb.sb([128, 256], BF16)
        kb.dma('pool', mask[:], maskd[:, :], mask, writes=[mask])
        eb32 = kb.sb([128, 25 * 128], F32)
        E = kb.sb([128, 25 * 128], BF16)
        kb.dma('sp', eb32[:], ebias[:, :], eb32, writes=[eb32])
        kb.op('act', lambda e: e.activation(out=E[:], in_=eb32[:], func=AF.Exp), reads=[eb32], writes=[E])
        KMAX = 16 * (512 + 128)
        qT = [kb.sb([64, S], BF16) for _ in range(2)]
        kT = [kb.sb([64, KMAX], BF16) for _ in range(2)]
        V = [kb.sb([128, 80 * 65], BF16) for _ in range(2)]
        osb = [kb.sb([128, 64 * 65], F32) for _ in range(2)]
        pT = [kb.sb([128, 256], BF16) for _ in range(6)]
        rden = [kb.sb([128, 1], F32) for _ in range(2)]
        sbk = [kb.ps([128, 512], F32) for _ in range(4)]
        obk = [kb.ps([128, 512], F32) for _ in range(4)]
        stages = []
        for b in range(NB):
            for di in range(3):
                stages.append(('A', b, di))
            stages.append(('B', b, None))

        def load(si):
            kind, b, di = stages[si]
            sl = si % 2
            if kind == 'A':
                d = DILS[di]
                Ls = S // d
                nt = Ls // 128
                kb.dma('sp', qT[sl][:], qA[di][b], qT[sl], writes=[qT[sl]])
                kb.dma('sp', kT[sl][:, 0:d * (Ls + 128)], kA[di][b], kT[sl], writes=[kT[sl]])
                kb.dma('sp', V[sl][:, 0:d * (nt + 1) * 65], vA[di][b], V[sl], writes=[V[sl]])
            else:
                kb.dma('sp', qT[sl][:], qB[b], qT[sl], writes=[qT[sl]])
                kb.dma('sp', kT[sl][:, 0:S], kB[b], kT[sl], writes=[kT[sl]])
                kb.dma('sp', V[sl][:, 0:64 * 65], vB[b], V[sl], writes=[V[sl]])

        load(0)
        out_toks = []
        fronts, backs = [], []
        LOOK = 3

        def add_step(si, first, last, front_fn, back_fn):
            n = len(fronts)

            def front(n=n):
                front_fn(n)

            def back(n=n):
                if first and si + 1 < len(stages):
                    load(si + 1)
                back_fn(n)
                if last:
                    kind, b, di = stages[si]
                    ob = osb[si % 2]
                    if kind == 'A':
                        out_toks.append(kb.dma('sp', oA[di][b], ob[:], ob, reads=[ob]))
                    else:
                        out_toks.append(kb.dma('sp', oB[b], ob[:, 0:64 * 64], ob, reads=[ob]))
            fronts.append(front)
            backs.append(back)

        for si, (kind, b, di) in enumerate(stages):
            sl = si % 2
            q, k, v, ob = qT[sl], kT[sl], V[sl], osb[sl]
            if kind == 'A':
                d = DILS[di]
                Ls = S // d
                nt = Ls // 128
                for r in range(d):
                    for j in range(nt + 1):
                        q0 = 128 * (j - 1) if j >= 1 else 0
                        q1 = 128 * (j + 1) if j <= nt - 1 else 128 * nt
                        w = q1 - q0
                        moff = 128 if j == 0 else 0
                        koff = r * (Ls + 128) + 128 * j
                        vt = r * (nt + 1) + j
                        qa = r * Ls + q0

                        def front_fn(step, k=k, q=q, koff=koff, qa=qa, w=w, moff=moff):
                            sb_ = sbk[step % 4]
                            p = pT[step % 6]
                            kb.op('pe', lambda e: e.matmul(sb_[:, 0:w], k[:, koff:koff + 128], q[:, qa:qa + w], start=True, stop=True),
                                  reads=[k, q], writes=[sb_])
                            kb.op('act', lambda e: e.activation(out=p[:, 0:w], in_=sb_[:, 0:w], func=AF.Exp), reads=[sb_], writes=[p])
                            meng = 'dve' if step % 2 == 0 else 'pool'
                            kb.op(meng, lambda e: e.tensor_tensor(out=p[:, 0:w], in0=p[:, 0:w], in1=mask[:, moff:moff + w], op=ALU.mult),
                                  reads=[p, mask], writes=[p])

                        def back_fn(step, j=j, nt=nt, r=r, v=v, vt=vt, ob=ob):
                            p = pT[step % 6]
                            if j >= 1:
                                o_ = obk[(j - 1) % 4]
                                kb.op('pe', lambda e: e.matmul(o_[:, 0:65], p[:, 0:128], v[:, vt * 65:(vt + 1) * 65], start=False, stop=True),
                                      reads=[p, v], writes=[], deps=o_.w + list(o_.r.values()))
                                o_.w = [Tok(kb.sems['pe'], kb.cnt['pe'])]
                                blk = r * nt + (j - 1)
                                kb.op('dve', lambda e: e.tensor_copy(out=ob[:, blk * 65:(blk + 1) * 65], in_=o_[:, 0:65]),
                                      reads=[o_], writes=[], deps=list(ob.r.values()))
                                ob.w = [Tok(kb.sems['dve'], kb.cnt['dve'])]
                            if j <= nt - 1:
                                o2 = obk[j % 4]
                                pc = 128 if j >= 1 else 0
                                kb.op('pe', lambda e: e.matmul(o2[:, 0:65], p[:, pc:pc + 128], v[:, vt * 65:(vt + 1) * 65], start=True, stop=False),
                                      reads=[p, v], writes=[o2])
                        add_step(si, r == 0 and j == 0, r == d - 1 and j == nt, front_fn, back_fn)
            else:
                for m in range(64):
                    cls = {0: 0, 1: 1, 62: 3, 63: 4}.get(m, 2)
                    bt = min(max(m - 2, 0), 59)
                    for j in range(5):
                        tile = bt + j
                        ecol = (cls * 5 + j) * 128

                        def front_fn(step, k=k, q=q, tile=tile, m=m, ecol=ecol):
                            sb_ = sbk[step % 4]
                            p = pT[step % 6]
                            kb.op('pe', lambda e: e.matmul(sb_[:, 0:128], k[:, tile * 128:(tile + 1) * 128], q[:, m * 128:(m + 1) * 128],
                                                           start=True, stop=True), reads=[k, q], writes=[sb_])
                            kb.op('act', lambda e: e.activation(out=p[:, 0:128], in_=sb_[:, 0:128], func=AF.Exp), reads=[sb_], writes=[p])
                            meng = 'dve' if step % 2 == 0 else 'pool'
                            kb.op(meng, lambda e: e.tensor_tensor(out=p[:, 0:128], in0=p[:, 0:128], in1=E[:, ecol:ecol + 128], op=ALU.mult),
                                  reads=[p, E], writes=[p])

                        def back_fn(step, j=j, m=m, v=v, tile=tile, ob=ob):
                            p = pT[step % 6]
                            o_ = obk[m % 4]
                            if j == 0:
                                kb.op('pe', lambda e: e.matmul(o_[:, 0:65], p[:, 0:128], v[:, tile * 65:(tile + 1) * 65], start=True, stop=False),
                                      reads=[p, v], writes=[o_])
                            else:
                                kb.op('pe', lambda e: e.matmul(o_[:, 0:65], p[:, 0:128], v[:, tile * 65:(tile + 1) * 65], start=False, stop=(j == 4)),
                                      reads=[p, v], writes=[], deps=o_.w)
                                o_.w = [Tok(kb.sems['pe'], kb.cnt['pe'])]
                            if j == 4:
                                rd = rden[m % 2]
                                kb.op('dve', lambda e: e.reciprocal(out=rd[:], in_=o_[:, 64:65]), reads=[o_], writes=[rd])
                                kb.op('dve', lambda e: e.tensor_scalar(out=ob[:, m * 64:(m + 1) * 64], in0=o_[:, 0:64], scalar1=rd[:, 0:1],
                                                                       scalar2=None, op0=ALU.mult),
                                      reads=[o_, rd], writes=[], deps=list(ob.r.values()))
                                ob.w = [Tok(kb.sems['dve'], kb.cnt['dve'])]
                        add_step(si, m == 0 and j == 0, m == 63 and j == 4, front_fn, back_fn)
        nsteps = len(fronts)
        for n in range(min(LOOK, nsteps)):
            fronts[n]()
        for n in range(nsteps):
            if n + LOOK < nsteps:
                fronts[n + LOOK]()
            backs[n]()
        kb.wait_only('sp', out_toks)
        kb.emit()
    return nc


def build_oproj(NT=32):
    nc = bass.Bass("TRN2", target_bir_lowering=False)
    T = NT * 128
    x = nc.dram_tensor("x", [T, D], F32, kind="ExternalInput").ap()
    oa = nc.dram_tensor("oa", [3, T, 8 * 65], F32, kind="ExternalInput").ap()
    obd = nc.dram_tensor("ob", [T, 512], F32, kind="ExternalInput").ap()
    wout = nc.dram_tensor("wout", [D, D], F32, kind="ExternalInput").ap()
    lng = nc.dram_tensor("lng", [128, D], F32, kind="ExternalInput").ap()
    lnb = nc.dram_tensor("lnb", [128, D], F32, kind="ExternalInput").ap()
    y = nc.dram_tensor("y", [T, D], F32, kind="ExternalOutput").ap()
    with ExitStack() as es:
        kb = KB(nc, es)
        ident = kb.sb([128, 128], F32)
        make_ident(kb, ident)
        g = kb.sb([128, D], F32)
        b = kb.sb([128, D], F32)
        kb.dma('sp', g[:], lng[:, :], g, writes=[g])
        kb.dma('sp', b[:], lnb[:, :], b, writes=[b])
        L = LNBufs(kb, nbuf=2)
        wb = kb.sb([128, 8, D], BF16)
        kb.dma('pool', wb[:], wout.rearrange("(c p) f -> p c f", p=128), wb, writes=[wb])
        xin = [kb.sb([128, D], F32) for _ in range(3)]
        oat = [[kb.sb([128, 8, 65], F32) for _ in range(3)] for _ in range(2)]
        rd = [kb.sb([128, 8], F32) for _ in range(2)]
        ot = [kb.sb([128, D], F32) for _ in range(2)]
        ota = [kb.view() for _ in range(2)]
        otb = [kb.view() for _ in range(2)]
        oT = [kb.sb([128, 8, 128], BF16) for _ in range(2)]
        oTa = [kb.view() for _ in range(2)]
        oTb = [kb.view() for _ in range(2)]
        pt = [[kb.ps([128, 512], F32) for _ in range(2)] for _ in range(2)]
        py = [[kb.ps([128, 512], F32) for _ in range(2)] for _ in range(2)]
        def front(i):
            xi = xin[i % 3]
            a3 = oat[i % 2]
            o = ot[i % 2]
            r_ = rd[i % 2]
            oTi = oT[i % 2]
            rows = slice(i * 128, (i + 1) * 128)
            kb.dma('sp', xi[:], x[rows, :], xi, writes=[xi])
            for br in range(3):
                kb.dma('act' if br == 1 else 'sp', a3[br][:], oa[br, rows, :].rearrange("p (h c) -> p h c", c=65), a3[br], writes=[a3[br]])
            kb.dma('act', o[:, 512:1024], obd[rows, :], otb[i % 2], writes=[otb[i % 2]])
            kb.op('dve', lambda e, a3=a3: e.tensor_tensor(out=a3[0][:], in0=a3[0][:], in1=a3[1][:], op=ALU.add), reads=[a3[1]], writes=[a3[0]])
            kb.op('dve', lambda e, a3=a3: e.tensor_tensor(out=a3[0][:], in0=a3[0][:], in1=a3[2][:], op=ALU.add), reads=[a3[2]], writes=[a3[0]])
            kb.op('dve', lambda e, a3=a3, r_=r_: e.reciprocal(out=r_[:], in_=a3[0][:, :, 64]), reads=[a3[0]], writes=[r_])
            kb.op('dve', lambda e, a3=a3, r_=r_, o=o: e.tensor_tensor(out=o[:, 0:512].rearrange("p (h c) -> p h c", c=64), in0=a3[0][:, :, 0:64],
                                                                   in1=r_[:].unsqueeze(2).broadcast_to([128, 8, 64]), op=ALU.mult),
                  reads=[a3[0], r_], writes=[ota[i % 2]])
            for hf in range(2):
                bk = pt[i % 2][hf]
                kb.group('pe', [lambda e, c=c, bk=bk, o=o: e.transpose(bk[:, (c % 4) * 128:(c % 4 + 1) * 128],
                                                                        o[:, c * 128:(c + 1) * 128], ident[:])
                                for c in range(hf * 4, hf * 4 + 4)], reads=[ota[i % 2] if hf == 0 else otb[i % 2], ident], writes=[bk])
                if hf == 0:
                    kb.op('act', lambda e, bk=bk, oTi=oTi: e.copy(out=oTi[:, 0:4, :], in_=bk[:].rearrange("p (c t) -> p c t", c=4)),
                          reads=[bk], writes=[oTa[i % 2]])
                else:
                    kb.op('dve', lambda e, bk=bk, oTi=oTi: e.tensor_copy(out=oTi[:, 4:8, :], in_=bk[:].rearrange("p (c t) -> p c t", c=4)),
                          reads=[bk], writes=[oTb[i % 2]])
            for oh in range(2):
                bk = py[i % 2][oh]
                kb.group('pe', [lambda e, c=c, bk=bk, oTi=oTi, oh=oh: e.matmul(bk[:], oTi[:, c, :], wb[:, c, oh * 512:(oh + 1) * 512],
                                                                               start=(c == 0), stop=(c == 7)) for c in range(8)],
                         reads=[oTa[i % 2], oTb[i % 2], wb], writes=[bk])

        def ln1(i):
            xi = xin[i % 3]
            return emit_ln1(kb, L, xi, xi, [py[i % 2][0][:], py[i % 2][1][:]], py[i % 2])

        def ln2(i, slot):
            return emit_ln2(kb, L, slot, g, b, y[i * 128:(i + 1) * 128, :], dma_eng='sp' if i % 2 == 0 else 'act')
        out_toks = skewed(NT, front, ln1, ln2)
        kb.wait_only('sp', out_toks)
        kb.emit()
    return nc

import ml_dtypes
BF = ml_dtypes.bfloat16
S_LEN = 8192
DILS = (1, 4, 16)


def rope_cs(pos):
    pos = pos.astype(np.float32)
    inv = (np.float32(10000.0) ** (-np.arange(0, 64, 2, dtype=np.float32) / np.float32(64))).astype(np.float32)
    ang = (pos[:, None] * inv[None, :]).astype(np.float32)
    return np.concatenate([np.cos(ang), np.sin(ang)], 1).astype(np.float32)


def attn_mask():
    p = np.arange(128)[:, None]
    a = np.arange(128)[None, :]
    return np.concatenate([(p <= a), (p >= a)], 1).astype(np.float32)


def na_bias_table(rpb_h):
    out = np.full((128, 25, 128), -30000.0, np.float32)
    kk = np.arange(128)
    qq = np.arange(128)
    for cls, m in enumerate((0, 1, 2, 62, 63)):
        base = min(max(2 * m - 4, 0), 118)
        qrow = 2 * m + qq // 64
        qcol = qq % 64
        rstart = np.clip(qrow - 4, 0, 120)
        cstart = np.clip(qcol - 8, 0, 48)
        for j in range(5):
            krow = base + 2 * j + kk // 64
            kcol = kk % 64
            ok = ((krow[:, None] >= rstart[None, :]) & (krow[:, None] < rstart[None, :] + 8)
                  & (kcol[:, None] >= cstart[None, :]) & (kcol[:, None] < cstart[None, :] + 16))
            roff = np.clip(krow[:, None] - qrow[None, :] + 7, 0, 14)
            coff = np.clip(kcol[:, None] - qcol[None, :], -15, 15) + 15
            vals = rpb_h[roff, coff]
            out[:, cls * 5 + j, :] = np.where(ok, vals, np.float32(-30000.0))
    return out.reshape(128, 25 * 128)


def attn_core_inputs(qkv, c, rpb_i):
    B, S = qkv.shape[0], qkv.shape[1]
    ha, hb = c, 8 + c
    im = {}
    q_h, k_h, v_h = qkv[:, :, 0, ha], qkv[:, :, 1, ha], qkv[:, :, 2, ha]
    one = np.ones((), BF)
    for di, d in enumerate(DILS):
        Ls = S // d
        nt = Ls // 128
        im[f"qA{di}"] = np.ascontiguousarray(q_h.reshape(B, Ls, d, 64).transpose(0, 3, 2, 1)).reshape(B, 64, d * Ls)
        kk = np.zeros((B, 64, d, Ls + 128), BF)
        kk[:, :, :, 64:64 + Ls] = k_h.reshape(B, Ls, d, 64).transpose(0, 3, 2, 1)
        im[f"kA{di}"] = kk.reshape(B, 64, d * (Ls + 128))
        vv = np.zeros((B, d, Ls + 128, 65), BF)
        vv[:, :, 64:64 + Ls, :64] = v_h.reshape(B, Ls, d, 64).transpose(0, 2, 1, 3)
        vv[:, :, 64:64 + Ls, 64] = one
        im[f"vA{di}"] = np.ascontiguousarray(vv.reshape(B, d, nt + 1, 128, 65).transpose(0, 3, 1, 2, 4)).reshape(B, 128, d * (nt + 1) * 65)
    im["qB"] = np.ascontiguousarray(qkv[:, :, 0, hb].transpose(0, 2, 1))
    im["kB"] = np.ascontiguousarray(qkv[:, :, 1, hb].transpose(0, 2, 1))
    vb = np.zeros((B, S, 65), BF)
    vb[:, :, :64] = qkv[:, :, 2, hb]
    vb[:, :, 64] = one
    im["vB"] = np.ascontiguousarray(vb.reshape(B, 64, 128, 65).transpose(0, 2, 1, 3)).reshape(B, 128, 64 * 65)
    im["ebias"] = na_bias_table(rpb_i[c])
    im["maskd"] = attn_mask()
    return im


def attn_core_outputs(results, B, S):
    oa = np.zeros((3, B, S, 8, 65), np.float32)
    ob = np.zeros((B, S, 8, 64), np.float32)
    for c, r in enumerate(results):
        for di, d in enumerate(DILS):
            Ls = S // d
            nt = Ls // 128
            a = r[f"oA{di}"].reshape(B, 128, d, nt, 65).transpose(0, 3, 1, 2, 4).reshape(B, S, 65)
            oa[di, :, :, c, :] = a
        ob[:, :, c, :] = r["oB"].reshape(B, 128, 64, 64).transpose(0, 2, 1, 3).reshape(B, S, 64)
    return oa.reshape(3, B, S, 8 * 65), ob.reshape(B, S, 512)


N_CORES = 8
_PROGS = {}


def _prog(name, fn):
    if name not in _PROGS:
        _PROGS[name] = fn()
    return _PROGS[name]


def _run(nc, in_maps):
    return run_bass_kernel_spmd(nc, in_maps, core_ids=list(range(N_CORES))).results


def _rep(v):
    return np.ascontiguousarray(np.broadcast_to(np.asarray(v, np.float32)[None], (128, 1024)))


def _attn_layer(xf, w_in, w_out, rpb_i, g, b, B, S):
    T = xf.shape[0] // N_CORES
    NT = T // 128
    halves = S // T
    nc1 = _prog('qkv', lambda: build_qkv(NT=NT))
    ims = []
    for c in range(N_CORES):
        p0 = (c % halves) * T
        ims.append({"x": xf[c * T:(c + 1) * T], "win": w_in, "cs": rope_cs(np.arange(p0, p0 + T))})
    r1 = _run(nc1, ims)
    qkv = np.concatenate([r["qkv"] for r in r1], 0).reshape(B, S, 3, 16, 64)
    nc2 = _prog('attn', lambda: build_attn(NB=B))
    r2 = _run(nc2, [attn_core_inputs(qkv, c, rpb_i) for c in range(N_CORES)])
    oa, ob = attn_core_outputs(r2, B, S)
    oa = oa.reshape(3, B * S, 8 * 65)
    ob = ob.reshape(B * S, 512)
    nc3 = _prog('oproj', lambda: build_oproj(NT=NT))
    ims = [{"x": xf[c * T:(c + 1) * T], "oa": np.ascontiguousarray(oa[:, c * T:(c + 1) * T]),
            "ob": np.ascontiguousarray(ob[c * T:(c + 1) * T]), "wout": w_out, "lng": _rep(g), "lnb": _rep(b)}
           for c in range(N_CORES)]
    r3 = _run(nc3, ims)
    return np.concatenate([r["y"] for r in r3], 0)


def _pool_layer(xf, pw, psc, g, b, B, S):
    T = xf.shape[0] // N_CORES
    NT = T // 128
    halves = S // T
    ncp = _prog('pool', lambda: build_pool(NT=NT))
    ims = []
    for c in range(N_CORES):
        h = c % halves
        xp = np.zeros((T + 16, 1024), np.float32)
        lo = c * T - (8 if h > 0 else 0)
        hi = (c + 1) * T + (8 if h < halves - 1 else 0)
        xp[8 - (c * T - lo): 8 + T + (hi - (c + 1) * T)] = xf[lo:hi]
        Bm, Bh = pool_tables(h == 0, h == halves - 1)
        ims.append({"xp": xp, "bm": np.ascontiguousarray(Bm.transpose(2, 0, 1, 3).reshape(128, 12 * 128)),
                    "bh": np.ascontiguousarray(Bh.transpose(2, 0, 1, 3).reshape(16, 12 * 128)),
                    "pw": pw, "psc": _rep(psc), "lng": _rep(g), "lnb": _rep(b)})
    r = _run(ncp, ims)
    return np.concatenate([q["y"] for q in r], 0)


MOE_CAP = 768


def _moe_layer(xf, rw, rbias, w1, w3, w2, g, b):
    T = xf.shape[0] // N_CORES
    NT = T // 128
    ncm = _prog('moe', lambda: build_moe_sparse(NT=NT, C=MOE_CAP, NE=16))
    rb = np.ascontiguousarray(np.broadcast_to(np.tile(np.asarray(rbias, np.float32), NT)[None], (128, NT * 16)))
    eoff = np.ascontiguousarray(np.broadcast_to(np.tile(np.arange(16, dtype=np.float32) * MOE_CAP, NT)[None], (128, NT * 16)))
    ims = [{"x": xf[c * T:(c + 1) * T], "rw": rw, "rbias": rb, "eoff": eoff, "w1": w1, "w3": w3, "w2": w2,
            "lng": _rep(g), "lnb": _rep(b)} for c in range(N_CORES)]
    r = _run(ncm, ims)
    return np.concatenate([q["y"] for q in r], 0)


def kernel(x, w_in, w_out, rpb, pool_w, pool_scale, router_w, router_bias, moe_w1, moe_w3, moe_w2, ln_g, ln_b):
    f = lambda a: np.ascontiguousarray(np.asarray(a, np.float32))
    x = f(x)
    B, S, Dm = x.shape
    xf = x.reshape(B * S, Dm)
    depth = moe_w1.shape[0]
    for layer in range(depth):
        i = layer // 2
        if layer % 2 == 0:
            xf = _attn_layer(xf, f(w_in[i]), f(w_out[i]), f(rpb[i]), ln_g[layer, 0], ln_b[layer, 0], B, S)
        else:
            xf = _pool_layer(xf, f(pool_w[i]), pool_scale[i], ln_g[layer, 0], ln_b[layer, 0], B, S)
        xf = _moe_layer(xf, f(router_w), router_bias, f(moe_w1[layer]), f(moe_w3[layer]), f(moe_w2[layer]),
                        ln_g[layer, 1], ln_b[layer, 1])
    return xf.reshape(B, S, Dm).astype(np.float32)
```
